# Optimizing a Trainium2 kernel written in Bass

```python
import math
import jax, jax.numpy as jnp
from jax import lax
import numpy as np

D_MODEL = 1024
BATCH = 8
SEQ = 2048
DEPTH = 4

N_MEM = 256
EPS = 1e-6
DA_HEADS = 8
DA_HEAD_DIM = D_MODEL // (2 * DA_HEADS)
DA_V_DIM = 2 * DA_HEAD_DIM
DA_QK_WIDTH = 2 * DA_HEADS * DA_HEAD_DIM
DA_V_WIDTH = DA_HEADS * DA_V_DIM
ROPE_THETA = 500000.0
ROT_DIM = DA_HEAD_DIM // 4
Q_BLOCK = 128
CONV_WIDTH = D_MODEL
CONV_K = 3
N_BRANCH = 2
SPLITS = tuple(np.cumsum([DA_QK_WIDTH, DA_QK_WIDTH, DA_V_WIDTH, CONV_WIDTH, CONV_WIDTH, CONV_WIDTH]).tolist())
IN_WIDTH = 2 * DA_QK_WIDTH + DA_V_WIDTH + 3 * CONV_WIDTH + N_BRANCH * D_MODEL
MEM_HEADS = 4
MEM_HEAD_DIM = D_MODEL // MEM_HEADS
N_GROUPS = 4
EXPERTS_PER_GROUP = 8
N_EXPERTS = N_GROUPS * EXPERTS_PER_GROUP
TOP_K = 2
D_EXPERT = 3 * D_MODEL // 4
MOE_BLOCK = 128

kernel_name = 'hybrid_gated_conv_diffattn_hmoe_encoder'


def rms_norm(x, g):
    xf = x.astype(jnp.float32)
    y = xf * lax.rsqrt(jnp.mean(xf * xf, axis=-1, keepdims=True) + EPS)
    return (y * g.astype(jnp.float32)).astype(x.dtype)


def rotary_tables(seq_len):
    inv = jnp.float32(ROPE_THETA) ** (-jnp.arange(0, ROT_DIM, 2, dtype=jnp.float32) / ROT_DIM)
    ang = jnp.arange(seq_len, dtype=jnp.float32)[:, None] * inv[None, :]
    return jnp.cos(ang), jnp.sin(ang)


def partial_rotary(x, cos, sin):
    half = ROT_DIM // 2
    xr = x[..., :ROT_DIM].astype(jnp.float32)
    x1, x2 = xr[..., :half], xr[..., half:]
    c, s = cos[:, None, :], sin[:, None, :]
    rot = jnp.concatenate([x1 * c - x2 * s, x2 * c + x1 * s], axis=-1).astype(x.dtype)
    return jnp.concatenate([rot, x[..., ROT_DIM:]], axis=-1)


def diff_attention(q, k, v, lam):
    _, b, s, h, dh = q.shape
    nb = s // Q_BLOCK
    qb = q.reshape(2, b, nb, Q_BLOCK, h, dh).transpose(2, 0, 1, 3, 4, 5)
    scale = dh ** -0.5

    def one_block(qblk):
        sc = jnp.einsum('cbqhd,cbkhd->cbhqk', qblk, k).astype(jnp.float32) * scale
        p = jax.nn.softmax(sc, axis=-1)
        w = p[0] - lam * p[1]
        return jnp.einsum('bhqk,bkhe->bqhe', w.astype(v.dtype), v)

    o = lax.map(one_block, qb)
    return o.transpose(1, 0, 2, 3, 4).reshape(b, s, h, 2 * dh)


def centred_depthwise_conv(u, w):
    c = u.shape[-1]
    return lax.conv_general_dilated(u, w[:, None, :].astype(u.dtype), window_strides=(1,),
                                    padding=[(CONV_K // 2, CONV_K // 2)],
                                    dimension_numbers=('NWC', 'WIO', 'NWC'), feature_group_count=c)


def hybrid_mixer(h, w_in, lq1, lk1, lq2, lk2, subln, conv_w, w_branch, w_o, lambda_init, cos, sin):
    b, s, _ = h.shape
    proj = h @ w_in
    q, k, v, c_b, c_c, c_x, gates = jnp.split(proj, SPLITS, axis=-1)
    q = jnp.moveaxis(q.reshape(b, s, DA_HEADS, 2, DA_HEAD_DIM), 3, 0)
    k = jnp.moveaxis(k.reshape(b, s, DA_HEADS, 2, DA_HEAD_DIM), 3, 0)
    q = partial_rotary(q, cos, sin)
    k = partial_rotary(k, cos, sin)
    v = v.reshape(b, s, DA_HEADS, DA_V_DIM)
    f32 = jnp.float32
    lam = (jnp.exp(jnp.sum(lq1.astype(f32) * lk1.astype(f32)))
           - jnp.exp(jnp.sum(lq2.astype(f32) * lk2.astype(f32))) + lambda_init)
    o = diff_attention(q, k, v, lam)
    o = rms_norm(o, subln) * (1.0 - lambda_init)
    y_attn = o.reshape(b, s, DA_V_WIDTH)
    y_conv = c_b * centred_depthwise_conv(c_c * c_x, conv_w)
    ys = jnp.stack([y_attn, y_conv], axis=2)
    br = jnp.einsum('bsnc,ncd->bsnd', ys, w_branch)
    g = jax.nn.sigmoid(gates.reshape(b, s, N_BRANCH, D_MODEL))
    return jnp.sum(g * br, axis=2) @ w_o


def memory_cross_attention(h, mem_n, w_cq, w_ckv, w_co):
    b, s, _ = h.shape
    m = mem_n.shape[1]
    q = (h @ w_cq).reshape(b, s, MEM_HEADS, MEM_HEAD_DIM)
    kv = (mem_n @ w_ckv).reshape(b, m, 2, MEM_HEADS, MEM_HEAD_DIM)
    k, v = kv[:, :, 0], kv[:, :, 1]
    sc = jnp.einsum('bshd,bmhd->bhsm', q, k).astype(jnp.float32) * (MEM_HEAD_DIM ** -0.5)
    p = jax.nn.softmax(sc, axis=-1)
    o = jnp.einsum('bhsm,bmhd->bshd', p.astype(v.dtype), v).reshape(b, s, MEM_HEADS * MEM_HEAD_DIM)
    return o @ w_co


def routed_experts(ht, expert_idx, expert_w, w_gate, w_up, w_down):
    t, d = ht.shape
    a = t * TOP_K
    flat_e = expert_idx.reshape(a).astype(jnp.int32)
    order = jnp.argsort(flat_e)
    sorted_e = flat_e[order]
    counts = jnp.bincount(flat_e, length=N_EXPERTS)
    padded = (counts + MOE_BLOCK - 1) // MOE_BLOCK * MOE_BLOCK
    pad_end = jnp.cumsum(padded)
    pad_start = pad_end - padded
    start = jnp.cumsum(counts) - counts
    dest = (pad_start[sorted_e] + jnp.arange(a, dtype=jnp.int32) - start[sorted_e]).astype(jnp.int32)
    n_blocks = -(-a // MOE_BLOCK) + N_EXPERTS
    rows = n_blocks * MOE_BLOCK
    row_tok = jnp.zeros((rows,), jnp.int32).at[dest].set((order // TOP_K).astype(jnp.int32))
    block_expert = jnp.minimum(
        jnp.searchsorted(pad_end, jnp.arange(n_blocks, dtype=jnp.int32) * MOE_BLOCK, side='right'),
        N_EXPERTS - 1)
    xb = ht[row_tok].reshape(n_blocks, MOE_BLOCK, d)

    def expert_block(args):
        xblk, e = args
        return (jax.nn.silu(xblk @ w_gate[e]) * (xblk @ w_up[e])) @ w_down[e]

    yb = lax.map(expert_block, (xb, block_expert)).reshape(rows, d)
    dest_flat = jnp.zeros((a,), jnp.int32).at[order].set(dest)
    y = yb[dest_flat].reshape(t, TOP_K, d)
    return jnp.einsum('tk,tkd->td', expert_w.astype(y.dtype), y)


def hierarchical_moe(h, w_rg, w_re, w_gate, w_up, w_down):
    b, s, d = h.shape
    ht = h.reshape(b * s, d)
    g_prob = jax.nn.softmax((ht @ w_rg).astype(jnp.float32), axis=-1)
    g_w, g_idx = lax.top_k(g_prob, 1)
    e_logits = (ht @ w_re).astype(jnp.float32).reshape(-1, N_GROUPS, EXPERTS_PER_GROUP)
    e_logits = jnp.take_along_axis(e_logits, g_idx[:, :, None], axis=1)[:, 0]
    e_prob = jax.nn.softmax(e_logits, axis=-1)
    e_w, e_idx = lax.top_k(e_prob, TOP_K)
    e_w = e_w / jnp.sum(e_w, axis=-1, keepdims=True)
    weights = g_w * e_w
    expert_idx = g_idx * EXPERTS_PER_GROUP + e_idx
    return routed_experts(ht, expert_idx, weights, w_gate, w_up, w_down).reshape(b, s, d)


def setup_inputs(seed: int = 0) -> dict:
    key = jax.random.key(seed)
    ks = jax.random.split(key, 24)
    f32 = jnp.float32
    res = (2.0 * DEPTH) ** -0.5

    def nrm(k, shape, scale):
        return jax.random.normal(k, shape, f32) * scale

    def gain(k, shape):
        return 1.0 + 0.02 * jax.random.normal(k, shape, f32)

    return {
        'x': nrm(ks[0], (BATCH, SEQ, D_MODEL), 1.0),
        'mem': nrm(ks[1], (BATCH, N_MEM, D_MODEL), 1.0),
        'norm_mix': gain(ks[2], (DEPTH, D_MODEL)),
        'w_in': nrm(ks[3], (DEPTH, D_MODEL, IN_WIDTH), D_MODEL ** -0.5),
        'lambda_q1': nrm(ks[4], (DEPTH, DA_HEAD_DIM), 0.1),
        'lambda_k1': nrm(ks[5], (DEPTH, DA_HEAD_DIM), 0.1),
        'lambda_q2': nrm(ks[6], (DEPTH, DA_HEAD_DIM), 0.1),
        'lambda_k2': nrm(ks[7], (DEPTH, DA_HEAD_DIM), 0.1),
        'subln': gain(ks[8], (DEPTH, DA_V_DIM)),
        'conv_w': nrm(ks[9], (DEPTH, CONV_K, CONV_WIDTH), CONV_K ** -0.5),
        'w_branch': nrm(ks[10], (DEPTH, N_BRANCH, D_MODEL, D_MODEL), D_MODEL ** -0.5),
        'w_o': nrm(ks[11], (DEPTH, D_MODEL, D_MODEL), res * D_MODEL ** -0.5),
        'norm_cross': gain(ks[12], (DEPTH, D_MODEL)),
        'norm_mem': gain(ks[13], (D_MODEL,)),
        'w_cq': nrm(ks[14], (DEPTH, D_MODEL, MEM_HEADS * MEM_HEAD_DIM), D_MODEL ** -0.5),
        'w_ckv': nrm(ks[15], (DEPTH, D_MODEL, 2 * MEM_HEADS * MEM_HEAD_DIM), D_MODEL ** -0.5),
        'w_co': nrm(ks[16], (DEPTH, MEM_HEADS * MEM_HEAD_DIM, D_MODEL), res * D_MODEL ** -0.5),
        'norm_ffn': gain(ks[17], (DEPTH, D_MODEL)),
        'w_router_group': nrm(ks[18], (DEPTH, D_MODEL, N_GROUPS), D_MODEL ** -0.5),
        'w_router_expert': nrm(ks[19], (DEPTH, D_MODEL, N_EXPERTS), D_MODEL ** -0.5),
        'w_exp_gate': nrm(ks[20], (DEPTH, N_EXPERTS, D_MODEL, D_EXPERT), D_MODEL ** -0.5),
        'w_exp_up': nrm(ks[21], (DEPTH, N_EXPERTS, D_MODEL, D_EXPERT), D_MODEL ** -0.5),
        'w_exp_down': nrm(ks[22], (DEPTH, N_EXPERTS, D_EXPERT, D_MODEL), res * D_EXPERT ** -0.5),
        'norm_final': gain(ks[23], (D_MODEL,)),
    }


def reference(x, mem, norm_mix, w_in, lambda_q1, lambda_k1, lambda_q2, lambda_k2, subln, conv_w,
              w_branch, w_o, norm_cross, norm_mem, w_cq, w_ckv, w_co, norm_ffn, w_router_group,
              w_router_expert, w_exp_gate, w_exp_up, w_exp_down, norm_final):
    cos, sin = rotary_tables(x.shape[1])
    mem_n = rms_norm(mem, norm_mem)
    for l in range(DEPTH):
        lambda_init = 0.8 - 0.6 * math.exp(-0.3 * l)
        x = x + hybrid_mixer(rms_norm(x, norm_mix[l]), w_in[l], lambda_q1[l], lambda_k1[l],
                             lambda_q2[l], lambda_k2[l], subln[l], conv_w[l], w_branch[l], w_o[l],
                             lambda_init, cos, sin)
        x = x + memory_cross_attention(rms_norm(x, norm_cross[l]), mem_n, w_cq[l], w_ckv[l], w_co[l])
        x = x + hierarchical_moe(rms_norm(x, norm_ffn[l]), w_router_group[l], w_router_expert[l],
                                 w_exp_gate[l], w_exp_up[l], w_exp_down[l])
    return rms_norm(x, norm_final)
```

```python
import math
import os
import numpy as np
import ml_dtypes
HEADCUT = int(os.environ.get('HEADCUT', '99'))
from contextlib import ExitStack
import concourse.bass as bass
import concourse.mybir as mybir
from concourse.bass_utils import run_bass_kernel_spmd

F32 = mybir.dt.float32
BF16 = mybir.dt.bfloat16
I32 = mybir.dt.int32
ALU = mybir.AluOpType
AF = mybir.ActivationFunctionType
AX = mybir.AxisListType

D = 1024
S_LEN = 2048
NT = 16
KC = 8
NMEM = 256
DEPTH = 4
NEXP = 32
DEXP = 768
NBLK = 64
EPS = 1e-6
NEG = -1.0e30


class Buf:
    __slots__ = ("name", "w", "r", "excl")

    def __init__(self, name):
        self.name = name
        self.w = None
        self.r = {}
        self.excl = False


class Sched:
    ENG = ("pe", "act", "dve", "pool", "sp")

    def __init__(self, nc, stack):
        self.nc = nc
        self.stack = stack
        self.prog = {e: [] for e in self.ENG}
        self.meta = {e: [] for e in self.ENG}
        self.sem = {e: stack.enter_context(nc.semaphore("s_" + e)) for e in self.ENG}
        self.dummy = [stack.enter_context(nc.semaphore("dummy%d" % i)) for i in range(int(os.environ.get('NDUMMY', '0')))]
        self.cnt = {e: 0 for e in self.ENG}
        self.seen = {e: {} for e in self.ENG}
        self.dsem = {}
        self.dcnt = {}
        self.nbuf = 0
        self.ninst = 0

    def buf(self, name=None):
        self.nbuf += 1
        return Buf(name or "b%d" % self.nbuf)

    def bufs(self, n, name="b"):
        return [self.buf("%s%d" % (name, i)) for i in range(n)]

    def _deps(self, eng, reads, writes, skipkey=None):
        deps = {}

        def add(k, v):
            if k == skipkey:
                return
            if deps.get(k, 0) < v:
                deps[k] = v

        for b in reads:
            if b.w is not None:
                add(*b.w)
            if b.excl:
                for k, v in b.r.items():
                    if k != ("e", eng):
                        add(k, v)
        for b in writes:
            if b.w is not None:
                add(*b.w)
            for k, v in b.r.items():
                add(k, v)
        out = []
        seen = self.seen[eng]
        for k, v in deps.items():
            if eng == "pe" and k == ("e", "pe"):
                continue
            if k[0] == "d":
                v = self.dcnt[k[1]]
            if seen.get(k, 0) >= v:
                continue
            seen[k] = v
            out.append((self._semof(k), v))
        return out

    def _semof(self, k):
        return self.sem[k[1]] if k[0] == "e" else self.dsem[k[1]]

    def _post(self, ev, reads, writes):
        k, v = ev
        for b in reads:
            if b.r.get(k, 0) < v:
                b.r[k] = v
        for b in writes:
            b.w = ev
            b.r = {}

    def op(self, eng, fn, reads=(), writes=()):
        waits = self._deps(eng, reads, writes)
        self.cnt[eng] += 1
        ev = (("e", eng), self.cnt[eng])
        sem = self.sem[eng]

        def thunk(e, waits=waits, fn=fn, sem=sem):
            for s, v in waits:
                e.wait_ge(s, v)
            fn(e).then_inc(sem, 1)

        self.prog[eng].append(thunk)
        self.meta[eng].append((waits, sem, 1, getattr(fn, "__name__", "op")))
        self._post(ev, reads, writes)
        self.ninst += 1
        return ev

    def dma(self, eng, key, fn, reads=(), writes=()):
        if key not in self.dsem:
            self.dsem[key] = self.stack.enter_context(self.nc.semaphore("d_" + str(key)))
            self.dcnt[key] = 0
        waits = self._deps(eng, reads, writes, skipkey=("d", key))
        self.dcnt[key] += 16
        ev = (("d", key), self.dcnt[key])
        sem = self.dsem[key]

        def thunk(e, waits=waits, fn=fn, sem=sem):
            for s, v in waits:
                e.wait_ge(s, v)
            fn(e).then_inc(sem, 16)

        self.prog[eng].append(thunk)
        self.meta[eng].append((waits, sem, 16, "dma:" + str(key)))
        self._post(ev, reads, writes)
        self.ninst += 1
        return ev

    def simulate(self):
        val = {}
        pc = {e: 0 for e in self.ENG}
        prog = self.meta
        while True:
            progressed = False
            for e in self.ENG:
                while pc[e] < len(prog[e]):
                    waits, sem, inc, nm = prog[e][pc[e]]
                    if all(val.get(id(s_), 0) >= v for s_, v in waits):
                        val[id(sem)] = val.get(id(sem), 0) + inc
                        pc[e] += 1
                        progressed = True
                    else:
                        break
            if all(pc[e] == len(prog[e]) for e in self.ENG):
                return None
            if not progressed:
                return {e: (pc[e], len(prog[e]), prog[e][pc[e]][3] if pc[e] < len(prog[e]) else None,
                            [(str(s_), v, val.get(id(s_), 0)) for s_, v in (prog[e][pc[e]][0] if pc[e] < len(prog[e]) else [])]) for e in self.ENG}

    def alias(self, new_bufs, old_bufs):
        for nb in new_bufs:
            for ob in old_bufs:
                if ob.w is not None:
                    k, v = ob.w
                    if nb.r.get(k, 0) < v:
                        nb.r[k] = v
                for k, v in ob.r.items():
                    if nb.r.get(k, 0) < v:
                        nb.r[k] = v

    def wait_all(self, eng, bufs):
        waits = self._deps(eng, bufs, bufs)

        def thunk(e, waits=waits):
            for s, v in waits:
                e.wait_ge(s, v)

        self.prog[eng].append(thunk)

    def emit(self):
        nc = self.nc
        prog = self.prog
        with nc.Block() as block:
            @block.tensor
            def _(e):
                for t in prog["pe"]:
                    t(e)

            @block.scalar
            def _(e):
                for t in prog["act"]:
                    t(e)

            @block.vector
            def _(e):
                for t in prog["dve"]:
                    t(e)

            @block.gpsimd
            def _(e):
                for t in prog["pool"]:
                    t(e)

            @block.sync
            def _(e):
                for t in prog["sp"]:
                    t(e)


def lambda_init(l):
    return 0.8 - 0.6 * math.exp(-0.3 * l)


GC_MIX, GC_CROSS, GC_FFN, GC_CONV, GC_SUBLN, GC_PER = 0, 8, 16, 24, 48, 49


def build(layer_ids, final, stop_after=None, max_steps=None):
    L = len(layer_ids)
    nc = bass.Bass("TRN2", target_bir_lowering=False)

    def din(name, shape, dt=F32):
        return nc.dram_tensor(name, shape, dt, kind="ExternalInput").ap()

    x_in = din("x", [S_LEN, D])
    mem_in = din("mem", [NMEM, D])
    w_in = din("w_in", [L, D, 8192])
    w_br = din("w_branch", [L, 2, D, D])
    w_o = din("w_o", [L, D, D])
    w_cq = din("w_cq", [L, D, D])
    w_ckv = din("w_ckv", [L, D, 2 * D])
    w_co = din("w_co", [L, D, D])
    w_rt = din("w_rt", [L, D, 36])
    w_eg = din("w_exp_gate", [L, NEXP, D, DEXP])
    w_eu = din("w_exp_up", [L, NEXP, D, DEXP])
    w_ed = din("w_exp_down", [L, NEXP, DEXP, D])
    gcols_d = din("gcols", [128, L * GC_PER + 8])
    lamv_d = din("lamv", [128, L * 256])
    gfin_d = din("gfinal", [128, D])
    cmat_d = din("cmat", [128, 4 * 128], BF16)
    cossin_d = din("cossin", [128, 2 * S_LEN], BF16)
    cmisc_d = din("cmisc", [128, 64 + 8])
    out_d = nc.dram_tensor("out", [S_LEN, D], F32, kind="ExternalOutput").ap()
    xb_d = nc.dram_tensor("xb_scr", [NBLK * 128, D], BF16, kind="Internal").ap()
    yb_d = nc.dram_tensor("yb_scr", [NBLK * 128, D], F32, kind="Internal").ap()

    st = ExitStack()
    with st:
        S = Sched(nc, st)

        def sb(name, shape, dt):
            return st.enter_context(nc.sbuf_tensor("sb_" + name, shape, dt))

        X = sb("X", [128, NT, D], F32)
        HT = sb("HT", [128, KC * S_LEN], BF16)
        YR = sb("YR", [128, KC * S_LEN], BF16)
        NBIG = 19 * 1024
        BIG = sb("BIG", [128, NBIG], BF16)
        NSLOT = 3
        WS = [sb("WS%d" % i, [128, 4096], BF16) for i in range(NSLOT)]
        cmat = sb("cmat", [128, 4 * 128], BF16)
        gcols = sb("gcols", [128, L * GC_PER + 8], F32)
        cmisc = sb("cmisc", [128, 72], F32)
        memT = sb("memT", [128, KC, NMEM], BF16)
        stat = sb("stat", [128, 64], F32)
        lamc = sb("lamc", [128, 2 * L], F32)
        sublnS = sb("sublnS", [128, L], F32)
        PS = [st.enter_context(nc.psum_tensor("ps%d" % i, [128, 512], F32)) for i in range(8)]
        PSB = S.bufs(8, "ps")
        for b_ in PSB:
            b_.excl = True

        ident = cmat[:, 0:128]
        ones = cmat[:, 128:256]
        rrot = cmat[:, 256:384]
        ustr = cmat[:, 384:512]
        thr = cmisc[:, 0:64]
        pcv = cmisc[:, 64:72]

        Xb = S.bufs(NT, "X")
        hTb = S.buf("hT")
        YRb = S.buf("YR")
        WSb = S.bufs(NSLOT, "WS")
        b_const = S.buf("const")
        b_memT = S.buf("memT")
        b_stat = S.buf("stat")

        hT = HT[:, :].rearrange("p (c t) -> p c t", c=KC)

        def carve(region, off, n, dt=BF16):
            if dt == BF16:
                return region[:, off:off + n], off + n
            assert off % 2 == 0
            return region[:, off:off + 2 * n].bitcast(dt), off + 2 * n

        psrr = [0]

        def psbank():
            i = psrr[0] % 8
            psrr[0] += 1
            return PS[i], PSB[i]

        def dma_sp(key, out, in_, reads=(), writes=()):
            return S.dma("sp", key, lambda e: e.dma_start(out=out, in_=in_), reads=reads, writes=writes)

        def dma_pool(key, out, in_, reads=(), writes=()):
            return S.dma("pool", key, lambda e: e.dma_start(out=out, in_=in_), reads=reads, writes=writes)

        def mm(out, lhsT, rhs, start, stop, reads, writes):
            S.op("pe", lambda e: e.matmul(out, lhsT=lhsT, rhs=rhs, start=start, stop=stop),
                 reads=reads, writes=writes)

        def tr(out, in_, reads, writes):
            S.op("pe", lambda e: e.transpose(out=out, in_=in_, identity=ident), reads=list(reads) + [b_const], writes=writes)

        def act(out, in_, func, reads, writes, scale=1.0, bias=None, accum=None):
            kw = {}
            if bias is not None:
                kw["bias"] = bias
            if accum is not None:
                kw["accum_out"] = accum
            S.op("act", lambda e: e.activation(out=out, in_=in_, func=func, scale=scale, **kw), reads=reads, writes=writes)

        def tt(eng, out, in0, in1, op, reads, writes):
            S.op(eng, lambda e: e.tensor_tensor(out=out, in0=in0, in1=in1, op=op), reads=reads, writes=writes)

        def ts(eng, out, in0, s1, op0, reads, writes, s2=None, op1=None, accum=None):
            kw = {}
            if accum is not None:
                kw["accum_out"] = accum
            if op1 is None:
                S.op(eng, lambda e: e.tensor_scalar(out=out, in0=in0, scalar1=s1, scalar2=None, op0=op0, **kw), reads=reads, writes=writes)
            else:
                S.op(eng, lambda e: e.tensor_scalar(out=out, in0=in0, scalar1=s1, scalar2=s2, op0=op0, op1=op1, **kw), reads=reads, writes=writes)

        def stt(eng, out, in0, scalar, in1, op0, op1, reads, writes):
            S.op(eng, lambda e: e.scalar_tensor_tensor(out=out, in0=in0, scalar=scalar, in1=in1, op0=op0, op1=op1), reads=reads, writes=writes)

        def cp(eng, out, in_, reads, writes):
            S.op(eng, lambda e: e.tensor_copy(out=out, in_=in_), reads=reads, writes=writes)

        dma_sp("c0", cmat[:, :], cmat_d[:, :], writes=[b_const])
        dma_sp("c1", gcols[:, :], gcols_d[:, :], writes=[b_const])
        dma_sp("c1", cmisc[:, :], cmisc_d[:, :], writes=[b_const])
        for i in range(NT):
            dma_sp("xin", X[:, i, :], x_in[i * 128:(i + 1) * 128, :], writes=[Xb[i]])

        b_big = S.buf("bigscratch")
        lamv, _ = carve(BIG, 0, L * 256, F32)
        ltmp, _ = carve(BIG, 2 * L * 256, 64, F32)
        dma_sp("c3", lamv, lamv_d[:, :], writes=[b_big])
        for li_, l in enumerate(layer_ids):
            base = li_ * 256
            for j in range(2):
                S.op("dve", lambda e, base=base, j=j: e.tensor_tensor(out=ltmp, in0=lamv[:, base + j * 128:base + j * 128 + 64],
                                                                      in1=lamv[:, base + j * 128 + 64:base + j * 128 + 128], op=ALU.mult),
                     reads=[b_big], writes=[b_big])
                S.op("dve", lambda e, j=j: e.tensor_reduce(out=stat[:, j:j + 1], in_=ltmp, axis=AX.X, op=ALU.add),
                     reads=[b_big], writes=[b_stat])
            act(stat[:, 2:4], stat[:, 0:2], AF.Exp, [b_stat], [b_stat])
            tt("dve", stat[:, 4:5], stat[:, 3:4], stat[:, 2:3], ALU.subtract, [b_stat], [b_stat])
            ts("dve", lamc[:, li_:li_ + 1], stat[:, 4:5], -lambda_init(l), ALU.add, [b_stat], [b_const])
            ts("dve", sublnS[:, li_:li_ + 1], gcols[:, li_ * GC_PER + GC_SUBLN:li_ * GC_PER + GC_SUBLN + 1],
               1.0 - lambda_init(l), ALU.mult, [b_const], [b_const])

        ssq = sb("ssq", [128, NT], F32)
        rstd = sb("rstd", [128, NT], F32)
        b_ssq = S.buf("ssq")
        b_rstd = S.buf("rstd")
        hn_t = [sb("hn%d" % i, [128, D], BF16) for i in range(2)]
        hn_b = S.bufs(2, "hn")

        def rstd_tiles(src_ap_fn, src_bufs, ntiles, inv_n):
            for i in range(ntiles):
                act(hn_t[0][:, :], src_ap_fn(i), AF.Square, [src_bufs[i]], [hn_b[0], b_ssq], accum=ssq[:, i:i + 1])
            ts("dve", rstd[:, 0:ntiles], ssq[:, 0:ntiles], inv_n, ALU.mult, [b_ssq], [b_rstd], s2=EPS, op1=ALU.add)
            act(rstd[:, 0:ntiles], rstd[:, 0:ntiles], AF.Ln, [b_rstd], [b_rstd])
            act(rstd[:, 0:ntiles], rstd[:, 0:ntiles], AF.Exp, [b_rstd], [b_rstd], scale=-0.5)

        def norm_to_fm(src_ap_fn, src_bufs, ntiles, gcol0, dstT, dst_buf, keep_rows=None):
            rstd_tiles(src_ap_fn, src_bufs, ntiles, 1.0 / D)
            for i in range(ntiles):
                if keep_rows is None:
                    hn, hb = hn_t[i % 2], hn_b[i % 2]
                    hn_ap = hn[:, :]
                else:
                    hn_ap, hb = keep_rows(i)
                act(hn_ap, src_ap_fn(i), AF.Copy, [src_bufs[i], b_rstd], [hb], scale=rstd[:, i:i + 1])
                pt, pb = psbank()
                ptb = pt[:, :].bitcast(BF16).rearrange("p (c t) -> p c t", t=128)
                for c in range(KC):
                    tr(ptb[:, c, :], hn_ap[:, c * 128:(c + 1) * 128], [hb], [pb])
                for c in range(KC):
                    eng = "dve" if c % 2 == 0 else "act"
                    if eng == "dve":
                        ts("dve", dstT[:, c, i * 128:(i + 1) * 128], ptb[:, c, :], gcols[:, gcol0 + c:gcol0 + c + 1], ALU.mult,
                           [pb, b_const], [dst_buf])
                    else:
                        act(dstT[:, c, i * 128:(i + 1) * 128], ptb[:, c, :], AF.Copy, [pb, b_const], [dst_buf],
                            scale=gcols[:, gcol0 + c:gcol0 + c + 1])

        memrows, _ = carve(BIG, 4096, 2 * D, F32)
        memrows = memrows.rearrange("p (i d) -> p i d", i=2)
        b_memrows = S.bufs(2, "memrows")
        for i in range(2):
            dma_sp("c2", memrows[:, i, :], mem_in[i * 128:(i + 1) * 128, :], writes=[b_memrows[i]])
        norm_to_fm(lambda i: memrows[:, i, :], b_memrows, 2, L * GC_PER, memT, b_memT)

        dbg_bufs = []
        steps = []

        def wload(slot, sbuf_, pieces):
            key = "w%d" % [i for i in range(NSLOT) if WS[i] is slot][0]
            for dst, src in pieces:
                dma_pool(key, dst(slot), src, writes=[sbuf_])

        def w3(slot, off, ncols):
            return slot[:, off:off + KC * ncols].rearrange("p (c n) -> p c n", c=KC)

        def dram_cols(w2d, c0, ncols):
            return w2d[:, c0:c0 + ncols].rearrange("(c p) n -> p c n", p=128)

        def layer_steps(li_, l):
            g0 = li_ * GC_PER
            win_l = w_in[li_]
            off = 0
            cosb, off = carve(BIG, off, S_LEN)
            sinb, off = carve(BIG, off, S_LEN)
            qT, off = carve(BIG, off, S_LEN)
            kT, off = carve(BIG, off, S_LEN)
            vtm, off = carve(BIG, off, NT * 128)
            vtm = vtm.rearrange("p (i e) -> p i e", i=NT)
            NE = 6
            Et = []
            for _ in range(NE):
                a, off = carve(BIG, off, 512)
                Et.append(a)
            NTMP = 6
            Tm = []
            for _ in range(NTMP):
                a, off = carve(BIG, off, 512, F32)
                Tm.append(a)
            assert off <= NBIG, off
            b_cs = S.buf("cossin")
            dbg_bufs.append(b_cs)
            b_q = S.buf("qT")
            b_k = S.buf("kT")
            b_v = S.buf("v")
            Eb = S.bufs(NE, "E")
            Tb = S.bufs(NTMP, "T")
            rrE = [0]
            rrT = [0]

            def getE():
                i = rrE[0] % NE
                rrE[0] += 1
                return Et[i], Eb[i]

            def getT():
                i = rrT[0] % NTMP
                rrT[0] += 1
                return Tm[i], Tb[i]

            yT = YR[:, :].rearrange("p (c t) -> p c t", c=KC)
            b_y = [S.buf("y%d" % c) for c in range(KC)]

            def mixer_begin(slot, sbuf_):
                S.alias([b_cs, b_q, b_k, b_v] + Eb + Tb, [b_big])
                S.alias(b_y, [YRb])
                dma_sp("cs", cosb, cossin_d[:, 0:S_LEN], writes=[b_cs])
                dma_sp("cs", sinb, cossin_d[:, S_LEN:2 * S_LEN], writes=[b_cs])
                if os.environ.get('CSTOUCH'):
                    cp("dve", stat[:, 60:61], cosb[:, 0:1], [b_cs], [b_stat])
                norm_to_fm(lambda i: X[:, i, :], Xb, NT, g0 + GC_MIX, hT, hTb)

            steps.append((None, mixer_begin))

            def head_step(h):
                def load(slot, sbuf_):
                    wload(slot, sbuf_, [
                        (lambda s: w3(s, 0, 128), dram_cols(win_l, h * 128, 128)),
                        (lambda s: w3(s, 1024, 128), dram_cols(win_l, 1024 + h * 128, 128)),
                        (lambda s: w3(s, 2048, 128), dram_cols(win_l, 2048 + h * 128, 128)),
                    ])

                def compute(slot, sbuf_):
                    wq = w3(slot, 0, 128)
                    wk = w3(slot, 1024, 128)
                    wv = w3(slot, 2048, 128)
                    for (wt, dst, db) in ((wq, qT, b_q), (wk, kT, b_k)):
                        for tb in range(4):
                            tsl = slice(tb * 512, (tb + 1) * 512)
                            pp, pb = psbank()
                            for c in range(KC):
                                mm(pp[:, :], wt[:, c, :], hT[:, c, tsl], c == 0, c == KC - 1, [sbuf_, hTb], [pb])
                            qb, qbb = getE()
                            act(qb, pp[:, :], AF.Copy, [pb], [qbb])
                            if HEADCUT == 0:
                                continue
                            ROT = int(os.environ.get('ROT', '9'))
                            p2, p2b = psbank()
                            P2V = os.environ.get('P2V', '')
                            if P2V == 'ident':
                                mm(p2[:, :], ident, qb, True, True, [b_const, qbb], [p2b])
                            elif P2V == 'rhs':
                                mm(p2[:, :], rrot, hT[:, 0, tsl], True, True, [b_const, hTb], [p2b])
                            elif P2V == 'evac':
                                mm(p2[:, :], rrot, qb, True, True, [b_const, qbb], [p2b])
                                t9, t9b = getT()
                                cp("dve", t9, p2[:, :], [p2b], [t9b])
                            else:
                                mm(p2[:, :], rrot, qb, True, True, [b_const, qbb], [p2b])
                            if ROT < 2:
                                continue
                            t1, t1b = getT()
                            if os.environ.get('ROTV') == 'sb':
                                tt("dve", t1, qb, cosb[:, tsl], ALU.mult, [qbb, b_cs], [t1b])
                            elif os.environ.get('ROTV') == 'hT':
                                tt("dve", t1, pp[:, :], hT[:, 0, tsl], ALU.mult, [pb, hTb], [t1b])
                            elif os.environ.get('ROTV') == 'nocs':
                                tt("dve", t1, pp[:, :], qb, ALU.mult, [pb, qbb], [t1b])
                            else:
                                tt("dve", t1, pp[:, :], cosb[:, tsl], ALU.mult, [pb, b_cs], [t1b])
                            if ROT < 3:
                                continue
                            t2, t2b = getT()
                            tt("dve", t2, p2[:, :], sinb[:, tsl], ALU.mult, [p2b, b_cs], [t2b])
                            if ROT < 4:
                                continue
                            tt("pool", dst[:, tsl], t1, t2, ALU.add, [t1b, t2b], [db])
                    if HEADCUT < 2:
                        return
                    for i4 in range(4):
                        pp, pb = psbank()
                        for ii in range(4):
                            i = i4 * 4 + ii
                            for c in range(KC):
                                mm(pp[:, ii * 128:(ii + 1) * 128], hT[:, c, i * 128:(i + 1) * 128], wv[:, c, :],
                                   c == 0, c == KC - 1, [sbuf_, hTb], [pb])
                        act(vtm[:, i4 * 4:(i4 + 1) * 4, :], pp[:, :].rearrange("p (i e) -> p i e", i=4), AF.Copy, [pb], [b_v])
                    if HEADCUT < 3:
                        return
                    for qb_ in range(4 if HEADCUT > 4 else 1):
                        qsl = slice(qb_ * 512, (qb_ + 1) * 512)
                        pO = [(PS[4], PSB[4]), (PS[5], PSB[5])]
                        pR = [(PS[6], PSB[6]), (PS[7], PSB[7])]
                        for kb in range(NT):
                            for c in range(2):
                                pS, pSb = psS[(kb * 2 + c) % 4]
                                mm(pS[:, :], kT[c * 64:(c + 1) * 64, kb * 128:(kb + 1) * 128], qT[c * 64:(c + 1) * 64, qsl],
                                   True, True, [b_k, b_q], [pSb])
                                E, Eb_ = getE()
                                act(E, pS[:, :], AF.Exp, [pSb], [Eb_], scale=0.125)
                                mm(pO[c][0][:, :], vtm[:, kb, :], E, kb == 0, kb == NT - 1, [b_v, Eb_], [pO[c][1]])
                                mm(pR[c][0][:, :], ones, E, kb == 0, kb == NT - 1, [b_const, Eb_], [pR[c][1]])
                        if HEADCUT < 4:
                            continue
                        ri = []
                        for c in range(2):
                            r_, rb_ = getT()
                            S.op("dve", lambda e, r_=r_, c=c: e.reciprocal(out=r_, in_=pR[c][0][:, :]), reads=[pR[c][1]], writes=[rb_])
                            ri.append((r_, rb_))
                        t0, t0b = getT()
                        tt("dve", t0, pO[0][0][:, :], ri[0][0], ALU.mult, [pO[0][1], ri[0][1]], [t0b])
                        t1, t1b = getT()
                        tt("dve", t1, pO[1][0][:, :], ri[1][0], ALU.mult, [pO[1][1], ri[1][1]], [t1b])
                        o_, ob_ = getT()
                        stt("dve", o_, t1, lamc[:, li_:li_ + 1], t0, ALU.mult, ALU.add, [t1b, t0b, b_const], [ob_])
                        sq, sqb = getE()
                        tt("pool", sq, o_, o_, ALU.mult, [ob_], [sqb])
                        pq, pqb = psS[0]
                        mm(pq[:, :], ones, sq, True, True, [b_const, sqb], [pqb])
                        rs, rsb = getT()
                        ts("dve", rs, pq[:, :], 1.0 / 128, ALU.mult, [pqb], [rsb], s2=EPS, op1=ALU.add)
                        act(rs, rs, AF.Ln, [rsb], [rsb])
                        act(rs, rs, AF.Exp, [rsb], [rsb], scale=-0.5)
                        stt("dve", yT[:, h, qsl], o_, sublnS[:, li_:li_ + 1], rs, ALU.mult, ALU.mult, [ob_, rsb, b_const], [b_y[h]])

                return load, compute

            psS = [(PS[i], PSB[i]) for i in range(4)]

            for h in range(8):
                steps.append(head_step(h))

            offc = 0
            Mreg, offc = carve(BIG, offc, KC * S_LEN)
            Mv = Mreg.rearrange("p (c t) -> p c t", c=KC)
            CT = []
            for _ in range(3):
                a, offc = carve(BIG, offc, 512, F32)
                CT.append(a)
            assert offc <= NBIG
            b_M = [S.buf("M%d" % c) for c in range(KC)]
            CTb = S.bufs(3, "CT")
            rrC = [0]

            def getC():
                i = rrC[0] % 3
                rrC[0] += 1
                return CT[i], CTb[i]

            def phaseC_begin(slot, sbuf_):
                S.alias(b_M + CTb, [b_cs, b_q, b_k, b_v] + Eb + Tb)

            steps.append((None, phaseC_begin))

            def branch_step(n, j):
                def load(slot, sbuf_):
                    wload(slot, sbuf_, [
                        (lambda s: w3(s, 0, 128), dram_cols(w_br[li_, n], j * 128, 128)),
                        (lambda s: w3(s, 1024, 128), dram_cols(win_l, 6144 + n * 1024 + j * 128, 128)),
                    ])

                def compute(slot, sbuf_):
                    wb = w3(slot, 0, 128)
                    wg = w3(slot, 1024, 128)
                    for tb in range(4):
                        tsl = slice(tb * 512, (tb + 1) * 512)
                        pg, pgb = psbank()
                        for c in range(KC):
                            mm(pg[:, :], wg[:, c, :], hT[:, c, tsl], c == 0, c == KC - 1, [sbuf_, hTb], [pgb])
                        pbr, pbrb = psbank()
                        for c in range(KC):
                            mm(pbr[:, :], wb[:, c, :], yT[:, c, tsl], c == 0, c == KC - 1, [sbuf_, b_y[c]], [pbrb])
                        sg, sgb = getC()
                        act(sg, pg[:, :], AF.Sigmoid, [pgb], [sgb])
                        if n == 0:
                            tt("dve", Mv[:, j, tsl], sg, pbr[:, :], ALU.mult, [sgb, pbrb], [b_M[j]])
                        else:
                            t_, tb_ = getC()
                            tt("dve", t_, sg, pbr[:, :], ALU.mult, [sgb, pbrb], [tb_])
                            tt("pool", Mv[:, j, tsl], Mv[:, j, tsl], t_, ALU.add, [b_M[j], tb_], [b_M[j]])

                return load, compute

            for j in range(KC):
                steps.append(branch_step(0, j))


            def conv_step(j):
                def load(slot, sbuf_):
                    wload(slot, sbuf_, [
                        (lambda s: w3(s, 0, 128), dram_cols(win_l, 3072 + j * 128, 128)),
                        (lambda s: w3(s, 1024, 128), dram_cols(win_l, 4096 + j * 128, 128)),
                        (lambda s: w3(s, 2048, 128), dram_cols(win_l, 5120 + j * 128, 128)),
                    ])

                def compute(slot, sbuf_):
                    wcb = w3(slot, 0, 128)
                    wcc = w3(slot, 1024, 128)
                    wcx = w3(slot, 2048, 128)
                    u = U_pad
                    for tb in range(4):
                        tsl = slice(tb * 512, (tb + 1) * 512)
                        pc_, pcb = psbank()
                        for c in range(KC):
                            mm(pc_[:, :], wcc[:, c, :], hT[:, c, tsl], c == 0, c == KC - 1, [sbuf_, hTb], [pcb])
                        px, pxb = psbank()
                        for c in range(KC):
                            mm(px[:, :], wcx[:, c, :], hT[:, c, tsl], c == 0, c == KC - 1, [sbuf_, hTb], [pxb])
                        t_, tb_ = getC()
                        act(t_, pc_[:, :], AF.Copy, [pcb], [tb_])
                        tt("dve", u[:, 1 + tb * 512:1 + (tb + 1) * 512], t_, px[:, :], ALU.mult, [tb_, pxb], [b_u])
                    cw = g0 + GC_CONV
                    for tb in range(4):
                        tsl = slice(tb * 512, (tb + 1) * 512)
                        acc, accb = getC()
                        act(acc, u[:, 1 + tb * 512:1 + (tb + 1) * 512], AF.Copy, [b_u, b_const], [accb], scale=gcols[:, cw + 8 + j:cw + 8 + j + 1])
                        stt("dve", acc, u[:, tb * 512:(tb + 1) * 512], gcols[:, cw + j:cw + j + 1], acc, ALU.mult, ALU.add, [b_u, accb, b_const], [accb])
                        stt("dve", acc, u[:, 2 + tb * 512:2 + (tb + 1) * 512], gcols[:, cw + 16 + j:cw + 16 + j + 1], acc, ALU.mult, ALU.add, [b_u, accb, b_const], [accb])
                        pb_, pbb = psbank()
                        for c in range(KC):
                            mm(pb_[:, :], wcb[:, c, :], hT[:, c, tsl], c == 0, c == KC - 1, [sbuf_, hTb], [pbb])
                        tt("dve", yT[:, j, tsl], pb_[:, :], acc, ALU.mult, [pbb, accb], [b_y[j]])

                return load, compute

            for j in range(KC):
                steps.append(conv_step(j))
            for j in range(KC):
                steps.append(branch_step(1, j))

            def out_proj_step(wmat, half, srcT, src_bufs):
                def load(slot, sbuf_):
                    wload(slot, sbuf_, [(lambda s: w3(s, 0, 512), dram_cols(wmat, half * 512, 512))])

                def compute(slot, sbuf_):
                    wt = w3(slot, 0, 512)
                    for i in range(NT):
                        pp, pb = psbank()
                        for c in range(KC):
                            mm(pp[:, :], srcT[:, c, i * 128:(i + 1) * 128], wt[:, c, :], c == 0, c == KC - 1, [sbuf_, src_bufs[c]], [pb])
                        tt("dve", X[:, i, half * 512:(half + 1) * 512], X[:, i, half * 512:(half + 1) * 512], pp[:, :], ALU.add,
                           [Xb[i], pb], [Xb[i]])

                return load, compute

            for half in range(2):
                steps.append(out_proj_step(w_o[li_], half, Mv, b_M))

            def mixer_end(slot, sbuf_):
                S.alias([b_big], b_M + CTb)
                S.alias([YRb], b_y)

            steps.append((None, mixer_end))
            if stop_after == (l, "mix"):
                return True

            offx = 0
            kcT, offx = carve(BIG, offx, KC * NMEM)
            kcT = kcT.rearrange("p (c m) -> p c m", c=KC)
            vc, offx = carve(BIG, offx, 2 * D)
            vc = vc.rearrange("p (i d) -> p i d", i=2)
            XE = []
            for _ in range(6):
                a, offx = carve(BIG, offx, 512)
                XE.append(a)
            XT = []
            for _ in range(4):
                a, offx = carve(BIG, offx, 512, F32)
                XT.append(a)
            assert offx <= NBIG
            b_kc = S.buf("kcT")
            b_vc = S.buf("vc")
            XEb = S.bufs(6, "XE")
            XTb = S.bufs(4, "XT")
            rrx = [0, 0]

            def getXE():
                i = rrx[0] % 6
                rrx[0] += 1
                return XE[i], XEb[i]

            def getXT():
                i = rrx[1] % 4
                rrx[1] += 1
                return XT[i], XTb[i]

            qcT = YR[:, :].rearrange("p (c t) -> p c t", c=KC)
            b_qc = [S.buf("qc%d" % c) for c in range(KC)]
            ocT = HT[:, :].rearrange("p (c t) -> p c t", c=KC)
            b_oc = [S.buf("oc%d" % c) for c in range(KC)]

            def cross_begin(slot, sbuf_):
                S.alias([b_kc, b_vc] + XEb + XTb, [b_big])
                S.alias(b_qc, [YRb])
                norm_to_fm(lambda i: X[:, i, :], Xb, NT, g0 + GC_CROSS, hT, hTb)

            steps.append((None, cross_begin))

            def ckv_step(part):
                def load(slot, sbuf_):
                    wload(slot, sbuf_, [(lambda s: w3(s, 0, 512), dram_cols(w_ckv[li_], part * 512, 512))])

                def compute(slot, sbuf_):
                    wt = w3(slot, 0, 512)
                    if part < 2:
                        for jj in range(4):
                            j = part * 4 + jj
                            pp, pb = psbank()
                            for c in range(KC):
                                mm(pp[:, 0:NMEM], wt[:, c, jj * 128:(jj + 1) * 128], memT[:, c, :], c == 0, c == KC - 1, [sbuf_, b_memT], [pb])
                            act(kcT[:, j, :], pp[:, 0:NMEM], AF.Copy, [pb], [b_kc])
                    else:
                        for i in range(2):
                            pp, pb = psbank()
                            for c in range(KC):
                                mm(pp[:, :], memT[:, c, i * 128:(i + 1) * 128], wt[:, c, :], c == 0, c == KC - 1, [sbuf_, b_memT], [pb])
                            act(vc[:, i, (part - 2) * 512:(part - 1) * 512], pp[:, :], AF.Copy, [pb], [b_vc])

                return load, compute

            for part in range(4):
                steps.append(ckv_step(part))

            def cq_step(half):
                def load(slot, sbuf_):
                    wload(slot, sbuf_, [(lambda s: w3(s, 0, 512), dram_cols(w_cq[li_], half * 512, 512))])

                def compute(slot, sbuf_):
                    wt = w3(slot, 0, 512)
                    for jj in range(4):
                        j = half * 4 + jj
                        for tb in range(4):
                            tsl = slice(tb * 512, (tb + 1) * 512)
                            pp, pb = psbank()
                            for c in range(KC):
                                mm(pp[:, :], wt[:, c, jj * 128:(jj + 1) * 128], hT[:, c, tsl], c == 0, c == KC - 1, [sbuf_, hTb], [pb])
                            if (jj + tb) % 2 == 0:
                                act(qcT[:, j, tsl], pp[:, :], AF.Copy, [pb], [b_qc[j]])
                            else:
                                cp("dve", qcT[:, j, tsl], pp[:, :], [pb], [b_qc[j]])

                return load, compute

            for half in range(2):
                steps.append(cq_step(half))

            def cross_attn(slot, sbuf_):
                S.alias(b_oc, [hTb])
                for h in range(4):
                    for tb in range(4):
                        tsl = slice(tb * 512, (tb + 1) * 512)
                        pO = [psbank(), psbank()]
                        pR = psbank()
                        for mb in range(2):
                            pS, pSb = psbank()
                            for cc in range(2):
                                mm(pS[:, :], kcT[:, 2 * h + cc, mb * 128:(mb + 1) * 128], qcT[:, 2 * h + cc, tsl], cc == 0, cc == 1,
                                   [b_kc, b_qc[2 * h + cc]], [pSb])
                            E, Eb_ = getXE()
                            act(E, pS[:, :], AF.Exp, [pSb], [Eb_], scale=1.0 / 16)
                            for ee in range(2):
                                mm(pO[ee][0][:, :], vc[:, mb, h * 256 + ee * 128:h * 256 + (ee + 1) * 128], E, mb == 0, mb == 1,
                                   [b_vc, Eb_], [pO[ee][1]])
                            mm(pR[0][:, :], ones, E, mb == 0, mb == 1, [b_const, Eb_], [pR[1]])
                        r_, rb_ = getXT()
                        S.op("dve", lambda e, r_=r_, pR=pR: e.reciprocal(out=r_, in_=pR[0][:, :]), reads=[pR[1]], writes=[rb_])
                        for ee in range(2):
                            tt("dve", ocT[:, 2 * h + ee, tsl], pO[ee][0][:, :], r_, ALU.mult, [pO[ee][1], rb_], [b_oc[2 * h + ee]])

            steps.append((None, cross_attn))
            for half in range(2):
                steps.append(out_proj_step(w_co[li_], half, ocT, b_oc))

            def cross_end(slot, sbuf_):
                S.alias([b_big], [b_kc, b_vc] + XEb + XTb)
                S.alias([YRb], b_qc)
                S.alias([hTb], b_oc)

            steps.append((None, cross_end))
            if stop_after == (l, "cross"):
                return True

            hrows = YR[:, :].rearrange("p (i d) -> p i d", i=NT)
            b_hr = S.bufs(NT, "hrows")
            offm = 0
            oh12, offm = carve(BIG, offm, NT * 64, F32)
            oh12 = oh12.rearrange("p (i e) -> p i e", i=NT)
            mohb, offm = carve(BIG, offm, NT * 32)
            mohb = mohb.rearrange("p (i e) -> p i e", i=NT)
            wts, offm = carve(BIG, offm, NT * 2, F32)
            wts = wts.rearrange("p (i k) -> p i k", i=NT)
            desti, offm = carve(BIG, offm, NT * 2, I32)
            desti = desti.rearrange("p (i k) -> p i k", i=NT)
            rt, offm = carve(BIG, offm, 256, F32)
            cnt, offm = carve(BIG, offm, 32, F32)
            padA, offm = carve(BIG, offm, 32, F32)
            padB, offm = carve(BIG, offm, 32, F32)
            pstart, offm = carve(BIG, offm, 32, F32)
            pend, offm = carve(BIG, offm, 32, F32)
            eblk, offm = carve(BIG, offm, NBLK, F32)
            idxgu, offm = carve(BIG, offm, NBLK * 8, I32)
            idxd, offm = carve(BIG, offm, NBLK * 8, I32)
            idxgu = idxgu.rearrange("p (b c) -> p b c", b=NBLK)
            idxd = idxd.rearrange("p (b c) -> p b c", b=NBLK)
            idxgu_f, offt = carve(BIG, offm, NBLK * 8, F32)
            idxd_f, offt = carve(BIG, offt, NBLK * 8, F32)
            idxgu_f = idxgu_f.rearrange("p (b c) -> p b c", b=NBLK)
            idxd_f = idxd_f.rearrange("p (b c) -> p b c", b=NBLK)
            offm0 = offm
            b_rt = S.buf("rt")
            b_oh = S.buf("oh")
            b_route = S.buf("route")

            def moe_begin(slot, sbuf_):
                for r in range(NBLK):
                    for hh in range(2):
                        dma_sp("xbz", xb_d[r * 128:(r + 1) * 128, hh * 512:(hh + 1) * 512], ztile[:, :], reads=[b_zt], writes=[b_xb])
                S.alias([b_rt, b_oh, b_route], [b_big])
                S.alias(b_hr, [YRb])

            steps.append((None, moe_begin))

            def router_step():
                def load(slot, sbuf_):
                    wload(slot, sbuf_, [(lambda s: s[:, 0:KC * 36].rearrange("p (c n) -> p c n", c=KC),
                                         w_rt[li_].rearrange("(c p) n -> p c n", p=128))])

                def compute(slot, sbuf_):
                    wr = slot[:, 0:KC * 36].rearrange("p (c n) -> p c n", c=KC)
                    norm_to_fm(lambda i: X[:, i, :], Xb, NT, g0 + GC_FFN, hT, hTb,
                               keep_rows=lambda i: (hrows[:, i, :], b_hr[i]))
                    lg = rt[:, 0:36]
                    for i in range(NT):
                        pp, pb = psbank()
                        for c in range(KC):
                            mm(pp[:, 0:36], hT[:, c, i * 128:(i + 1) * 128], wr[:, c, :], c == 0, c == KC - 1, [sbuf_, hTb], [pb])
                        R_ = [b_rt]
                        act(lg, pp[:, 0:36], AF.Copy, [pb], R_)
                        mxg = rt[:, 40:41]
                        S.op("dve", lambda e, mxg=mxg: e.tensor_reduce(out=mxg, in_=rt[:, 0:4], axis=AX.X, op=ALU.max), reads=R_, writes=R_)
                        ohg = rt[:, 44:48]
                        ts("dve", ohg, rt[:, 0:4], mxg, ALU.is_equal, R_, R_)
                        nmx = rt[:, 41:42]
                        ts("dve", nmx, mxg, -1.0, ALU.mult, R_, R_)
                        sumg = rt[:, 42:43]
                        act(rt[:, 48:52], rt[:, 0:4], AF.Exp, R_, R_, bias=nmx, accum=sumg)
                        gw = rt[:, 43:44]
                        S.op("dve", lambda e, gw=gw, sumg=sumg: e.reciprocal(out=gw, in_=sumg), reads=R_, writes=R_)
                        pen = rt[:, 52:56]
                        ts("dve", pen, ohg, -1.0, ALU.add, R_, R_, s2=-NEG, op1=ALU.mult)
                        lem = rt[:, 64:96]
                        for g in range(4):
                            ts("dve", lem[:, g * 8:(g + 1) * 8], rt[:, 4 + g * 8:4 + (g + 1) * 8], pen[:, g:g + 1], ALU.add, R_, R_)
                        m1 = rt[:, 56:57]
                        S.op("dve", lambda e, m1=m1, lem=lem: e.tensor_reduce(out=m1, in_=lem, axis=AX.X, op=ALU.max), reads=R_, writes=R_)
                        ts("dve", oh12[:, i, 0:32], lem, m1, ALU.is_equal, R_, [b_oh])
                        lem2 = rt[:, 96:128]
                        stt("dve", lem2, oh12[:, i, 0:32], NEG, lem, ALU.mult, ALU.add, [b_oh, b_rt], R_)
                        m2 = rt[:, 57:58]
                        S.op("dve", lambda e, m2=m2, lem2=lem2: e.tensor_reduce(out=m2, in_=lem2, axis=AX.X, op=ALU.max), reads=R_, writes=R_)
                        ts("dve", oh12[:, i, 32:64], lem2, m2, ALU.is_equal, R_, [b_oh])
                        dd = rt[:, 58:59]
                        tt("dve", dd, m2, m1, ALU.subtract, R_, R_)
                        ed = rt[:, 59:60]
                        act(ed, dd, AF.Exp, R_, R_)
                        den = rt[:, 60:61]
                        ts("dve", den, ed, 1.0, ALU.add, R_, R_)
                        w1 = rt[:, 61:62]
                        S.op("dve", lambda e, w1=w1, den=den: e.reciprocal(out=w1, in_=den), reads=R_, writes=R_)
                        tt("dve", wts[:, i, 0:1], w1, gw, ALU.mult, R_, [b_oh])
                        w2 = rt[:, 62:63]
                        tt("dve", w2, ed, w1, ALU.mult, R_, R_)
                        tt("dve", wts[:, i, 1:2], w2, gw, ALU.mult, R_, [b_oh])
                        tt("dve", mohb[:, i, :], oh12[:, i, 0:32], oh12[:, i, 32:64], ALU.add, [b_oh], [b_oh])
                    pc_, pcb = psbank()
                    for i in range(NT):
                        mm(pc_[:, 0:32], ones, mohb[:, i, :], i == 0, i == NT - 1, [b_const, b_oh], [pcb])
                    Q_ = [b_route]
                    cp("dve", cnt, pc_[:, 0:32], [pcb], Q_)
                    ts("dve", padA, cnt, 1.0 / 128, ALU.mult, Q_, Q_, s2=127.0 / 128 - 0.49609375, op1=ALU.add)
                    cp("dve", padB.bitcast(I32), padA, Q_, Q_)
                    cp("dve", padA, padB.bitcast(I32), Q_, Q_)
                    ts("dve", padA, padA, 128.0, ALU.mult, Q_, Q_)
                    cp("dve", pstart, padA, Q_, Q_)
                    a, b_ = padA, padB
                    for s_ in (1, 2, 4, 8, 16):
                        cp("dve", b_[:, 0:s_], a[:, 0:s_], Q_, Q_)
                        tt("dve", b_[:, s_:32], a[:, s_:32], a[:, 0:32 - s_], ALU.add, Q_, Q_)
                        a, b_ = b_, a
                    cp("dve", pend, a, Q_, Q_)
                    tt("dve", pstart, pend, pstart, ALU.subtract, Q_, Q_)
                    for i in range(NT):
                        pp, pb = psbank()
                        mm(pp[:, 0:32], ustr, mohb[:, i, :], True, i == 0, [b_const, b_oh], [pb])
                        for j in range(i):
                            mm(pp[:, 0:32], ones, mohb[:, j, :], False, j == i - 1, [b_const, b_oh], [pb])
                        tmp = rt[:, 128:160]
                        tt("dve", tmp, pp[:, 0:32], pstart, ALU.add, [pb, b_route], [b_rt])
                        for k in range(2):
                            prod = rt[:, 160:192]
                            df = rt[:, 192 + k:193 + k]
                            tt("dve", prod, tmp, oh12[:, i, k * 32:(k + 1) * 32], ALU.mult, [b_rt, b_oh], [b_rt])
                            S.op("dve", lambda e, df=df, prod=prod: e.tensor_reduce(out=df, in_=prod, axis=AX.X, op=ALU.add), reads=[b_rt], writes=[b_rt])
                            cp("dve", desti[:, i, k:k + 1], df, [b_rt], [b_route])
                    S.op("dve", lambda e: e.memset(eblk, 0.0), writes=Q_)
                    for e_ in range(NEXP):
                        stt("dve", eblk, thr, pend[:, e_:e_ + 1], eblk, ALU.is_ge, ALU.add, [b_const] + Q_, Q_)
                    ts("dve", eblk, eblk, float(NEXP - 1), ALU.min, Q_, Q_)
                    for c in range(8):
                        ts("dve", idxgu_f[:, :, c], eblk, float(D), ALU.mult, Q_, Q_, s2=pcv[:, c:c + 1], op1=ALU.add)
                    for c in range(6):
                        ts("dve", idxd_f[:, :, c], eblk, float(DEXP), ALU.mult, Q_, Q_, s2=pcv[:, c:c + 1], op1=ALU.add)
                    cp("dve", idxgu[:, :, :], idxgu_f[:, :, :], Q_, Q_)
                    cp("dve", idxd[:, :, 0:6], idxd_f[:, :, 0:6], Q_, Q_)
                    for i in range(NT):
                        for k in range(2):
                            S.dma("pool", "scat", lambda e, i=i, k=k: e.indirect_dma_start(
                                out=xb_d[:, :], out_offset=bass.IndirectOffsetOnAxis(ap=desti[:, i, k:k + 1], axis=0),
                                in_=hrows[:, i, :], in_offset=None), reads=[b_hr[i], b_route], writes=[b_xb])

                return load, compute

            steps.append(router_step())

            Gw = [HT[:, g * 6144:(g + 1) * 6144].rearrange("p (c n) -> p c n", c=8) for g in range(2)]
            xg_t = [HT[:, 12288 + i * 1024:12288 + (i + 1) * 1024] for i in range(2)]
            xgT_t = [HT[:, 14336 + i * 1024:14336 + (i + 1) * 1024].rearrange("p (c t) -> p c t", c=8) for i in range(2)]
            Uw = [YR[:, g * 6144:(g + 1) * 6144].rearrange("p (c n) -> p c n", c=8) for g in range(2)]
            ybt = [YR[:, 12288 + i * 2048:12288 + (i + 1) * 2048].bitcast(F32) for i in range(2)]
            offd = offm0 + (offm0 % 2)
            Dw = []
            for g in range(2):
                a, offd = carve(BIG, offd, 6 * D)
                Dw.append(a.rearrange("p (c n) -> p c n", c=6))
            sgt = U_pad[:, 2:2 + 2 * DEXP].bitcast(F32)
            hmt = hn_t[0][:, 0:DEXP]
            hmT = [hn_t[1][:, 0:DEXP].rearrange("p (c t) -> p c t", c=6)]
            a, offd = carve(BIG, offd, DEXP)
            hmT.append(a.rearrange("p (c t) -> p c t", c=6))
            assert offd <= NBIG, offd
            b_G = S.bufs(2, "Gw")
            b_U = S.bufs(2, "Uw")
            b_D = S.bufs(2, "Dw")
            b_xg = S.bufs(2, "xg")
            b_xgT = S.bufs(2, "xgT")
            b_ybt = S.bufs(2, "ybt")
            b_sg = S.buf("sg")
            b_hm = S.buf("hm")
            b_hmT = S.bufs(2, "hmT")
            weg = w_eg[li_].rearrange("e d n -> (e d) n")
            weu = w_eu[li_].rearrange("e d n -> (e d) n")
            wed = w_ed[li_].rearrange("e d n -> (e d) n")

            def moe_blocks(slot, sbuf_):
                S.alias(b_G + b_xg + b_xgT, [hTb])
                S.alias(b_U + b_ybt, b_hr)
                S.alias(b_D + [b_hmT[1]], [b_big, b_route])
                S.alias([b_sg], [b_u])
                S.alias([b_hm, b_hmT[0]], hn_b)

                def issue_loads(b):
                    p = b % 2
                    dma_sp("xg%d" % p, xg_t[p], xb_d[b * 128:(b + 1) * 128, :], reads=[b_xb], writes=[b_xg[p]])
                    for c in range(8):
                        S.dma("pool", "G%d" % p, lambda e, c=c, p=p, b=b: e.indirect_dma_start(
                            out=Gw[p][:, c, :], out_offset=None, in_=weg,
                            in_offset=bass.IndirectOffsetOnAxis(ap=idxgu[:, b, c:c + 1], axis=0)), reads=[b_route], writes=[b_G[p]])
                    for c in range(8):
                        S.dma("pool", "U%d" % p, lambda e, c=c, p=p, b=b: e.indirect_dma_start(
                            out=Uw[p][:, c, :], out_offset=None, in_=weu,
                            in_offset=bass.IndirectOffsetOnAxis(ap=idxgu[:, b, c:c + 1], axis=0)), reads=[b_route], writes=[b_U[p]])
                    for c in range(6):
                        S.dma("pool", "D%d" % p, lambda e, c=c, p=p, b=b: e.indirect_dma_start(
                            out=Dw[p][:, c, :], out_offset=None, in_=wed,
                            in_offset=bass.IndirectOffsetOnAxis(ap=idxd[:, b, c:c + 1], axis=0)), reads=[b_route], writes=[b_D[p]])

                issue_loads(0)
                for b in range(NBLK):
                    p = b % 2
                    if b + 1 < NBLK:
                        issue_loads(b + 1)
                    pt, pb = psbank()
                    ptb = pt[:, :].bitcast(BF16).rearrange("p (c t) -> p c t", t=128)
                    for c in range(KC):
                        tr(ptb[:, c, :], xg_t[p][:, c * 128:(c + 1) * 128], [b_xg[p]], [pb])
                    for c in range(KC):
                        gc = gcols[:, g0 + GC_FFN + c:g0 + GC_FFN + c + 1]
                        if c % 2 == 0:
                            ts("dve", xgT_t[p][:, c, :], ptb[:, c, :], gc, ALU.mult, [pb, b_const], [b_xgT[p]])
                        else:
                            act(xgT_t[p][:, c, :], ptb[:, c, :], AF.Copy, [pb, b_const], [b_xgT[p]], scale=gc)
                    for (n0, nn) in ((0, 512), (512, 256)):
                        pg, pgb = psbank()
                        for c in range(KC):
                            mm(pg[:, 0:nn], xgT_t[p][:, c, :], Gw[p][:, c, n0:n0 + nn], c == 0, c == KC - 1, [b_xgT[p], b_G[p]], [pgb])
                        pu, pub = psbank()
                        for c in range(KC):
                            mm(pu[:, 0:nn], xgT_t[p][:, c, :], Uw[p][:, c, n0:n0 + nn], c == 0, c == KC - 1, [b_xgT[p], b_U[p]], [pub])
                        act(sgt[:, n0:n0 + nn], pg[:, 0:nn], AF.Silu, [pgb], [b_sg])
                        tt("dve", hmt[:, n0:n0 + nn], sgt[:, n0:n0 + nn], pu[:, 0:nn], ALU.mult, [b_sg, pub], [b_hm])
                    pt2, pb2 = psbank()
                    pt2b = pt2[:, :].bitcast(BF16).rearrange("p (c t) -> p c t", t=128)
                    for c in range(6):
                        tr(pt2b[:, c, :], hmt[:, c * 128:(c + 1) * 128], [b_hm], [pb2])
                    act(hmT[p][:, 0:3, :], pt2b[:, 0:3, :], AF.Copy, [pb2], [b_hmT[p]])
                    cp("dve", hmT[p][:, 3:6, :], pt2b[:, 3:6, :], [pb2], [b_hmT[p]])
                    for n in range(2):
                        py, pyb = psbank()
                        for c in range(6):
                            mm(py[:, :], hmT[p][:, c, :], Dw[p][:, c, n * 512:(n + 1) * 512], c == 0, c == 5, [b_hmT[p], b_D[p]], [pyb])
                        if n == 0:
                            act(ybt[p][:, 0:512], py[:, :], AF.Copy, [pyb], [b_ybt[p]])
                        else:
                            cp("dve", ybt[p][:, 512:1024], py[:, :], [pyb], [b_ybt[p]])
                    dma_sp("ybst", yb_d[b * 128:(b + 1) * 128, :], ybt[p], reads=[b_ybt[p]], writes=[b_yb])
                S.alias(b_gat, b_G + b_xg + b_xgT)
                for i in range(NT):
                    for k in range(2):
                        gi = (i * 2 + k) % 4
                        S.dma("pool", "gat%d" % gi, lambda e, i=i, k=k, gi=gi: e.indirect_dma_start(
                            out=gat_t[gi], out_offset=None, in_=yb_d[:, :],
                            in_offset=bass.IndirectOffsetOnAxis(ap=desti[:, i, k:k + 1], axis=0)), reads=[b_yb, b_route], writes=[b_gat[gi]])
                        stt("dve", X[:, i, :], gat_t[gi], wts[:, i, k:k + 1], X[:, i, :], ALU.mult, ALU.add, [b_gat[gi], b_oh, Xb[i]], [Xb[i]])

            gat_t = [HT[:, i * 2048:(i + 1) * 2048].bitcast(F32) for i in range(4)]
            b_gat = S.bufs(4, "gat")
            steps.append((None, moe_blocks))

            def moe_end(slot, sbuf_):
                S.alias([b_big], [b_rt, b_oh, b_route] + b_D + b_hmT)
                S.alias([b_u], [b_sg])
                S.alias(hn_b, [b_hm, b_hmT[0]])
                S.alias([YRb], b_U + b_ybt + b_hr)
                S.alias([hTb], b_gat + b_G + b_xg + b_xgT)

            steps.append((None, moe_end))
            if stop_after == (l, "moe"):
                return True
            return False

        ztile = sb("ztile", [128, 512], BF16)
        b_zt = S.buf("zt")
        S.op("pool", lambda e: e.memset(ztile[:, :], 0.0), writes=[b_zt])
        U_pad = sb("U_pad", [128, S_LEN + 4], BF16)
        b_u = S.buf("u")
        b_xb = S.buf("xb_d")
        b_yb = S.buf("yb_d")
        S.op("pool", lambda e: e.memset(U_pad[:, :], 0.0), writes=[b_u])

        stopped = False
        for li_, l in enumerate(layer_ids):
            stopped = layer_steps(li_, l)
            if stopped:
                break

        def epilogue(slot, sbuf_):
            if final and not stopped:
                gfin, _ = carve(BIG, 0, D, F32)
                otile = [carve(BIG, 2 * D + i * 2 * D, D, F32)[0] for i in range(2)]
                b_gf = S.buf("gfin")
                b_ot = S.bufs(2, "otile")
                S.alias([b_gf] + b_ot, [b_big])
                dma_sp("c2", gfin, gfin_d[:, :], writes=[b_gf])
                rstd_tiles(lambda i: X[:, i, :], Xb, NT, 1.0 / D)
                for i in range(NT):
                    p = i % 2
                    act(otile[p], X[:, i, :], AF.Copy, [Xb[i], b_rstd], [b_ot[p]], scale=rstd[:, i:i + 1])
                    tt("dve", otile[p], otile[p], gfin, ALU.mult, [b_ot[p], b_gf], [b_ot[p]])
                    dma_sp("out", out_d[i * 128:(i + 1) * 128, :], otile[p], reads=[b_ot[p]], writes=[b_out])
            else:
                for i in range(NT):
                    dma_sp("out", out_d[i * 128:(i + 1) * 128, :], X[:, i, :], reads=[Xb[i]], writes=[b_out])

        b_out = S.buf("out")
        if max_steps is not None:
            del steps[max_steps:]
        steps.append((None, epilogue))

        wsteps = [k for k, (ld, _) in enumerate(steps) if ld is not None]
        issued = 0
        done = 0
        for k, (ld, cpf) in enumerate(steps):
            while issued < len(wsteps) and issued < done + NSLOT:
                sl = issued % NSLOT
                steps[wsteps[issued]][0](WS[sl], WSb[sl])
                issued += 1
            if ld is not None:
                sl = done % NSLOT
                cpf(WS[sl], WSb[sl])
                done += 1
            else:
                cpf(None, None)
        S.wait_all("sp", [b_out])
        if os.environ.get('WAITCS'):
            for b_ in dbg_bufs:
                S.wait_all("sp", [b_])
        dl = S.simulate()
        if dl is not None:
            raise RuntimeError("sync deadlock: %r" % (dl,))
        S.emit()
    return nc, S.ninst


def _consts():
    ident = np.eye(128, dtype=np.float32)
    ones = np.ones((128, 128), np.float32)
    rrot = np.zeros((128, 128), np.float32)
    for c in range(2):
        for i in range(8):
            rrot[c * 64 + 8 + i, c * 64 + i] = -1.0
            rrot[c * 64 + i, c * 64 + 8 + i] = 1.0
    ustr = np.triu(np.ones((128, 128), np.float32), 1)
    cmat = np.concatenate([ident, ones, rrot, ustr], axis=1)
    inv = np.float32(500000.0) ** (-np.arange(0, 16, 2, dtype=np.float32) / np.float32(16))
    ang = np.arange(S_LEN, dtype=np.float32)[:, None] * inv[None, :]
    cs, sn = np.cos(ang).astype(np.float32), np.sin(ang).astype(np.float32)
    cosT = np.ones((128, S_LEN), np.float32)
    sinT = np.zeros((128, S_LEN), np.float32)
    for c in range(2):
        for i in range(16):
            cosT[c * 64 + i] = cs[:, i % 8]
            sinT[c * 64 + i] = sn[:, i % 8]
    cossin = np.concatenate([cosT, sinT], axis=1)
    thr = np.broadcast_to((np.arange(64, dtype=np.float32) * 128.0)[None, :], (128, 64))
    pc = np.arange(8, dtype=np.float32)[None, :] * 128.0 + np.arange(128, dtype=np.float32)[:, None]
    cmisc = np.ascontiguousarray(np.concatenate([thr, pc], axis=1))
    return np.ascontiguousarray(cmat.astype(ml_dtypes.bfloat16)), np.ascontiguousarray(cossin.astype(ml_dtypes.bfloat16)), cmisc


def _pack_small(inp, layer_ids):
    cols = []
    for l in layer_ids:
        def colmaj(v):
            return np.asarray(v, np.float32).reshape(8, 128).T
        cols.append(colmaj(inp["norm_mix"][l]))
        cols.append(colmaj(inp["norm_cross"][l]))
        cols.append(colmaj(inp["norm_ffn"][l]))
        for k in range(3):
            cols.append(colmaj(inp["conv_w"][l][k]))
        cols.append(np.asarray(inp["subln"][l], np.float32).reshape(128, 1))
    cols.append(np.asarray(inp["norm_mem"], np.float32).reshape(8, 128).T)
    gcols = np.ascontiguousarray(np.concatenate(cols, axis=1))
    lam = []
    for l in layer_ids:
        lam.append(np.concatenate([inp["lambda_q1"][l], inp["lambda_k1"][l], inp["lambda_q2"][l], inp["lambda_k2"][l]]))
    lamv = np.ascontiguousarray(np.broadcast_to(np.concatenate(lam)[None, :].astype(np.float32), (128, len(layer_ids) * 256)))
    gfin = np.ascontiguousarray(np.broadcast_to(np.asarray(inp["norm_final"], np.float32)[None, :], (128, D)))
    return gcols, lamv, gfin


_PROG_CACHE = {}


def _run(inp, x_shards, layer_ids, final, stop_after=None, cores=None, max_steps=None):
    key = (tuple(layer_ids), final, stop_after, max_steps)
    if key not in _PROG_CACHE:
        _PROG_CACHE[key] = build(list(layer_ids), final, stop_after, max_steps)[0]
    nc = _PROG_CACHE[key]
    cmat, cossin, cmisc = _consts()
    gcols, lamv, gfin = _pack_small(inp, layer_ids)
    ls = list(layer_ids)
    sl = slice(ls[0], ls[-1] + 1)
    w_rt = np.ascontiguousarray(np.concatenate([inp["w_router_group"][sl], inp["w_router_expert"][sl]], axis=-1))
    shared = {
        "w_in": np.ascontiguousarray(inp["w_in"][sl]), "w_branch": np.ascontiguousarray(inp["w_branch"][sl]),
        "w_o": np.ascontiguousarray(inp["w_o"][sl]), "w_cq": np.ascontiguousarray(inp["w_cq"][sl]),
        "w_ckv": np.ascontiguousarray(inp["w_ckv"][sl]), "w_co": np.ascontiguousarray(inp["w_co"][sl]),
        "w_rt": w_rt, "w_exp_gate": np.ascontiguousarray(inp["w_exp_gate"][sl]),
        "w_exp_up": np.ascontiguousarray(inp["w_exp_up"][sl]), "w_exp_down": np.ascontiguousarray(inp["w_exp_down"][sl]),
        "gcols": gcols, "lamv": lamv, "gfinal": gfin, "cmat": cmat, "cossin": cossin, "cmisc": cmisc,
    }
    cores = list(range(len(x_shards))) if cores is None else cores
    in_maps = []
    for b in range(len(x_shards)):
        m = dict(shared)
        m["x"] = np.ascontiguousarray(x_shards[b])
        m["mem"] = np.ascontiguousarray(inp["mem"][b])
        in_maps.append(m)
    res = run_bass_kernel_spmd(nc, in_maps, core_ids=cores)
    return [r["out"] for r in res.results]


MODE = "unfused"


def kernel(**inputs):
    inp = {k: np.asarray(v) for k, v in inputs.items()}
    xs = [inp["x"][b] for b in range(inp["x"].shape[0])]
    if MODE == "fused":
        outs = _run(inp, xs, list(range(DEPTH)), True)
    else:
        for l in range(DEPTH):
            xs = _run(inp, xs, [l], l == DEPTH - 1)
        outs = xs
    return np.stack(outs, axis=0).astype(np.float32)
```

```python
import math
import os
import numpy as np
import ml_dtypes
HEADCUT = int(os.environ.get('HEADCUT', '99'))
from contextlib import ExitStack
import concourse.bass as bass
import concourse.mybir as mybir
from concourse.bass_utils import run_bass_kernel_spmd

F32 = mybir.dt.float32
BF16 = mybir.dt.bfloat16
I32 = mybir.dt.int32
ALU = mybir.AluOpType
AF = mybir.ActivationFunctionType
AX = mybir.AxisListType

D = 1024
S_LEN = 2048
NT = 16
KC = 8
NMEM = 256
DEPTH = 4
NEXP = 32
DEXP = 768
NBLK = 64
EPS = 1e-6
NEG = -1.0e30


class Buf:
    __slots__ = ("name", "w", "r", "excl")

    def __init__(self, name):
        self.name = name
        self.w = None
        self.r = {}
        self.excl = False


class Sched:
    ENG = ("pe", "act", "dve", "pool", "sp")

    def __init__(self, nc, stack):
        self.nc = nc
        self.stack = stack
        self.rec = {e: [] for e in self.ENG}
        self.sem = {e: stack.enter_context(nc.semaphore("s_" + e)) for e in self.ENG}
        self.cnt = {e: 0 for e in self.ENG}
        self.seen = {e: {} for e in self.ENG}
        self.dsem = {}
        self.dcnt = {}
        self.nbuf = 0
        self.ninst = 0

    def buf(self, name=None):
        self.nbuf += 1
        return Buf(name or "b%d" % self.nbuf)

    def bufs(self, n, name="b"):
        return [self.buf("%s%d" % (name, i)) for i in range(n)]

    def _deps(self, eng, reads, writes, skipkey=None):
        deps = {}

        def add(k, v):
            if k == skipkey:
                return
            if deps.get(k, 0) < v:
                deps[k] = v

        for b in reads:
            if b.w is not None:
                add(*b.w)
            if b.excl:
                for k, v in b.r.items():
                    if k != ("e", eng):
                        add(k, v)
        for b in writes:
            if b.w is not None:
                add(*b.w)
            for k, v in b.r.items():
                add(k, v)
        out = []
        seen = self.seen[eng]
        for k, v in deps.items():
            if eng == "pe" and k == ("e", "pe"):
                continue
            if k[0] == "d":
                v = self.dcnt[k[1]]
            if seen.get(k, 0) >= v:
                continue
            seen[k] = v
            out.append((k, v))
        return out

    def _post(self, ev, reads, writes):
        k, v = ev
        for b in reads:
            if b.r.get(k, 0) < v:
                b.r[k] = v
        for b in writes:
            b.w = ev
            b.r = {}

    def op(self, eng, fn, reads=(), writes=()):
        waits = self._deps(eng, reads, writes)
        self.cnt[eng] += 1
        ev = (("e", eng), self.cnt[eng])
        self.rec[eng].append(("op", waits, fn, self.cnt[eng]))
        self._post(ev, reads, writes)
        self.ninst += 1
        return ev

    def dma(self, eng, key, fn, reads=(), writes=()):
        if key not in self.dsem:
            self.dsem[key] = self.stack.enter_context(self.nc.semaphore("d_" + str(key)))
            self.dcnt[key] = 0
        waits = self._deps(eng, reads, writes, skipkey=("d", key))
        self.dcnt[key] += 16
        ev = (("d", key), self.dcnt[key])
        self.rec[eng].append(("dma", waits, fn, key))
        self._post(ev, reads, writes)
        self.ninst += 1
        return ev

    def alias(self, new_bufs, old_bufs):
        for nb in new_bufs:
            for ob in old_bufs:
                if ob.w is not None:
                    k, v = ob.w
                    if nb.r.get(k, 0) < v:
                        nb.r[k] = v
                for k, v in ob.r.items():
                    if nb.r.get(k, 0) < v:
                        nb.r[k] = v

    def wait_all(self, eng, bufs):
        waits = self._deps(eng, bufs, bufs)
        self.rec[eng].append(("wait", waits, None, None))

    def finalize(self):
        waited = {e: set() for e in self.ENG}
        for eng in self.ENG:
            for kind, waits, fn, x in self.rec[eng]:
                for k, v in waits:
                    if k[0] == "e":
                        waited[k[1]].add(v)
        rank = {e: {v: i + 1 for i, v in enumerate(sorted(waited[e]))} for e in self.ENG}
        self.prog = {}
        for eng in self.ENG:
            pl = []
            for kind, waits, fn, x in self.rec[eng]:
                rw = []
                for k, v in waits:
                    if k[0] == "e":
                        rw.append((self.sem[k[1]], rank[k[1]][v]))
                    else:
                        rw.append((self.dsem[k[1]], v))
                if kind == "op":
                    pl.append((rw, fn, self.sem[eng] if x in waited[eng] else None, 1))
                elif kind == "dma":
                    pl.append((rw, fn, self.dsem[x], 16))
                else:
                    pl.append((rw, None, None, 0))
            self.prog[eng] = pl

    def simulate(self):
        val = {}
        pc = {e: 0 for e in self.ENG}
        prog = self.prog
        while True:
            progressed = False
            for e in self.ENG:
                while pc[e] < len(prog[e]):
                    waits, fn, sem, inc = prog[e][pc[e]]
                    if all(val.get(id(s_), 0) >= v for s_, v in waits):
                        if sem is not None:
                            val[id(sem)] = val.get(id(sem), 0) + inc
                        pc[e] += 1
                        progressed = True
                    else:
                        break
            if all(pc[e] == len(prog[e]) for e in self.ENG):
                return None
            if not progressed:
                return {e: (pc[e], len(prog[e])) for e in self.ENG}

    def emit(self):
        nc = self.nc
        prog = self.prog

        def run(e, pl):
            for waits, fn, sem, inc in pl:
                for s_, v in waits:
                    e.wait_ge(s_, v)
                if fn is not None:
                    ins = fn(e)
                    if sem is not None:
                        ins.then_inc(sem, inc)

        with nc.Block() as block:
            @block.tensor
            def _(e):
                run(e, prog["pe"])

            @block.scalar
            def _(e):
                run(e, prog["act"])

            @block.vector
            def _(e):
                run(e, prog["dve"])

            @block.gpsimd
            def _(e):
                run(e, prog["pool"])

            @block.sync
            def _(e):
                run(e, prog["sp"])


def lambda_init(l):
    return 0.8 - 0.6 * math.exp(-0.3 * l)


GC_MIX, GC_CROSS, GC_FFN, GC_CONV, GC_SUBLN, GC_PER = 0, 8, 16, 24, 48, 49


def build(layer_ids, final, stop_after=None, max_steps=None):
    L = len(layer_ids)
    nc = bass.Bass("TRN2", target_bir_lowering=False)

    def din(name, shape, dt=F32):
        return nc.dram_tensor(name, shape, dt, kind="ExternalInput").ap()

    x_in = din("x", [S_LEN, D])
    mem_in = din("mem", [NMEM, D])
    w_in = din("w_in", [L, D, 8192])
    w_br = din("w_branch", [L, 2, D, D])
    w_o = din("w_o", [L, D, D])
    w_cq = din("w_cq", [L, D, D])
    w_ckv = din("w_ckv", [L, D, 2 * D])
    w_co = din("w_co", [L, D, D])
    w_rt = din("w_rt", [L, D, 36])
    w_eg = din("w_exp_gate", [L, NEXP, D, DEXP])
    w_eu = din("w_exp_up", [L, NEXP, D, DEXP])
    w_ed = din("w_exp_down", [L, NEXP, DEXP, D])
    gcols_d = din("gcols", [128, L * GC_PER + 8])
    lamv_d = din("lamv", [128, L * 256])
    gfin_d = din("gfinal", [128, D])
    cmat_d = din("cmat", [128, 4 * 128], BF16)
    cossin_d = din("cossin", [128, 2 * S_LEN], BF16)
    cmisc_d = din("cmisc", [128, 64 + 8])
    out_d = nc.dram_tensor("out", [S_LEN, D], F32, kind="ExternalOutput").ap()
    xb_d = nc.dram_tensor("xb_scr", [NBLK * 128, D], BF16, kind="Internal").ap()
    yb_d = nc.dram_tensor("yb_scr", [NBLK * 128, D], F32, kind="Internal").ap()

    st = ExitStack()
    with st:
        S = Sched(nc, st)

        def sb(name, shape, dt):
            return st.enter_context(nc.sbuf_tensor("sb_" + name, shape, dt))

        X = sb("X", [128, NT, D], F32)
        HT = sb("HT", [128, KC * S_LEN], BF16)
        YR = sb("YR", [128, KC * S_LEN], BF16)
        NBIG = 19 * 1024
        BIG = sb("BIG", [128, NBIG], BF16)
        NSLOT = 3
        WS = [sb("WS%d" % i, [128, 4096], BF16) for i in range(NSLOT)]
        cmat = sb("cmat", [128, 4 * 128], BF16)
        gcols = sb("gcols", [128, L * GC_PER + 8], F32)
        cmisc = sb("cmisc", [128, 72], F32)
        memT = sb("memT", [128, KC, NMEM], BF16)
        stat = sb("stat", [128, 64], F32)
        lamc = sb("lamc", [128, 2 * L], F32)
        sublnS = sb("sublnS", [128, L], F32)
        PS = [st.enter_context(nc.psum_tensor("ps%d" % i, [128, 512], F32)) for i in range(8)]
        PSB = S.bufs(8, "ps")
        for b_ in PSB:
            b_.excl = True

        ident = cmat[:, 0:128]
        ones = cmat[:, 128:256]
        rrot = cmat[:, 256:384]
        ustr = cmat[:, 384:512]
        thr = cmisc[:, 0:64]
        pcv = cmisc[:, 64:72]

        Xb = S.bufs(NT, "X")
        hTb = S.buf("hT")
        YRb = S.buf("YR")
        WSb = S.bufs(NSLOT, "WS")
        b_const = S.buf("const")
        b_memT = S.buf("memT")
        b_stat = S.buf("stat")

        hT = HT[:, :].rearrange("p (c t) -> p c t", c=KC)

        def carve(region, off, n, dt=BF16):
            if dt == BF16:
                return region[:, off:off + n], off + n
            assert off % 2 == 0
            return region[:, off:off + 2 * n].bitcast(dt), off + 2 * n

        psrr = [0]

        def psbank():
            i = psrr[0] % 8
            psrr[0] += 1
            return PS[i], PSB[i]

        def dma_sp(key, out, in_, reads=(), writes=()):
            return S.dma("sp", key, lambda e: e.dma_start(out=out, in_=in_), reads=reads, writes=writes)

        def dma_pool(key, out, in_, reads=(), writes=()):
            return S.dma("pool", key, lambda e: e.dma_start(out=out, in_=in_), reads=reads, writes=writes)

        def mm(out, lhsT, rhs, start, stop, reads, writes):
            S.op("pe", lambda e: e.matmul(out, lhsT=lhsT, rhs=rhs, start=start, stop=stop),
                 reads=reads, writes=writes)

        def tr(out, in_, reads, writes):
            S.op("pe", lambda e: e.transpose(out=out, in_=in_, identity=ident), reads=list(reads) + [b_const], writes=writes)

        def act(out, in_, func, reads, writes, scale=1.0, bias=None, accum=None):
            kw = {}
            if bias is not None:
                kw["bias"] = bias
            if accum is not None:
                kw["accum_out"] = accum
            S.op("act", lambda e: e.activation(out=out, in_=in_, func=func, scale=scale, **kw), reads=reads, writes=writes)

        def tt(eng, out, in0, in1, op, reads, writes):
            S.op(eng, lambda e: e.tensor_tensor(out=out, in0=in0, in1=in1, op=op), reads=reads, writes=writes)

        def ts(eng, out, in0, s1, op0, reads, writes, s2=None, op1=None, accum=None):
            kw = {}
            if accum is not None:
                kw["accum_out"] = accum
            if op1 is None:
                S.op(eng, lambda e: e.tensor_scalar(out=out, in0=in0, scalar1=s1, scalar2=None, op0=op0, **kw), reads=reads, writes=writes)
            else:
                S.op(eng, lambda e: e.tensor_scalar(out=out, in0=in0, scalar1=s1, scalar2=s2, op0=op0, op1=op1, **kw), reads=reads, writes=writes)

        def stt(eng, out, in0, scalar, in1, op0, op1, reads, writes):
            S.op(eng, lambda e: e.scalar_tensor_tensor(out=out, in0=in0, scalar=scalar, in1=in1, op0=op0, op1=op1), reads=reads, writes=writes)

        def cp(eng, out, in_, reads, writes):
            S.op(eng, lambda e: e.tensor_copy(out=out, in_=in_), reads=reads, writes=writes)

        dma_sp("c0", cmat[:, :], cmat_d[:, :], writes=[b_const])
        dma_sp("c1", gcols[:, :], gcols_d[:, :], writes=[b_const])
        dma_sp("c1", cmisc[:, :], cmisc_d[:, :], writes=[b_const])
        for i in range(NT):
            dma_sp("xin", X[:, i, :], x_in[i * 128:(i + 1) * 128, :], writes=[Xb[i]])

        b_big = S.buf("bigscratch")
        lamv, _ = carve(BIG, 0, L * 256, F32)
        ltmp, _ = carve(BIG, 2 * L * 256, 64, F32)
        dma_sp("c3", lamv, lamv_d[:, :], writes=[b_big])
        for li_, l in enumerate(layer_ids):
            base = li_ * 256
            for j in range(2):
                S.op("dve", lambda e, base=base, j=j: e.tensor_tensor(out=ltmp, in0=lamv[:, base + j * 128:base + j * 128 + 64],
                                                                      in1=lamv[:, base + j * 128 + 64:base + j * 128 + 128], op=ALU.mult),
                     reads=[b_big], writes=[b_big])
                S.op("dve", lambda e, j=j: e.tensor_reduce(out=stat[:, j:j + 1], in_=ltmp, axis=AX.X, op=ALU.add),
                     reads=[b_big], writes=[b_stat])
            act(stat[:, 2:4], stat[:, 0:2], AF.Exp, [b_stat], [b_stat])
            tt("dve", stat[:, 4:5], stat[:, 3:4], stat[:, 2:3], ALU.subtract, [b_stat], [b_stat])
            ts("dve", lamc[:, li_:li_ + 1], stat[:, 4:5], -lambda_init(l), ALU.add, [b_stat], [b_const])
            ts("dve", sublnS[:, li_:li_ + 1], gcols[:, li_ * GC_PER + GC_SUBLN:li_ * GC_PER + GC_SUBLN + 1],
               1.0 - lambda_init(l), ALU.mult, [b_const], [b_const])

        ssq = sb("ssq", [128, NT], F32)
        rstd = sb("rstd", [128, NT], F32)
        b_ssq = S.buf("ssq")
        b_rstd = S.buf("rstd")
        hn_t = [sb("hn%d" % i, [128, D], BF16) for i in range(2)]
        hn_b = S.bufs(2, "hn")

        def rstd_tiles(src_ap_fn, src_bufs, ntiles, inv_n):
            for i in range(ntiles):
                act(hn_t[0][:, :], src_ap_fn(i), AF.Square, [src_bufs[i]], [hn_b[0], b_ssq], accum=ssq[:, i:i + 1])
            ts("dve", rstd[:, 0:ntiles], ssq[:, 0:ntiles], inv_n, ALU.mult, [b_ssq], [b_rstd], s2=EPS, op1=ALU.add)
            act(rstd[:, 0:ntiles], rstd[:, 0:ntiles], AF.Ln, [b_rstd], [b_rstd])
            act(rstd[:, 0:ntiles], rstd[:, 0:ntiles], AF.Exp, [b_rstd], [b_rstd], scale=-0.5)

        def norm_to_fm(src_ap_fn, src_bufs, ntiles, gcol0, dstT, dst_buf, keep_rows=None):
            rstd_tiles(src_ap_fn, src_bufs, ntiles, 1.0 / D)
            for i in range(ntiles):
                if keep_rows is None:
                    hn, hb = hn_t[i % 2], hn_b[i % 2]
                    hn_ap = hn[:, :]
                else:
                    hn_ap, hb = keep_rows(i)
                act(hn_ap, src_ap_fn(i), AF.Copy, [src_bufs[i], b_rstd], [hb], scale=rstd[:, i:i + 1])
                pt, pb = psbank()
                ptb = pt[:, :].bitcast(BF16).rearrange("p (c t) -> p c t", t=128)
                for c in range(KC):
                    tr(ptb[:, c, :], hn_ap[:, c * 128:(c + 1) * 128], [hb], [pb])
                for c in range(KC):
                    eng = "dve" if c % 2 == 0 else "act"
                    if eng == "dve":
                        ts("dve", dstT[:, c, i * 128:(i + 1) * 128], ptb[:, c, :], gcols[:, gcol0 + c:gcol0 + c + 1], ALU.mult,
                           [pb, b_const], [dst_buf])
                    else:
                        act(dstT[:, c, i * 128:(i + 1) * 128], ptb[:, c, :], AF.Copy, [pb, b_const], [dst_buf],
                            scale=gcols[:, gcol0 + c:gcol0 + c + 1])

        memrows, _ = carve(BIG, 4096, 2 * D, F32)
        memrows = memrows.rearrange("p (i d) -> p i d", i=2)
        b_memrows = S.bufs(2, "memrows")
        for i in range(2):
            dma_sp("c2", memrows[:, i, :], mem_in[i * 128:(i + 1) * 128, :], writes=[b_memrows[i]])
        norm_to_fm(lambda i: memrows[:, i, :], b_memrows, 2, L * GC_PER, memT, b_memT)

        dbg_bufs = []
        steps = []

        def wload(slot, sbuf_, pieces):
            key = "w%d" % [i for i in range(NSLOT) if WS[i] is slot][0]
            for dst, src in pieces:
                dma_pool(key, dst(slot), src, writes=[sbuf_])

        def w3(slot, off, ncols):
            return slot[:, off:off + KC * ncols].rearrange("p (c n) -> p c n", c=KC)

        def dram_cols(w2d, c0, ncols):
            return w2d[:, c0:c0 + ncols].rearrange("(c p) n -> p c n", p=128)

        def layer_steps(li_, l):
            g0 = li_ * GC_PER
            win_l = w_in[li_]
            off = 0
            cosb, off = carve(BIG, off, S_LEN)
            sinb, off = carve(BIG, off, S_LEN)
            qT, off = carve(BIG, off, S_LEN)
            kT, off = carve(BIG, off, S_LEN)
            vtm, off = carve(BIG, off, NT * 128)
            vtm = vtm.rearrange("p (i e) -> p i e", i=NT)
            NE = 6
            Et = []
            for _ in range(NE):
                a, off = carve(BIG, off, 512)
                Et.append(a)
            NTMP = 6
            Tm = []
            for _ in range(NTMP):
                a, off = carve(BIG, off, 512, F32)
                Tm.append(a)
            assert off <= NBIG, off
            b_cs = S.buf("cossin")
            dbg_bufs.append(b_cs)
            b_q = S.buf("qT")
            b_k = S.buf("kT")
            b_v = S.buf("v")
            Eb = S.bufs(NE, "E")
            Tb = S.bufs(NTMP, "T")
            rrE = [0]
            rrT = [0]

            def getE():
                i = rrE[0] % NE
                rrE[0] += 1
                return Et[i], Eb[i]

            def getT():
                i = rrT[0] % NTMP
                rrT[0] += 1
                return Tm[i], Tb[i]

            yT = YR[:, :].rearrange("p (c t) -> p c t", c=KC)
            b_y = [S.buf("y%d" % c) for c in range(KC)]

            def mixer_begin(slot, sbuf_):
                S.alias([b_cs, b_q, b_k, b_v] + Eb + Tb, [b_big])
                S.alias(b_y, [YRb])
                dma_sp("cs", cosb, cossin_d[:, 0:S_LEN], writes=[b_cs])
                dma_sp("cs", sinb, cossin_d[:, S_LEN:2 * S_LEN], writes=[b_cs])
                if os.environ.get('CSTOUCH'):
                    cp("dve", stat[:, 60:61], cosb[:, 0:1], [b_cs], [b_stat])
                norm_to_fm(lambda i: X[:, i, :], Xb, NT, g0 + GC_MIX, hT, hTb)

            steps.append((None, mixer_begin))

            def head_step(h):
                def load(slot, sbuf_):
                    wload(slot, sbuf_, [
                        (lambda s: w3(s, 0, 128), dram_cols(win_l, h * 128, 128)),
                        (lambda s: w3(s, 1024, 128), dram_cols(win_l, 1024 + h * 128, 128)),
                        (lambda s: w3(s, 2048, 128), dram_cols(win_l, 2048 + h * 128, 128)),
                    ])

                def compute(slot, sbuf_):
                    wq = w3(slot, 0, 128)
                    wk = w3(slot, 1024, 128)
                    wv = w3(slot, 2048, 128)
                    for (wt, dst, db) in ((wq, qT, b_q), (wk, kT, b_k)):
                        for tb in range(4):
                            tsl = slice(tb * 512, (tb + 1) * 512)
                            pp, pb = psbank()
                            for c in range(KC):
                                mm(pp[:, :], wt[:, c, :], hT[:, c, tsl], c == 0, c == KC - 1, [sbuf_, hTb], [pb])
                            qb, qbb = getE()
                            act(qb, pp[:, :], AF.Copy, [pb], [qbb])
                            if HEADCUT == 0:
                                continue
                            ROT = int(os.environ.get('ROT', '9'))
                            p2, p2b = psbank()
                            P2V = os.environ.get('P2V', '')
                            if P2V == 'ident':
                                mm(p2[:, :], ident, qb, True, True, [b_const, qbb], [p2b])
                            elif P2V == 'rhs':
                                mm(p2[:, :], rrot, hT[:, 0, tsl], True, True, [b_const, hTb], [p2b])
                            elif P2V == 'evac':
                                mm(p2[:, :], rrot, qb, True, True, [b_const, qbb], [p2b])
                                t9, t9b = getT()
                                cp("dve", t9, p2[:, :], [p2b], [t9b])
                            else:
                                mm(p2[:, :], rrot, qb, True, True, [b_const, qbb], [p2b])
                            if ROT < 2:
                                continue
                            t1, t1b = getT()
                            if os.environ.get('ROTV') == 'sb':
                                tt("dve", t1, qb, cosb[:, tsl], ALU.mult, [qbb, b_cs], [t1b])
                            elif os.environ.get('ROTV') == 'hT':
                                tt("dve", t1, pp[:, :], hT[:, 0, tsl], ALU.mult, [pb, hTb], [t1b])
                            elif os.environ.get('ROTV') == 'nocs':
                                tt("dve", t1, pp[:, :], qb, ALU.mult, [pb, qbb], [t1b])
                            else:
                                tt("dve", t1, pp[:, :], cosb[:, tsl], ALU.mult, [pb, b_cs], [t1b])
                            if ROT < 3:
                                continue
                            t2, t2b = getT()
                            tt("dve", t2, p2[:, :], sinb[:, tsl], ALU.mult, [p2b, b_cs], [t2b])
                            if ROT < 4:
                                continue
                            tt("pool", dst[:, tsl], t1, t2, ALU.add, [t1b, t2b], [db])
                    if HEADCUT < 2:
                        return
                    for i4 in range(4):
                        pp, pb = psbank()
                        for ii in range(4):
                            i = i4 * 4 + ii
                            for c in range(KC):
                                mm(pp[:, ii * 128:(ii + 1) * 128], hT[:, c, i * 128:(i + 1) * 128], wv[:, c, :],
                                   c == 0, c == KC - 1, [sbuf_, hTb], [pb])
                        act(vtm[:, i4 * 4:(i4 + 1) * 4, :], pp[:, :].rearrange("p (i e) -> p i e", i=4), AF.Copy, [pb], [b_v])
                    if HEADCUT < 3:
                        return
                    for qb_ in range(4 if HEADCUT > 4 else 1):
                        qsl = slice(qb_ * 512, (qb_ + 1) * 512)
                        pO = [(PS[4], PSB[4]), (PS[5], PSB[5])]
                        pR = [(PS[6], PSB[6]), (PS[7], PSB[7])]
                        for kb in range(NT):
                            for c in range(2):
                                pS, pSb = psS[(kb * 2 + c) % 4]
                                mm(pS[:, :], kT[c * 64:(c + 1) * 64, kb * 128:(kb + 1) * 128], qT[c * 64:(c + 1) * 64, qsl],
                                   True, True, [b_k, b_q], [pSb])
                                E, Eb_ = getE()
                                act(E, pS[:, :], AF.Exp, [pSb], [Eb_], scale=0.125)
                                mm(pO[c][0][:, :], vtm[:, kb, :], E, kb == 0, kb == NT - 1, [b_v, Eb_], [pO[c][1]])
                                mm(pR[c][0][:, :], ones, E, kb == 0, kb == NT - 1, [b_const, Eb_], [pR[c][1]])
                        if HEADCUT < 4:
                            continue
                        ri = []
                        for c in range(2):
                            r_, rb_ = getT()
                            S.op("dve", lambda e, r_=r_, c=c: e.reciprocal(out=r_, in_=pR[c][0][:, :]), reads=[pR[c][1]], writes=[rb_])
                            ri.append((r_, rb_))
                        t0, t0b = getT()
                        tt("dve", t0, pO[0][0][:, :], ri[0][0], ALU.mult, [pO[0][1], ri[0][1]], [t0b])
                        t1, t1b = getT()
                        tt("dve", t1, pO[1][0][:, :], ri[1][0], ALU.mult, [pO[1][1], ri[1][1]], [t1b])
                        o_, ob_ = getT()
                        stt("dve", o_, t1, lamc[:, li_:li_ + 1], t0, ALU.mult, ALU.add, [t1b, t0b, b_const], [ob_])
                        sq, sqb = getE()
                        tt("pool", sq, o_, o_, ALU.mult, [ob_], [sqb])
                        pq, pqb = psS[0]
                        mm(pq[:, :], ones, sq, True, True, [b_const, sqb], [pqb])
                        rs, rsb = getT()
                        ts("dve", rs, pq[:, :], 1.0 / 128, ALU.mult, [pqb], [rsb], s2=EPS, op1=ALU.add)
                        act(rs, rs, AF.Ln, [rsb], [rsb])
                        act(rs, rs, AF.Exp, [rsb], [rsb], scale=-0.5)
                        stt("dve", yT[:, h, qsl], o_, sublnS[:, li_:li_ + 1], rs, ALU.mult, ALU.mult, [ob_, rsb, b_const], [b_y[h]])

                return load, compute

            psS = [(PS[i], PSB[i]) for i in range(4)]

            for h in range(8):
                steps.append(head_step(h))

            offc = 0
            Mreg, offc = carve(BIG, offc, KC * S_LEN)
            Mv = Mreg.rearrange("p (c t) -> p c t", c=KC)
            CT = []
            for _ in range(3):
                a, offc = carve(BIG, offc, 512, F32)
                CT.append(a)
            assert offc <= NBIG
            b_M = [S.buf("M%d" % c) for c in range(KC)]
            CTb = S.bufs(3, "CT")
            rrC = [0]

            def getC():
                i = rrC[0] % 3
                rrC[0] += 1
                return CT[i], CTb[i]

            def phaseC_begin(slot, sbuf_):
                S.alias(b_M + CTb, [b_cs, b_q, b_k, b_v] + Eb + Tb)

            steps.append((None, phaseC_begin))

            def branch_step(n, j):
                def load(slot, sbuf_):
                    wload(slot, sbuf_, [
                        (lambda s: w3(s, 0, 128), dram_cols(w_br[li_, n], j * 128, 128)),
                        (lambda s: w3(s, 1024, 128), dram_cols(win_l, 6144 + n * 1024 + j * 128, 128)),
                    ])

                def compute(slot, sbuf_):
                    wb = w3(slot, 0, 128)
                    wg = w3(slot, 1024, 128)
                    for tb in range(4):
                        tsl = slice(tb * 512, (tb + 1) * 512)
                        pg, pgb = psbank()
                        for c in range(KC):
                            mm(pg[:, :], wg[:, c, :], hT[:, c, tsl], c == 0, c == KC - 1, [sbuf_, hTb], [pgb])
                        pbr, pbrb = psbank()
                        for c in range(KC):
                            mm(pbr[:, :], wb[:, c, :], yT[:, c, tsl], c == 0, c == KC - 1, [sbuf_, b_y[c]], [pbrb])
                        sg, sgb = getC()
                        act(sg, pg[:, :], AF.Sigmoid, [pgb], [sgb])
                        if n == 0:
                            tt("dve", Mv[:, j, tsl], sg, pbr[:, :], ALU.mult, [sgb, pbrb], [b_M[j]])
                        else:
                            t_, tb_ = getC()
                            tt("dve", t_, sg, pbr[:, :], ALU.mult, [sgb, pbrb], [tb_])
                            tt("pool", Mv[:, j, tsl], Mv[:, j, tsl], t_, ALU.add, [b_M[j], tb_], [b_M[j]])

                return load, compute

            for j in range(KC):
                steps.append(branch_step(0, j))


            def conv_step(j):
                def load(slot, sbuf_):
                    wload(slot, sbuf_, [
                        (lambda s: w3(s, 0, 128), dram_cols(win_l, 3072 + j * 128, 128)),
                        (lambda s: w3(s, 1024, 128), dram_cols(win_l, 4096 + j * 128, 128)),
                        (lambda s: w3(s, 2048, 128), dram_cols(win_l, 5120 + j * 128, 128)),
                    ])

                def compute(slot, sbuf_):
                    wcb = w3(slot, 0, 128)
                    wcc = w3(slot, 1024, 128)
                    wcx = w3(slot, 2048, 128)
                    u = U_pad
                    for tb in range(4):
                        tsl = slice(tb * 512, (tb + 1) * 512)
                        pc_, pcb = psbank()
                        for c in range(KC):
                            mm(pc_[:, :], wcc[:, c, :], hT[:, c, tsl], c == 0, c == KC - 1, [sbuf_, hTb], [pcb])
                        px, pxb = psbank()
                        for c in range(KC):
                            mm(px[:, :], wcx[:, c, :], hT[:, c, tsl], c == 0, c == KC - 1, [sbuf_, hTb], [pxb])
                        t_, tb_ = getC()
                        act(t_, pc_[:, :], AF.Copy, [pcb], [tb_])
                        tt("dve", u[:, 1 + tb * 512:1 + (tb + 1) * 512], t_, px[:, :], ALU.mult, [tb_, pxb], [b_u])
                    cw = g0 + GC_CONV
                    for tb in range(4):
                        tsl = slice(tb * 512, (tb + 1) * 512)
                        acc, accb = getC()
                        act(acc, u[:, 1 + tb * 512:1 + (tb + 1) * 512], AF.Copy, [b_u, b_const], [accb], scale=gcols[:, cw + 8 + j:cw + 8 + j + 1])
                        stt("dve", acc, u[:, tb * 512:(tb + 1) * 512], gcols[:, cw + j:cw + j + 1], acc, ALU.mult, ALU.add, [b_u, accb, b_const], [accb])
                        stt("dve", acc, u[:, 2 + tb * 512:2 + (tb + 1) * 512], gcols[:, cw + 16 + j:cw + 16 + j + 1], acc, ALU.mult, ALU.add, [b_u, accb, b_const], [accb])
                        pb_, pbb = psbank()
                        for c in range(KC):
                            mm(pb_[:, :], wcb[:, c, :], hT[:, c, tsl], c == 0, c == KC - 1, [sbuf_, hTb], [pbb])
                        tt("dve", yT[:, j, tsl], pb_[:, :], acc, ALU.mult, [pbb, accb], [b_y[j]])

                return load, compute

            for j in range(KC):
                steps.append(conv_step(j))
            for j in range(KC):
                steps.append(branch_step(1, j))

            def out_proj_step(wmat, half, srcT, src_bufs):
                def load(slot, sbuf_):
                    wload(slot, sbuf_, [(lambda s: w3(s, 0, 512), dram_cols(wmat, half * 512, 512))])

                def compute(slot, sbuf_):
                    wt = w3(slot, 0, 512)
                    for i in range(NT):
                        pp, pb = psbank()
                        for c in range(KC):
                            mm(pp[:, :], srcT[:, c, i * 128:(i + 1) * 128], wt[:, c, :], c == 0, c == KC - 1, [sbuf_, src_bufs[c]], [pb])
                        tt("dve", X[:, i, half * 512:(half + 1) * 512], X[:, i, half * 512:(half + 1) * 512], pp[:, :], ALU.add,
                           [Xb[i], pb], [Xb[i]])

                return load, compute

            for half in range(2):
                steps.append(out_proj_step(w_o[li_], half, Mv, b_M))

            def mixer_end(slot, sbuf_):
                S.alias([b_big], b_M + CTb)
                S.alias([YRb], b_y)

            steps.append((None, mixer_end))
            if stop_after == (l, "mix"):
                return True

            offx = 0
            kcT, offx = carve(BIG, offx, KC * NMEM)
            kcT = kcT.rearrange("p (c m) -> p c m", c=KC)
            vc, offx = carve(BIG, offx, 2 * D)
            vc = vc.rearrange("p (i d) -> p i d", i=2)
            XE = []
            for _ in range(6):
                a, offx = carve(BIG, offx, 512)
                XE.append(a)
            XT = []
            for _ in range(4):
                a, offx = carve(BIG, offx, 512, F32)
                XT.append(a)
            assert offx <= NBIG
            b_kc = S.buf("kcT")
            b_vc = S.buf("vc")
            XEb = S.bufs(6, "XE")
            XTb = S.bufs(4, "XT")
            rrx = [0, 0]

            def getXE():
                i = rrx[0] % 6
                rrx[0] += 1
                return XE[i], XEb[i]

            def getXT():
                i = rrx[1] % 4
                rrx[1] += 1
                return XT[i], XTb[i]

            qcT = YR[:, :].rearrange("p (c t) -> p c t", c=KC)
            b_qc = [S.buf("qc%d" % c) for c in range(KC)]
            ocT = HT[:, :].rearrange("p (c t) -> p c t", c=KC)
            b_oc = [S.buf("oc%d" % c) for c in range(KC)]

            def cross_begin(slot, sbuf_):
                S.alias([b_kc, b_vc] + XEb + XTb, [b_big])
                S.alias(b_qc, [YRb])
                norm_to_fm(lambda i: X[:, i, :], Xb, NT, g0 + GC_CROSS, hT, hTb)

            steps.append((None, cross_begin))

            def ckv_step(part):
                def load(slot, sbuf_):
                    wload(slot, sbuf_, [(lambda s: w3(s, 0, 512), dram_cols(w_ckv[li_], part * 512, 512))])

                def compute(slot, sbuf_):
                    wt = w3(slot, 0, 512)
                    if part < 2:
                        for jj in range(4):
                            j = part * 4 + jj
                            pp, pb = psbank()
                            for c in range(KC):
                                mm(pp[:, 0:NMEM], wt[:, c, jj * 128:(jj + 1) * 128], memT[:, c, :], c == 0, c == KC - 1, [sbuf_, b_memT], [pb])
                            act(kcT[:, j, :], pp[:, 0:NMEM], AF.Copy, [pb], [b_kc])
                    else:
                        for i in range(2):
                            pp, pb = psbank()
                            for c in range(KC):
                                mm(pp[:, :], memT[:, c, i * 128:(i + 1) * 128], wt[:, c, :], c == 0, c == KC - 1, [sbuf_, b_memT], [pb])
                            act(vc[:, i, (part - 2) * 512:(part - 1) * 512], pp[:, :], AF.Copy, [pb], [b_vc])

                return load, compute

            for part in range(4):
                steps.append(ckv_step(part))

            def cq_step(half):
                def load(slot, sbuf_):
                    wload(slot, sbuf_, [(lambda s: w3(s, 0, 512), dram_cols(w_cq[li_], half * 512, 512))])

                def compute(slot, sbuf_):
                    wt = w3(slot, 0, 512)
                    for jj in range(4):
                        j = half * 4 + jj
                        for tb in range(4):
                            tsl = slice(tb * 512, (tb + 1) * 512)
                            pp, pb = psbank()
                            for c in range(KC):
                                mm(pp[:, :], wt[:, c, jj * 128:(jj + 1) * 128], hT[:, c, tsl], c == 0, c == KC - 1, [sbuf_, hTb], [pb])
                            if (jj + tb) % 2 == 0:
                                act(qcT[:, j, tsl], pp[:, :], AF.Copy, [pb], [b_qc[j]])
                            else:
                                cp("dve", qcT[:, j, tsl], pp[:, :], [pb], [b_qc[j]])

                return load, compute

            for half in range(2):
                steps.append(cq_step(half))

            def cross_attn(slot, sbuf_):
                S.alias(b_oc, [hTb])
                for h in range(4):
                    for tb in range(4):
                        tsl = slice(tb * 512, (tb + 1) * 512)
                        pO = [psbank(), psbank()]
                        pR = psbank()
                        for mb in range(2):
                            pS, pSb = psbank()
                            for cc in range(2):
                                mm(pS[:, :], kcT[:, 2 * h + cc, mb * 128:(mb + 1) * 128], qcT[:, 2 * h + cc, tsl], cc == 0, cc == 1,
                                   [b_kc, b_qc[2 * h + cc]], [pSb])
                            E, Eb_ = getXE()
                            act(E, pS[:, :], AF.Exp, [pSb], [Eb_], scale=1.0 / 16)
                            for ee in range(2):
                                mm(pO[ee][0][:, :], vc[:, mb, h * 256 + ee * 128:h * 256 + (ee + 1) * 128], E, mb == 0, mb == 1,
                                   [b_vc, Eb_], [pO[ee][1]])
                            mm(pR[0][:, :], ones, E, mb == 0, mb == 1, [b_const, Eb_], [pR[1]])
                        r_, rb_ = getXT()
                        S.op("dve", lambda e, r_=r_, pR=pR: e.reciprocal(out=r_, in_=pR[0][:, :]), reads=[pR[1]], writes=[rb_])
                        for ee in range(2):
                            tt("dve", ocT[:, 2 * h + ee, tsl], pO[ee][0][:, :], r_, ALU.mult, [pO[ee][1], rb_], [b_oc[2 * h + ee]])

            steps.append((None, cross_attn))
            for half in range(2):
                steps.append(out_proj_step(w_co[li_], half, ocT, b_oc))

            def cross_end(slot, sbuf_):
                S.alias([b_big], [b_kc, b_vc] + XEb + XTb)
                S.alias([YRb], b_qc)
                S.alias([hTb], b_oc)

            steps.append((None, cross_end))
            if stop_after == (l, "cross"):
                return True

            hrows = YR[:, :].rearrange("p (i d) -> p i d", i=NT)
            b_hr = S.bufs(NT, "hrows")
            offm = 0
            oh12, offm = carve(BIG, offm, NT * 64, F32)
            oh12 = oh12.rearrange("p (i e) -> p i e", i=NT)
            mohb, offm = carve(BIG, offm, NT * 32)
            mohb = mohb.rearrange("p (i e) -> p i e", i=NT)
            wts, offm = carve(BIG, offm, NT * 2, F32)
            wts = wts.rearrange("p (i k) -> p i k", i=NT)
            desti, offm = carve(BIG, offm, NT * 2, I32)
            desti = desti.rearrange("p (i k) -> p i k", i=NT)
            rt, offm = carve(BIG, offm, 256, F32)
            cnt, offm = carve(BIG, offm, 32, F32)
            padA, offm = carve(BIG, offm, 32, F32)
            padB, offm = carve(BIG, offm, 32, F32)
            pstart, offm = carve(BIG, offm, 32, F32)
            pend, offm = carve(BIG, offm, 32, F32)
            eblk, offm = carve(BIG, offm, NBLK, F32)
            idxgu, offm = carve(BIG, offm, NBLK * 8, I32)
            idxd, offm = carve(BIG, offm, NBLK * 8, I32)
            idxgu = idxgu.rearrange("p (b c) -> p b c", b=NBLK)
            idxd = idxd.rearrange("p (b c) -> p b c", b=NBLK)
            idxgu_f, offt = carve(BIG, offm, NBLK * 8, F32)
            idxd_f, offt = carve(BIG, offt, NBLK * 8, F32)
            idxgu_f = idxgu_f.rearrange("p (b c) -> p b c", b=NBLK)
            idxd_f = idxd_f.rearrange("p (b c) -> p b c", b=NBLK)
            offm0 = offm
            b_rt = S.buf("rt")
            b_oh = S.buf("oh")
            b_route = S.buf("route")

            def moe_begin(slot, sbuf_):
                for r in range(NBLK):
                    for hh in range(2):
                        dma_sp("xbz", xb_d[r * 128:(r + 1) * 128, hh * 512:(hh + 1) * 512], ztile[:, :], reads=[b_zt], writes=[b_xb])
                S.alias([b_rt, b_oh, b_route], [b_big])
                S.alias(b_hr, [YRb])

            steps.append((None, moe_begin))

            def router_step():
                def load(slot, sbuf_):
                    wload(slot, sbuf_, [(lambda s: s[:, 0:KC * 36].rearrange("p (c n) -> p c n", c=KC),
                                         w_rt[li_].rearrange("(c p) n -> p c n", p=128))])

                def compute(slot, sbuf_):
                    wr = slot[:, 0:KC * 36].rearrange("p (c n) -> p c n", c=KC)
                    norm_to_fm(lambda i: X[:, i, :], Xb, NT, g0 + GC_FFN, hT, hTb,
                               keep_rows=lambda i: (hrows[:, i, :], b_hr[i]))
                    lg = rt[:, 0:36]
                    for i in range(NT):
                        pp, pb = psbank()
                        for c in range(KC):
                            mm(pp[:, 0:36], hT[:, c, i * 128:(i + 1) * 128], wr[:, c, :], c == 0, c == KC - 1, [sbuf_, hTb], [pb])
                        R_ = [b_rt]
                        act(lg, pp[:, 0:36], AF.Copy, [pb], R_)
                        mxg = rt[:, 40:41]
                        S.op("dve", lambda e, mxg=mxg: e.tensor_reduce(out=mxg, in_=rt[:, 0:4], axis=AX.X, op=ALU.max), reads=R_, writes=R_)
                        ohg = rt[:, 44:48]
                        ts("dve", ohg, rt[:, 0:4], mxg, ALU.is_equal, R_, R_)
                        nmx = rt[:, 41:42]
                        ts("dve", nmx, mxg, -1.0, ALU.mult, R_, R_)
                        sumg = rt[:, 42:43]
                        act(rt[:, 48:52], rt[:, 0:4], AF.Exp, R_, R_, bias=nmx, accum=sumg)
                        gw = rt[:, 43:44]
                        S.op("dve", lambda e, gw=gw, sumg=sumg: e.reciprocal(out=gw, in_=sumg), reads=R_, writes=R_)
                        pen = rt[:, 52:56]
                        ts("dve", pen, ohg, -1.0, ALU.add, R_, R_, s2=-NEG, op1=ALU.mult)
                        lem = rt[:, 64:96]
                        for g in range(4):
                            ts("dve", lem[:, g * 8:(g + 1) * 8], rt[:, 4 + g * 8:4 + (g + 1) * 8], pen[:, g:g + 1], ALU.add, R_, R_)
                        m1 = rt[:, 56:57]
                        S.op("dve", lambda e, m1=m1, lem=lem: e.tensor_reduce(out=m1, in_=lem, axis=AX.X, op=ALU.max), reads=R_, writes=R_)
                        ts("dve", oh12[:, i, 0:32], lem, m1, ALU.is_equal, R_, [b_oh])
                        lem2 = rt[:, 96:128]
                        stt("dve", lem2, oh12[:, i, 0:32], NEG, lem, ALU.mult, ALU.add, [b_oh, b_rt], R_)
                        m2 = rt[:, 57:58]
                        S.op("dve", lambda e, m2=m2, lem2=lem2: e.tensor_reduce(out=m2, in_=lem2, axis=AX.X, op=ALU.max), reads=R_, writes=R_)
                        ts("dve", oh12[:, i, 32:64], lem2, m2, ALU.is_equal, R_, [b_oh])
                        dd = rt[:, 58:59]
                        tt("dve", dd, m2, m1, ALU.subtract, R_, R_)
                        ed = rt[:, 59:60]
                        act(ed, dd, AF.Exp, R_, R_)
                        den = rt[:, 60:61]
                        ts("dve", den, ed, 1.0, ALU.add, R_, R_)
                        w1 = rt[:, 61:62]
                        S.op("dve", lambda e, w1=w1, den=den: e.reciprocal(out=w1, in_=den), reads=R_, writes=R_)
                        tt("dve", wts[:, i, 0:1], w1, gw, ALU.mult, R_, [b_oh])
                        w2 = rt[:, 62:63]
                        tt("dve", w2, ed, w1, ALU.mult, R_, R_)
                        tt("dve", wts[:, i, 1:2], w2, gw, ALU.mult, R_, [b_oh])
                        tt("dve", mohb[:, i, :], oh12[:, i, 0:32], oh12[:, i, 32:64], ALU.add, [b_oh], [b_oh])
                    pc_, pcb = psbank()
                    for i in range(NT):
                        mm(pc_[:, 0:32], ones, mohb[:, i, :], i == 0, i == NT - 1, [b_const, b_oh], [pcb])
                    Q_ = [b_route]
                    cp("dve", cnt, pc_[:, 0:32], [pcb], Q_)
                    ts("dve", padA, cnt, 1.0 / 128, ALU.mult, Q_, Q_, s2=127.0 / 128 - 0.49609375, op1=ALU.add)
                    cp("dve", padB.bitcast(I32), padA, Q_, Q_)
                    cp("dve", padA, padB.bitcast(I32), Q_, Q_)
                    ts("dve", padA, padA, 128.0, ALU.mult, Q_, Q_)
                    cp("dve", pstart, padA, Q_, Q_)
                    a, b_ = padA, padB
                    for s_ in (1, 2, 4, 8, 16):
                        cp("dve", b_[:, 0:s_], a[:, 0:s_], Q_, Q_)
                        tt("dve", b_[:, s_:32], a[:, s_:32], a[:, 0:32 - s_], ALU.add, Q_, Q_)
                        a, b_ = b_, a
                    cp("dve", pend, a, Q_, Q_)
                    tt("dve", pstart, pend, pstart, ALU.subtract, Q_, Q_)
                    for i in range(NT):
                        pp, pb = psbank()
                        mm(pp[:, 0:32], ustr, mohb[:, i, :], True, i == 0, [b_const, b_oh], [pb])
                        for j in range(i):
                            mm(pp[:, 0:32], ones, mohb[:, j, :], False, j == i - 1, [b_const, b_oh], [pb])
                        tmp = rt[:, 128:160]
                        tt("dve", tmp, pp[:, 0:32], pstart, ALU.add, [pb, b_route], [b_rt])
                        for k in range(2):
                            prod = rt[:, 160:192]
                            df = rt[:, 192 + k:193 + k]
                            tt("dve", prod, tmp, oh12[:, i, k * 32:(k + 1) * 32], ALU.mult, [b_rt, b_oh], [b_rt])
                            S.op("dve", lambda e, df=df, prod=prod: e.tensor_reduce(out=df, in_=prod, axis=AX.X, op=ALU.add), reads=[b_rt], writes=[b_rt])
                            cp("dve", desti[:, i, k:k + 1], df, [b_rt], [b_route])
                    S.op("dve", lambda e: e.memset(eblk, 0.0), writes=Q_)
                    for e_ in range(NEXP):
                        stt("dve", eblk, thr, pend[:, e_:e_ + 1], eblk, ALU.is_ge, ALU.add, [b_const] + Q_, Q_)
                    ts("dve", eblk, eblk, float(NEXP - 1), ALU.min, Q_, Q_, s2=float(li_ * NEXP), op1=ALU.add)
                    for c in range(8):
                        ts("dve", idxgu_f[:, :, c], eblk, float(D), ALU.mult, Q_, Q_, s2=pcv[:, c:c + 1], op1=ALU.add)
                    for c in range(6):
                        ts("dve", idxd_f[:, :, c], eblk, float(DEXP), ALU.mult, Q_, Q_, s2=pcv[:, c:c + 1], op1=ALU.add)
                    cp("dve", idxgu[:, :, :], idxgu_f[:, :, :], Q_, Q_)
                    cp("dve", idxd[:, :, 0:6], idxd_f[:, :, 0:6], Q_, Q_)
                    for i in range(NT):
                        for k in range(2):
                            S.dma("pool", "scat", lambda e, i=i, k=k: e.indirect_dma_start(
                                out=xb_d[:, :], out_offset=bass.IndirectOffsetOnAxis(ap=desti[:, i, k:k + 1], axis=0),
                                in_=hrows[:, i, :], in_offset=None), reads=[b_hr[i], b_route], writes=[b_xb])

                return load, compute

            steps.append(router_step())

            Gw = [HT[:, g * 6144:(g + 1) * 6144].rearrange("p (c n) -> p c n", c=8) for g in range(2)]
            xg_t = [HT[:, 12288 + i * 1024:12288 + (i + 1) * 1024] for i in range(2)]
            xgT_t = [HT[:, 14336 + i * 1024:14336 + (i + 1) * 1024].rearrange("p (c t) -> p c t", c=8) for i in range(2)]
            Uw = [YR[:, g * 6144:(g + 1) * 6144].rearrange("p (c n) -> p c n", c=8) for g in range(2)]
            ybt = [YR[:, 12288 + i * 2048:12288 + (i + 1) * 2048].bitcast(F32) for i in range(2)]
            offd = offm0 + (offm0 % 2)
            Dw = []
            for g in range(2):
                a, offd = carve(BIG, offd, 6 * D)
                Dw.append(a.rearrange("p (c n) -> p c n", c=6))
            sgt = U_pad[:, 2:2 + 2 * DEXP].bitcast(F32)
            hmt = hn_t[0][:, 0:DEXP]
            hmT = [hn_t[1][:, 0:DEXP].rearrange("p (c t) -> p c t", c=6)]
            a, offd = carve(BIG, offd, DEXP)
            hmT.append(a.rearrange("p (c t) -> p c t", c=6))
            assert offd <= NBIG, offd
            b_G = S.bufs(2, "Gw")
            b_U = S.bufs(2, "Uw")
            b_D = S.bufs(2, "Dw")
            b_xg = S.bufs(2, "xg")
            b_xgT = S.bufs(2, "xgT")
            b_ybt = S.bufs(2, "ybt")
            b_sg = S.buf("sg")
            b_hm = S.buf("hm")
            b_hmT = S.bufs(2, "hmT")
            weg = w_eg.rearrange("l e d n -> (l e d) n")
            weu = w_eu.rearrange("l e d n -> (l e d) n")
            wed = w_ed.rearrange("l e d n -> (l e d) n")

            def moe_blocks(slot, sbuf_):
                S.alias(b_G + b_xg + b_xgT, [hTb])
                S.alias(b_U + b_ybt, b_hr)
                S.alias(b_D + [b_hmT[1]], [b_big, b_route])
                S.alias([b_sg], [b_u])
                S.alias([b_hm, b_hmT[0]], hn_b)

                def issue_loads(b):
                    p = b % 2
                    dma_sp("xg%d" % p, xg_t[p], xb_d[b * 128:(b + 1) * 128, :], reads=[b_xb], writes=[b_xg[p]])
                    for c in range(8):
                        S.dma("pool", "G%d" % p, lambda e, c=c, p=p, b=b: e.indirect_dma_start(
                            out=Gw[p][:, c, :], out_offset=None, in_=weg,
                            in_offset=bass.IndirectOffsetOnAxis(ap=idxgu[:, b, c:c + 1], axis=0)), reads=[b_route], writes=[b_G[p]])
                    for c in range(8):
                        S.dma("pool", "U%d" % p, lambda e, c=c, p=p, b=b: e.indirect_dma_start(
                            out=Uw[p][:, c, :], out_offset=None, in_=weu,
                            in_offset=bass.IndirectOffsetOnAxis(ap=idxgu[:, b, c:c + 1], axis=0)), reads=[b_route], writes=[b_U[p]])
                    for c in range(6):
                        S.dma("pool", "D%d" % p, lambda e, c=c, p=p, b=b: e.indirect_dma_start(
                            out=Dw[p][:, c, :], out_offset=None, in_=wed,
                            in_offset=bass.IndirectOffsetOnAxis(ap=idxd[:, b, c:c + 1], axis=0)), reads=[b_route], writes=[b_D[p]])

                issue_loads(0)
                for b in range(NBLK):
                    p = b % 2
                    if b + 1 < NBLK:
                        issue_loads(b + 1)
                    pt, pb = psbank()
                    ptb = pt[:, :].bitcast(BF16).rearrange("p (c t) -> p c t", t=128)
                    for c in range(KC):
                        tr(ptb[:, c, :], xg_t[p][:, c * 128:(c + 1) * 128], [b_xg[p]], [pb])
                    for c in range(KC):
                        gc = gcols[:, g0 + GC_FFN + c:g0 + GC_FFN + c + 1]
                        if c % 2 == 0:
                            ts("dve", xgT_t[p][:, c, :], ptb[:, c, :], gc, ALU.mult, [pb, b_const], [b_xgT[p]])
                        else:
                            act(xgT_t[p][:, c, :], ptb[:, c, :], AF.Copy, [pb, b_const], [b_xgT[p]], scale=gc)
                    for (n0, nn) in ((0, 512), (512, 256)):
                        pg, pgb = psbank()
                        for c in range(KC):
                            mm(pg[:, 0:nn], xgT_t[p][:, c, :], Gw[p][:, c, n0:n0 + nn], c == 0, c == KC - 1, [b_xgT[p], b_G[p]], [pgb])
                        pu, pub = psbank()
                        for c in range(KC):
                            mm(pu[:, 0:nn], xgT_t[p][:, c, :], Uw[p][:, c, n0:n0 + nn], c == 0, c == KC - 1, [b_xgT[p], b_U[p]], [pub])
                        act(sgt[:, n0:n0 + nn], pg[:, 0:nn], AF.Silu, [pgb], [b_sg])
                        tt("dve", hmt[:, n0:n0 + nn], sgt[:, n0:n0 + nn], pu[:, 0:nn], ALU.mult, [b_sg, pub], [b_hm])
                    pt2, pb2 = psbank()
                    pt2b = pt2[:, :].bitcast(BF16).rearrange("p (c t) -> p c t", t=128)
                    for c in range(6):
                        tr(pt2b[:, c, :], hmt[:, c * 128:(c + 1) * 128], [b_hm], [pb2])
                    act(hmT[p][:, 0:3, :], pt2b[:, 0:3, :], AF.Copy, [pb2], [b_hmT[p]])
                    cp("dve", hmT[p][:, 3:6, :], pt2b[:, 3:6, :], [pb2], [b_hmT[p]])
                    for n in range(2):
                        py, pyb = psbank()
                        for c in range(6):
                            mm(py[:, :], hmT[p][:, c, :], Dw[p][:, c, n * 512:(n + 1) * 512], c == 0, c == 5, [b_hmT[p], b_D[p]], [pyb])
                        if n == 0:
                            act(ybt[p][:, 0:512], py[:, :], AF.Copy, [pyb], [b_ybt[p]])
                        else:
                            cp("dve", ybt[p][:, 512:1024], py[:, :], [pyb], [b_ybt[p]])
                    dma_sp("ybst", yb_d[b * 128:(b + 1) * 128, :], ybt[p], reads=[b_ybt[p]], writes=[b_yb])
                S.alias(b_gat, b_G + b_xg + b_xgT)
                for i in range(NT):
                    for k in range(2):
                        gi = (i * 2 + k) % 4
                        S.dma("pool", "gat%d" % gi, lambda e, i=i, k=k, gi=gi: e.indirect_dma_start(
                            out=gat_t[gi], out_offset=None, in_=yb_d[:, :],
                            in_offset=bass.IndirectOffsetOnAxis(ap=desti[:, i, k:k + 1], axis=0)), reads=[b_yb, b_route], writes=[b_gat[gi]])
                        stt("dve", X[:, i, :], gat_t[gi], wts[:, i, k:k + 1], X[:, i, :], ALU.mult, ALU.add, [b_gat[gi], b_oh, Xb[i]], [Xb[i]])

            gat_t = [HT[:, i * 2048:(i + 1) * 2048].bitcast(F32) for i in range(4)]
            b_gat = S.bufs(4, "gat")
            steps.append((None, moe_blocks))

            def moe_end(slot, sbuf_):
                S.alias([b_big], [b_rt, b_oh, b_route] + b_D + b_hmT)
                S.alias([b_u], [b_sg])
                S.alias(hn_b, [b_hm, b_hmT[0]])
                S.alias([YRb], b_U + b_ybt + b_hr)
                S.alias([hTb], b_gat + b_G + b_xg + b_xgT)

            steps.append((None, moe_end))
            if stop_after == (l, "moe"):
                return True
            return False

        ztile = sb("ztile", [128, 512], BF16)
        b_zt = S.buf("zt")
        S.op("pool", lambda e: e.memset(ztile[:, :], 0.0), writes=[b_zt])
        U_pad = sb("U_pad", [128, S_LEN + 4], BF16)
        b_u = S.buf("u")
        b_xb = S.buf("xb_d")
        b_yb = S.buf("yb_d")
        S.op("pool", lambda e: e.memset(U_pad[:, :], 0.0), writes=[b_u])

        stopped = False
        for li_, l in enumerate(layer_ids):
            stopped = layer_steps(li_, l)
            if stopped:
                break

        def epilogue(slot, sbuf_):
            if final and not stopped:
                gfin, _ = carve(BIG, 0, D, F32)
                otile = [carve(BIG, 2 * D + i * 2 * D, D, F32)[0] for i in range(2)]
                b_gf = S.buf("gfin")
                b_ot = S.bufs(2, "otile")
                S.alias([b_gf] + b_ot, [b_big])
                dma_sp("c2", gfin, gfin_d[:, :], writes=[b_gf])
                rstd_tiles(lambda i: X[:, i, :], Xb, NT, 1.0 / D)
                for i in range(NT):
                    p = i % 2
                    act(otile[p], X[:, i, :], AF.Copy, [Xb[i], b_rstd], [b_ot[p]], scale=rstd[:, i:i + 1])
                    tt("dve", otile[p], otile[p], gfin, ALU.mult, [b_ot[p], b_gf], [b_ot[p]])
                    dma_sp("out", out_d[i * 128:(i + 1) * 128, :], otile[p], reads=[b_ot[p]], writes=[b_out])
            else:
                for i in range(NT):
                    dma_sp("out", out_d[i * 128:(i + 1) * 128, :], X[:, i, :], reads=[Xb[i]], writes=[b_out])

        b_out = S.buf("out")
        if max_steps is not None:
            del steps[max_steps:]
        steps.append((None, epilogue))

        wsteps = [k for k, (ld, _) in enumerate(steps) if ld is not None]
        issued = 0
        done = 0
        for k, (ld, cpf) in enumerate(steps):
            while issued < len(wsteps) and issued < done + NSLOT:
                sl = issued % NSLOT
                steps[wsteps[issued]][0](WS[sl], WSb[sl])
                issued += 1
            if ld is not None:
                sl = done % NSLOT
                cpf(WS[sl], WSb[sl])
                done += 1
            else:
                cpf(None, None)
        S.wait_all("sp", [b_out])
        if os.environ.get('WAITCS'):
            for b_ in dbg_bufs:
                S.wait_all("sp", [b_])
        S.finalize()
        dl = S.simulate()
        if dl is not None:
            raise RuntimeError("sync deadlock: %r" % (dl,))
        S.emit()
    return nc, S.ninst


def _consts():
    ident = np.eye(128, dtype=np.float32)
    ones = np.ones((128, 128), np.float32)
    rrot = np.zeros((128, 128), np.float32)
    for c in range(2):
        for i in range(8):
            rrot[c * 64 + 8 + i, c * 64 + i] = -1.0
            rrot[c * 64 + i, c * 64 + 8 + i] = 1.0
    ustr = np.triu(np.ones((128, 128), np.float32), 1)
    cmat = np.concatenate([ident, ones, rrot, ustr], axis=1)
    inv = np.float32(500000.0) ** (-np.arange(0, 16, 2, dtype=np.float32) / np.float32(16))
    ang = np.arange(S_LEN, dtype=np.float32)[:, None] * inv[None, :]
    cs, sn = np.cos(ang).astype(np.float32), np.sin(ang).astype(np.float32)
    cosT = np.ones((128, S_LEN), np.float32)
    sinT = np.zeros((128, S_LEN), np.float32)
    for c in range(2):
        for i in range(16):
            cosT[c * 64 + i] = cs[:, i % 8]
            sinT[c * 64 + i] = sn[:, i % 8]
    cossin = np.concatenate([cosT, sinT], axis=1)
    thr = np.broadcast_to((np.arange(64, dtype=np.float32) * 128.0)[None, :], (128, 64))
    pc = np.arange(8, dtype=np.float32)[None, :] * 128.0 + np.arange(128, dtype=np.float32)[:, None]
    cmisc = np.ascontiguousarray(np.concatenate([thr, pc], axis=1))
    return np.ascontiguousarray(cmat.astype(ml_dtypes.bfloat16)), np.ascontiguousarray(cossin.astype(ml_dtypes.bfloat16)), cmisc


def _pack_small(inp, layer_ids):
    cols = []
    for l in layer_ids:
        def colmaj(v):
            return np.asarray(v, np.float32).reshape(8, 128).T
        cols.append(colmaj(inp["norm_mix"][l]))
        cols.append(colmaj(inp["norm_cross"][l]))
        cols.append(colmaj(inp["norm_ffn"][l]))
        for k in range(3):
            cols.append(colmaj(inp["conv_w"][l][k]))
        cols.append(np.asarray(inp["subln"][l], np.float32).reshape(128, 1))
    cols.append(np.asarray(inp["norm_mem"], np.float32).reshape(8, 128).T)
    gcols = np.ascontiguousarray(np.concatenate(cols, axis=1))
    lam = []
    for l in layer_ids:
        lam.append(np.concatenate([inp["lambda_q1"][l], inp["lambda_k1"][l], inp["lambda_q2"][l], inp["lambda_k2"][l]]))
    lamv = np.ascontiguousarray(np.broadcast_to(np.concatenate(lam)[None, :].astype(np.float32), (128, len(layer_ids) * 256)))
    gfin = np.ascontiguousarray(np.broadcast_to(np.asarray(inp["norm_final"], np.float32)[None, :], (128, D)))
    return gcols, lamv, gfin


_PROG_CACHE = {}


def _run(inp, x_shards, layer_ids, final, stop_after=None, cores=None, max_steps=None):
    key = (tuple(layer_ids), final, stop_after, max_steps)
    if key not in _PROG_CACHE:
        _PROG_CACHE[key] = build(list(layer_ids), final, stop_after, max_steps)[0]
    nc = _PROG_CACHE[key]
    cmat, cossin, cmisc = _consts()
    gcols, lamv, gfin = _pack_small(inp, layer_ids)
    ls = list(layer_ids)
    sl = slice(ls[0], ls[-1] + 1)
    w_rt = np.ascontiguousarray(np.concatenate([inp["w_router_group"][sl], inp["w_router_expert"][sl]], axis=-1))
    shared = {
        "w_in": np.ascontiguousarray(inp["w_in"][sl]), "w_branch": np.ascontiguousarray(inp["w_branch"][sl]),
        "w_o": np.ascontiguousarray(inp["w_o"][sl]), "w_cq": np.ascontiguousarray(inp["w_cq"][sl]),
        "w_ckv": np.ascontiguousarray(inp["w_ckv"][sl]), "w_co": np.ascontiguousarray(inp["w_co"][sl]),
        "w_rt": w_rt, "w_exp_gate": np.ascontiguousarray(inp["w_exp_gate"][sl]),
        "w_exp_up": np.ascontiguousarray(inp["w_exp_up"][sl]), "w_exp_down": np.ascontiguousarray(inp["w_exp_down"][sl]),
        "gcols": gcols, "lamv": lamv, "gfinal": gfin, "cmat": cmat, "cossin": cossin, "cmisc": cmisc,
    }
    cores = list(range(len(x_shards))) if cores is None else cores
    in_maps = []
    for b in range(len(x_shards)):
        m = dict(shared)
        m["x"] = np.ascontiguousarray(x_shards[b])
        m["mem"] = np.ascontiguousarray(inp["mem"][b])
        in_maps.append(m)
    res = run_bass_kernel_spmd(nc, in_maps, core_ids=cores)
    return [r["out"] for r in res.results]


MODE = "fused"


def kernel(**inputs):
    inp = {k: np.asarray(v) for k, v in inputs.items()}
    xs = [inp["x"][b] for b in range(inp["x"].shape[0])]
    if MODE == "fused":
        outs = _run(inp, xs, list(range(DEPTH)), True)
    else:
        for l in range(DEPTH):
            xs = _run(inp, xs, [l], l == DEPTH - 1)
        outs = xs
    return np.stack(outs, axis=0).astype(np.float32)
```

```python
import math
import os
import numpy as np
import ml_dtypes
HEADCUT = int(os.environ.get('HEADCUT', '99'))
from contextlib import ExitStack
import concourse.bass as bass
import concourse.mybir as mybir
from concourse.bass_utils import run_bass_kernel_spmd

F32 = mybir.dt.float32
BF16 = mybir.dt.bfloat16
I32 = mybir.dt.int32
ALU = mybir.AluOpType
AF = mybir.ActivationFunctionType
AX = mybir.AxisListType

D = 1024
S_LEN = 2048
NT = 16
KC = 8
NMEM = 256
DEPTH = 4
NEXP = 32
DEXP = 768
NBLK = 64
EPS = 1e-6
NEG = -1.0e30


class Buf:
    __slots__ = ("name", "w", "r", "excl")

    def __init__(self, name):
        self.name = name
        self.w = None
        self.r = {}
        self.excl = False


class Sched:
    ENG = ("pe", "act", "dve", "pool", "sp")

    def __init__(self, nc, stack):
        self.nc = nc
        self.stack = stack
        self.rec = {e: [] for e in self.ENG}
        self.sem = {e: stack.enter_context(nc.semaphore("s_" + e)) for e in self.ENG}
        self.cnt = {e: 0 for e in self.ENG}
        self.seen = {e: {} for e in self.ENG}
        self.dsem = {}
        self.dcnt = {}
        self.nbuf = 0
        self.ninst = 0

    def buf(self, name=None):
        self.nbuf += 1
        return Buf(name or "b%d" % self.nbuf)

    def bufs(self, n, name="b"):
        return [self.buf("%s%d" % (name, i)) for i in range(n)]

    def _deps(self, eng, reads, writes, skipkey=None):
        deps = {}

        def add(k, v):
            if k == skipkey:
                return
            if deps.get(k, 0) < v:
                deps[k] = v

        for b in reads:
            if b.w is not None:
                add(*b.w)
            if b.excl:
                for k, v in b.r.items():
                    if k != ("e", eng):
                        add(k, v)
        for b in writes:
            if b.w is not None:
                add(*b.w)
            for k, v in b.r.items():
                add(k, v)
        out = []
        seen = self.seen[eng]
        for k, v in deps.items():
            if eng == "pe" and k == ("e", "pe"):
                continue
            if k[0] == "d":
                v = self.dcnt[k[1]]
            if seen.get(k, 0) >= v:
                continue
            seen[k] = v
            out.append((k, v))
        return out

    def _post(self, ev, reads, writes):
        k, v = ev
        for b in reads:
            if b.r.get(k, 0) < v:
                b.r[k] = v
        for b in writes:
            b.w = ev
            b.r = {}

    def op(self, eng, fn, reads=(), writes=()):
        waits = self._deps(eng, reads, writes)
        self.cnt[eng] += 1
        ev = (("e", eng), self.cnt[eng])
        self.rec[eng].append(("op", waits, fn, self.cnt[eng]))
        self._post(ev, reads, writes)
        self.ninst += 1
        return ev

    def dma(self, eng, key, fn, reads=(), writes=()):
        if key not in self.dsem:
            self.dsem[key] = self.stack.enter_context(self.nc.semaphore("d_" + str(key)))
            self.dcnt[key] = 0
        waits = self._deps(eng, reads, writes, skipkey=("d", key))
        self.dcnt[key] += 16
        ev = (("d", key), self.dcnt[key])
        self.rec[eng].append(("dma", waits, fn, key))
        self._post(ev, reads, writes)
        self.ninst += 1
        return ev

    def alias(self, new_bufs, old_bufs):
        for nb in new_bufs:
            for ob in old_bufs:
                if ob.w is not None:
                    k, v = ob.w
                    if nb.r.get(k, 0) < v:
                        nb.r[k] = v
                for k, v in ob.r.items():
                    if nb.r.get(k, 0) < v:
                        nb.r[k] = v

    def wait_all(self, eng, bufs):
        waits = self._deps(eng, bufs, bufs)
        self.rec[eng].append(("wait", waits, None, None))

    def finalize(self):
        waited = {e: set() for e in self.ENG}
        for eng in self.ENG:
            for kind, waits, fn, x in self.rec[eng]:
                for k, v in waits:
                    if k[0] == "e":
                        waited[k[1]].add(v)
        rank = {e: {v: i + 1 for i, v in enumerate(sorted(waited[e]))} for e in self.ENG}
        self.prog = {}
        for eng in self.ENG:
            pl = []
            for kind, waits, fn, x in self.rec[eng]:
                rw = []
                for k, v in waits:
                    if k[0] == "e":
                        rw.append((self.sem[k[1]], rank[k[1]][v]))
                    else:
                        rw.append((self.dsem[k[1]], v))
                if kind == "op":
                    pl.append((rw, fn, self.sem[eng] if x in waited[eng] else None, 1))
                elif kind == "dma":
                    pl.append((rw, fn, self.dsem[x], 16))
                else:
                    pl.append((rw, None, None, 0))
            self.prog[eng] = pl

    def simulate(self):
        val = {}
        pc = {e: 0 for e in self.ENG}
        prog = self.prog
        while True:
            progressed = False
            for e in self.ENG:
                while pc[e] < len(prog[e]):
                    waits, fn, sem, inc = prog[e][pc[e]]
                    if all(val.get(id(s_), 0) >= v for s_, v in waits):
                        if sem is not None:
                            val[id(sem)] = val.get(id(sem), 0) + inc
                        pc[e] += 1
                        progressed = True
                    else:
                        break
            if all(pc[e] == len(prog[e]) for e in self.ENG):
                return None
            if not progressed:
                return {e: (pc[e], len(prog[e])) for e in self.ENG}

    def emit(self):
        nc = self.nc
        prog = self.prog

        def run(e, pl):
            for waits, fn, sem, inc in pl:
                for s_, v in waits:
                    e.wait_ge(s_, v)
                if fn is not None:
                    ins = fn(e)
                    if sem is not None:
                        ins.then_inc(sem, inc)

        with nc.Block() as block:
            @block.tensor
            def _(e):
                run(e, prog["pe"])

            @block.scalar
            def _(e):
                run(e, prog["act"])

            @block.vector
            def _(e):
                run(e, prog["dve"])

            @block.gpsimd
            def _(e):
                run(e, prog["pool"])

            @block.sync
            def _(e):
                run(e, prog["sp"])


def lambda_init(l):
    return 0.8 - 0.6 * math.exp(-0.3 * l)


GC_MIX, GC_CROSS, GC_FFN, GC_CONV, GC_SUBLN, GC_PER = 0, 8, 16, 24, 48, 49


def build(layer_ids, final, stop_after=None, max_steps=None):
    L = len(layer_ids)
    nc = bass.Bass("TRN2", target_bir_lowering=False)

    def din(name, shape, dt=F32):
        return nc.dram_tensor(name, shape, dt, kind="ExternalInput").ap()

    x_in = din("x", [S_LEN, D])
    mem_in = din("mem", [NMEM, D])
    w_in = din("w_in", [L, D, 8192])
    w_br = din("w_branch", [L, 2, D, D])
    w_o = din("w_o", [L, D, D])
    w_cq = din("w_cq", [L, D, D])
    w_ckv = din("w_ckv", [L, D, 2 * D])
    w_co = din("w_co", [L, D, D])
    w_rt = din("w_rt", [L, D, 36])
    w_eg = din("w_exp_gate", [L, NEXP, D, DEXP])
    w_eu = din("w_exp_up", [L, NEXP, D, DEXP])
    w_ed = din("w_exp_down", [L, NEXP, DEXP, D])
    gcols_d = din("gcols", [128, L * GC_PER + 8])
    lamv_d = din("lamv", [128, L * 256])
    gfin_d = din("gfinal", [128, D])
    cmat_d = din("cmat", [128, 4 * 128], BF16)
    cossin_d = din("cossin", [128, 2 * S_LEN], BF16)
    cmisc_d = din("cmisc", [128, 64 + 8])
    out_d = nc.dram_tensor("out", [S_LEN, D], F32, kind="ExternalOutput").ap()
    xb_d = nc.dram_tensor("xb_scr", [NBLK * 128, D], BF16, kind="Internal").ap()
    yb_d = nc.dram_tensor("yb_scr", [NBLK * 128, D], F32, kind="Internal").ap()

    st = ExitStack()
    with st:
        S = Sched(nc, st)

        def sb(name, shape, dt):
            return st.enter_context(nc.sbuf_tensor("sb_" + name, shape, dt))

        X = sb("X", [128, NT, D], F32)
        HT = sb("HT", [128, KC * S_LEN], BF16)
        YR = sb("YR", [128, KC * S_LEN], BF16)
        NBIG = 19 * 1024
        BIG = sb("BIG", [128, NBIG], BF16)
        NSLOT = 3
        WS = [sb("WS%d" % i, [128, 4096], BF16) for i in range(NSLOT)]
        cmat = sb("cmat", [128, 4 * 128], BF16)
        gcols = sb("gcols", [128, L * GC_PER + 8], F32)
        cmisc = sb("cmisc", [128, 72], F32)
        memT = sb("memT", [128, KC, NMEM], BF16)
        stat = sb("stat", [128, 64], F32)
        lamc = sb("lamc", [128, 2 * L], F32)
        sublnS = sb("sublnS", [128, L], F32)
        PS = [st.enter_context(nc.psum_tensor("ps%d" % i, [128, 512], F32)) for i in range(8)]
        PSB = S.bufs(8, "ps")
        for b_ in PSB:
            b_.excl = True

        ident = cmat[:, 0:128]
        ones = cmat[:, 128:256]
        rrot = cmat[:, 256:384]
        ustr = cmat[:, 384:512]
        thr = cmisc[:, 0:64]
        pcv = cmisc[:, 64:72]

        Xb = S.bufs(NT, "X")
        hTb = S.buf("hT")
        YRb = S.buf("YR")
        WSb = S.bufs(NSLOT, "WS")
        b_const = S.buf("const")
        b_memT = S.buf("memT")
        b_stat = S.buf("stat")

        hT = HT[:, :].rearrange("p (c t) -> p c t", c=KC)

        def carve(region, off, n, dt=BF16):
            if dt == BF16:
                return region[:, off:off + n], off + n
            assert off % 2 == 0
            return region[:, off:off + 2 * n].bitcast(dt), off + 2 * n

        psrr = [0]

        def psbank():
            i = psrr[0] % 8
            psrr[0] += 1
            return PS[i], PSB[i]

        def dma_sp(key, out, in_, reads=(), writes=()):
            return S.dma("sp", key, lambda e: e.dma_start(out=out, in_=in_), reads=reads, writes=writes)

        def dma_pool(key, out, in_, reads=(), writes=()):
            return S.dma("pool", key, lambda e: e.dma_start(out=out, in_=in_), reads=reads, writes=writes)

        def mm(out, lhsT, rhs, start, stop, reads, writes):
            S.op("pe", lambda e: e.matmul(out, lhsT=lhsT, rhs=rhs, start=start, stop=stop),
                 reads=reads, writes=writes)

        def tr(out, in_, reads, writes):
            S.op("pe", lambda e: e.transpose(out=out, in_=in_, identity=ident), reads=list(reads) + [b_const], writes=writes)

        def act(out, in_, func, reads, writes, scale=1.0, bias=None, accum=None):
            kw = {}
            if bias is not None:
                kw["bias"] = bias
            if accum is not None:
                kw["accum_out"] = accum
            S.op("act", lambda e: e.activation(out=out, in_=in_, func=func, scale=scale, **kw), reads=reads, writes=writes)

        def tt(eng, out, in0, in1, op, reads, writes):
            S.op(eng, lambda e: e.tensor_tensor(out=out, in0=in0, in1=in1, op=op), reads=reads, writes=writes)

        def ts(eng, out, in0, s1, op0, reads, writes, s2=None, op1=None, accum=None):
            kw = {}
            if accum is not None:
                kw["accum_out"] = accum
            if op1 is None:
                S.op(eng, lambda e: e.tensor_scalar(out=out, in0=in0, scalar1=s1, scalar2=None, op0=op0, **kw), reads=reads, writes=writes)
            else:
                S.op(eng, lambda e: e.tensor_scalar(out=out, in0=in0, scalar1=s1, scalar2=s2, op0=op0, op1=op1, **kw), reads=reads, writes=writes)

        def stt(eng, out, in0, scalar, in1, op0, op1, reads, writes):
            S.op(eng, lambda e: e.scalar_tensor_tensor(out=out, in0=in0, scalar=scalar, in1=in1, op0=op0, op1=op1), reads=reads, writes=writes)

        def cp(eng, out, in_, reads, writes):
            S.op(eng, lambda e: e.tensor_copy(out=out, in_=in_), reads=reads, writes=writes)

        dma_sp("c0", cmat[:, :], cmat_d[:, :], writes=[b_const])
        dma_sp("c1", gcols[:, :], gcols_d[:, :], writes=[b_const])
        dma_sp("c1", cmisc[:, :], cmisc_d[:, :], writes=[b_const])
        for i in range(NT):
            dma_sp("xin", X[:, i, :], x_in[i * 128:(i + 1) * 128, :], writes=[Xb[i]])

        b_big = S.buf("bigscratch")
        lamv, _ = carve(BIG, 0, L * 256, F32)
        ltmp, _ = carve(BIG, 2 * L * 256, 64, F32)
        dma_sp("c3", lamv, lamv_d[:, :], writes=[b_big])
        for li_, l in enumerate(layer_ids):
            base = li_ * 256
            for j in range(2):
                S.op("dve", lambda e, base=base, j=j: e.tensor_tensor(out=ltmp, in0=lamv[:, base + j * 128:base + j * 128 + 64],
                                                                      in1=lamv[:, base + j * 128 + 64:base + j * 128 + 128], op=ALU.mult),
                     reads=[b_big], writes=[b_big])
                S.op("dve", lambda e, j=j: e.tensor_reduce(out=stat[:, j:j + 1], in_=ltmp, axis=AX.X, op=ALU.add),
                     reads=[b_big], writes=[b_stat])
            act(stat[:, 2:4], stat[:, 0:2], AF.Exp, [b_stat], [b_stat])
            tt("dve", stat[:, 4:5], stat[:, 3:4], stat[:, 2:3], ALU.subtract, [b_stat], [b_stat])
            ts("dve", lamc[:, li_:li_ + 1], stat[:, 4:5], -lambda_init(l), ALU.add, [b_stat], [b_const])
            ts("dve", sublnS[:, li_:li_ + 1], gcols[:, li_ * GC_PER + GC_SUBLN:li_ * GC_PER + GC_SUBLN + 1],
               1.0 - lambda_init(l), ALU.mult, [b_const], [b_const])

        ssq = sb("ssq", [128, NT], F32)
        rstd = sb("rstd", [128, NT], F32)
        b_ssq = S.buf("ssq")
        b_rstd = S.buf("rstd")
        hn_t = [sb("hn%d" % i, [128, D], BF16) for i in range(2)]
        hn_b = S.bufs(2, "hn")

        def rstd_tiles(src_ap_fn, src_bufs, ntiles, inv_n):
            for i in range(ntiles):
                act(hn_t[0][:, :], src_ap_fn(i), AF.Square, [src_bufs[i]], [hn_b[0], b_ssq], accum=ssq[:, i:i + 1])
            ts("dve", rstd[:, 0:ntiles], ssq[:, 0:ntiles], inv_n, ALU.mult, [b_ssq], [b_rstd], s2=EPS, op1=ALU.add)
            act(rstd[:, 0:ntiles], rstd[:, 0:ntiles], AF.Ln, [b_rstd], [b_rstd])
            act(rstd[:, 0:ntiles], rstd[:, 0:ntiles], AF.Exp, [b_rstd], [b_rstd], scale=-0.5)

        def norm_to_fm(src_ap_fn, src_bufs, ntiles, gcol0, dstT, dst_buf, keep_rows=None):
            rstd_tiles(src_ap_fn, src_bufs, ntiles, 1.0 / D)
            for i in range(ntiles):
                if keep_rows is None:
                    hn, hb = hn_t[i % 2], hn_b[i % 2]
                    hn_ap = hn[:, :]
                else:
                    hn_ap, hb = keep_rows(i)
                act(hn_ap, src_ap_fn(i), AF.Copy, [src_bufs[i], b_rstd], [hb], scale=rstd[:, i:i + 1])
                pt, pb = psbank()
                ptb = pt[:, :].bitcast(BF16).rearrange("p (c t) -> p c t", t=128)
                for c in range(KC):
                    tr(ptb[:, c, :], hn_ap[:, c * 128:(c + 1) * 128], [hb], [pb])
                for c in range(KC):
                    eng = "dve" if c % 2 == 0 else "act"
                    if eng == "dve":
                        ts("dve", dstT[:, c, i * 128:(i + 1) * 128], ptb[:, c, :], gcols[:, gcol0 + c:gcol0 + c + 1], ALU.mult,
                           [pb, b_const], [dst_buf])
                    else:
                        act(dstT[:, c, i * 128:(i + 1) * 128], ptb[:, c, :], AF.Copy, [pb, b_const], [dst_buf],
                            scale=gcols[:, gcol0 + c:gcol0 + c + 1])

        memrows, _ = carve(BIG, 4096, 2 * D, F32)
        memrows = memrows.rearrange("p (i d) -> p i d", i=2)
        b_memrows = S.bufs(2, "memrows")
        for i in range(2):
            dma_sp("c2", memrows[:, i, :], mem_in[i * 128:(i + 1) * 128, :], writes=[b_memrows[i]])
        norm_to_fm(lambda i: memrows[:, i, :], b_memrows, 2, L * GC_PER, memT, b_memT)

        dbg_bufs = []
        steps = []

        def wload(slot, sbuf_, pieces):
            key = "w%d" % [i for i in range(NSLOT) if WS[i] is slot][0]
            for dst, src in pieces:
                dma_pool(key, dst(slot), src, writes=[sbuf_])

        def w3(slot, off, ncols):
            return slot[:, off:off + KC * ncols].rearrange("p (c n) -> p c n", c=KC)

        def dram_cols(w2d, c0, ncols):
            return w2d[:, c0:c0 + ncols].rearrange("(c p) n -> p c n", p=128)

        def layer_steps(li_, l):
            g0 = li_ * GC_PER
            win_l = w_in[li_]
            off = 0
            cosb, off = carve(BIG, off, S_LEN)
            sinb, off = carve(BIG, off, S_LEN)
            qT, off = carve(BIG, off, S_LEN)
            kT, off = carve(BIG, off, S_LEN)
            vtm, off = carve(BIG, off, NT * 128)
            vtm = vtm.rearrange("p (i e) -> p i e", i=NT)
            NE = 6
            Et = []
            for _ in range(NE):
                a, off = carve(BIG, off, 512)
                Et.append(a)
            NTMP = 6
            Tm = []
            for _ in range(NTMP):
                a, off = carve(BIG, off, 512, F32)
                Tm.append(a)
            assert off <= NBIG, off
            b_cs = S.buf("cossin")
            dbg_bufs.append(b_cs)
            b_q = S.buf("qT")
            b_k = S.buf("kT")
            b_v = S.buf("v")
            Eb = S.bufs(NE, "E")
            Tb = S.bufs(NTMP, "T")
            rrE = [0]
            rrT = [0]

            def getE():
                i = rrE[0] % NE
                rrE[0] += 1
                return Et[i], Eb[i]

            def getT():
                i = rrT[0] % NTMP
                rrT[0] += 1
                return Tm[i], Tb[i]

            yT = YR[:, :].rearrange("p (c t) -> p c t", c=KC)
            b_y = [S.buf("y%d" % c) for c in range(KC)]

            def mixer_begin(slot, sbuf_):
                S.alias([b_cs, b_q, b_k, b_v] + Eb + Tb, [b_big])
                S.alias(b_y, [YRb])
                dma_sp("cs", cosb, cossin_d[:, 0:S_LEN], writes=[b_cs])
                dma_sp("cs", sinb, cossin_d[:, S_LEN:2 * S_LEN], writes=[b_cs])
                if os.environ.get('CSTOUCH'):
                    cp("dve", stat[:, 60:61], cosb[:, 0:1], [b_cs], [b_stat])
                norm_to_fm(lambda i: X[:, i, :], Xb, NT, g0 + GC_MIX, hT, hTb)
                for r in range(NBLK):
                    for hh in range(2):
                        dma_sp("xbz", xb_d[r * 128:(r + 1) * 128, hh * 512:(hh + 1) * 512], ztile[:, :], reads=[b_zt], writes=[b_xb])

            steps.append((None, mixer_begin))

            def head_step(h):
                def load(slot, sbuf_):
                    wload(slot, sbuf_, [
                        (lambda s: w3(s, 0, 128), dram_cols(win_l, h * 128, 128)),
                        (lambda s: w3(s, 1024, 128), dram_cols(win_l, 1024 + h * 128, 128)),
                        (lambda s: w3(s, 2048, 128), dram_cols(win_l, 2048 + h * 128, 128)),
                    ])

                def compute(slot, sbuf_):
                    wq = w3(slot, 0, 128)
                    wk = w3(slot, 1024, 128)
                    wv = w3(slot, 2048, 128)
                    for (wt, dst, db) in ((wq, qT, b_q), (wk, kT, b_k)):
                        for tb in range(4):
                            tsl = slice(tb * 512, (tb + 1) * 512)
                            pp, pb = psbank()
                            for c in range(KC):
                                mm(pp[:, :], wt[:, c, :], hT[:, c, tsl], c == 0, c == KC - 1, [sbuf_, hTb], [pb])
                            qb, qbb = getE()
                            act(qb, pp[:, :], AF.Copy, [pb], [qbb])
                            if HEADCUT == 0:
                                continue
                            ROT = int(os.environ.get('ROT', '9'))
                            p2, p2b = psbank()
                            P2V = os.environ.get('P2V', '')
                            if P2V == 'ident':
                                mm(p2[:, :], ident, qb, True, True, [b_const, qbb], [p2b])
                            elif P2V == 'rhs':
                                mm(p2[:, :], rrot, hT[:, 0, tsl], True, True, [b_const, hTb], [p2b])
                            elif P2V == 'evac':
                                mm(p2[:, :], rrot, qb, True, True, [b_const, qbb], [p2b])
                                t9, t9b = getT()
                                cp("dve", t9, p2[:, :], [p2b], [t9b])
                            else:
                                mm(p2[:, :], rrot, qb, True, True, [b_const, qbb], [p2b])
                            if ROT < 2:
                                continue
                            t1, t1b = getT()
                            if os.environ.get('ROTV') == 'sb':
                                tt("dve", t1, qb, cosb[:, tsl], ALU.mult, [qbb, b_cs], [t1b])
                            elif os.environ.get('ROTV') == 'hT':
                                tt("dve", t1, pp[:, :], hT[:, 0, tsl], ALU.mult, [pb, hTb], [t1b])
                            elif os.environ.get('ROTV') == 'nocs':
                                tt("dve", t1, pp[:, :], qb, ALU.mult, [pb, qbb], [t1b])
                            else:
                                tt("dve", t1, pp[:, :], cosb[:, tsl], ALU.mult, [pb, b_cs], [t1b])
                            if ROT < 3:
                                continue
                            t2, t2b = getT()
                            tt("dve", t2, p2[:, :], sinb[:, tsl], ALU.mult, [p2b, b_cs], [t2b])
                            if ROT < 4:
                                continue
                            tt("pool", dst[:, tsl], t1, t2, ALU.add, [t1b, t2b], [db])
                    if HEADCUT < 2:
                        return
                    for i4 in range(4):
                        pp, pb = psbank()
                        for ii in range(4):
                            i = i4 * 4 + ii
                            for c in range(KC):
                                mm(pp[:, ii * 128:(ii + 1) * 128], hT[:, c, i * 128:(i + 1) * 128], wv[:, c, :],
                                   c == 0, c == KC - 1, [sbuf_, hTb], [pb])
                        act(vtm[:, i4 * 4:(i4 + 1) * 4, :], pp[:, :].rearrange("p (i e) -> p i e", i=4), AF.Copy, [pb], [b_v])
                    if HEADCUT < 3:
                        return
                    for qb_ in range(4 if HEADCUT > 4 else 1):
                        qsl = slice(qb_ * 512, (qb_ + 1) * 512)
                        pO = [(PS[4], PSB[4]), (PS[5], PSB[5])]
                        pR = [(PS[6], PSB[6]), (PS[7], PSB[7])]
                        its = [(kb, c) for kb in range(NT) for c in range(2)]
                        LA = 3
                        Es = {}
                        for i in range(len(its) + LA):
                            if i < len(its):
                                kb, c = its[i]
                                pS, pSb = psS[i % 4]
                                mm(pS[:, :], kT[c * 64:(c + 1) * 64, kb * 128:(kb + 1) * 128], qT[c * 64:(c + 1) * 64, qsl],
                                   True, True, [b_k, b_q], [pSb])
                                E, Eb_ = getE()
                                act(E, pS[:, :], AF.Exp, [pSb], [Eb_], scale=0.125)
                                Es[i] = (E, Eb_)
                            j = i - LA
                            if j >= 0:
                                kb, c = its[j]
                                E, Eb_ = Es.pop(j)
                                mm(pO[c][0][:, :], vtm[:, kb, :], E, kb == 0, kb == NT - 1, [b_v, Eb_], [pO[c][1]])
                                mm(pR[c][0][:, :], ones, E, kb == 0, kb == NT - 1, [b_const, Eb_], [pR[c][1]])
                        if HEADCUT < 4:
                            continue
                        ri = []
                        for c in range(2):
                            r_, rb_ = getT()
                            S.op("dve", lambda e, r_=r_, c=c: e.reciprocal(out=r_, in_=pR[c][0][:, :]), reads=[pR[c][1]], writes=[rb_])
                            ri.append((r_, rb_))
                        t0, t0b = getT()
                        tt("dve", t0, pO[0][0][:, :], ri[0][0], ALU.mult, [pO[0][1], ri[0][1]], [t0b])
                        t1, t1b = getT()
                        tt("dve", t1, pO[1][0][:, :], ri[1][0], ALU.mult, [pO[1][1], ri[1][1]], [t1b])
                        o_, ob_ = getT()
                        stt("dve", o_, t1, lamc[:, li_:li_ + 1], t0, ALU.mult, ALU.add, [t1b, t0b, b_const], [ob_])
                        sq, sqb = getE()
                        tt("pool", sq, o_, o_, ALU.mult, [ob_], [sqb])
                        pq, pqb = psS[0]
                        mm(pq[:, :], ones, sq, True, True, [b_const, sqb], [pqb])
                        rs, rsb = getT()
                        ts("dve", rs, pq[:, :], 1.0 / 128, ALU.mult, [pqb], [rsb], s2=EPS, op1=ALU.add)
                        act(rs, rs, AF.Ln, [rsb], [rsb])
                        act(rs, rs, AF.Exp, [rsb], [rsb], scale=-0.5)
                        stt("dve", yT[:, h, qsl], o_, sublnS[:, li_:li_ + 1], rs, ALU.mult, ALU.mult, [ob_, rsb, b_const], [b_y[h]])

                return load, compute

            psS = [(PS[i], PSB[i]) for i in range(4)]

            for h in range(8):
                steps.append(head_step(h))

            offc = 0
            Mreg, offc = carve(BIG, offc, KC * S_LEN)
            Mv = Mreg.rearrange("p (c t) -> p c t", c=KC)
            CT = []
            for _ in range(3):
                a, offc = carve(BIG, offc, 512, F32)
                CT.append(a)
            assert offc <= NBIG
            b_M = [S.buf("M%d" % c) for c in range(KC)]
            CTb = S.bufs(3, "CT")
            rrC = [0]

            def getC():
                i = rrC[0] % 3
                rrC[0] += 1
                return CT[i], CTb[i]

            def phaseC_begin(slot, sbuf_):
                S.alias(b_M + CTb, [b_cs, b_q, b_k, b_v] + Eb + Tb)

            steps.append((None, phaseC_begin))

            def branch_step(n, j):
                def load(slot, sbuf_):
                    wload(slot, sbuf_, [
                        (lambda s: w3(s, 0, 128), dram_cols(w_br[li_, n], j * 128, 128)),
                        (lambda s: w3(s, 1024, 128), dram_cols(win_l, 6144 + n * 1024 + j * 128, 128)),
                    ])

                def compute(slot, sbuf_):
                    wb = w3(slot, 0, 128)
                    wg = w3(slot, 1024, 128)
                    for tb in range(4):
                        tsl = slice(tb * 512, (tb + 1) * 512)
                        pg, pgb = psbank()
                        for c in range(KC):
                            mm(pg[:, :], wg[:, c, :], hT[:, c, tsl], c == 0, c == KC - 1, [sbuf_, hTb], [pgb])
                        pbr, pbrb = psbank()
                        for c in range(KC):
                            mm(pbr[:, :], wb[:, c, :], yT[:, c, tsl], c == 0, c == KC - 1, [sbuf_, b_y[c]], [pbrb])
                        sg, sgb = getC()
                        act(sg, pg[:, :], AF.Sigmoid, [pgb], [sgb])
                        if n == 0:
                            tt("dve", Mv[:, j, tsl], sg, pbr[:, :], ALU.mult, [sgb, pbrb], [b_M[j]])
                        else:
                            t_, tb_ = getC()
                            tt("dve", t_, sg, pbr[:, :], ALU.mult, [sgb, pbrb], [tb_])
                            tt("pool", Mv[:, j, tsl], Mv[:, j, tsl], t_, ALU.add, [b_M[j], tb_], [b_M[j]])

                return load, compute

            for j in range(KC):
                steps.append(branch_step(0, j))


            def conv_step(j):
                def load(slot, sbuf_):
                    wload(slot, sbuf_, [
                        (lambda s: w3(s, 0, 128), dram_cols(win_l, 3072 + j * 128, 128)),
                        (lambda s: w3(s, 1024, 128), dram_cols(win_l, 4096 + j * 128, 128)),
                        (lambda s: w3(s, 2048, 128), dram_cols(win_l, 5120 + j * 128, 128)),
                    ])

                def compute(slot, sbuf_):
                    wcb = w3(slot, 0, 128)
                    wcc = w3(slot, 1024, 128)
                    wcx = w3(slot, 2048, 128)
                    u = U_pad
                    for tb in range(4):
                        tsl = slice(tb * 512, (tb + 1) * 512)
                        pc_, pcb = psbank()
                        for c in range(KC):
                            mm(pc_[:, :], wcc[:, c, :], hT[:, c, tsl], c == 0, c == KC - 1, [sbuf_, hTb], [pcb])
                        px, pxb = psbank()
                        for c in range(KC):
                            mm(px[:, :], wcx[:, c, :], hT[:, c, tsl], c == 0, c == KC - 1, [sbuf_, hTb], [pxb])
                        t_, tb_ = getC()
                        act(t_, pc_[:, :], AF.Copy, [pcb], [tb_])
                        tt("dve", u[:, 1 + tb * 512:1 + (tb + 1) * 512], t_, px[:, :], ALU.mult, [tb_, pxb], [b_u])
                    cw = g0 + GC_CONV
                    for tb in range(4):
                        tsl = slice(tb * 512, (tb + 1) * 512)
                        acc, accb = getC()
                        act(acc, u[:, 1 + tb * 512:1 + (tb + 1) * 512], AF.Copy, [b_u, b_const], [accb], scale=gcols[:, cw + 8 + j:cw + 8 + j + 1])
                        stt("dve", acc, u[:, tb * 512:(tb + 1) * 512], gcols[:, cw + j:cw + j + 1], acc, ALU.mult, ALU.add, [b_u, accb, b_const], [accb])
                        stt("dve", acc, u[:, 2 + tb * 512:2 + (tb + 1) * 512], gcols[:, cw + 16 + j:cw + 16 + j + 1], acc, ALU.mult, ALU.add, [b_u, accb, b_const], [accb])
                        pb_, pbb = psbank()
                        for c in range(KC):
                            mm(pb_[:, :], wcb[:, c, :], hT[:, c, tsl], c == 0, c == KC - 1, [sbuf_, hTb], [pbb])
                        tt("dve", yT[:, j, tsl], pb_[:, :], acc, ALU.mult, [pbb, accb], [b_y[j]])

                return load, compute

            for j in range(KC):
                steps.append(conv_step(j))
            for j in range(KC):
                steps.append(branch_step(1, j))

            def out_proj_step(wmat, half, srcT, src_bufs):
                def load(slot, sbuf_):
                    wload(slot, sbuf_, [(lambda s: w3(s, 0, 512), dram_cols(wmat, half * 512, 512))])

                def compute(slot, sbuf_):
                    wt = w3(slot, 0, 512)
                    for i in range(NT):
                        pp, pb = psbank()
                        for c in range(KC):
                            mm(pp[:, :], srcT[:, c, i * 128:(i + 1) * 128], wt[:, c, :], c == 0, c == KC - 1, [sbuf_, src_bufs[c]], [pb])
                        tt("dve", X[:, i, half * 512:(half + 1) * 512], X[:, i, half * 512:(half + 1) * 512], pp[:, :], ALU.add,
                           [Xb[i], pb], [Xb[i]])

                return load, compute

            for half in range(2):
                steps.append(out_proj_step(w_o[li_], half, Mv, b_M))

            def mixer_end(slot, sbuf_):
                S.alias([b_big], b_M + CTb)
                S.alias([YRb], b_y)

            steps.append((None, mixer_end))
            if stop_after == (l, "mix"):
                return True

            offx = 0
            kcT, offx = carve(BIG, offx, KC * NMEM)
            kcT = kcT.rearrange("p (c m) -> p c m", c=KC)
            vc, offx = carve(BIG, offx, 2 * D)
            vc = vc.rearrange("p (i d) -> p i d", i=2)
            XE = []
            for _ in range(6):
                a, offx = carve(BIG, offx, 512)
                XE.append(a)
            XT = []
            for _ in range(4):
                a, offx = carve(BIG, offx, 512, F32)
                XT.append(a)
            assert offx <= NBIG
            b_kc = S.buf("kcT")
            b_vc = S.buf("vc")
            XEb = S.bufs(6, "XE")
            XTb = S.bufs(4, "XT")
            rrx = [0, 0]

            def getXE():
                i = rrx[0] % 6
                rrx[0] += 1
                return XE[i], XEb[i]

            def getXT():
                i = rrx[1] % 4
                rrx[1] += 1
                return XT[i], XTb[i]

            qcT = YR[:, :].rearrange("p (c t) -> p c t", c=KC)
            b_qc = [S.buf("qc%d" % c) for c in range(KC)]
            ocT = HT[:, :].rearrange("p (c t) -> p c t", c=KC)
            b_oc = [S.buf("oc%d" % c) for c in range(KC)]

            def cross_begin(slot, sbuf_):
                S.alias([b_kc, b_vc] + XEb + XTb, [b_big])
                S.alias(b_qc, [YRb])
                norm_to_fm(lambda i: X[:, i, :], Xb, NT, g0 + GC_CROSS, hT, hTb)

            steps.append((None, cross_begin))

            def ckv_step(part):
                def load(slot, sbuf_):
                    wload(slot, sbuf_, [(lambda s: w3(s, 0, 512), dram_cols(w_ckv[li_], part * 512, 512))])

                def compute(slot, sbuf_):
                    wt = w3(slot, 0, 512)
                    if part < 2:
                        for jj in range(4):
                            j = part * 4 + jj
                            pp, pb = psbank()
                            for c in range(KC):
                                mm(pp[:, 0:NMEM], wt[:, c, jj * 128:(jj + 1) * 128], memT[:, c, :], c == 0, c == KC - 1, [sbuf_, b_memT], [pb])
                            act(kcT[:, j, :], pp[:, 0:NMEM], AF.Copy, [pb], [b_kc])
                    else:
                        for i in range(2):
                            pp, pb = psbank()
                            for c in range(KC):
                                mm(pp[:, :], memT[:, c, i * 128:(i + 1) * 128], wt[:, c, :], c == 0, c == KC - 1, [sbuf_, b_memT], [pb])
                            act(vc[:, i, (part - 2) * 512:(part - 1) * 512], pp[:, :], AF.Copy, [pb], [b_vc])

                return load, compute

            for part in range(4):
                steps.append(ckv_step(part))

            def cq_step(half):
                def load(slot, sbuf_):
                    wload(slot, sbuf_, [(lambda s: w3(s, 0, 512), dram_cols(w_cq[li_], half * 512, 512))])

                def compute(slot, sbuf_):
                    wt = w3(slot, 0, 512)
                    for jj in range(4):
                        j = half * 4 + jj
                        for tb in range(4):
                            tsl = slice(tb * 512, (tb + 1) * 512)
                            pp, pb = psbank()
                            for c in range(KC):
                                mm(pp[:, :], wt[:, c, jj * 128:(jj + 1) * 128], hT[:, c, tsl], c == 0, c == KC - 1, [sbuf_, hTb], [pb])
                            if (jj + tb) % 2 == 0:
                                act(qcT[:, j, tsl], pp[:, :], AF.Copy, [pb], [b_qc[j]])
                            else:
                                cp("dve", qcT[:, j, tsl], pp[:, :], [pb], [b_qc[j]])

                return load, compute

            for half in range(2):
                steps.append(cq_step(half))

            def cross_attn(slot, sbuf_):
                S.alias(b_oc, [hTb])
                for h in range(4):
                    for tb in range(4):
                        tsl = slice(tb * 512, (tb + 1) * 512)
                        pO = [psbank(), psbank()]
                        pR = psbank()
                        for mb in range(2):
                            pS, pSb = psbank()
                            for cc in range(2):
                                mm(pS[:, :], kcT[:, 2 * h + cc, mb * 128:(mb + 1) * 128], qcT[:, 2 * h + cc, tsl], cc == 0, cc == 1,
                                   [b_kc, b_qc[2 * h + cc]], [pSb])
                            E, Eb_ = getXE()
                            act(E, pS[:, :], AF.Exp, [pSb], [Eb_], scale=1.0 / 16)
                            for ee in range(2):
                                mm(pO[ee][0][:, :], vc[:, mb, h * 256 + ee * 128:h * 256 + (ee + 1) * 128], E, mb == 0, mb == 1,
                                   [b_vc, Eb_], [pO[ee][1]])
                            mm(pR[0][:, :], ones, E, mb == 0, mb == 1, [b_const, Eb_], [pR[1]])
                        r_, rb_ = getXT()
                        S.op("dve", lambda e, r_=r_, pR=pR: e.reciprocal(out=r_, in_=pR[0][:, :]), reads=[pR[1]], writes=[rb_])
                        for ee in range(2):
                            tt("dve", ocT[:, 2 * h + ee, tsl], pO[ee][0][:, :], r_, ALU.mult, [pO[ee][1], rb_], [b_oc[2 * h + ee]])

            steps.append((None, cross_attn))
            for half in range(2):
                steps.append(out_proj_step(w_co[li_], half, ocT, b_oc))

            def cross_end(slot, sbuf_):
                S.alias([b_big], [b_kc, b_vc] + XEb + XTb)
                S.alias([YRb], b_qc)
                S.alias([hTb], b_oc)

            steps.append((None, cross_end))
            if stop_after == (l, "cross"):
                return True

            hrows = YR[:, :].rearrange("p (i d) -> p i d", i=NT)
            b_hr = S.bufs(NT, "hrows")
            offm = 0
            oh12, offm = carve(BIG, offm, NT * 64, F32)
            oh12 = oh12.rearrange("p (i e) -> p i e", i=NT)
            mohb, offm = carve(BIG, offm, NT * 32)
            mohb = mohb.rearrange("p (i e) -> p i e", i=NT)
            wts, offm = carve(BIG, offm, NT * 2, F32)
            wts = wts.rearrange("p (i k) -> p i k", i=NT)
            desti, offm = carve(BIG, offm, NT * 2, I32)
            desti = desti.rearrange("p (i k) -> p i k", i=NT)
            rt, offm = carve(BIG, offm, 320, F32)
            cnt, offm = carve(BIG, offm, 32, F32)
            padA, offm = carve(BIG, offm, 32, F32)
            padB, offm = carve(BIG, offm, 32, F32)
            pstart, offm = carve(BIG, offm, 32, F32)
            pend, offm = carve(BIG, offm, 32, F32)
            eblk, offm = carve(BIG, offm, NBLK, F32)
            idxgu, offm = carve(BIG, offm, NBLK * 8, I32)
            idxd, offm = carve(BIG, offm, NBLK * 8, I32)
            idxgu = idxgu.rearrange("p (b c) -> p b c", b=NBLK)
            idxd = idxd.rearrange("p (b c) -> p b c", b=NBLK)
            idxgu_f, offt = carve(BIG, offm, NBLK * 8, F32)
            idxd_f, offt = carve(BIG, offt, NBLK * 8, F32)
            idxgu_f = idxgu_f.rearrange("p (b c) -> p b c", b=NBLK)
            idxd_f = idxd_f.rearrange("p (b c) -> p b c", b=NBLK)
            offm0 = offm
            b_rt = S.buf("rt")
            b_oh = S.buf("oh")
            b_route = S.buf("route")

            def moe_begin(slot, sbuf_):
                S.alias([b_rt, b_oh, b_route], [b_big])
                S.alias(b_hr, [YRb])

            steps.append((None, moe_begin))

            def router_step():
                def load(slot, sbuf_):
                    wload(slot, sbuf_, [(lambda s: s[:, 0:KC * 36].rearrange("p (c n) -> p c n", c=KC),
                                         w_rt[li_].rearrange("(c p) n -> p c n", p=128))])

                def compute(slot, sbuf_):
                    wr = slot[:, 0:KC * 36].rearrange("p (c n) -> p c n", c=KC)
                    norm_to_fm(lambda i: X[:, i, :], Xb, NT, g0 + GC_FFN, hT, hTb,
                               keep_rows=lambda i: (hrows[:, i, :], b_hr[i]))
                    lg = rt[:, 0:36]
                    for i in range(NT):
                        pp, pb = psbank()
                        for c in range(KC):
                            mm(pp[:, 0:36], hT[:, c, i * 128:(i + 1) * 128], wr[:, c, :], c == 0, c == KC - 1, [sbuf_, hTb], [pb])
                        R_ = [b_rt]
                        act(lg, pp[:, 0:36], AF.Copy, [pb], R_)
                        mxg = rt[:, 40:41]
                        S.op("dve", lambda e, mxg=mxg: e.tensor_reduce(out=mxg, in_=rt[:, 0:4], axis=AX.X, op=ALU.max), reads=R_, writes=R_)
                        ohg = rt[:, 44:48]
                        ts("dve", ohg, rt[:, 0:4], mxg, ALU.is_equal, R_, R_)
                        nmx = rt[:, 41:42]
                        ts("dve", nmx, mxg, -1.0, ALU.mult, R_, R_)
                        sumg = rt[:, 42:43]
                        act(rt[:, 48:52], rt[:, 0:4], AF.Exp, R_, R_, bias=nmx, accum=sumg)
                        gw = rt[:, 43:44]
                        S.op("dve", lambda e, gw=gw, sumg=sumg: e.reciprocal(out=gw, in_=sumg), reads=R_, writes=R_)
                        pen = rt[:, 52:56]
                        ts("dve", pen, ohg, -1.0, ALU.add, R_, R_, s2=-NEG, op1=ALU.mult)
                        lem = rt[:, 64:96]
                        for g in range(4):
                            ts("dve", lem[:, g * 8:(g + 1) * 8], rt[:, 4 + g * 8:4 + (g + 1) * 8], pen[:, g:g + 1], ALU.add, R_, R_)
                        m1 = rt[:, 56:57]
                        S.op("dve", lambda e, m1=m1, lem=lem: e.tensor_reduce(out=m1, in_=lem, axis=AX.X, op=ALU.max), reads=R_, writes=R_)
                        ts("dve", oh12[:, i, 0:32], lem, m1, ALU.is_equal, R_, [b_oh])
                        lem2 = rt[:, 96:128]
                        stt("dve", lem2, oh12[:, i, 0:32], NEG, lem, ALU.mult, ALU.add, [b_oh, b_rt], R_)
                        m2 = rt[:, 57:58]
                        S.op("dve", lambda e, m2=m2, lem2=lem2: e.tensor_reduce(out=m2, in_=lem2, axis=AX.X, op=ALU.max), reads=R_, writes=R_)
                        ts("dve", oh12[:, i, 32:64], lem2, m2, ALU.is_equal, R_, [b_oh])
                        dd = rt[:, 58:59]
                        tt("dve", dd, m2, m1, ALU.subtract, R_, R_)
                        ed = rt[:, 59:60]
                        act(ed, dd, AF.Exp, R_, R_)
                        den = rt[:, 60:61]
                        ts("dve", den, ed, 1.0, ALU.add, R_, R_)
                        w1 = rt[:, 61:62]
                        S.op("dve", lambda e, w1=w1, den=den: e.reciprocal(out=w1, in_=den), reads=R_, writes=R_)
                        tt("dve", wts[:, i, 0:1], w1, gw, ALU.mult, R_, [b_oh])
                        w2 = rt[:, 62:63]
                        tt("dve", w2, ed, w1, ALU.mult, R_, R_)
                        tt("dve", wts[:, i, 1:2], w2, gw, ALU.mult, R_, [b_oh])
                        tt("dve", mohb[:, i, :], oh12[:, i, 0:32], oh12[:, i, 32:64], ALU.add, [b_oh], [b_oh])
                    pc_, pcb = psbank()
                    for i in range(NT):
                        mm(pc_[:, 0:32], ones, mohb[:, i, :], i == 0, i == NT - 1, [b_const, b_oh], [pcb])
                    Q_ = [b_route]
                    cp("dve", cnt, pc_[:, 0:32], [pcb], Q_)
                    ts("dve", padA, cnt, 1.0 / 128, ALU.mult, Q_, Q_, s2=127.0 / 128 - 0.49609375, op1=ALU.add)
                    cp("dve", padB.bitcast(I32), padA, Q_, Q_)
                    cp("dve", padA, padB.bitcast(I32), Q_, Q_)
                    ts("dve", padA, padA, 128.0, ALU.mult, Q_, Q_)
                    cp("dve", pstart, padA, Q_, Q_)
                    a, b_ = padA, padB
                    for s_ in (1, 2, 4, 8, 16):
                        cp("dve", b_[:, 0:s_], a[:, 0:s_], Q_, Q_)
                        tt("dve", b_[:, s_:32], a[:, s_:32], a[:, 0:32 - s_], ALU.add, Q_, Q_)
                        a, b_ = b_, a
                    cp("dve", pend, a, Q_, Q_)
                    tt("dve", pstart, pend, pstart, ALU.subtract, Q_, Q_)
                    for i in range(NT):
                        pp, pb = psbank()
                        mm(pp[:, 0:32], ustr, mohb[:, i, :], True, i == 0, [b_const, b_oh], [pb])
                        for j in range(i):
                            mm(pp[:, 0:32], ones, mohb[:, j, :], False, j == i - 1, [b_const, b_oh], [pb])
                        tmp = rt[:, 128:160]
                        tt("dve", tmp, pp[:, 0:32], pstart, ALU.add, [pb, b_route], [b_rt])
                        for k in range(2):
                            prod = rt[:, 160:192]
                            df = rt[:, 192 + k:193 + k]
                            tt("dve", prod, tmp, oh12[:, i, k * 32:(k + 1) * 32], ALU.mult, [b_rt, b_oh], [b_rt])
                            S.op("dve", lambda e, df=df, prod=prod: e.tensor_reduce(out=df, in_=prod, axis=AX.X, op=ALU.add), reads=[b_rt], writes=[b_rt])
                            cp("dve", desti[:, i, k:k + 1], df, [b_rt], [b_route])
                    S.op("dve", lambda e: e.memset(eblk, 0.0), writes=Q_)
                    for e_ in range(NEXP):
                        stt("dve", eblk, thr, pend[:, e_:e_ + 1], eblk, ALU.is_ge, ALU.add, [b_const] + Q_, Q_)
                    ts("dve", eblk, eblk, float(NEXP - 1), ALU.min, Q_, Q_, s2=float(li_ * NEXP), op1=ALU.add)
                    for c in range(8):
                        ts("dve", idxgu_f[:, :, c], eblk, float(D), ALU.mult, Q_, Q_, s2=pcv[:, c:c + 1], op1=ALU.add)
                    for c in range(6):
                        ts("dve", idxd_f[:, :, c], eblk, float(DEXP), ALU.mult, Q_, Q_, s2=pcv[:, c:c + 1], op1=ALU.add)
                    cp("dve", idxgu[:, :, :], idxgu_f[:, :, :], Q_, Q_)
                    cp("dve", idxd[:, :, 0:6], idxd_f[:, :, 0:6], Q_, Q_)
                    for i in range(NT):
                        for k in range(2):
                            S.dma("pool", "scat", lambda e, i=i, k=k: e.indirect_dma_start(
                                out=xb_d[:, :], out_offset=bass.IndirectOffsetOnAxis(ap=desti[:, i, k:k + 1], axis=0),
                                in_=hrows[:, i, :], in_offset=None), reads=[b_hr[i], b_route], writes=[b_xb])

                return load, compute

            steps.append(router_step())

            Gw = [HT[:, g * 6144:(g + 1) * 6144].rearrange("p (c n) -> p c n", c=8) for g in range(2)]
            xg_t = [HT[:, 12288 + i * 1024:12288 + (i + 1) * 1024] for i in range(2)]
            xgT_t = [HT[:, 14336 + i * 1024:14336 + (i + 1) * 1024].rearrange("p (c t) -> p c t", c=8) for i in range(2)]
            Uw = [YR[:, g * 6144:(g + 1) * 6144].rearrange("p (c n) -> p c n", c=8) for g in range(2)]
            ybt = [YR[:, 12288 + i * 2048:12288 + (i + 1) * 2048].bitcast(F32) for i in range(2)]
            offd = offm0 + (offm0 % 2)
            Dw = []
            for g in range(2):
                a, offd = carve(BIG, offd, 6 * D)
                Dw.append(a.rearrange("p (c n) -> p c n", c=6))
            sgt = U_pad[:, 2:2 + 2 * DEXP].bitcast(F32)
            hmt = hn_t[0][:, 0:DEXP]
            hmT = [hn_t[1][:, 0:DEXP].rearrange("p (c t) -> p c t", c=6)]
            a, offd = carve(BIG, offd, DEXP)
            hmT.append(a.rearrange("p (c t) -> p c t", c=6))
            assert offd <= NBIG, offd
            b_G = S.bufs(2, "Gw")
            b_U = S.bufs(2, "Uw")
            b_D = S.bufs(2, "Dw")
            b_xg = S.bufs(2, "xg")
            b_xgT = S.bufs(2, "xgT")
            b_ybt = S.bufs(2, "ybt")
            b_sg = S.buf("sg")
            b_hm = S.buf("hm")
            b_hmT = S.bufs(2, "hmT")
            weg = w_eg.rearrange("l e d n -> (l e d) n")
            weu = w_eu.rearrange("l e d n -> (l e d) n")
            wed = w_ed.rearrange("l e d n -> (l e d) n")

            def moe_blocks(slot, sbuf_):
                S.alias(b_G + b_xg + b_xgT, [hTb])
                S.alias(b_U + b_ybt, b_hr)
                S.alias(b_D + [b_hmT[1]], [b_big, b_route])
                S.alias([b_sg], [b_u])
                S.alias([b_hm, b_hmT[0]], hn_b)

                def issue_loads(b):
                    p = b % 2
                    dma_sp("xg%d" % p, xg_t[p], xb_d[b * 128:(b + 1) * 128, :], reads=[b_xb], writes=[b_xg[p]])
                    for c in range(8):
                        S.dma("pool", "G%d" % p, lambda e, c=c, p=p, b=b: e.indirect_dma_start(
                            out=Gw[p][:, c, :], out_offset=None, in_=weg,
                            in_offset=bass.IndirectOffsetOnAxis(ap=idxgu[:, b, c:c + 1], axis=0)), reads=[b_route], writes=[b_G[p]])
                    for c in range(8):
                        S.dma("pool", "U%d" % p, lambda e, c=c, p=p, b=b: e.indirect_dma_start(
                            out=Uw[p][:, c, :], out_offset=None, in_=weu,
                            in_offset=bass.IndirectOffsetOnAxis(ap=idxgu[:, b, c:c + 1], axis=0)), reads=[b_route], writes=[b_U[p]])
                    for c in range(6):
                        S.dma("pool", "D%d" % p, lambda e, c=c, p=p, b=b: e.indirect_dma_start(
                            out=Dw[p][:, c, :], out_offset=None, in_=wed,
                            in_offset=bass.IndirectOffsetOnAxis(ap=idxd[:, b, c:c + 1], axis=0)), reads=[b_route], writes=[b_D[p]])

                issue_loads(0)
                for b in range(NBLK):
                    p = b % 2
                    if b + 1 < NBLK:
                        issue_loads(b + 1)
                    pt, pb = psbank()
                    ptb = pt[:, :].bitcast(BF16).rearrange("p (c t) -> p c t", t=128)
                    for c in range(KC):
                        tr(ptb[:, c, :], xg_t[p][:, c * 128:(c + 1) * 128], [b_xg[p]], [pb])
                    for c in range(KC):
                        gc = gcols[:, g0 + GC_FFN + c:g0 + GC_FFN + c + 1]
                        if c % 2 == 0:
                            ts("dve", xgT_t[p][:, c, :], ptb[:, c, :], gc, ALU.mult, [pb, b_const], [b_xgT[p]])
                        else:
                            act(xgT_t[p][:, c, :], ptb[:, c, :], AF.Copy, [pb, b_const], [b_xgT[p]], scale=gc)
                    for (n0, nn) in ((0, 512), (512, 256)):
                        pg, pgb = psbank()
                        for c in range(KC):
                            mm(pg[:, 0:nn], xgT_t[p][:, c, :], Gw[p][:, c, n0:n0 + nn], c == 0, c == KC - 1, [b_xgT[p], b_G[p]], [pgb])
                        pu, pub = psbank()
                        for c in range(KC):
                            mm(pu[:, 0:nn], xgT_t[p][:, c, :], Uw[p][:, c, n0:n0 + nn], c == 0, c == KC - 1, [b_xgT[p], b_U[p]], [pub])
                        act(sgt[:, n0:n0 + nn], pg[:, 0:nn], AF.Silu, [pgb], [b_sg])
                        tt("dve", hmt[:, n0:n0 + nn], sgt[:, n0:n0 + nn], pu[:, 0:nn], ALU.mult, [b_sg, pub], [b_hm])
                    pt2, pb2 = psbank()
                    pt2b = pt2[:, :].bitcast(BF16).rearrange("p (c t) -> p c t", t=128)
                    for c in range(6):
                        tr(pt2b[:, c, :], hmt[:, c * 128:(c + 1) * 128], [b_hm], [pb2])
                    act(hmT[p][:, 0:3, :], pt2b[:, 0:3, :], AF.Copy, [pb2], [b_hmT[p]])
                    cp("dve", hmT[p][:, 3:6, :], pt2b[:, 3:6, :], [pb2], [b_hmT[p]])
                    for n in range(2):
                        py, pyb = psbank()
                        for c in range(6):
                            mm(py[:, :], hmT[p][:, c, :], Dw[p][:, c, n * 512:(n + 1) * 512], c == 0, c == 5, [b_hmT[p], b_D[p]], [pyb])
                        if n == 0:
                            act(ybt[p][:, 0:512], py[:, :], AF.Copy, [pyb], [b_ybt[p]])
                        else:
                            cp("dve", ybt[p][:, 512:1024], py[:, :], [pyb], [b_ybt[p]])
                    dma_sp("ybst", yb_d[b * 128:(b + 1) * 128, :], ybt[p], reads=[b_ybt[p]], writes=[b_yb])
                S.alias(b_gat, b_G + b_xg + b_xgT)
                for i in range(NT):
                    for k in range(2):
                        gi = (i * 2 + k) % 4
                        S.dma("pool", "gat%d" % gi, lambda e, i=i, k=k, gi=gi: e.indirect_dma_start(
                            out=gat_t[gi], out_offset=None, in_=yb_d[:, :],
                            in_offset=bass.IndirectOffsetOnAxis(ap=desti[:, i, k:k + 1], axis=0)), reads=[b_yb, b_route], writes=[b_gat[gi]])
                        stt("dve", X[:, i, :], gat_t[gi], wts[:, i, k:k + 1], X[:, i, :], ALU.mult, ALU.add, [b_gat[gi], b_oh, Xb[i]], [Xb[i]])

            gat_t = [HT[:, i * 2048:(i + 1) * 2048].bitcast(F32) for i in range(4)]
            b_gat = S.bufs(4, "gat")
            steps.append((None, moe_blocks))

            def moe_end(slot, sbuf_):
                S.alias([b_big], [b_rt, b_oh, b_route] + b_D + b_hmT)
                S.alias([b_u], [b_sg])
                S.alias(hn_b, [b_hm, b_hmT[0]])
                S.alias([YRb], b_U + b_ybt + b_hr)
                S.alias([hTb], b_gat + b_G + b_xg + b_xgT)

            steps.append((None, moe_end))
            if stop_after == (l, "moe"):
                return True
            return False

        ztile = sb("ztile", [128, 512], BF16)
        b_zt = S.buf("zt")
        S.op("pool", lambda e: e.memset(ztile[:, :], 0.0), writes=[b_zt])
        U_pad = sb("U_pad", [128, S_LEN + 4], BF16)
        b_u = S.buf("u")
        b_xb = S.buf("xb_d")
        b_yb = S.buf("yb_d")
        S.op("pool", lambda e: e.memset(U_pad[:, :], 0.0), writes=[b_u])

        stopped = False
        for li_, l in enumerate(layer_ids):
            stopped = layer_steps(li_, l)
            if stopped:
                break

        def epilogue(slot, sbuf_):
            if final and not stopped:
                gfin, _ = carve(BIG, 0, D, F32)
                otile = [carve(BIG, 2 * D + i * 2 * D, D, F32)[0] for i in range(2)]
                b_gf = S.buf("gfin")
                b_ot = S.bufs(2, "otile")
                S.alias([b_gf] + b_ot, [b_big])
                dma_sp("c2", gfin, gfin_d[:, :], writes=[b_gf])
                rstd_tiles(lambda i: X[:, i, :], Xb, NT, 1.0 / D)
                for i in range(NT):
                    p = i % 2
                    act(otile[p], X[:, i, :], AF.Copy, [Xb[i], b_rstd], [b_ot[p]], scale=rstd[:, i:i + 1])
                    tt("dve", otile[p], otile[p], gfin, ALU.mult, [b_ot[p], b_gf], [b_ot[p]])
                    dma_sp("out", out_d[i * 128:(i + 1) * 128, :], otile[p], reads=[b_ot[p]], writes=[b_out])
            else:
                for i in range(NT):
                    dma_sp("out", out_d[i * 128:(i + 1) * 128, :], X[:, i, :], reads=[Xb[i]], writes=[b_out])

        b_out = S.buf("out")
        if max_steps is not None:
            del steps[max_steps:]
        steps.append((None, epilogue))

        wsteps = [k for k, (ld, _) in enumerate(steps) if ld is not None]
        issued = 0
        done = 0
        for k, (ld, cpf) in enumerate(steps):
            while issued < len(wsteps) and issued < done + NSLOT:
                sl = issued % NSLOT
                steps[wsteps[issued]][0](WS[sl], WSb[sl])
                issued += 1
            if ld is not None:
                sl = done % NSLOT
                cpf(WS[sl], WSb[sl])
                done += 1
            else:
                cpf(None, None)
        S.wait_all("sp", [b_out])
        if os.environ.get('WAITCS'):
            for b_ in dbg_bufs:
                S.wait_all("sp", [b_])
        S.finalize()
        dl = S.simulate()
        if dl is not None:
            raise RuntimeError("sync deadlock: %r" % (dl,))
        S.emit()
    return nc, S.ninst


def _consts():
    ident = np.eye(128, dtype=np.float32)
    ones = np.ones((128, 128), np.float32)
    rrot = np.zeros((128, 128), np.float32)
    for c in range(2):
        for i in range(8):
            rrot[c * 64 + 8 + i, c * 64 + i] = -1.0
            rrot[c * 64 + i, c * 64 + 8 + i] = 1.0
    ustr = np.triu(np.ones((128, 128), np.float32), 1)
    cmat = np.concatenate([ident, ones, rrot, ustr], axis=1)
    inv = np.float32(500000.0) ** (-np.arange(0, 16, 2, dtype=np.float32) / np.float32(16))
    ang = np.arange(S_LEN, dtype=np.float32)[:, None] * inv[None, :]
    cs, sn = np.cos(ang).astype(np.float32), np.sin(ang).astype(np.float32)
    cosT = np.ones((128, S_LEN), np.float32)
    sinT = np.zeros((128, S_LEN), np.float32)
    for c in range(2):
        for i in range(16):
            cosT[c * 64 + i] = cs[:, i % 8]
            sinT[c * 64 + i] = sn[:, i % 8]
    cossin = np.concatenate([cosT, sinT], axis=1)
    thr = np.broadcast_to((np.arange(64, dtype=np.float32) * 128.0)[None, :], (128, 64))
    pc = np.arange(8, dtype=np.float32)[None, :] * 128.0 + np.arange(128, dtype=np.float32)[:, None]
    cmisc = np.ascontiguousarray(np.concatenate([thr, pc], axis=1))
    return np.ascontiguousarray(cmat.astype(ml_dtypes.bfloat16)), np.ascontiguousarray(cossin.astype(ml_dtypes.bfloat16)), cmisc


def _pack_small(inp, layer_ids):
    cols = []
    for l in layer_ids:
        def colmaj(v):
            return np.asarray(v, np.float32).reshape(8, 128).T
        cols.append(colmaj(inp["norm_mix"][l]))
        cols.append(colmaj(inp["norm_cross"][l]))
        cols.append(colmaj(inp["norm_ffn"][l]))
        for k in range(3):
            cols.append(colmaj(inp["conv_w"][l][k]))
        cols.append(np.asarray(inp["subln"][l], np.float32).reshape(128, 1))
    cols.append(np.asarray(inp["norm_mem"], np.float32).reshape(8, 128).T)
    gcols = np.ascontiguousarray(np.concatenate(cols, axis=1))
    lam = []
    for l in layer_ids:
        lam.append(np.concatenate([inp["lambda_q1"][l], inp["lambda_k1"][l], inp["lambda_q2"][l], inp["lambda_k2"][l]]))
    lamv = np.ascontiguousarray(np.broadcast_to(np.concatenate(lam)[None, :].astype(np.float32), (128, len(layer_ids) * 256)))
    gfin = np.ascontiguousarray(np.broadcast_to(np.asarray(inp["norm_final"], np.float32)[None, :], (128, D)))
    return gcols, lamv, gfin


_PROG_CACHE = {}


def _run(inp, x_shards, layer_ids, final, stop_after=None, cores=None, max_steps=None):
    key = (tuple(layer_ids), final, stop_after, max_steps)
    if key not in _PROG_CACHE:
        _PROG_CACHE[key] = build(list(layer_ids), final, stop_after, max_steps)[0]
    nc = _PROG_CACHE[key]
    cmat, cossin, cmisc = _consts()
    gcols, lamv, gfin = _pack_small(inp, layer_ids)
    ls = list(layer_ids)
    sl = slice(ls[0], ls[-1] + 1)
    w_rt = np.ascontiguousarray(np.concatenate([inp["w_router_group"][sl], inp["w_router_expert"][sl]], axis=-1))
    shared = {
        "w_in": np.ascontiguousarray(inp["w_in"][sl]), "w_branch": np.ascontiguousarray(inp["w_branch"][sl]),
        "w_o": np.ascontiguousarray(inp["w_o"][sl]), "w_cq": np.ascontiguousarray(inp["w_cq"][sl]),
        "w_ckv": np.ascontiguousarray(inp["w_ckv"][sl]), "w_co": np.ascontiguousarray(inp["w_co"][sl]),
        "w_rt": w_rt, "w_exp_gate": np.ascontiguousarray(inp["w_exp_gate"][sl]),
        "w_exp_up": np.ascontiguousarray(inp["w_exp_up"][sl]), "w_exp_down": np.ascontiguousarray(inp["w_exp_down"][sl]),
        "gcols": gcols, "lamv": lamv, "gfinal": gfin, "cmat": cmat, "cossin": cossin, "cmisc": cmisc,
    }
    cores = list(range(len(x_shards))) if cores is None else cores
    in_maps = []
    for b in range(len(x_shards)):
        m = dict(shared)
        m["x"] = np.ascontiguousarray(x_shards[b])
        m["mem"] = np.ascontiguousarray(inp["mem"][b])
        in_maps.append(m)
    res = run_bass_kernel_spmd(nc, in_maps, core_ids=cores)
    return [r["out"] for r in res.results]


MODE = "fused"


def kernel(**inputs):
    inp = {k: np.asarray(v) for k, v in inputs.items()}
    xs = [inp["x"][b] for b in range(inp["x"].shape[0])]
    if MODE == "fused":
        outs = _run(inp, xs, list(range(DEPTH)), True)
    else:
        for l in range(DEPTH):
            xs = _run(inp, xs, [l], l == DEPTH - 1)
        outs = xs
    return np.stack(outs, axis=0).astype(np.float32)
```

```python
import math
import os
import numpy as np
import ml_dtypes
HEADCUT = int(os.environ.get('HEADCUT', '99'))
from contextlib import ExitStack
import concourse.bass as bass
import concourse.mybir as mybir
from concourse.bass_utils import run_bass_kernel_spmd

F32 = mybir.dt.float32
BF16 = mybir.dt.bfloat16
I32 = mybir.dt.int32
ALU = mybir.AluOpType
AF = mybir.ActivationFunctionType
AX = mybir.AxisListType

D = 1024
S_LEN = 2048
NT = 16
KC = 8
NMEM = 256
DEPTH = 4
NEXP = 32
DEXP = 768
NBLK = 48
RB = 256
NROW = NBLK * RB
EPS = 1e-6
NEG = -1.0e30


class Buf:
    __slots__ = ("name", "w", "r", "excl")

    def __init__(self, name):
        self.name = name
        self.w = None
        self.r = {}
        self.excl = False


class Sched:
    ENG = ("pe", "act", "dve", "pool", "sp")

    def __init__(self, nc, stack):
        self.nc = nc
        self.stack = stack
        self.rec = {e: [] for e in self.ENG}
        self.sem = {e: stack.enter_context(nc.semaphore("s_" + e)) for e in self.ENG}
        self.cnt = {e: 0 for e in self.ENG}
        self.seen = {e: {} for e in self.ENG}
        self.dsem = {}
        self.dcnt = {}
        self.nbuf = 0
        self.ninst = 0

    def buf(self, name=None):
        self.nbuf += 1
        return Buf(name or "b%d" % self.nbuf)

    def bufs(self, n, name="b"):
        return [self.buf("%s%d" % (name, i)) for i in range(n)]

    def _deps(self, eng, reads, writes, skipkey=None):
        deps = {}

        def add(k, v):
            if k == skipkey:
                return
            if deps.get(k, 0) < v:
                deps[k] = v

        for b in reads:
            if b.w is not None:
                add(*b.w)
            if b.excl:
                for k, v in b.r.items():
                    if k != ("e", eng):
                        add(k, v)
        for b in writes:
            if b.w is not None:
                add(*b.w)
            for k, v in b.r.items():
                add(k, v)
        out = []
        seen = self.seen[eng]
        for k, v in deps.items():
            if eng == "pe" and k == ("e", "pe"):
                continue
            if k[0] == "d":
                v = self.dcnt[k[1]]
            if seen.get(k, 0) >= v:
                continue
            seen[k] = v
            out.append((k, v))
        return out

    def _post(self, ev, reads, writes):
        k, v = ev
        for b in reads:
            if b.r.get(k, 0) < v:
                b.r[k] = v
        for b in writes:
            b.w = ev
            b.r = {}

    def op(self, eng, fn, reads=(), writes=()):
        waits = self._deps(eng, reads, writes)
        self.cnt[eng] += 1
        ev = (("e", eng), self.cnt[eng])
        self.rec[eng].append(("op", waits, fn, self.cnt[eng]))
        self._post(ev, reads, writes)
        self.ninst += 1
        return ev

    def dma(self, eng, key, fn, reads=(), writes=()):
        if key not in self.dsem:
            self.dsem[key] = self.stack.enter_context(self.nc.semaphore("d_" + str(key)))
            self.dcnt[key] = 0
        waits = self._deps(eng, reads, writes, skipkey=("d", key))
        self.dcnt[key] += 16
        ev = (("d", key), self.dcnt[key])
        self.rec[eng].append(("dma", waits, fn, key))
        self._post(ev, reads, writes)
        self.ninst += 1
        return ev

    def alias(self, new_bufs, old_bufs):
        for nb in new_bufs:
            for ob in old_bufs:
                if ob.w is not None:
                    k, v = ob.w
                    if nb.r.get(k, 0) < v:
                        nb.r[k] = v
                for k, v in ob.r.items():
                    if nb.r.get(k, 0) < v:
                        nb.r[k] = v

    def wait_all(self, eng, bufs):
        waits = self._deps(eng, bufs, bufs)
        self.rec[eng].append(("wait", waits, None, None))

    def finalize(self):
        waited = {e: set() for e in self.ENG}
        for eng in self.ENG:
            for kind, waits, fn, x in self.rec[eng]:
                for k, v in waits:
                    if k[0] == "e":
                        waited[k[1]].add(v)
        rank = {e: {v: i + 1 for i, v in enumerate(sorted(waited[e]))} for e in self.ENG}
        self.prog = {}
        for eng in self.ENG:
            pl = []
            for kind, waits, fn, x in self.rec[eng]:
                rw = []
                for k, v in waits:
                    if k[0] == "e":
                        rw.append((self.sem[k[1]], rank[k[1]][v]))
                    else:
                        rw.append((self.dsem[k[1]], v))
                if kind == "op":
                    pl.append((rw, fn, self.sem[eng] if x in waited[eng] else None, 1))
                elif kind == "dma":
                    pl.append((rw, fn, self.dsem[x], 16))
                else:
                    pl.append((rw, None, None, 0))
            self.prog[eng] = pl

    def simulate(self):
        val = {}
        pc = {e: 0 for e in self.ENG}
        prog = self.prog
        while True:
            progressed = False
            for e in self.ENG:
                while pc[e] < len(prog[e]):
                    waits, fn, sem, inc = prog[e][pc[e]]
                    if all(val.get(id(s_), 0) >= v for s_, v in waits):
                        if sem is not None:
                            val[id(sem)] = val.get(id(sem), 0) + inc
                        pc[e] += 1
                        progressed = True
                    else:
                        break
            if all(pc[e] == len(prog[e]) for e in self.ENG):
                return None
            if not progressed:
                return {e: (pc[e], len(prog[e])) for e in self.ENG}

    def emit(self):
        nc = self.nc
        prog = self.prog

        def run(e, pl):
            for waits, fn, sem, inc in pl:
                for s_, v in waits:
                    e.wait_ge(s_, v)
                if fn is not None:
                    ins = fn(e)
                    if sem is not None:
                        ins.then_inc(sem, inc)

        with nc.Block() as block:
            @block.tensor
            def _(e):
                run(e, prog["pe"])

            @block.scalar
            def _(e):
                run(e, prog["act"])

            @block.vector
            def _(e):
                run(e, prog["dve"])

            @block.gpsimd
            def _(e):
                run(e, prog["pool"])

            @block.sync
            def _(e):
                run(e, prog["sp"])


def lambda_init(l):
    return 0.8 - 0.6 * math.exp(-0.3 * l)


GC_MIX, GC_CROSS, GC_FFN, GC_CONV, GC_SUBLN, GC_PER = 0, 8, 16, 24, 48, 49


def build(layer_ids, final, stop_after=None, max_steps=None):
    L = len(layer_ids)
    nc = bass.Bass("TRN2", target_bir_lowering=False)

    def din(name, shape, dt=F32):
        return nc.dram_tensor(name, shape, dt, kind="ExternalInput").ap()

    x_in = din("x", [S_LEN, D])
    mem_in = din("mem", [NMEM, D])
    w_in = din("w_in", [L, D, 8192])
    w_br = din("w_branch", [L, 2, D, D])
    w_o = din("w_o", [L, D, D])
    w_cq = din("w_cq", [L, D, D])
    w_ckv = din("w_ckv", [L, D, 2 * D])
    w_co = din("w_co", [L, D, D])
    w_rt = din("w_rt", [L, D, 36])
    w_eg = din("w_exp_gate", [L, NEXP, D, DEXP])
    w_eu = din("w_exp_up", [L, NEXP, D, DEXP])
    w_ed = din("w_exp_down", [L, NEXP, DEXP, D])
    gcols_d = din("gcols", [128, L * GC_PER + 8])
    lamv_d = din("lamv", [128, L * 256])
    gfin_d = din("gfinal", [128, D])
    cmat_d = din("cmat", [128, 4 * 128], BF16)
    cossin_d = din("cossin", [128, 2 * S_LEN], BF16)
    cmisc_d = din("cmisc", [128, 64 + 8])
    out_d = nc.dram_tensor("out", [S_LEN, D], F32, kind="ExternalOutput").ap()
    xb_d = nc.dram_tensor("xb_scr", [NROW, D], BF16, kind="Internal").ap()
    yb_d = nc.dram_tensor("yb_scr", [NROW, D], F32, kind="Internal").ap()

    st = ExitStack()
    with st:
        S = Sched(nc, st)

        def sb(name, shape, dt):
            return st.enter_context(nc.sbuf_tensor("sb_" + name, shape, dt))

        X = sb("X", [128, NT, D], F32)
        HT = sb("HT", [128, KC * S_LEN], BF16)
        YR = sb("YR", [128, KC * S_LEN], BF16)
        NBIG = 19 * 1024
        BIG = sb("BIG", [128, NBIG], BF16)
        NSLOT = 3
        WS = [sb("WS%d" % i, [128, 4096], BF16) for i in range(NSLOT)]
        cmat = sb("cmat", [128, 4 * 128], BF16)
        gcols = sb("gcols", [128, L * GC_PER + 8], F32)
        cmisc = sb("cmisc", [128, 72], F32)
        memT = sb("memT", [128, KC, NMEM], BF16)
        stat = sb("stat", [128, 64], F32)
        lamc = sb("lamc", [128, 2 * L], F32)
        sublnS = sb("sublnS", [128, L], F32)
        PS = [st.enter_context(nc.psum_tensor("ps%d" % i, [128, 512], F32)) for i in range(8)]
        PSB = S.bufs(8, "ps")
        for b_ in PSB:
            b_.excl = True

        ident = cmat[:, 0:128]
        ones = cmat[:, 128:256]
        rrot = cmat[:, 256:384]
        ustr = cmat[:, 384:512]
        thr = cmisc[:, 0:64]
        pcv = cmisc[:, 64:72]

        Xb = S.bufs(NT, "X")
        hTb = S.buf("hT")
        YRb = S.buf("YR")
        WSb = S.bufs(NSLOT, "WS")
        b_const = S.buf("const")
        b_memT = S.buf("memT")
        b_stat = S.buf("stat")

        hT = HT[:, :].rearrange("p (c t) -> p c t", c=KC)

        def carve(region, off, n, dt=BF16):
            if dt == BF16:
                return region[:, off:off + n], off + n
            assert off % 2 == 0
            return region[:, off:off + 2 * n].bitcast(dt), off + 2 * n

        psrr = [0]

        def psbank():
            i = psrr[0] % 8
            psrr[0] += 1
            return PS[i], PSB[i]

        def dma_sp(key, out, in_, reads=(), writes=()):
            return S.dma("sp", key, lambda e: e.dma_start(out=out, in_=in_), reads=reads, writes=writes)

        def dma_pool(key, out, in_, reads=(), writes=()):
            return S.dma("pool", key, lambda e: e.dma_start(out=out, in_=in_), reads=reads, writes=writes)

        def mm(out, lhsT, rhs, start, stop, reads, writes):
            S.op("pe", lambda e: e.matmul(out, lhsT=lhsT, rhs=rhs, start=start, stop=stop),
                 reads=reads, writes=writes)

        def tr(out, in_, reads, writes):
            S.op("pe", lambda e: e.transpose(out=out, in_=in_, identity=ident), reads=list(reads) + [b_const], writes=writes)

        def act(out, in_, func, reads, writes, scale=1.0, bias=None, accum=None):
            kw = {}
            if bias is not None:
                kw["bias"] = bias
            if accum is not None:
                kw["accum_out"] = accum
            S.op("act", lambda e: e.activation(out=out, in_=in_, func=func, scale=scale, **kw), reads=reads, writes=writes)

        def tt(eng, out, in0, in1, op, reads, writes):
            S.op(eng, lambda e: e.tensor_tensor(out=out, in0=in0, in1=in1, op=op), reads=reads, writes=writes)

        def ts(eng, out, in0, s1, op0, reads, writes, s2=None, op1=None, accum=None):
            kw = {}
            if accum is not None:
                kw["accum_out"] = accum
            if op1 is None:
                S.op(eng, lambda e: e.tensor_scalar(out=out, in0=in0, scalar1=s1, scalar2=None, op0=op0, **kw), reads=reads, writes=writes)
            else:
                S.op(eng, lambda e: e.tensor_scalar(out=out, in0=in0, scalar1=s1, scalar2=s2, op0=op0, op1=op1, **kw), reads=reads, writes=writes)

        def stt(eng, out, in0, scalar, in1, op0, op1, reads, writes):
            S.op(eng, lambda e: e.scalar_tensor_tensor(out=out, in0=in0, scalar=scalar, in1=in1, op0=op0, op1=op1), reads=reads, writes=writes)

        def cp(eng, out, in_, reads, writes):
            S.op(eng, lambda e: e.tensor_copy(out=out, in_=in_), reads=reads, writes=writes)

        dma_sp("c0", cmat[:, :], cmat_d[:, :], writes=[b_const])
        dma_sp("c1", gcols[:, :], gcols_d[:, :], writes=[b_const])
        dma_sp("c1", cmisc[:, :], cmisc_d[:, :], writes=[b_const])
        for i in range(NT):
            dma_sp("xin", X[:, i, :], x_in[i * 128:(i + 1) * 128, :], writes=[Xb[i]])

        b_big = S.buf("bigscratch")
        lamv, _ = carve(BIG, 0, L * 256, F32)
        ltmp, _ = carve(BIG, 2 * L * 256, 64, F32)
        dma_sp("c3", lamv, lamv_d[:, :], writes=[b_big])
        for li_, l in enumerate(layer_ids):
            base = li_ * 256
            for j in range(2):
                S.op("dve", lambda e, base=base, j=j: e.tensor_tensor(out=ltmp, in0=lamv[:, base + j * 128:base + j * 128 + 64],
                                                                      in1=lamv[:, base + j * 128 + 64:base + j * 128 + 128], op=ALU.mult),
                     reads=[b_big], writes=[b_big])
                S.op("dve", lambda e, j=j: e.tensor_reduce(out=stat[:, j:j + 1], in_=ltmp, axis=AX.X, op=ALU.add),
                     reads=[b_big], writes=[b_stat])
            act(stat[:, 2:4], stat[:, 0:2], AF.Exp, [b_stat], [b_stat])
            tt("dve", stat[:, 4:5], stat[:, 3:4], stat[:, 2:3], ALU.subtract, [b_stat], [b_stat])
            ts("dve", lamc[:, li_:li_ + 1], stat[:, 4:5], -lambda_init(l), ALU.add, [b_stat], [b_const])
            ts("dve", sublnS[:, li_:li_ + 1], gcols[:, li_ * GC_PER + GC_SUBLN:li_ * GC_PER + GC_SUBLN + 1],
               1.0 - lambda_init(l), ALU.mult, [b_const], [b_const])

        ssq = sb("ssq", [128, NT], F32)
        rstd = sb("rstd", [128, NT], F32)
        b_ssq = S.buf("ssq")
        b_rstd = S.buf("rstd")
        hn_t = [sb("hn%d" % i, [128, D], BF16) for i in range(2)]
        hn_b = S.bufs(2, "hn")

        def rstd_tiles(src_ap_fn, src_bufs, ntiles, inv_n):
            for i in range(ntiles):
                act(hn_t[0][:, :], src_ap_fn(i), AF.Square, [src_bufs[i]], [hn_b[0], b_ssq], accum=ssq[:, i:i + 1])
            ts("dve", rstd[:, 0:ntiles], ssq[:, 0:ntiles], inv_n, ALU.mult, [b_ssq], [b_rstd], s2=EPS, op1=ALU.add)
            act(rstd[:, 0:ntiles], rstd[:, 0:ntiles], AF.Ln, [b_rstd], [b_rstd])
            act(rstd[:, 0:ntiles], rstd[:, 0:ntiles], AF.Exp, [b_rstd], [b_rstd], scale=-0.5)

        def norm_to_fm(src_ap_fn, src_bufs, ntiles, gcol0, dstT, dst_buf, keep_rows=None):
            rstd_tiles(src_ap_fn, src_bufs, ntiles, 1.0 / D)
            for i in range(ntiles):
                if keep_rows is None:
                    hn, hb = hn_t[i % 2], hn_b[i % 2]
                    hn_ap = hn[:, :]
                else:
                    hn_ap, hb = keep_rows(i)
                act(hn_ap, src_ap_fn(i), AF.Copy, [src_bufs[i], b_rstd], [hb], scale=rstd[:, i:i + 1])
                pt, pb = psbank()
                ptb = pt[:, :].bitcast(BF16).rearrange("p (c t) -> p c t", t=128)
                for c in range(KC):
                    tr(ptb[:, c, :], hn_ap[:, c * 128:(c + 1) * 128], [hb], [pb])
                for c in range(KC):
                    eng = "dve" if c % 2 == 0 else "act"
                    if eng == "dve":
                        ts("dve", dstT[:, c, i * 128:(i + 1) * 128], ptb[:, c, :], gcols[:, gcol0 + c:gcol0 + c + 1], ALU.mult,
                           [pb, b_const], [dst_buf])
                    else:
                        act(dstT[:, c, i * 128:(i + 1) * 128], ptb[:, c, :], AF.Copy, [pb, b_const], [dst_buf],
                            scale=gcols[:, gcol0 + c:gcol0 + c + 1])

        memrows, _ = carve(BIG, 4096, 2 * D, F32)
        memrows = memrows.rearrange("p (i d) -> p i d", i=2)
        b_memrows = S.bufs(2, "memrows")
        for i in range(2):
            dma_sp("c2", memrows[:, i, :], mem_in[i * 128:(i + 1) * 128, :], writes=[b_memrows[i]])
        norm_to_fm(lambda i: memrows[:, i, :], b_memrows, 2, L * GC_PER, memT, b_memT)

        dbg_bufs = []
        steps = []

        def wload(slot, sbuf_, pieces):
            key = "w%d" % [i for i in range(NSLOT) if WS[i] is slot][0]
            for dst, src in pieces:
                dma_pool(key, dst(slot), src, writes=[sbuf_])

        def w3(slot, off, ncols):
            return slot[:, off:off + KC * ncols].rearrange("p (c n) -> p c n", c=KC)

        def dram_cols(w2d, c0, ncols):
            return w2d[:, c0:c0 + ncols].rearrange("(c p) n -> p c n", p=128)

        def layer_steps(li_, l):
            g0 = li_ * GC_PER
            win_l = w_in[li_]
            off = 0
            cosb, off = carve(BIG, off, S_LEN)
            sinb, off = carve(BIG, off, S_LEN)
            qT, off = carve(BIG, off, S_LEN)
            kT, off = carve(BIG, off, S_LEN)
            vtm, off = carve(BIG, off, NT * 128)
            vtm = vtm.rearrange("p (i e) -> p i e", i=NT)
            NE = 6
            Et = []
            for _ in range(NE):
                a, off = carve(BIG, off, 512)
                Et.append(a)
            NTMP = 6
            Tm = []
            for _ in range(NTMP):
                a, off = carve(BIG, off, 512, F32)
                Tm.append(a)
            assert off <= NBIG, off
            b_cs = S.buf("cossin")
            dbg_bufs.append(b_cs)
            b_q = S.buf("qT")
            b_k = S.buf("kT")
            b_v = S.buf("v")
            Eb = S.bufs(NE, "E")
            Tb = S.bufs(NTMP, "T")
            rrE = [0]
            rrT = [0]

            def getE():
                i = rrE[0] % NE
                rrE[0] += 1
                return Et[i], Eb[i]

            def getT():
                i = rrT[0] % NTMP
                rrT[0] += 1
                return Tm[i], Tb[i]

            yT = YR[:, :].rearrange("p (c t) -> p c t", c=KC)
            b_y = [S.buf("y%d" % c) for c in range(KC)]

            def mixer_begin(slot, sbuf_):
                S.alias([b_cs, b_q, b_k, b_v] + Eb + Tb, [b_big])
                S.alias(b_y, [YRb])
                dma_sp("cs", cosb, cossin_d[:, 0:S_LEN], writes=[b_cs])
                dma_sp("cs", sinb, cossin_d[:, S_LEN:2 * S_LEN], writes=[b_cs])
                if os.environ.get('CSTOUCH'):
                    cp("dve", stat[:, 60:61], cosb[:, 0:1], [b_cs], [b_stat])
                norm_to_fm(lambda i: X[:, i, :], Xb, NT, g0 + GC_MIX, hT, hTb)
                for r in range(NROW // 128):
                    for hh in range(2):
                        dma_sp("xbz", xb_d[r * 128:(r + 1) * 128, hh * 512:(hh + 1) * 512], ztile[:, :], reads=[b_zt], writes=[b_xb])

            steps.append((None, mixer_begin))

            def head_step(h):
                def load(slot, sbuf_):
                    wload(slot, sbuf_, [
                        (lambda s: w3(s, 0, 128), dram_cols(win_l, h * 128, 128)),
                        (lambda s: w3(s, 1024, 128), dram_cols(win_l, 1024 + h * 128, 128)),
                        (lambda s: w3(s, 2048, 128), dram_cols(win_l, 2048 + h * 128, 128)),
                    ])

                def compute(slot, sbuf_):
                    wq = w3(slot, 0, 128)
                    wk = w3(slot, 1024, 128)
                    wv = w3(slot, 2048, 128)
                    for (wt, dst, db) in ((wq, qT, b_q), (wk, kT, b_k)):
                        for tb in range(4):
                            tsl = slice(tb * 512, (tb + 1) * 512)
                            pp, pb = psbank()
                            for c in range(KC):
                                mm(pp[:, :], wt[:, c, :], hT[:, c, tsl], c == 0, c == KC - 1, [sbuf_, hTb], [pb])
                            qb, qbb = getE()
                            act(qb, pp[:, :], AF.Copy, [pb], [qbb])
                            if HEADCUT == 0:
                                continue
                            ROT = int(os.environ.get('ROT', '9'))
                            p2, p2b = psbank()
                            P2V = os.environ.get('P2V', '')
                            if P2V == 'ident':
                                mm(p2[:, :], ident, qb, True, True, [b_const, qbb], [p2b])
                            elif P2V == 'rhs':
                                mm(p2[:, :], rrot, hT[:, 0, tsl], True, True, [b_const, hTb], [p2b])
                            elif P2V == 'evac':
                                mm(p2[:, :], rrot, qb, True, True, [b_const, qbb], [p2b])
                                t9, t9b = getT()
                                cp("dve", t9, p2[:, :], [p2b], [t9b])
                            else:
                                mm(p2[:, :], rrot, qb, True, True, [b_const, qbb], [p2b])
                            if ROT < 2:
                                continue
                            t1, t1b = getT()
                            if os.environ.get('ROTV') == 'sb':
                                tt("dve", t1, qb, cosb[:, tsl], ALU.mult, [qbb, b_cs], [t1b])
                            elif os.environ.get('ROTV') == 'hT':
                                tt("dve", t1, pp[:, :], hT[:, 0, tsl], ALU.mult, [pb, hTb], [t1b])
                            elif os.environ.get('ROTV') == 'nocs':
                                tt("dve", t1, pp[:, :], qb, ALU.mult, [pb, qbb], [t1b])
                            else:
                                tt("dve", t1, pp[:, :], cosb[:, tsl], ALU.mult, [pb, b_cs], [t1b])
                            if ROT < 3:
                                continue
                            t2, t2b = getT()
                            tt("dve", t2, p2[:, :], sinb[:, tsl], ALU.mult, [p2b, b_cs], [t2b])
                            if ROT < 4:
                                continue
                            tt("pool", dst[:, tsl], t1, t2, ALU.add, [t1b, t2b], [db])
                    if HEADCUT < 2:
                        return
                    for i4 in range(4):
                        pp, pb = psbank()
                        for ii in range(4):
                            i = i4 * 4 + ii
                            for c in range(KC):
                                mm(pp[:, ii * 128:(ii + 1) * 128], hT[:, c, i * 128:(i + 1) * 128], wv[:, c, :],
                                   c == 0, c == KC - 1, [sbuf_, hTb], [pb])
                        act(vtm[:, i4 * 4:(i4 + 1) * 4, :], pp[:, :].rearrange("p (i e) -> p i e", i=4), AF.Copy, [pb], [b_v])
                    if HEADCUT < 3:
                        return
                    pending = [None]
                    for qb_ in range(4):
                        qsl = slice(qb_ * 512, (qb_ + 1) * 512)
                        pO = [(PS[4], PSB[4]), (PS[5], PSB[5])]
                        pR = [(PS[6], PSB[6]), (PS[7], PSB[7])]
                        its = [(kb, c) for kb in range(NT) for c in range(2)]
                        LA = 2
                        Es = {}
                        for i in range(len(its) + LA):
                            if i < len(its):
                                kb, c = its[i]
                                pS, pSb = psS[i % 3]
                                mm(pS[:, :], kT[c * 64:(c + 1) * 64, kb * 128:(kb + 1) * 128], qT[c * 64:(c + 1) * 64, qsl],
                                   True, True, [b_k, b_q], [pSb])
                                E, Eb_ = getE()
                                act(E, pS[:, :], AF.Exp, [pSb], [Eb_], scale=0.125)
                                Es[i] = (E, Eb_)
                            j = i - LA
                            if j >= 0:
                                kb, c = its[j]
                                E, Eb_ = Es.pop(j)
                                mm(pO[c][0][:, :], vtm[:, kb, :], E, kb == 0, kb == NT - 1, [b_v, Eb_], [pO[c][1]])
                                mm(pR[c][0][:, :], ones, E, kb == 0, kb == NT - 1, [b_const, Eb_], [pR[c][1]])
                            if i == 10 and pending[0] is not None:
                                pending[0]()
                                pending[0] = None
                        ri = []
                        for c in range(2):
                            r_, rb_ = getT()
                            act(r_, pR[c][0][:, :], AF.Ln, [pR[c][1]], [rb_])
                            act(r_, r_, AF.Exp, [rb_], [rb_], scale=-1.0)
                            ri.append((r_, rb_))
                        t0, t0b = getT()
                        tt("dve", t0, pO[0][0][:, :], ri[0][0], ALU.mult, [pO[0][1], ri[0][1]], [t0b])
                        t1, t1b = getT()
                        tt("dve", t1, pO[1][0][:, :], ri[1][0], ALU.mult, [pO[1][1], ri[1][1]], [t1b])
                        o_, ob_ = getT()
                        stt("dve", o_, t1, lamc[:, li_:li_ + 1], t0, ALU.mult, ALU.add, [t1b, t0b, b_const], [ob_])
                        sqf, sqb = getT()
                        sq = sqf.bitcast(BF16)[:, 0:512]
                        tt("pool", sq, o_, o_, ALU.mult, [ob_], [sqb])

                        def fin2(o_=o_, ob_=ob_, sq=sq, sqb=sqb, qsl=qsl):
                            pq, pqb = psS[3]
                            mm(pq[:, :], ones, sq, True, True, [b_const, sqb], [pqb])
                            rs, rsb = getT()
                            ts("dve", rs, pq[:, :], 1.0 / 128, ALU.mult, [pqb], [rsb], s2=EPS, op1=ALU.add)
                            act(rs, rs, AF.Ln, [rsb], [rsb])
                            act(rs, rs, AF.Exp, [rsb], [rsb], scale=-0.5)
                            stt("dve", yT[:, h, qsl], o_, sublnS[:, li_:li_ + 1], rs, ALU.mult, ALU.mult, [ob_, rsb, b_const], [b_y[h]])

                        pending[0] = fin2
                    if pending[0] is not None:
                        pending[0]()
                        pending[0] = None

                return load, compute

            psS = [(PS[i], PSB[i]) for i in range(4)]

            for h in range(8):
                steps.append(head_step(h))

            offc = 0
            Mreg, offc = carve(BIG, offc, KC * S_LEN)
            Mv = Mreg.rearrange("p (c t) -> p c t", c=KC)
            CT = []
            for _ in range(3):
                a, offc = carve(BIG, offc, 512, F32)
                CT.append(a)
            assert offc <= NBIG
            b_M = [S.buf("M%d" % c) for c in range(KC)]
            CTb = S.bufs(3, "CT")
            rrC = [0]

            def getC():
                i = rrC[0] % 3
                rrC[0] += 1
                return CT[i], CTb[i]

            def phaseC_begin(slot, sbuf_):
                S.alias(b_M + CTb, [b_cs, b_q, b_k, b_v] + Eb + Tb)

            steps.append((None, phaseC_begin))

            def branch_step(n, j):
                def load(slot, sbuf_):
                    wload(slot, sbuf_, [
                        (lambda s: w3(s, 0, 128), dram_cols(w_br[li_, n], j * 128, 128)),
                        (lambda s: w3(s, 1024, 128), dram_cols(win_l, 6144 + n * 1024 + j * 128, 128)),
                    ])

                def compute(slot, sbuf_):
                    wb = w3(slot, 0, 128)
                    wg = w3(slot, 1024, 128)
                    for tb in range(4):
                        tsl = slice(tb * 512, (tb + 1) * 512)
                        pg, pgb = psbank()
                        for c in range(KC):
                            mm(pg[:, :], wg[:, c, :], hT[:, c, tsl], c == 0, c == KC - 1, [sbuf_, hTb], [pgb])
                        pbr, pbrb = psbank()
                        for c in range(KC):
                            mm(pbr[:, :], wb[:, c, :], yT[:, c, tsl], c == 0, c == KC - 1, [sbuf_, b_y[c]], [pbrb])
                        sg, sgb = getC()
                        act(sg, pg[:, :], AF.Sigmoid, [pgb], [sgb])
                        if n == 0:
                            tt("dve", Mv[:, j, tsl], sg, pbr[:, :], ALU.mult, [sgb, pbrb], [b_M[j]])
                        else:
                            t_, tb_ = getC()
                            tt("dve", t_, sg, pbr[:, :], ALU.mult, [sgb, pbrb], [tb_])
                            tt("pool", Mv[:, j, tsl], Mv[:, j, tsl], t_, ALU.add, [b_M[j], tb_], [b_M[j]])

                return load, compute

            for j in range(KC):
                steps.append(branch_step(0, j))


            def conv_step(j):
                def load(slot, sbuf_):
                    wload(slot, sbuf_, [
                        (lambda s: w3(s, 0, 128), dram_cols(win_l, 3072 + j * 128, 128)),
                        (lambda s: w3(s, 1024, 128), dram_cols(win_l, 4096 + j * 128, 128)),
                        (lambda s: w3(s, 2048, 128), dram_cols(win_l, 5120 + j * 128, 128)),
                    ])

                def compute(slot, sbuf_):
                    wcb = w3(slot, 0, 128)
                    wcc = w3(slot, 1024, 128)
                    wcx = w3(slot, 2048, 128)
                    u = U_pad
                    for tb in range(4):
                        tsl = slice(tb * 512, (tb + 1) * 512)
                        pc_, pcb = psbank()
                        for c in range(KC):
                            mm(pc_[:, :], wcc[:, c, :], hT[:, c, tsl], c == 0, c == KC - 1, [sbuf_, hTb], [pcb])
                        px, pxb = psbank()
                        for c in range(KC):
                            mm(px[:, :], wcx[:, c, :], hT[:, c, tsl], c == 0, c == KC - 1, [sbuf_, hTb], [pxb])
                        t_, tb_ = getC()
                        act(t_, pc_[:, :], AF.Copy, [pcb], [tb_])
                        tt("dve", u[:, 1 + tb * 512:1 + (tb + 1) * 512], t_, px[:, :], ALU.mult, [tb_, pxb], [b_u])
                    cw = g0 + GC_CONV
                    for tb in range(4):
                        tsl = slice(tb * 512, (tb + 1) * 512)
                        acc, accb = getC()
                        act(acc, u[:, 1 + tb * 512:1 + (tb + 1) * 512], AF.Copy, [b_u, b_const], [accb], scale=gcols[:, cw + 8 + j:cw + 8 + j + 1])
                        stt("dve", acc, u[:, tb * 512:(tb + 1) * 512], gcols[:, cw + j:cw + j + 1], acc, ALU.mult, ALU.add, [b_u, accb, b_const], [accb])
                        stt("dve", acc, u[:, 2 + tb * 512:2 + (tb + 1) * 512], gcols[:, cw + 16 + j:cw + 16 + j + 1], acc, ALU.mult, ALU.add, [b_u, accb, b_const], [accb])
                        pb_, pbb = psbank()
                        for c in range(KC):
                            mm(pb_[:, :], wcb[:, c, :], hT[:, c, tsl], c == 0, c == KC - 1, [sbuf_, hTb], [pbb])
                        tt("dve", yT[:, j, tsl], pb_[:, :], acc, ALU.mult, [pbb, accb], [b_y[j]])

                return load, compute

            for j in range(KC):
                steps.append(conv_step(j))
            for j in range(KC):
                steps.append(branch_step(1, j))

            def out_proj_step(wmat, half, srcT, src_bufs):
                def load(slot, sbuf_):
                    wload(slot, sbuf_, [(lambda s: w3(s, 0, 512), dram_cols(wmat, half * 512, 512))])

                def compute(slot, sbuf_):
                    wt = w3(slot, 0, 512)
                    for i in range(NT):
                        pp, pb = psbank()
                        for c in range(KC):
                            mm(pp[:, :], srcT[:, c, i * 128:(i + 1) * 128], wt[:, c, :], c == 0, c == KC - 1, [sbuf_, src_bufs[c]], [pb])
                        tt("dve", X[:, i, half * 512:(half + 1) * 512], X[:, i, half * 512:(half + 1) * 512], pp[:, :], ALU.add,
                           [Xb[i], pb], [Xb[i]])

                return load, compute

            for half in range(2):
                steps.append(out_proj_step(w_o[li_], half, Mv, b_M))

            def mixer_end(slot, sbuf_):
                S.alias([b_big], b_M + CTb)
                S.alias([YRb], b_y)

            steps.append((None, mixer_end))
            if stop_after == (l, "mix"):
                return True

            offx = 0
            kcT, offx = carve(BIG, offx, KC * NMEM)
            kcT = kcT.rearrange("p (c m) -> p c m", c=KC)
            vc, offx = carve(BIG, offx, 2 * D)
            vc = vc.rearrange("p (i d) -> p i d", i=2)
            XE = []
            for _ in range(6):
                a, offx = carve(BIG, offx, 512)
                XE.append(a)
            XT = []
            for _ in range(4):
                a, offx = carve(BIG, offx, 512, F32)
                XT.append(a)
            assert offx <= NBIG
            b_kc = S.buf("kcT")
            b_vc = S.buf("vc")
            XEb = S.bufs(6, "XE")
            XTb = S.bufs(4, "XT")
            rrx = [0, 0]

            def getXE():
                i = rrx[0] % 6
                rrx[0] += 1
                return XE[i], XEb[i]

            def getXT():
                i = rrx[1] % 4
                rrx[1] += 1
                return XT[i], XTb[i]

            qcT = YR[:, :].rearrange("p (c t) -> p c t", c=KC)
            b_qc = [S.buf("qc%d" % c) for c in range(KC)]
            ocT = HT[:, :].rearrange("p (c t) -> p c t", c=KC)
            b_oc = [S.buf("oc%d" % c) for c in range(KC)]

            def cross_begin(slot, sbuf_):
                S.alias([b_kc, b_vc] + XEb + XTb, [b_big])
                S.alias(b_qc, [YRb])
                norm_to_fm(lambda i: X[:, i, :], Xb, NT, g0 + GC_CROSS, hT, hTb)

            steps.append((None, cross_begin))

            def ckv_step(part):
                def load(slot, sbuf_):
                    wload(slot, sbuf_, [(lambda s: w3(s, 0, 512), dram_cols(w_ckv[li_], part * 512, 512))])

                def compute(slot, sbuf_):
                    wt = w3(slot, 0, 512)
                    if part < 2:
                        for jj in range(4):
                            j = part * 4 + jj
                            pp, pb = psbank()
                            for c in range(KC):
                                mm(pp[:, 0:NMEM], wt[:, c, jj * 128:(jj + 1) * 128], memT[:, c, :], c == 0, c == KC - 1, [sbuf_, b_memT], [pb])
                            act(kcT[:, j, :], pp[:, 0:NMEM], AF.Copy, [pb], [b_kc])
                    else:
                        for i in range(2):
                            pp, pb = psbank()
                            for c in range(KC):
                                mm(pp[:, :], memT[:, c, i * 128:(i + 1) * 128], wt[:, c, :], c == 0, c == KC - 1, [sbuf_, b_memT], [pb])
                            act(vc[:, i, (part - 2) * 512:(part - 1) * 512], pp[:, :], AF.Copy, [pb], [b_vc])

                return load, compute

            for part in range(4):
                steps.append(ckv_step(part))

            def cq_step(half):
                def load(slot, sbuf_):
                    wload(slot, sbuf_, [(lambda s: w3(s, 0, 512), dram_cols(w_cq[li_], half * 512, 512))])

                def compute(slot, sbuf_):
                    wt = w3(slot, 0, 512)
                    for jj in range(4):
                        j = half * 4 + jj
                        for tb in range(4):
                            tsl = slice(tb * 512, (tb + 1) * 512)
                            pp, pb = psbank()
                            for c in range(KC):
                                mm(pp[:, :], wt[:, c, jj * 128:(jj + 1) * 128], hT[:, c, tsl], c == 0, c == KC - 1, [sbuf_, hTb], [pb])
                            if (jj + tb) % 2 == 0:
                                act(qcT[:, j, tsl], pp[:, :], AF.Copy, [pb], [b_qc[j]])
                            else:
                                cp("dve", qcT[:, j, tsl], pp[:, :], [pb], [b_qc[j]])

                return load, compute

            for half in range(2):
                steps.append(cq_step(half))

            def cross_attn(slot, sbuf_):
                S.alias(b_oc, [hTb])
                for h in range(4):
                    for tb in range(4):
                        tsl = slice(tb * 512, (tb + 1) * 512)
                        pO = [psbank(), psbank()]
                        pR = psbank()
                        for mb in range(2):
                            pS, pSb = psbank()
                            for cc in range(2):
                                mm(pS[:, :], kcT[:, 2 * h + cc, mb * 128:(mb + 1) * 128], qcT[:, 2 * h + cc, tsl], cc == 0, cc == 1,
                                   [b_kc, b_qc[2 * h + cc]], [pSb])
                            E, Eb_ = getXE()
                            act(E, pS[:, :], AF.Exp, [pSb], [Eb_], scale=1.0 / 16)
                            for ee in range(2):
                                mm(pO[ee][0][:, :], vc[:, mb, h * 256 + ee * 128:h * 256 + (ee + 1) * 128], E, mb == 0, mb == 1,
                                   [b_vc, Eb_], [pO[ee][1]])
                            mm(pR[0][:, :], ones, E, mb == 0, mb == 1, [b_const, Eb_], [pR[1]])
                        r_, rb_ = getXT()
                        S.op("dve", lambda e, r_=r_, pR=pR: e.reciprocal(out=r_, in_=pR[0][:, :]), reads=[pR[1]], writes=[rb_])
                        for ee in range(2):
                            tt("dve", ocT[:, 2 * h + ee, tsl], pO[ee][0][:, :], r_, ALU.mult, [pO[ee][1], rb_], [b_oc[2 * h + ee]])

            steps.append((None, cross_attn))
            for half in range(2):
                steps.append(out_proj_step(w_co[li_], half, ocT, b_oc))

            def cross_end(slot, sbuf_):
                S.alias([b_big], [b_kc, b_vc] + XEb + XTb)
                S.alias([YRb], b_qc)
                S.alias([hTb], b_oc)

            steps.append((None, cross_end))
            if stop_after == (l, "cross"):
                return True

            hrows = YR[:, :].rearrange("p (i d) -> p i d", i=NT)
            b_hr = S.bufs(NT, "hrows")
            offm = 0
            oh12, offm = carve(BIG, offm, NT * 64, F32)
            oh12 = oh12.rearrange("p (i e) -> p i e", i=NT)
            mohb, offm = carve(BIG, offm, NT * 32)
            mohb = mohb.rearrange("p (i e) -> p i e", i=NT)
            wts, offm = carve(BIG, offm, NT * 2, F32)
            wts = wts.rearrange("p (i k) -> p i k", i=NT)
            desti, offm = carve(BIG, offm, NT * 2, I32)
            desti = desti.rearrange("p (i k) -> p i k", i=NT)
            rt, offm = carve(BIG, offm, 320, F32)
            cnt, offm = carve(BIG, offm, 32, F32)
            padA, offm = carve(BIG, offm, 32, F32)
            padB, offm = carve(BIG, offm, 32, F32)
            pstart, offm = carve(BIG, offm, 32, F32)
            pend, offm = carve(BIG, offm, 32, F32)
            eblk, offm = carve(BIG, offm, NBLK, F32)
            idxgu, offm = carve(BIG, offm, NBLK * 8, I32)
            idxd, offm = carve(BIG, offm, NBLK * 8, I32)
            idxgu = idxgu.rearrange("p (b c) -> p b c", b=NBLK)
            idxd = idxd.rearrange("p (b c) -> p b c", b=NBLK)
            idxgu_f, offt = carve(BIG, offm, NBLK * 8, F32)
            idxd_f, offt = carve(BIG, offt, NBLK * 8, F32)
            idxgu_f = idxgu_f.rearrange("p (b c) -> p b c", b=NBLK)
            idxd_f = idxd_f.rearrange("p (b c) -> p b c", b=NBLK)
            rtall, offt = carve(BIG, offt, NT * 208, F32)
            rtall = rtall.rearrange("p (i n) -> p i n", i=NT)
            assert offt <= NBIG, offt
            offm0 = offm
            b_rt = S.buf("rt")
            b_rt_i = S.bufs(NT, "rti")
            b_oh_i = S.bufs(NT, "ohi")
            b_dest_i = S.bufs(NT, "desti")
            b_oh = S.buf("oh")
            b_route = S.buf("route")

            def moe_begin(slot, sbuf_):
                S.alias([b_rt, b_oh, b_route] + b_rt_i + b_oh_i + b_dest_i, [b_big])
                S.alias(b_hr, [YRb])

            steps.append((None, moe_begin))

            def router_step():
                def load(slot, sbuf_):
                    wload(slot, sbuf_, [(lambda s: s[:, 0:KC * 36].rearrange("p (c n) -> p c n", c=KC),
                                         w_rt[li_].rearrange("(c p) n -> p c n", p=128))])

                def compute(slot, sbuf_):
                    wr = slot[:, 0:KC * 36].rearrange("p (c n) -> p c n", c=KC)
                    norm_to_fm(lambda i: X[:, i, :], Xb, NT, g0 + GC_FFN, hT, hTb,
                               keep_rows=lambda i: (hrows[:, i, :], b_hr[i]))
                    def rt_tile(i):
                        rt = rtall[:, i, :]
                        yield
                        lg = rt[:, 0:36]
                        yield
                        pp, pb = psbank()
                        yield
                        for c in range(KC):
                            mm(pp[:, 0:36], hT[:, c, i * 128:(i + 1) * 128], wr[:, c, :], c == 0, c == KC - 1, [sbuf_, hTb], [pb])
                        yield
                        R_ = [b_rt_i[i]]
                        yield
                        act(lg, pp[:, 0:36], AF.Copy, [pb], R_)
                        yield
                        mxg = rt[:, 40:41]
                        yield
                        S.op("dve", lambda e, mxg=mxg: e.tensor_reduce(out=mxg, in_=rt[:, 0:4], axis=AX.X, op=ALU.max), reads=R_, writes=R_)
                        yield
                        ohg = rt[:, 44:48]
                        yield
                        ts("dve", ohg, rt[:, 0:4], mxg, ALU.is_equal, R_, R_)
                        yield
                        nmx = rt[:, 41:42]
                        yield
                        ts("dve", nmx, mxg, -1.0, ALU.mult, R_, R_)
                        yield
                        sumg = rt[:, 42:43]
                        yield
                        act(rt[:, 48:52], rt[:, 0:4], AF.Exp, R_, R_, bias=nmx, accum=sumg)
                        yield
                        gw = rt[:, 43:44]
                        yield
                        S.op("dve", lambda e, gw=gw, sumg=sumg: e.reciprocal(out=gw, in_=sumg), reads=R_, writes=R_)
                        yield
                        pen = rt[:, 52:56]
                        yield
                        ts("dve", pen, ohg, -1.0, ALU.add, R_, R_, s2=-NEG, op1=ALU.mult)
                        yield
                        lem = rt[:, 64:96]
                        yield
                        for g in range(4):
                            ts("dve", lem[:, g * 8:(g + 1) * 8], rt[:, 4 + g * 8:4 + (g + 1) * 8], pen[:, g:g + 1], ALU.add, R_, R_)
                        yield
                        m1 = rt[:, 56:57]
                        yield
                        S.op("dve", lambda e, m1=m1, lem=lem: e.tensor_reduce(out=m1, in_=lem, axis=AX.X, op=ALU.max), reads=R_, writes=R_)
                        yield
                        ts("dve", oh12[:, i, 0:32], lem, m1, ALU.is_equal, R_, [b_oh_i[i]])
                        yield
                        lem2 = rt[:, 96:128]
                        yield
                        stt("dve", lem2, oh12[:, i, 0:32], NEG, lem, ALU.mult, ALU.add, [b_oh_i[i], b_rt_i[i]], R_)
                        yield
                        m2 = rt[:, 57:58]
                        yield
                        S.op("dve", lambda e, m2=m2, lem2=lem2: e.tensor_reduce(out=m2, in_=lem2, axis=AX.X, op=ALU.max), reads=R_, writes=R_)
                        yield
                        ts("dve", oh12[:, i, 32:64], lem2, m2, ALU.is_equal, R_, [b_oh_i[i]])
                        yield
                        dd = rt[:, 58:59]
                        yield
                        tt("dve", dd, m2, m1, ALU.subtract, R_, R_)
                        yield
                        ed = rt[:, 59:60]
                        yield
                        act(ed, dd, AF.Exp, R_, R_)
                        yield
                        den = rt[:, 60:61]
                        yield
                        ts("dve", den, ed, 1.0, ALU.add, R_, R_)
                        yield
                        w1 = rt[:, 61:62]
                        yield
                        S.op("dve", lambda e, w1=w1, den=den: e.reciprocal(out=w1, in_=den), reads=R_, writes=R_)
                        yield
                        tt("dve", wts[:, i, 0:1], w1, gw, ALU.mult, R_, [b_oh_i[i]])
                        yield
                        w2 = rt[:, 62:63]
                        yield
                        tt("dve", w2, ed, w1, ALU.mult, R_, R_)
                        yield
                        tt("dve", wts[:, i, 1:2], w2, gw, ALU.mult, R_, [b_oh_i[i]])
                        yield
                        tt("dve", mohb[:, i, :], oh12[:, i, 0:32], oh12[:, i, 32:64], ALU.add, [b_oh_i[i]], [b_oh_i[i]])
                        yield


                    def run_interleaved(gens):
                        gens = list(gens)
                        while gens:
                            for g_ in list(gens):
                                try:
                                    next(g_)
                                except StopIteration:
                                    gens.remove(g_)

                    run_interleaved([rt_tile(i) for i in range(0, 8)])
                    run_interleaved([rt_tile(i) for i in range(8, NT)])
                    pc_, pcb = psbank()
                    for i in range(NT):
                        mm(pc_[:, 0:32], ones, mohb[:, i, :], i == 0, i == NT - 1, [b_const, b_oh_i[i]], [pcb])
                    Q_ = [b_route]
                    cp("dve", cnt, pc_[:, 0:32], [pcb], Q_)
                    ts("dve", padA, cnt, 1.0 / RB, ALU.mult, Q_, Q_, s2=(RB - 1.0) / RB - 0.5 + 0.5 / RB, op1=ALU.add)
                    cp("dve", padB.bitcast(I32), padA, Q_, Q_)
                    cp("dve", padA, padB.bitcast(I32), Q_, Q_)
                    ts("dve", padA, padA, float(RB), ALU.mult, Q_, Q_)
                    cp("dve", pstart, padA, Q_, Q_)
                    a, b_ = padA, padB
                    for s_ in (1, 2, 4, 8, 16):
                        cp("dve", b_[:, 0:s_], a[:, 0:s_], Q_, Q_)
                        tt("dve", b_[:, s_:32], a[:, s_:32], a[:, 0:32 - s_], ALU.add, Q_, Q_)
                        a, b_ = b_, a
                    cp("dve", pend, a, Q_, Q_)
                    tt("dve", pstart, pend, pstart, ALU.subtract, Q_, Q_)
                    def dest_tile(i):
                        rt = rtall[:, i, :]
                        yield
                        pp, pb = psbank()
                        yield
                        mm(pp[:, 0:32], ustr, mohb[:, i, :], True, i == 0, [b_const, b_oh_i[i]], [pb])
                        yield
                        for j in range(i):
                            mm(pp[:, 0:32], ones, mohb[:, j, :], False, j == i - 1, [b_const, b_oh_i[j]], [pb])
                        yield
                        tmp = rt[:, 128:160]
                        yield
                        tt("dve", tmp, pp[:, 0:32], pstart, ALU.add, [pb, b_route], [b_rt_i[i]])
                        yield
                        for k in range(2):
                            prod = rt[:, 160:192]
                            df = rt[:, 192 + k:193 + k]
                            tt("dve", prod, tmp, oh12[:, i, k * 32:(k + 1) * 32], ALU.mult, [b_rt_i[i], b_oh_i[i]], [b_rt_i[i]])
                            S.op("dve", lambda e, df=df, prod=prod: e.tensor_reduce(out=df, in_=prod, axis=AX.X, op=ALU.add), reads=[b_rt_i[i]], writes=[b_rt_i[i]])
                            cp("dve", desti[:, i, k:k + 1], df, [b_rt_i[i]], [b_dest_i[i]])
                        yield


                    run_interleaved([dest_tile(i) for i in range(0, 8)])
                    run_interleaved([dest_tile(i) for i in range(8, NT)])
                    S.op("dve", lambda e: e.memset(eblk, 0.0), writes=Q_)
                    for e_ in range(NEXP):
                        stt("dve", eblk, thr[:, 0:NBLK], pend[:, e_:e_ + 1], eblk, ALU.is_ge, ALU.add, [b_const] + Q_, Q_)
                    ts("dve", eblk, eblk, float(NEXP - 1), ALU.min, Q_, Q_, s2=float(li_ * NEXP), op1=ALU.add)
                    for c in range(8):
                        ts("dve", idxgu_f[:, :, c], eblk, float(D), ALU.mult, Q_, Q_, s2=pcv[:, c:c + 1], op1=ALU.add)
                    for c in range(6):
                        ts("dve", idxd_f[:, :, c], eblk, float(DEXP), ALU.mult, Q_, Q_, s2=pcv[:, c:c + 1], op1=ALU.add)
                    cp("dve", idxgu[:, :, :], idxgu_f[:, :, :], Q_, Q_)
                    cp("dve", idxd[:, :, 0:6], idxd_f[:, :, 0:6], Q_, Q_)
                    for i in range(NT):
                        for k in range(2):
                            S.dma("pool", "scat", lambda e, i=i, k=k: e.indirect_dma_start(
                                out=xb_d[:, :], out_offset=bass.IndirectOffsetOnAxis(ap=desti[:, i, k:k + 1], axis=0),
                                in_=hrows[:, i, :], in_offset=None), reads=[b_hr[i], b_dest_i[i]], writes=[b_xb])

                return load, compute

            steps.append(router_step())

            Gw = [HT[:, g * 6144:(g + 1) * 6144].rearrange("p (c n) -> p c n", c=8) for g in range(2)]
            xg_t = [HT[:, 12288 + i * 1024:12288 + (i + 1) * 1024] for i in range(2)]
            xgT_t = [HT[:, 14336 + i * 1024:14336 + (i + 1) * 1024].rearrange("p (c t) -> p c t", c=8) for i in range(2)]
            Uw = [YR[:, g * 6144:(g + 1) * 6144].rearrange("p (c n) -> p c n", c=8) for g in range(2)]
            ybt = [YR[:, 12288 + i * 2048:12288 + (i + 1) * 2048].bitcast(F32) for i in range(2)]
            offd = offm0 + (offm0 % 2)
            Dw = []
            for g in range(2):
                a, offd = carve(BIG, offd, 6 * D)
                Dw.append(a.rearrange("p (c n) -> p c n", c=6))
            sgt = U_pad[:, 2:2 + 2 * DEXP].bitcast(F32)
            hmt = hn_t[0][:, 0:DEXP]
            hmT = [hn_t[1][:, 0:DEXP].rearrange("p (c t) -> p c t", c=6)]
            a, offd = carve(BIG, offd, DEXP)
            hmT.append(a.rearrange("p (c t) -> p c t", c=6))
            assert offd <= NBIG, offd
            b_G = S.bufs(2, "Gw")
            b_U = S.bufs(2, "Uw")
            b_D = S.bufs(2, "Dw")
            b_xg = S.bufs(2, "xg")
            b_xgT = S.bufs(2, "xgT")
            b_ybt = S.bufs(2, "ybt")
            b_sg = S.buf("sg")
            b_hm = S.buf("hm")
            b_hmT = S.bufs(2, "hmT")
            weg = w_eg.rearrange("l e d n -> (l e d) n")
            weu = w_eu.rearrange("l e d n -> (l e d) n")
            wed = w_ed.rearrange("l e d n -> (l e d) n")

            def moe_blocks(slot, sbuf_):
                S.alias(b_G + b_xg + b_xgT, [hTb])
                S.alias(b_U + b_ybt, b_hr)
                S.alias(b_D + [b_hmT[1]], [b_big, b_route] + b_rt_i)
                S.alias([b_sg], [b_u])
                S.alias([b_hm, b_hmT[0]], hn_b)

                def issue_w(b):
                    p = b % 2
                    for c in range(8):
                        S.dma("pool", "G%d" % p, lambda e, c=c, p=p, b=b: e.indirect_dma_start(
                            out=Gw[p][:, c, :], out_offset=None, in_=weg,
                            in_offset=bass.IndirectOffsetOnAxis(ap=idxgu[:, b, c:c + 1], axis=0)), reads=[b_route], writes=[b_G[p]])
                    for c in range(8):
                        S.dma("pool", "U%d" % p, lambda e, c=c, p=p, b=b: e.indirect_dma_start(
                            out=Uw[p][:, c, :], out_offset=None, in_=weu,
                            in_offset=bass.IndirectOffsetOnAxis(ap=idxgu[:, b, c:c + 1], axis=0)), reads=[b_route], writes=[b_U[p]])
                    for c in range(6):
                        S.dma("pool", "D%d" % p, lambda e, c=c, p=p, b=b: e.indirect_dma_start(
                            out=Dw[p][:, c, :], out_offset=None, in_=wed,
                            in_offset=bass.IndirectOffsetOnAxis(ap=idxd[:, b, c:c + 1], axis=0)), reads=[b_route], writes=[b_D[p]])

                def issue_x(t):
                    q = t % 2
                    dma_sp("xg%d" % q, xg_t[q], xb_d[t * 128:(t + 1) * 128, :], reads=[b_xb], writes=[b_xg[q]])

                NTIL = NROW // 128
                SUB = RB // 128
                issue_w(0)
                issue_x(0)
                for t in range(NTIL):
                    b = t // SUB
                    p = b % 2
                    q = t % 2
                    if t % SUB == 0 and b + 1 < NBLK:
                        issue_w(b + 1)
                    if t + 1 < NTIL:
                        issue_x(t + 1)
                    pt, pb = psbank()
                    ptb = pt[:, :].bitcast(BF16).rearrange("p (c t) -> p c t", t=128)
                    for c in range(KC):
                        tr(ptb[:, c, :], xg_t[q][:, c * 128:(c + 1) * 128], [b_xg[q]], [pb])
                    for c in range(KC):
                        gc = gcols[:, g0 + GC_FFN + c:g0 + GC_FFN + c + 1]
                        if c % 2 == 0:
                            ts("dve", xgT_t[q][:, c, :], ptb[:, c, :], gc, ALU.mult, [pb, b_const], [b_xgT[q]])
                        else:
                            act(xgT_t[q][:, c, :], ptb[:, c, :], AF.Copy, [pb, b_const], [b_xgT[q]], scale=gc)
                    for (n0, nn) in ((0, 512), (512, 256)):
                        pg, pgb = psbank()
                        for c in range(KC):
                            mm(pg[:, 0:nn], xgT_t[q][:, c, :], Gw[p][:, c, n0:n0 + nn], c == 0, c == KC - 1, [b_xgT[q], b_G[p]], [pgb])
                        pu, pub = psbank()
                        for c in range(KC):
                            mm(pu[:, 0:nn], xgT_t[q][:, c, :], Uw[p][:, c, n0:n0 + nn], c == 0, c == KC - 1, [b_xgT[q], b_U[p]], [pub])
                        act(sgt[:, n0:n0 + nn], pg[:, 0:nn], AF.Silu, [pgb], [b_sg])
                        tt("dve", hmt[:, n0:n0 + nn], sgt[:, n0:n0 + nn], pu[:, 0:nn], ALU.mult, [b_sg, pub], [b_hm])
                    pt2, pb2 = psbank()
                    pt2b = pt2[:, :].bitcast(BF16).rearrange("p (c t) -> p c t", t=128)
                    for c in range(6):
                        tr(pt2b[:, c, :], hmt[:, c * 128:(c + 1) * 128], [b_hm], [pb2])
                    act(hmT[q][:, 0:3, :], pt2b[:, 0:3, :], AF.Copy, [pb2], [b_hmT[q]])
                    cp("dve", hmT[q][:, 3:6, :], pt2b[:, 3:6, :], [pb2], [b_hmT[q]])
                    for n in range(2):
                        py, pyb = psbank()
                        for c in range(6):
                            mm(py[:, :], hmT[q][:, c, :], Dw[p][:, c, n * 512:(n + 1) * 512], c == 0, c == 5, [b_hmT[q], b_D[p]], [pyb])
                        if n == 0:
                            act(ybt[q][:, 0:512], py[:, :], AF.Copy, [pyb], [b_ybt[q]])
                        else:
                            cp("dve", ybt[q][:, 512:1024], py[:, :], [pyb], [b_ybt[q]])
                    dma_sp("ybst", yb_d[t * 128:(t + 1) * 128, :], ybt[q], reads=[b_ybt[q]], writes=[b_yb])
                S.alias(b_gat, b_G + b_xg + b_xgT)
                for i in range(NT):
                    for k in range(2):
                        gi = (i * 2 + k) % 4
                        S.dma("pool", "gat%d" % gi, lambda e, i=i, k=k, gi=gi: e.indirect_dma_start(
                            out=gat_t[gi], out_offset=None, in_=yb_d[:, :],
                            in_offset=bass.IndirectOffsetOnAxis(ap=desti[:, i, k:k + 1], axis=0)), reads=[b_yb, b_dest_i[i]], writes=[b_gat[gi]])
                        stt("dve", X[:, i, :], gat_t[gi], wts[:, i, k:k + 1], X[:, i, :], ALU.mult, ALU.add, [b_gat[gi], b_oh_i[i], Xb[i]], [Xb[i]])

            gat_t = [HT[:, i * 2048:(i + 1) * 2048].bitcast(F32) for i in range(4)]
            b_gat = S.bufs(4, "gat")
            steps.append((None, moe_blocks))

            def moe_end(slot, sbuf_):
                S.alias([b_big], [b_rt, b_oh, b_route] + b_rt_i + b_oh_i + b_dest_i + b_D + b_hmT)
                S.alias([b_u], [b_sg])
                S.alias(hn_b, [b_hm, b_hmT[0]])
                S.alias([YRb], b_U + b_ybt + b_hr)
                S.alias([hTb], b_gat + b_G + b_xg + b_xgT)

            steps.append((None, moe_end))
            if stop_after == (l, "moe"):
                return True
            return False

        ztile = sb("ztile", [128, 512], BF16)
        b_zt = S.buf("zt")
        S.op("pool", lambda e: e.memset(ztile[:, :], 0.0), writes=[b_zt])
        U_pad = sb("U_pad", [128, S_LEN + 4], BF16)
        b_u = S.buf("u")
        b_xb = S.buf("xb_d")
        b_yb = S.buf("yb_d")
        S.op("pool", lambda e: e.memset(U_pad[:, :], 0.0), writes=[b_u])

        stopped = False
        for li_, l in enumerate(layer_ids):
            stopped = layer_steps(li_, l)
            if stopped:
                break

        def epilogue(slot, sbuf_):
            if final and not stopped:
                gfin, _ = carve(BIG, 0, D, F32)
                otile = [carve(BIG, 2 * D + i * 2 * D, D, F32)[0] for i in range(2)]
                b_gf = S.buf("gfin")
                b_ot = S.bufs(2, "otile")
                S.alias([b_gf] + b_ot, [b_big])
                dma_sp("c2", gfin, gfin_d[:, :], writes=[b_gf])
                rstd_tiles(lambda i: X[:, i, :], Xb, NT, 1.0 / D)
                for i in range(NT):
                    p = i % 2
                    act(otile[p], X[:, i, :], AF.Copy, [Xb[i], b_rstd], [b_ot[p]], scale=rstd[:, i:i + 1])
                    tt("dve", otile[p], otile[p], gfin, ALU.mult, [b_ot[p], b_gf], [b_ot[p]])
                    dma_sp("out", out_d[i * 128:(i + 1) * 128, :], otile[p], reads=[b_ot[p]], writes=[b_out])
            else:
                for i in range(NT):
                    dma_sp("out", out_d[i * 128:(i + 1) * 128, :], X[:, i, :], reads=[Xb[i]], writes=[b_out])

        b_out = S.buf("out")
        if max_steps is not None:
            del steps[max_steps:]
        steps.append((None, epilogue))

        wsteps = [k for k, (ld, _) in enumerate(steps) if ld is not None]
        issued = 0
        done = 0
        for k, (ld, cpf) in enumerate(steps):
            while issued < len(wsteps) and issued < done + NSLOT:
                sl = issued % NSLOT
                steps[wsteps[issued]][0](WS[sl], WSb[sl])
                issued += 1
            if ld is not None:
                sl = done % NSLOT
                cpf(WS[sl], WSb[sl])
                done += 1
            else:
                cpf(None, None)
        S.wait_all("sp", [b_out])
        if os.environ.get('WAITCS'):
            for b_ in dbg_bufs:
                S.wait_all("sp", [b_])
        S.finalize()
        dl = S.simulate()
        if dl is not None:
            raise RuntimeError("sync deadlock: %r" % (dl,))
        S.emit()
    return nc, S.ninst


def _consts():
    ident = np.eye(128, dtype=np.float32)
    ones = np.ones((128, 128), np.float32)
    rrot = np.zeros((128, 128), np.float32)
    for c in range(2):
        for i in range(8):
            rrot[c * 64 + 8 + i, c * 64 + i] = -1.0
            rrot[c * 64 + i, c * 64 + 8 + i] = 1.0
    ustr = np.triu(np.ones((128, 128), np.float32), 1)
    cmat = np.concatenate([ident, ones, rrot, ustr], axis=1)
    inv = np.float32(500000.0) ** (-np.arange(0, 16, 2, dtype=np.float32) / np.float32(16))
    ang = np.arange(S_LEN, dtype=np.float32)[:, None] * inv[None, :]
    cs, sn = np.cos(ang).astype(np.float32), np.sin(ang).astype(np.float32)
    cosT = np.ones((128, S_LEN), np.float32)
    sinT = np.zeros((128, S_LEN), np.float32)
    for c in range(2):
        for i in range(16):
            cosT[c * 64 + i] = cs[:, i % 8]
            sinT[c * 64 + i] = sn[:, i % 8]
    cossin = np.concatenate([cosT, sinT], axis=1)
    thr = np.broadcast_to((np.arange(64, dtype=np.float32) * float(RB))[None, :], (128, 64))
    pc = np.arange(8, dtype=np.float32)[None, :] * 128.0 + np.arange(128, dtype=np.float32)[:, None]
    cmisc = np.ascontiguousarray(np.concatenate([thr, pc], axis=1))
    return np.ascontiguousarray(cmat.astype(ml_dtypes.bfloat16)), np.ascontiguousarray(cossin.astype(ml_dtypes.bfloat16)), cmisc


def _pack_small(inp, layer_ids):
    cols = []
    for l in layer_ids:
        def colmaj(v):
            return np.asarray(v, np.float32).reshape(8, 128).T
        cols.append(colmaj(inp["norm_mix"][l]))
        cols.append(colmaj(inp["norm_cross"][l]))
        cols.append(colmaj(inp["norm_ffn"][l]))
        for k in range(3):
            cols.append(colmaj(inp["conv_w"][l][k]))
        cols.append(np.asarray(inp["subln"][l], np.float32).reshape(128, 1))
    cols.append(np.asarray(inp["norm_mem"], np.float32).reshape(8, 128).T)
    gcols = np.ascontiguousarray(np.concatenate(cols, axis=1))
    lam = []
    for l in layer_ids:
        lam.append(np.concatenate([inp["lambda_q1"][l], inp["lambda_k1"][l], inp["lambda_q2"][l], inp["lambda_k2"][l]]))
    lamv = np.ascontiguousarray(np.broadcast_to(np.concatenate(lam)[None, :].astype(np.float32), (128, len(layer_ids) * 256)))
    gfin = np.ascontiguousarray(np.broadcast_to(np.asarray(inp["norm_final"], np.float32)[None, :], (128, D)))
    return gcols, lamv, gfin


_PROG_CACHE = {}


def _run(inp, x_shards, layer_ids, final, stop_after=None, cores=None, max_steps=None):
    key = (tuple(layer_ids), final, stop_after, max_steps)
    if key not in _PROG_CACHE:
        _PROG_CACHE[key] = build(list(layer_ids), final, stop_after, max_steps)[0]
    nc = _PROG_CACHE[key]
    cmat, cossin, cmisc = _consts()
    gcols, lamv, gfin = _pack_small(inp, layer_ids)
    ls = list(layer_ids)
    sl = slice(ls[0], ls[-1] + 1)
    w_rt = np.ascontiguousarray(np.concatenate([inp["w_router_group"][sl], inp["w_router_expert"][sl]], axis=-1))
    shared = {
        "w_in": np.ascontiguousarray(inp["w_in"][sl]), "w_branch": np.ascontiguousarray(inp["w_branch"][sl]),
        "w_o": np.ascontiguousarray(inp["w_o"][sl]), "w_cq": np.ascontiguousarray(inp["w_cq"][sl]),
        "w_ckv": np.ascontiguousarray(inp["w_ckv"][sl]), "w_co": np.ascontiguousarray(inp["w_co"][sl]),
        "w_rt": w_rt, "w_exp_gate": np.ascontiguousarray(inp["w_exp_gate"][sl]),
        "w_exp_up": np.ascontiguousarray(inp["w_exp_up"][sl]), "w_exp_down": np.ascontiguousarray(inp["w_exp_down"][sl]),
        "gcols": gcols, "lamv": lamv, "gfinal": gfin, "cmat": cmat, "cossin": cossin, "cmisc": cmisc,
    }
    cores = list(range(len(x_shards))) if cores is None else cores
    in_maps = []
    for b in range(len(x_shards)):
        m = dict(shared)
        m["x"] = np.ascontiguousarray(x_shards[b])
        m["mem"] = np.ascontiguousarray(inp["mem"][b])
        in_maps.append(m)
    res = run_bass_kernel_spmd(nc, in_maps, core_ids=cores)
    return [r["out"] for r in res.results]


MODE = "fused"


def kernel(**inputs):
    inp = {k: np.asarray(v) for k, v in inputs.items()}
    xs = [inp["x"][b] for b in range(inp["x"].shape[0])]
    if MODE == "fused":
        outs = _run(inp, xs, list(range(DEPTH)), True)
    else:
        for l in range(DEPTH):
            xs = _run(inp, xs, [l], l == DEPTH - 1)
        outs = xs
    return np.stack(outs, axis=0).astype(np.float32)
```

```python
import math
import os
import numpy as np
import ml_dtypes
HEADCUT = int(os.environ.get('HEADCUT', '99'))
from contextlib import ExitStack
import concourse.bass as bass
import concourse.mybir as mybir
from concourse.bass_utils import run_bass_kernel_spmd

F32 = mybir.dt.float32
BF16 = mybir.dt.bfloat16
I32 = mybir.dt.int32
ALU = mybir.AluOpType
AF = mybir.ActivationFunctionType
AX = mybir.AxisListType

D = 1024
S_LEN = 2048
NT = 16
KC = 8
NMEM = 256
DEPTH = 4
NEXP = 32
DEXP = 768
NBLK = 48
RB = 256
NROW = NBLK * RB
EPS = 1e-6
NEG = -1.0e30


class Buf:
    __slots__ = ("name", "w", "r", "excl")

    def __init__(self, name):
        self.name = name
        self.w = None
        self.r = {}
        self.excl = False


class Sched:
    ENG = ("pe", "act", "dve", "pool", "sp")

    def __init__(self, nc, stack):
        self.nc = nc
        self.stack = stack
        self.rec = {e: [] for e in self.ENG}
        self.sem = {e: stack.enter_context(nc.semaphore("s_" + e)) for e in self.ENG}
        self.cnt = {e: 0 for e in self.ENG}
        self.seen = {e: {} for e in self.ENG}
        self.dsem = {}
        self.dcnt = {}
        self.nbuf = 0
        self.ninst = 0

    def buf(self, name=None):
        self.nbuf += 1
        return Buf(name or "b%d" % self.nbuf)

    def bufs(self, n, name="b"):
        return [self.buf("%s%d" % (name, i)) for i in range(n)]

    def _deps(self, eng, reads, writes, skipkey=None):
        deps = {}

        def add(k, v):
            if k == skipkey:
                return
            if deps.get(k, 0) < v:
                deps[k] = v

        for b in reads:
            if b.w is not None:
                add(*b.w)
            if b.excl:
                for k, v in b.r.items():
                    if k != ("e", eng):
                        add(k, v)
        for b in writes:
            if b.w is not None:
                add(*b.w)
            for k, v in b.r.items():
                add(k, v)
        out = []
        seen = self.seen[eng]
        for k, v in deps.items():
            if eng == "pe" and k == ("e", "pe"):
                continue
            if k[0] == "d":
                v = self.dcnt[k[1]]
            if seen.get(k, 0) >= v:
                continue
            seen[k] = v
            out.append((k, v))
        return out

    def _post(self, ev, reads, writes):
        k, v = ev
        for b in reads:
            if b.r.get(k, 0) < v:
                b.r[k] = v
        for b in writes:
            b.w = ev
            b.r = {}

    def op(self, eng, fn, reads=(), writes=()):
        waits = self._deps(eng, reads, writes)
        self.cnt[eng] += 1
        ev = (("e", eng), self.cnt[eng])
        self.rec[eng].append(("op", waits, fn, self.cnt[eng]))
        self._post(ev, reads, writes)
        self.ninst += 1
        return ev

    def dma(self, eng, key, fn, reads=(), writes=()):
        if key not in self.dsem:
            self.dsem[key] = self.stack.enter_context(self.nc.semaphore("d_" + str(key)))
            self.dcnt[key] = 0
        waits = self._deps(eng, reads, writes, skipkey=("d", key))
        self.dcnt[key] += 16
        ev = (("d", key), self.dcnt[key])
        self.rec[eng].append(("dma", waits, fn, key))
        self._post(ev, reads, writes)
        self.ninst += 1
        return ev

    def alias(self, new_bufs, old_bufs):
        for nb in new_bufs:
            for ob in old_bufs:
                if ob.w is not None:
                    k, v = ob.w
                    if nb.r.get(k, 0) < v:
                        nb.r[k] = v
                for k, v in ob.r.items():
                    if nb.r.get(k, 0) < v:
                        nb.r[k] = v

    def wait_all(self, eng, bufs):
        waits = self._deps(eng, bufs, bufs)
        self.rec[eng].append(("wait", waits, None, None))

    def finalize(self):
        waited = {e: set() for e in self.ENG}
        for eng in self.ENG:
            for kind, waits, fn, x in self.rec[eng]:
                for k, v in waits:
                    if k[0] == "e":
                        waited[k[1]].add(v)
        rank = {e: {v: i + 1 for i, v in enumerate(sorted(waited[e]))} for e in self.ENG}
        self.prog = {}
        for eng in self.ENG:
            pl = []
            for kind, waits, fn, x in self.rec[eng]:
                rw = []
                for k, v in waits:
                    if k[0] == "e":
                        rw.append((self.sem[k[1]], rank[k[1]][v]))
                    else:
                        rw.append((self.dsem[k[1]], v))
                if kind == "op":
                    pl.append((rw, fn, self.sem[eng] if x in waited[eng] else None, 1))
                elif kind == "dma":
                    pl.append((rw, fn, self.dsem[x], 16))
                else:
                    pl.append((rw, None, None, 0))
            self.prog[eng] = pl

    def simulate(self):
        val = {}
        pc = {e: 0 for e in self.ENG}
        prog = self.prog
        while True:
            progressed = False
            for e in self.ENG:
                while pc[e] < len(prog[e]):
                    waits, fn, sem, inc = prog[e][pc[e]]
                    if all(val.get(id(s_), 0) >= v for s_, v in waits):
                        if sem is not None:
                            val[id(sem)] = val.get(id(sem), 0) + inc
                        pc[e] += 1
                        progressed = True
                    else:
                        break
            if all(pc[e] == len(prog[e]) for e in self.ENG):
                return None
            if not progressed:
                return {e: (pc[e], len(prog[e])) for e in self.ENG}

    def emit(self):
        nc = self.nc
        prog = self.prog

        def run(e, pl):
            for waits, fn, sem, inc in pl:
                for s_, v in waits:
                    e.wait_ge(s_, v)
                if fn is not None:
                    ins = fn(e)
                    if sem is not None:
                        ins.then_inc(sem, inc)

        with nc.Block() as block:
            @block.tensor
            def _(e):
                run(e, prog["pe"])

            @block.scalar
            def _(e):
                run(e, prog["act"])

            @block.vector
            def _(e):
                run(e, prog["dve"])

            @block.gpsimd
            def _(e):
                run(e, prog["pool"])

            @block.sync
            def _(e):
                run(e, prog["sp"])


def lambda_init(l):
    return 0.8 - 0.6 * math.exp(-0.3 * l)


GC_MIX, GC_CROSS, GC_FFN, GC_CONV, GC_SUBLN, GC_PER = 0, 8, 16, 24, 48, 49


def build(layer_ids, final, stop_after=None, max_steps=None):
    L = len(layer_ids)
    nc = bass.Bass("TRN2", target_bir_lowering=False)

    def din(name, shape, dt=F32):
        return nc.dram_tensor(name, shape, dt, kind="ExternalInput").ap()

    x_in = din("x", [S_LEN, D])
    mem_in = din("mem", [NMEM, D])
    w_in = din("w_in", [L, D, 8192])
    w_br = din("w_branch", [L, 2, D, D])
    w_o = din("w_o", [L, D, D])
    w_cq = din("w_cq", [L, D, D])
    w_ckv = din("w_ckv", [L, D, 2 * D])
    w_co = din("w_co", [L, D, D])
    w_rt = din("w_rt", [L, D, 36])
    w_eg = din("w_exp_gate", [L, NEXP, D, DEXP])
    w_eu = din("w_exp_up", [L, NEXP, D, DEXP])
    w_ed = din("w_exp_down", [L, NEXP, DEXP, D])
    gcols_d = din("gcols", [128, L * GC_PER + 8])
    lamv_d = din("lamv", [128, L * 256])
    gfin_d = din("gfinal", [128, D])
    cmat_d = din("cmat", [128, 4 * 128], BF16)
    cossin_d = din("cossin", [128, 2 * S_LEN], BF16)
    cmisc_d = din("cmisc", [128, 64 + 8])
    out_d = nc.dram_tensor("out", [S_LEN, D], F32, kind="ExternalOutput").ap()
    xb_d = nc.dram_tensor("xb_scr", [NROW, D], BF16, kind="Internal").ap()
    yb_d = nc.dram_tensor("yb_scr", [NROW, D], F32, kind="Internal").ap()

    st = ExitStack()
    with st:
        S = Sched(nc, st)

        def sb(name, shape, dt):
            return st.enter_context(nc.sbuf_tensor("sb_" + name, shape, dt))

        X = sb("X", [128, NT, D], F32)
        HT = sb("HT", [128, KC * S_LEN], BF16)
        YR = sb("YR", [128, KC * S_LEN], BF16)
        NBIG = 19 * 1024
        BIG = sb("BIG", [128, NBIG], BF16)
        NSLOT = 3
        WS = [sb("WS%d" % i, [128, 4096], BF16) for i in range(NSLOT)]
        cmat = sb("cmat", [128, 4 * 128], BF16)
        gcols = sb("gcols", [128, L * GC_PER + 8], F32)
        cmisc = sb("cmisc", [128, 72], F32)
        memT = sb("memT", [128, KC, NMEM], BF16)
        stat = sb("stat", [128, 64], F32)
        lamc = sb("lamc", [128, 2 * L], F32)
        sublnS = sb("sublnS", [128, L], F32)
        PS = [st.enter_context(nc.psum_tensor("ps%d" % i, [128, 512], F32)) for i in range(8)]
        PSB = S.bufs(8, "ps")
        for b_ in PSB:
            b_.excl = True

        ident = cmat[:, 0:128]
        ones = cmat[:, 128:256]
        rrot = cmat[:, 256:384]
        ustr = cmat[:, 384:512]
        thr = cmisc[:, 0:64]
        pcv = cmisc[:, 64:72]

        Xb = S.bufs(NT, "X")
        hTb = S.buf("hT")
        YRb = S.buf("YR")
        WSb = S.bufs(NSLOT, "WS")
        b_const = S.buf("const")
        b_memT = S.buf("memT")
        b_stat = S.buf("stat")

        hT = HT[:, :].rearrange("p (c t) -> p c t", c=KC)

        def carve(region, off, n, dt=BF16):
            if dt == BF16:
                return region[:, off:off + n], off + n
            assert off % 2 == 0
            return region[:, off:off + 2 * n].bitcast(dt), off + 2 * n

        psrr = [0]

        def psbank():
            i = psrr[0] % 8
            psrr[0] += 1
            return PS[i], PSB[i]

        def dma_sp(key, out, in_, reads=(), writes=()):
            return S.dma("sp", key, lambda e: e.dma_start(out=out, in_=in_), reads=reads, writes=writes)

        def dma_pool(key, out, in_, reads=(), writes=()):
            return S.dma("pool", key, lambda e: e.dma_start(out=out, in_=in_), reads=reads, writes=writes)

        def mm(out, lhsT, rhs, start, stop, reads, writes):
            S.op("pe", lambda e: e.matmul(out, lhsT=lhsT, rhs=rhs, start=start, stop=stop),
                 reads=reads, writes=writes)

        def tr(out, in_, reads, writes):
            S.op("pe", lambda e: e.transpose(out=out, in_=in_, identity=ident), reads=list(reads) + [b_const], writes=writes)

        def act(out, in_, func, reads, writes, scale=1.0, bias=None, accum=None):
            kw = {}
            if bias is not None:
                kw["bias"] = bias
            if accum is not None:
                kw["accum_out"] = accum
            S.op("act", lambda e: e.activation(out=out, in_=in_, func=func, scale=scale, **kw), reads=reads, writes=writes)

        def tt(eng, out, in0, in1, op, reads, writes):
            S.op(eng, lambda e: e.tensor_tensor(out=out, in0=in0, in1=in1, op=op), reads=reads, writes=writes)

        def ts(eng, out, in0, s1, op0, reads, writes, s2=None, op1=None, accum=None):
            kw = {}
            if accum is not None:
                kw["accum_out"] = accum
            if op1 is None:
                S.op(eng, lambda e: e.tensor_scalar(out=out, in0=in0, scalar1=s1, scalar2=None, op0=op0, **kw), reads=reads, writes=writes)
            else:
                S.op(eng, lambda e: e.tensor_scalar(out=out, in0=in0, scalar1=s1, scalar2=s2, op0=op0, op1=op1, **kw), reads=reads, writes=writes)

        def stt(eng, out, in0, scalar, in1, op0, op1, reads, writes):
            S.op(eng, lambda e: e.scalar_tensor_tensor(out=out, in0=in0, scalar=scalar, in1=in1, op0=op0, op1=op1), reads=reads, writes=writes)

        def cp(eng, out, in_, reads, writes):
            S.op(eng, lambda e: e.tensor_copy(out=out, in_=in_), reads=reads, writes=writes)

        dma_sp("c0", cmat[:, :], cmat_d[:, :], writes=[b_const])
        dma_sp("c1", gcols[:, :], gcols_d[:, :], writes=[b_const])
        dma_sp("c1", cmisc[:, :], cmisc_d[:, :], writes=[b_const])
        for i in range(NT):
            dma_sp("xin", X[:, i, :], x_in[i * 128:(i + 1) * 128, :], writes=[Xb[i]])

        b_big = S.buf("bigscratch")
        lamv, _ = carve(BIG, 0, L * 256, F32)
        ltmp, _ = carve(BIG, 2 * L * 256, 64, F32)
        dma_sp("c3", lamv, lamv_d[:, :], writes=[b_big])
        for li_, l in enumerate(layer_ids):
            base = li_ * 256
            for j in range(2):
                S.op("dve", lambda e, base=base, j=j: e.tensor_tensor(out=ltmp, in0=lamv[:, base + j * 128:base + j * 128 + 64],
                                                                      in1=lamv[:, base + j * 128 + 64:base + j * 128 + 128], op=ALU.mult),
                     reads=[b_big], writes=[b_big])
                S.op("dve", lambda e, j=j: e.tensor_reduce(out=stat[:, j:j + 1], in_=ltmp, axis=AX.X, op=ALU.add),
                     reads=[b_big], writes=[b_stat])
            act(stat[:, 2:4], stat[:, 0:2], AF.Exp, [b_stat], [b_stat])
            tt("dve", stat[:, 4:5], stat[:, 3:4], stat[:, 2:3], ALU.subtract, [b_stat], [b_stat])
            ts("dve", lamc[:, li_:li_ + 1], stat[:, 4:5], -lambda_init(l), ALU.add, [b_stat], [b_const])
            ts("dve", sublnS[:, li_:li_ + 1], gcols[:, li_ * GC_PER + GC_SUBLN:li_ * GC_PER + GC_SUBLN + 1],
               1.0 - lambda_init(l), ALU.mult, [b_const], [b_const])

        ssq = sb("ssq", [128, NT], F32)
        rstd = sb("rstd", [128, NT], F32)
        b_ssq = S.buf("ssq")
        b_rstd = S.buf("rstd")
        hn_t = [sb("hn%d" % i, [128, D], BF16) for i in range(2)]
        hn_b = S.bufs(2, "hn")

        def rstd_tiles(src_ap_fn, src_bufs, ntiles, inv_n):
            for i in range(ntiles):
                act(hn_t[0][:, :], src_ap_fn(i), AF.Square, [src_bufs[i]], [hn_b[0], b_ssq], accum=ssq[:, i:i + 1])
            ts("dve", rstd[:, 0:ntiles], ssq[:, 0:ntiles], inv_n, ALU.mult, [b_ssq], [b_rstd], s2=EPS, op1=ALU.add)
            act(rstd[:, 0:ntiles], rstd[:, 0:ntiles], AF.Ln, [b_rstd], [b_rstd])
            act(rstd[:, 0:ntiles], rstd[:, 0:ntiles], AF.Exp, [b_rstd], [b_rstd], scale=-0.5)

        def norm_to_fm(src_ap_fn, src_bufs, ntiles, gcol0, dstT, dst_buf, keep_rows=None):
            rstd_tiles(src_ap_fn, src_bufs, ntiles, 1.0 / D)
            for i in range(ntiles):
                if keep_rows is None:
                    hn, hb = hn_t[i % 2], hn_b[i % 2]
                    hn_ap = hn[:, :]
                else:
                    hn_ap, hb = keep_rows(i)
                act(hn_ap, src_ap_fn(i), AF.Copy, [src_bufs[i], b_rstd], [hb], scale=rstd[:, i:i + 1])
                pt, pb = psbank()
                ptb = pt[:, :].bitcast(BF16).rearrange("p (c t) -> p c t", t=128)
                for c in range(KC):
                    tr(ptb[:, c, :], hn_ap[:, c * 128:(c + 1) * 128], [hb], [pb])
                for c in range(KC):
                    eng = "dve" if c % 2 == 0 else "act"
                    if eng == "dve":
                        ts("dve", dstT[:, c, i * 128:(i + 1) * 128], ptb[:, c, :], gcols[:, gcol0 + c:gcol0 + c + 1], ALU.mult,
                           [pb, b_const], [dst_buf])
                    else:
                        act(dstT[:, c, i * 128:(i + 1) * 128], ptb[:, c, :], AF.Copy, [pb, b_const], [dst_buf],
                            scale=gcols[:, gcol0 + c:gcol0 + c + 1])

        memrows, _ = carve(BIG, 4096, 2 * D, F32)
        memrows = memrows.rearrange("p (i d) -> p i d", i=2)
        b_memrows = S.bufs(2, "memrows")
        for i in range(2):
            dma_sp("c2", memrows[:, i, :], mem_in[i * 128:(i + 1) * 128, :], writes=[b_memrows[i]])
        norm_to_fm(lambda i: memrows[:, i, :], b_memrows, 2, L * GC_PER, memT, b_memT)

        dbg_bufs = []
        steps = []

        def wload(slot, sbuf_, pieces):
            key = "w%d" % [i for i in range(NSLOT) if WS[i] is slot][0]
            for dst, src in pieces:
                dma_pool(key, dst(slot), src, writes=[sbuf_])

        def w3(slot, off, ncols):
            return slot[:, off:off + KC * ncols].rearrange("p (c n) -> p c n", c=KC)

        def dram_cols(w2d, c0, ncols):
            return w2d[:, c0:c0 + ncols].rearrange("(c p) n -> p c n", p=128)

        def layer_steps(li_, l):
            g0 = li_ * GC_PER
            win_l = w_in[li_]
            off = 0
            cosb, off = carve(BIG, off, S_LEN)
            sinb, off = carve(BIG, off, S_LEN)
            qT, off = carve(BIG, off, S_LEN)
            kT, off = carve(BIG, off, S_LEN)
            vtm, off = carve(BIG, off, NT * 128)
            vtm = vtm.rearrange("p (i e) -> p i e", i=NT)
            NE = 6
            Et = []
            for _ in range(NE):
                a, off = carve(BIG, off, 512)
                Et.append(a)
            NTMP = 6
            Tm = []
            for _ in range(NTMP):
                a, off = carve(BIG, off, 512, F32)
                Tm.append(a)
            assert off <= NBIG, off
            b_cs = S.buf("cossin")
            dbg_bufs.append(b_cs)
            b_q = S.buf("qT")
            b_k = S.buf("kT")
            b_v = S.buf("v")
            Eb = S.bufs(NE, "E")
            Tb = S.bufs(NTMP, "T")
            rrE = [0]
            rrT = [0]

            def getE():
                i = rrE[0] % NE
                rrE[0] += 1
                return Et[i], Eb[i]

            def getT():
                i = rrT[0] % (NTMP - 2)
                rrT[0] += 1
                return Tm[i], Tb[i]

            Esum = [(Tm[NTMP - 2], Tb[NTMP - 2]), (Tm[NTMP - 1], Tb[NTMP - 1])]

            yT = YR[:, :].rearrange("p (c t) -> p c t", c=KC)
            b_y = [S.buf("y%d" % c) for c in range(KC)]

            def mixer_begin(slot, sbuf_):
                S.alias([b_cs, b_q, b_k, b_v] + Eb + Tb, [b_big])
                S.alias(b_y, [YRb])
                dma_sp("cs", cosb, cossin_d[:, 0:S_LEN], writes=[b_cs])
                dma_sp("cs", sinb, cossin_d[:, S_LEN:2 * S_LEN], writes=[b_cs])
                if os.environ.get('CSTOUCH'):
                    cp("dve", stat[:, 60:61], cosb[:, 0:1], [b_cs], [b_stat])
                norm_to_fm(lambda i: X[:, i, :], Xb, NT, g0 + GC_MIX, hT, hTb)
                for r in range(NROW // 128):
                    for hh in range(2):
                        dma_sp("xbz", xb_d[r * 128:(r + 1) * 128, hh * 512:(hh + 1) * 512], ztile[:, :], reads=[b_zt], writes=[b_xb])

            steps.append((None, mixer_begin))

            def head_step(h):
                def load(slot, sbuf_):
                    wload(slot, sbuf_, [
                        (lambda s: w3(s, 0, 128), dram_cols(win_l, h * 128, 128)),
                        (lambda s: w3(s, 1024, 128), dram_cols(win_l, 1024 + h * 128, 128)),
                        (lambda s: w3(s, 2048, 128), dram_cols(win_l, 2048 + h * 128, 128)),
                    ])

                def compute(slot, sbuf_):
                    wq = w3(slot, 0, 128)
                    wk = w3(slot, 1024, 128)
                    wv = w3(slot, 2048, 128)
                    for (wt, dst, db) in ((wq, qT, b_q), (wk, kT, b_k)):
                        for tb in range(4):
                            tsl = slice(tb * 512, (tb + 1) * 512)
                            pp, pb = psbank()
                            for c in range(KC):
                                mm(pp[:, :], wt[:, c, :], hT[:, c, tsl], c == 0, c == KC - 1, [sbuf_, hTb], [pb])
                            qb, qbb = getE()
                            act(qb, pp[:, :], AF.Copy, [pb], [qbb])
                            if HEADCUT == 0:
                                continue
                            ROT = int(os.environ.get('ROT', '9'))
                            p2, p2b = psbank()
                            P2V = os.environ.get('P2V', '')
                            if P2V == 'ident':
                                mm(p2[:, :], ident, qb, True, True, [b_const, qbb], [p2b])
                            elif P2V == 'rhs':
                                mm(p2[:, :], rrot, hT[:, 0, tsl], True, True, [b_const, hTb], [p2b])
                            elif P2V == 'evac':
                                mm(p2[:, :], rrot, qb, True, True, [b_const, qbb], [p2b])
                                t9, t9b = getT()
                                cp("dve", t9, p2[:, :], [p2b], [t9b])
                            else:
                                mm(p2[:, :], rrot, qb, True, True, [b_const, qbb], [p2b])
                            if ROT < 2:
                                continue
                            t1, t1b = getT()
                            if os.environ.get('ROTV') == 'sb':
                                tt("dve", t1, qb, cosb[:, tsl], ALU.mult, [qbb, b_cs], [t1b])
                            elif os.environ.get('ROTV') == 'hT':
                                tt("dve", t1, pp[:, :], hT[:, 0, tsl], ALU.mult, [pb, hTb], [t1b])
                            elif os.environ.get('ROTV') == 'nocs':
                                tt("dve", t1, pp[:, :], qb, ALU.mult, [pb, qbb], [t1b])
                            else:
                                tt("dve", t1, pp[:, :], cosb[:, tsl], ALU.mult, [pb, b_cs], [t1b])
                            if ROT < 3:
                                continue
                            t2, t2b = getT()
                            tt("dve", t2, p2[:, :], sinb[:, tsl], ALU.mult, [p2b, b_cs], [t2b])
                            if ROT < 4:
                                continue
                            tt("pool", dst[:, tsl], t1, t2, ALU.add, [t1b, t2b], [db])
                    if HEADCUT < 2:
                        return
                    for i4 in range(4):
                        pp, pb = psbank()
                        for ii in range(4):
                            i = i4 * 4 + ii
                            for c in range(KC):
                                mm(pp[:, ii * 128:(ii + 1) * 128], hT[:, c, i * 128:(i + 1) * 128], wv[:, c, :],
                                   c == 0, c == KC - 1, [sbuf_, hTb], [pb])
                        act(vtm[:, i4 * 4:(i4 + 1) * 4, :], pp[:, :].rearrange("p (i e) -> p i e", i=4), AF.Copy, [pb], [b_v])
                    if HEADCUT < 3:
                        return
                    pending = [None]
                    for qb_ in range(4):
                        qsl = slice(qb_ * 512, (qb_ + 1) * 512)
                        pO = [(PS[4], PSB[4]), (PS[5], PSB[5])]
                        pR = [(PS[6], PSB[6]), (PS[7], PSB[7])]
                        its = [(kb, c) for kb in range(NT) for c in range(2)]
                        LA = 2
                        Es = {}
                        for i in range(len(its) + LA):
                            if i < len(its):
                                kb, c = its[i]
                                pS, pSb = psS[i % 3]
                                mm(pS[:, :], kT[c * 64:(c + 1) * 64, kb * 128:(kb + 1) * 128], qT[c * 64:(c + 1) * 64, qsl],
                                   True, True, [b_k, b_q], [pSb])
                                E, Eb_ = getE()
                                act(E, pS[:, :], AF.Exp, [pSb], [Eb_], scale=0.125)
                                Es[i] = (E, Eb_)
                            j = i - LA
                            if j >= 0:
                                kb, c = its[j]
                                E, Eb_ = Es.pop(j)
                                mm(pO[c][0][:, :], vtm[:, kb, :], E, kb == 0, kb == NT - 1, [b_v, Eb_], [pO[c][1]])
                                es_, esb_ = Esum[c]
                                eng_ = "dve" if c == 0 else "pool"
                                if kb == 0:
                                    cp(eng_, es_, E, [Eb_], [esb_])
                                else:
                                    tt(eng_, es_, es_, E, ALU.add, [esb_, Eb_], [esb_])
                            if i == 10 and pending[0] is not None:
                                pending[0]()
                                pending[0] = None
                        ri = []
                        for c in range(2):
                            e16, e16b = getE()
                            act(e16, Esum[c][0], AF.Copy, [Esum[c][1]], [e16b])
                            mm(pR[c][0][:, :], ones, e16, True, True, [b_const, e16b], [pR[c][1]])
                        for c in range(2):
                            r_, rb_ = getT()
                            act(r_, pR[c][0][:, :], AF.Ln, [pR[c][1]], [rb_])
                            act(r_, r_, AF.Exp, [rb_], [rb_], scale=-1.0)
                            ri.append((r_, rb_))
                        t0, t0b = getT()
                        tt("dve", t0, pO[0][0][:, :], ri[0][0], ALU.mult, [pO[0][1], ri[0][1]], [t0b])
                        t1, t1b = getT()
                        tt("dve", t1, pO[1][0][:, :], ri[1][0], ALU.mult, [pO[1][1], ri[1][1]], [t1b])
                        o_, ob_ = getT()
                        stt("dve", o_, t1, lamc[:, li_:li_ + 1], t0, ALU.mult, ALU.add, [t1b, t0b, b_const], [ob_])
                        sqf, sqb = getT()
                        sq = sqf.bitcast(BF16)[:, 0:512]
                        tt("pool", sq, o_, o_, ALU.mult, [ob_], [sqb])

                        def fin2(o_=o_, ob_=ob_, sq=sq, sqb=sqb, qsl=qsl):
                            pq, pqb = psS[3]
                            mm(pq[:, :], ones, sq, True, True, [b_const, sqb], [pqb])
                            rs, rsb = getT()
                            ts("dve", rs, pq[:, :], 1.0 / 128, ALU.mult, [pqb], [rsb], s2=EPS, op1=ALU.add)
                            act(rs, rs, AF.Ln, [rsb], [rsb])
                            act(rs, rs, AF.Exp, [rsb], [rsb], scale=-0.5)
                            stt("dve", yT[:, h, qsl], o_, sublnS[:, li_:li_ + 1], rs, ALU.mult, ALU.mult, [ob_, rsb, b_const], [b_y[h]])

                        pending[0] = fin2
                    if pending[0] is not None:
                        pending[0]()
                        pending[0] = None

                return load, compute

            psS = [(PS[i], PSB[i]) for i in range(4)]

            for h in range(8):
                steps.append(head_step(h))

            offc = 0
            Mreg, offc = carve(BIG, offc, KC * S_LEN)
            Mv = Mreg.rearrange("p (c t) -> p c t", c=KC)
            CT = []
            for _ in range(3):
                a, offc = carve(BIG, offc, 512, F32)
                CT.append(a)
            assert offc <= NBIG
            b_M = [S.buf("M%d" % c) for c in range(KC)]
            CTb = S.bufs(3, "CT")
            rrC = [0]

            def getC():
                i = rrC[0] % 3
                rrC[0] += 1
                return CT[i], CTb[i]

            def phaseC_begin(slot, sbuf_):
                S.alias(b_M + CTb, [b_cs, b_q, b_k, b_v] + Eb + Tb)

            steps.append((None, phaseC_begin))

            def branch_step(n, j):
                def load(slot, sbuf_):
                    wload(slot, sbuf_, [
                        (lambda s: w3(s, 0, 128), dram_cols(w_br[li_, n], j * 128, 128)),
                        (lambda s: w3(s, 1024, 128), dram_cols(win_l, 6144 + n * 1024 + j * 128, 128)),
                    ])

                def compute(slot, sbuf_):
                    wb = w3(slot, 0, 128)
                    wg = w3(slot, 1024, 128)
                    for tb in range(4):
                        tsl = slice(tb * 512, (tb + 1) * 512)
                        pg, pgb = psbank()
                        for c in range(KC):
                            mm(pg[:, :], wg[:, c, :], hT[:, c, tsl], c == 0, c == KC - 1, [sbuf_, hTb], [pgb])
                        pbr, pbrb = psbank()
                        for c in range(KC):
                            mm(pbr[:, :], wb[:, c, :], yT[:, c, tsl], c == 0, c == KC - 1, [sbuf_, b_y[c]], [pbrb])
                        sg, sgb = getC()
                        act(sg, pg[:, :], AF.Sigmoid, [pgb], [sgb])
                        if n == 0:
                            tt("dve", Mv[:, j, tsl], sg, pbr[:, :], ALU.mult, [sgb, pbrb], [b_M[j]])
                        else:
                            t_, tb_ = getC()
                            tt("dve", t_, sg, pbr[:, :], ALU.mult, [sgb, pbrb], [tb_])
                            tt("pool", Mv[:, j, tsl], Mv[:, j, tsl], t_, ALU.add, [b_M[j], tb_], [b_M[j]])

                return load, compute

            for j in range(KC):
                steps.append(branch_step(0, j))


            def conv_step(j):
                def load(slot, sbuf_):
                    wload(slot, sbuf_, [
                        (lambda s: w3(s, 0, 128), dram_cols(win_l, 3072 + j * 128, 128)),
                        (lambda s: w3(s, 1024, 128), dram_cols(win_l, 4096 + j * 128, 128)),
                        (lambda s: w3(s, 2048, 128), dram_cols(win_l, 5120 + j * 128, 128)),
                    ])

                def compute(slot, sbuf_):
                    wcb = w3(slot, 0, 128)
                    wcc = w3(slot, 1024, 128)
                    wcx = w3(slot, 2048, 128)
                    u = U_pad
                    for tb in range(4):
                        tsl = slice(tb * 512, (tb + 1) * 512)
                        pc_, pcb = psbank()
                        for c in range(KC):
                            mm(pc_[:, :], wcc[:, c, :], hT[:, c, tsl], c == 0, c == KC - 1, [sbuf_, hTb], [pcb])
                        px, pxb = psbank()
                        for c in range(KC):
                            mm(px[:, :], wcx[:, c, :], hT[:, c, tsl], c == 0, c == KC - 1, [sbuf_, hTb], [pxb])
                        t_, tb_ = getC()
                        act(t_, pc_[:, :], AF.Copy, [pcb], [tb_])
                        tt("dve", u[:, 1 + tb * 512:1 + (tb + 1) * 512], t_, px[:, :], ALU.mult, [tb_, pxb], [b_u])
                    cw = g0 + GC_CONV
                    for tb in range(4):
                        tsl = slice(tb * 512, (tb + 1) * 512)
                        acc, accb = getC()
                        act(acc, u[:, 1 + tb * 512:1 + (tb + 1) * 512], AF.Copy, [b_u, b_const], [accb], scale=gcols[:, cw + 8 + j:cw + 8 + j + 1])
                        stt("dve", acc, u[:, tb * 512:(tb + 1) * 512], gcols[:, cw + j:cw + j + 1], acc, ALU.mult, ALU.add, [b_u, accb, b_const], [accb])
                        stt("dve", acc, u[:, 2 + tb * 512:2 + (tb + 1) * 512], gcols[:, cw + 16 + j:cw + 16 + j + 1], acc, ALU.mult, ALU.add, [b_u, accb, b_const], [accb])
                        pb_, pbb = psbank()
                        for c in range(KC):
                            mm(pb_[:, :], wcb[:, c, :], hT[:, c, tsl], c == 0, c == KC - 1, [sbuf_, hTb], [pbb])
                        tt("dve", yT[:, j, tsl], pb_[:, :], acc, ALU.mult, [pbb, accb], [b_y[j]])

                return load, compute

            for j in range(KC):
                steps.append(conv_step(j))
            for j in range(KC):
                steps.append(branch_step(1, j))

            def out_proj_step(wmat, half, srcT, src_bufs):
                def load(slot, sbuf_):
                    wload(slot, sbuf_, [(lambda s: w3(s, 0, 512), dram_cols(wmat, half * 512, 512))])

                def compute(slot, sbuf_):
                    wt = w3(slot, 0, 512)
                    for i in range(NT):
                        pp, pb = psbank()
                        for c in range(KC):
                            mm(pp[:, :], srcT[:, c, i * 128:(i + 1) * 128], wt[:, c, :], c == 0, c == KC - 1, [sbuf_, src_bufs[c]], [pb])
                        tt("dve", X[:, i, half * 512:(half + 1) * 512], X[:, i, half * 512:(half + 1) * 512], pp[:, :], ALU.add,
                           [Xb[i], pb], [Xb[i]])

                return load, compute

            for half in range(2):
                steps.append(out_proj_step(w_o[li_], half, Mv, b_M))

            def mixer_end(slot, sbuf_):
                S.alias([b_big], b_M + CTb)
                S.alias([YRb], b_y)

            steps.append((None, mixer_end))
            if stop_after == (l, "mix"):
                return True

            offx = 0
            kcT, offx = carve(BIG, offx, KC * NMEM)
            kcT = kcT.rearrange("p (c m) -> p c m", c=KC)
            vc, offx = carve(BIG, offx, 2 * D)
            vc = vc.rearrange("p (i d) -> p i d", i=2)
            XE = []
            for _ in range(6):
                a, offx = carve(BIG, offx, 512)
                XE.append(a)
            XT = []
            for _ in range(4):
                a, offx = carve(BIG, offx, 512, F32)
                XT.append(a)
            assert offx <= NBIG
            b_kc = S.buf("kcT")
            b_vc = S.buf("vc")
            XEb = S.bufs(6, "XE")
            XTb = S.bufs(4, "XT")
            rrx = [0, 0]

            def getXE():
                i = rrx[0] % 6
                rrx[0] += 1
                return XE[i], XEb[i]

            def getXT():
                i = rrx[1] % 4
                rrx[1] += 1
                return XT[i], XTb[i]

            qcT = YR[:, :].rearrange("p (c t) -> p c t", c=KC)
            b_qc = [S.buf("qc%d" % c) for c in range(KC)]
            ocT = HT[:, :].rearrange("p (c t) -> p c t", c=KC)
            b_oc = [S.buf("oc%d" % c) for c in range(KC)]

            def cross_begin(slot, sbuf_):
                S.alias([b_kc, b_vc] + XEb + XTb, [b_big])
                S.alias(b_qc, [YRb])
                norm_to_fm(lambda i: X[:, i, :], Xb, NT, g0 + GC_CROSS, hT, hTb)

            steps.append((None, cross_begin))

            def ckv_step(part):
                def load(slot, sbuf_):
                    wload(slot, sbuf_, [(lambda s: w3(s, 0, 512), dram_cols(w_ckv[li_], part * 512, 512))])

                def compute(slot, sbuf_):
                    wt = w3(slot, 0, 512)
                    if part < 2:
                        for jj in range(4):
                            j = part * 4 + jj
                            pp, pb = psbank()
                            for c in range(KC):
                                mm(pp[:, 0:NMEM], wt[:, c, jj * 128:(jj + 1) * 128], memT[:, c, :], c == 0, c == KC - 1, [sbuf_, b_memT], [pb])
                            act(kcT[:, j, :], pp[:, 0:NMEM], AF.Copy, [pb], [b_kc])
                    else:
                        for i in range(2):
                            pp, pb = psbank()
                            for c in range(KC):
                                mm(pp[:, :], memT[:, c, i * 128:(i + 1) * 128], wt[:, c, :], c == 0, c == KC - 1, [sbuf_, b_memT], [pb])
                            act(vc[:, i, (part - 2) * 512:(part - 1) * 512], pp[:, :], AF.Copy, [pb], [b_vc])

                return load, compute

            for part in range(4):
                steps.append(ckv_step(part))

            def cq_step(half):
                def load(slot, sbuf_):
                    wload(slot, sbuf_, [(lambda s: w3(s, 0, 512), dram_cols(w_cq[li_], half * 512, 512))])

                def compute(slot, sbuf_):
                    wt = w3(slot, 0, 512)
                    for jj in range(4):
                        j = half * 4 + jj
                        for tb in range(4):
                            tsl = slice(tb * 512, (tb + 1) * 512)
                            pp, pb = psbank()
                            for c in range(KC):
                                mm(pp[:, :], wt[:, c, jj * 128:(jj + 1) * 128], hT[:, c, tsl], c == 0, c == KC - 1, [sbuf_, hTb], [pb])
                            if (jj + tb) % 2 == 0:
                                act(qcT[:, j, tsl], pp[:, :], AF.Copy, [pb], [b_qc[j]])
                            else:
                                cp("dve", qcT[:, j, tsl], pp[:, :], [pb], [b_qc[j]])

                return load, compute

            for half in range(2):
                steps.append(cq_step(half))

            def cross_attn(slot, sbuf_):
                S.alias(b_oc, [hTb])
                for h in range(4):
                    for tb in range(4):
                        tsl = slice(tb * 512, (tb + 1) * 512)
                        pO = [psbank(), psbank()]
                        pR = psbank()
                        for mb in range(2):
                            pS, pSb = psbank()
                            for cc in range(2):
                                mm(pS[:, :], kcT[:, 2 * h + cc, mb * 128:(mb + 1) * 128], qcT[:, 2 * h + cc, tsl], cc == 0, cc == 1,
                                   [b_kc, b_qc[2 * h + cc]], [pSb])
                            E, Eb_ = getXE()
                            act(E, pS[:, :], AF.Exp, [pSb], [Eb_], scale=1.0 / 16)
                            for ee in range(2):
                                mm(pO[ee][0][:, :], vc[:, mb, h * 256 + ee * 128:h * 256 + (ee + 1) * 128], E, mb == 0, mb == 1,
                                   [b_vc, Eb_], [pO[ee][1]])
                            mm(pR[0][:, :], ones, E, mb == 0, mb == 1, [b_const, Eb_], [pR[1]])
                        r_, rb_ = getXT()
                        S.op("dve", lambda e, r_=r_, pR=pR: e.reciprocal(out=r_, in_=pR[0][:, :]), reads=[pR[1]], writes=[rb_])
                        for ee in range(2):
                            tt("dve", ocT[:, 2 * h + ee, tsl], pO[ee][0][:, :], r_, ALU.mult, [pO[ee][1], rb_], [b_oc[2 * h + ee]])

            steps.append((None, cross_attn))
            for half in range(2):
                steps.append(out_proj_step(w_co[li_], half, ocT, b_oc))

            def cross_end(slot, sbuf_):
                S.alias([b_big], [b_kc, b_vc] + XEb + XTb)
                S.alias([YRb], b_qc)
                S.alias([hTb], b_oc)

            steps.append((None, cross_end))
            if stop_after == (l, "cross"):
                return True

            hrows = YR[:, :].rearrange("p (i d) -> p i d", i=NT)
            b_hr = S.bufs(NT, "hrows")
            offm = 0
            oh12, offm = carve(BIG, offm, NT * 64, F32)
            oh12 = oh12.rearrange("p (i e) -> p i e", i=NT)
            mohb, offm = carve(BIG, offm, NT * 32)
            mohb = mohb.rearrange("p (i e) -> p i e", i=NT)
            wts, offm = carve(BIG, offm, NT * 2, F32)
            wts = wts.rearrange("p (i k) -> p i k", i=NT)
            desti, offm = carve(BIG, offm, NT * 2, I32)
            desti = desti.rearrange("p (i k) -> p i k", i=NT)
            rt, offm = carve(BIG, offm, 320, F32)
            cnt, offm = carve(BIG, offm, 32, F32)
            padA, offm = carve(BIG, offm, 32, F32)
            padB, offm = carve(BIG, offm, 32, F32)
            pstart, offm = carve(BIG, offm, 32, F32)
            pend, offm = carve(BIG, offm, 32, F32)
            eblk, offm = carve(BIG, offm, NBLK, F32)
            idxgu, offm = carve(BIG, offm, NBLK * 8, I32)
            idxd, offm = carve(BIG, offm, NBLK * 8, I32)
            idxgu = idxgu.rearrange("p (b c) -> p b c", b=NBLK)
            idxd = idxd.rearrange("p (b c) -> p b c", b=NBLK)
            idxgu_f, offt = carve(BIG, offm, NBLK * 8, F32)
            idxd_f, offt = carve(BIG, offt, NBLK * 8, F32)
            idxgu_f = idxgu_f.rearrange("p (b c) -> p b c", b=NBLK)
            idxd_f = idxd_f.rearrange("p (b c) -> p b c", b=NBLK)
            rtall, offt = carve(BIG, offt, NT * 208, F32)
            rtall = rtall.rearrange("p (i n) -> p i n", i=NT)
            assert offt <= NBIG, offt
            offm0 = offm
            b_rt = S.buf("rt")
            b_rt_i = S.bufs(NT, "rti")
            b_oh_i = S.bufs(NT, "ohi")
            b_dest_i = S.bufs(NT, "desti")
            b_oh = S.buf("oh")
            b_route = S.buf("route")

            def moe_begin(slot, sbuf_):
                S.alias([b_rt, b_oh, b_route] + b_rt_i + b_oh_i + b_dest_i, [b_big])
                S.alias(b_hr, [YRb])

            steps.append((None, moe_begin))

            def router_step():
                def load(slot, sbuf_):
                    wload(slot, sbuf_, [(lambda s: s[:, 0:KC * 36].rearrange("p (c n) -> p c n", c=KC),
                                         w_rt[li_].rearrange("(c p) n -> p c n", p=128))])

                def compute(slot, sbuf_):
                    wr = slot[:, 0:KC * 36].rearrange("p (c n) -> p c n", c=KC)
                    norm_to_fm(lambda i: X[:, i, :], Xb, NT, g0 + GC_FFN, hT, hTb,
                               keep_rows=lambda i: (hrows[:, i, :], b_hr[i]))
                    def rt_tile(i):
                        rt = rtall[:, i, :]
                        yield
                        lg = rt[:, 0:36]
                        yield
                        pp, pb = psbank()
                        yield
                        for c in range(KC):
                            mm(pp[:, 0:36], hT[:, c, i * 128:(i + 1) * 128], wr[:, c, :], c == 0, c == KC - 1, [sbuf_, hTb], [pb])
                        yield
                        R_ = [b_rt_i[i]]
                        yield
                        act(lg, pp[:, 0:36], AF.Copy, [pb], R_)
                        yield
                        mxg = rt[:, 40:41]
                        yield
                        S.op("dve", lambda e, mxg=mxg: e.tensor_reduce(out=mxg, in_=rt[:, 0:4], axis=AX.X, op=ALU.max), reads=R_, writes=R_)
                        yield
                        ohg = rt[:, 44:48]
                        yield
                        ts("dve", ohg, rt[:, 0:4], mxg, ALU.is_equal, R_, R_)
                        yield
                        nmx = rt[:, 41:42]
                        yield
                        ts("dve", nmx, mxg, -1.0, ALU.mult, R_, R_)
                        yield
                        sumg = rt[:, 42:43]
                        yield
                        act(rt[:, 48:52], rt[:, 0:4], AF.Exp, R_, R_, bias=nmx, accum=sumg)
                        yield
                        gw = rt[:, 43:44]
                        yield
                        S.op("dve", lambda e, gw=gw, sumg=sumg: e.reciprocal(out=gw, in_=sumg), reads=R_, writes=R_)
                        yield
                        pen = rt[:, 52:56]
                        yield
                        ts("dve", pen, ohg, -1.0, ALU.add, R_, R_, s2=-NEG, op1=ALU.mult)
                        yield
                        lem = rt[:, 64:96]
                        yield
                        for g in range(4):
                            ts("dve", lem[:, g * 8:(g + 1) * 8], rt[:, 4 + g * 8:4 + (g + 1) * 8], pen[:, g:g + 1], ALU.add, R_, R_)
                        yield
                        m1 = rt[:, 56:57]
                        yield
                        S.op("dve", lambda e, m1=m1, lem=lem: e.tensor_reduce(out=m1, in_=lem, axis=AX.X, op=ALU.max), reads=R_, writes=R_)
                        yield
                        ts("dve", oh12[:, i, 0:32], lem, m1, ALU.is_equal, R_, [b_oh_i[i]])
                        yield
                        lem2 = rt[:, 96:128]
                        yield
                        stt("dve", lem2, oh12[:, i, 0:32], NEG, lem, ALU.mult, ALU.add, [b_oh_i[i], b_rt_i[i]], R_)
                        yield
                        m2 = rt[:, 57:58]
                        yield
                        S.op("dve", lambda e, m2=m2, lem2=lem2: e.tensor_reduce(out=m2, in_=lem2, axis=AX.X, op=ALU.max), reads=R_, writes=R_)
                        yield
                        ts("dve", oh12[:, i, 32:64], lem2, m2, ALU.is_equal, R_, [b_oh_i[i]])
                        yield
                        dd = rt[:, 58:59]
                        yield
                        tt("dve", dd, m2, m1, ALU.subtract, R_, R_)
                        yield
                        ed = rt[:, 59:60]
                        yield
                        act(ed, dd, AF.Exp, R_, R_)
                        yield
                        den = rt[:, 60:61]
                        yield
                        ts("dve", den, ed, 1.0, ALU.add, R_, R_)
                        yield
                        w1 = rt[:, 61:62]
                        yield
                        S.op("dve", lambda e, w1=w1, den=den: e.reciprocal(out=w1, in_=den), reads=R_, writes=R_)
                        yield
                        tt("dve", wts[:, i, 0:1], w1, gw, ALU.mult, R_, [b_oh_i[i]])
                        yield
                        w2 = rt[:, 62:63]
                        yield
                        tt("dve", w2, ed, w1, ALU.mult, R_, R_)
                        yield
                        tt("dve", wts[:, i, 1:2], w2, gw, ALU.mult, R_, [b_oh_i[i]])
                        yield
                        tt("dve", mohb[:, i, :], oh12[:, i, 0:32], oh12[:, i, 32:64], ALU.add, [b_oh_i[i]], [b_oh_i[i]])
                        yield


                    def run_interleaved(gens):
                        gens = list(gens)
                        while gens:
                            for g_ in list(gens):
                                try:
                                    next(g_)
                                except StopIteration:
                                    gens.remove(g_)

                    run_interleaved([rt_tile(i) for i in range(0, 8)])
                    run_interleaved([rt_tile(i) for i in range(8, NT)])
                    pc_, pcb = psbank()
                    for i in range(NT):
                        mm(pc_[:, 0:32], ones, mohb[:, i, :], i == 0, i == NT - 1, [b_const, b_oh_i[i]], [pcb])
                    Q_ = [b_route]
                    cp("dve", cnt, pc_[:, 0:32], [pcb], Q_)
                    ts("dve", padA, cnt, 1.0 / RB, ALU.mult, Q_, Q_, s2=(RB - 1.0) / RB - 0.5 + 0.5 / RB, op1=ALU.add)
                    cp("dve", padB.bitcast(I32), padA, Q_, Q_)
                    cp("dve", padA, padB.bitcast(I32), Q_, Q_)
                    ts("dve", padA, padA, float(RB), ALU.mult, Q_, Q_)
                    cp("dve", pstart, padA, Q_, Q_)
                    a, b_ = padA, padB
                    for s_ in (1, 2, 4, 8, 16):
                        cp("dve", b_[:, 0:s_], a[:, 0:s_], Q_, Q_)
                        tt("dve", b_[:, s_:32], a[:, s_:32], a[:, 0:32 - s_], ALU.add, Q_, Q_)
                        a, b_ = b_, a
                    cp("dve", pend, a, Q_, Q_)
                    tt("dve", pstart, pend, pstart, ALU.subtract, Q_, Q_)
                    def dest_tile(i):
                        rt = rtall[:, i, :]
                        yield
                        pp, pb = psbank()
                        yield
                        mm(pp[:, 0:32], ustr, mohb[:, i, :], True, i == 0, [b_const, b_oh_i[i]], [pb])
                        yield
                        for j in range(i):
                            mm(pp[:, 0:32], ones, mohb[:, j, :], False, j == i - 1, [b_const, b_oh_i[j]], [pb])
                        yield
                        tmp = rt[:, 128:160]
                        yield
                        tt("dve", tmp, pp[:, 0:32], pstart, ALU.add, [pb, b_route], [b_rt_i[i]])
                        yield
                        for k in range(2):
                            prod = rt[:, 160:192]
                            df = rt[:, 192 + k:193 + k]
                            tt("dve", prod, tmp, oh12[:, i, k * 32:(k + 1) * 32], ALU.mult, [b_rt_i[i], b_oh_i[i]], [b_rt_i[i]])
                            S.op("dve", lambda e, df=df, prod=prod: e.tensor_reduce(out=df, in_=prod, axis=AX.X, op=ALU.add), reads=[b_rt_i[i]], writes=[b_rt_i[i]])
                            cp("dve", desti[:, i, k:k + 1], df, [b_rt_i[i]], [b_dest_i[i]])
                        yield


                    run_interleaved([dest_tile(i) for i in range(0, 8)])
                    run_interleaved([dest_tile(i) for i in range(8, NT)])
                    S.op("dve", lambda e: e.memset(eblk, 0.0), writes=Q_)
                    for e_ in range(NEXP):
                        stt("dve", eblk, thr[:, 0:NBLK], pend[:, e_:e_ + 1], eblk, ALU.is_ge, ALU.add, [b_const] + Q_, Q_)
                    ts("dve", eblk, eblk, float(NEXP - 1), ALU.min, Q_, Q_, s2=float(li_ * NEXP), op1=ALU.add)
                    for c in range(8):
                        ts("dve", idxgu_f[:, :, c], eblk, float(D), ALU.mult, Q_, Q_, s2=pcv[:, c:c + 1], op1=ALU.add)
                    for c in range(6):
                        ts("dve", idxd_f[:, :, c], eblk, float(DEXP), ALU.mult, Q_, Q_, s2=pcv[:, c:c + 1], op1=ALU.add)
                    cp("dve", idxgu[:, :, :], idxgu_f[:, :, :], Q_, Q_)
                    cp("dve", idxd[:, :, 0:6], idxd_f[:, :, 0:6], Q_, Q_)
                    for i in range(NT):
                        for k in range(2):
                            S.dma("pool", "scat", lambda e, i=i, k=k: e.indirect_dma_start(
                                out=xb_d[:, :], out_offset=bass.IndirectOffsetOnAxis(ap=desti[:, i, k:k + 1], axis=0),
                                in_=hrows[:, i, :], in_offset=None), reads=[b_hr[i], b_dest_i[i]], writes=[b_xb])

                return load, compute

            steps.append(router_step())

            Gw = [HT[:, g * 6144:(g + 1) * 6144].rearrange("p (c n) -> p c n", c=8) for g in range(2)]
            xg_t = [HT[:, 12288 + i * 1024:12288 + (i + 1) * 1024] for i in range(2)]
            xgT_t = [HT[:, 14336 + i * 1024:14336 + (i + 1) * 1024].rearrange("p (c t) -> p c t", c=8) for i in range(2)]
            Uw = [YR[:, g * 6144:(g + 1) * 6144].rearrange("p (c n) -> p c n", c=8) for g in range(2)]
            ybt = [YR[:, 12288 + i * 2048:12288 + (i + 1) * 2048].bitcast(F32) for i in range(2)]
            offd = offm0 + (offm0 % 2)
            Dw = []
            for g in range(2):
                a, offd = carve(BIG, offd, 6 * D)
                Dw.append(a.rearrange("p (c n) -> p c n", c=6))
            sgt = U_pad[:, 2:2 + 2 * DEXP].bitcast(F32)
            hmt = hn_t[0][:, 0:DEXP]
            hmT = [hn_t[1][:, 0:DEXP].rearrange("p (c t) -> p c t", c=6)]
            a, offd = carve(BIG, offd, DEXP)
            hmT.append(a.rearrange("p (c t) -> p c t", c=6))
            assert offd <= NBIG, offd
            b_G = S.bufs(2, "Gw")
            b_U = S.bufs(2, "Uw")
            b_D = S.bufs(2, "Dw")
            b_xg = S.bufs(2, "xg")
            b_xgT = S.bufs(2, "xgT")
            b_ybt = S.bufs(2, "ybt")
            b_sg = S.buf("sg")
            b_hm = S.buf("hm")
            b_hmT = S.bufs(2, "hmT")
            weg = w_eg.rearrange("l e d n -> (l e d) n")
            weu = w_eu.rearrange("l e d n -> (l e d) n")
            wed = w_ed.rearrange("l e d n -> (l e d) n")

            def moe_blocks(slot, sbuf_):
                S.alias(b_G + b_xg + b_xgT, [hTb])
                S.alias(b_U + b_ybt, b_hr)
                S.alias(b_D + [b_hmT[1]], [b_big, b_route] + b_rt_i)
                S.alias([b_sg], [b_u])
                S.alias([b_hm, b_hmT[0]], hn_b)

                def issue_w(b):
                    p = b % 2
                    for c in range(8):
                        S.dma("pool", "G%d" % p, lambda e, c=c, p=p, b=b: e.indirect_dma_start(
                            out=Gw[p][:, c, :], out_offset=None, in_=weg,
                            in_offset=bass.IndirectOffsetOnAxis(ap=idxgu[:, b, c:c + 1], axis=0)), reads=[b_route], writes=[b_G[p]])
                    for c in range(8):
                        S.dma("pool", "U%d" % p, lambda e, c=c, p=p, b=b: e.indirect_dma_start(
                            out=Uw[p][:, c, :], out_offset=None, in_=weu,
                            in_offset=bass.IndirectOffsetOnAxis(ap=idxgu[:, b, c:c + 1], axis=0)), reads=[b_route], writes=[b_U[p]])
                    for c in range(6):
                        S.dma("pool", "D%d" % p, lambda e, c=c, p=p, b=b: e.indirect_dma_start(
                            out=Dw[p][:, c, :], out_offset=None, in_=wed,
                            in_offset=bass.IndirectOffsetOnAxis(ap=idxd[:, b, c:c + 1], axis=0)), reads=[b_route], writes=[b_D[p]])

                def issue_x(t):
                    q = t % 2
                    dma_sp("xg%d" % q, xg_t[q], xb_d[t * 128:(t + 1) * 128, :], reads=[b_xb], writes=[b_xg[q]])

                NTIL = NROW // 128
                SUB = RB // 128
                issue_w(0)
                issue_x(0)
                for t in range(NTIL):
                    b = t // SUB
                    p = b % 2
                    q = t % 2
                    if t % SUB == 0 and b + 1 < NBLK:
                        issue_w(b + 1)
                    if t + 1 < NTIL:
                        issue_x(t + 1)
                    pt, pb = psbank()
                    ptb = pt[:, :].bitcast(BF16).rearrange("p (c t) -> p c t", t=128)
                    for c in range(KC):
                        tr(ptb[:, c, :], xg_t[q][:, c * 128:(c + 1) * 128], [b_xg[q]], [pb])
                    for c in range(KC):
                        gc = gcols[:, g0 + GC_FFN + c:g0 + GC_FFN + c + 1]
                        if c % 2 == 0:
                            ts("dve", xgT_t[q][:, c, :], ptb[:, c, :], gc, ALU.mult, [pb, b_const], [b_xgT[q]])
                        else:
                            act(xgT_t[q][:, c, :], ptb[:, c, :], AF.Copy, [pb, b_const], [b_xgT[q]], scale=gc)
                    for (n0, nn) in ((0, 512), (512, 256)):
                        pg, pgb = psbank()
                        for c in range(KC):
                            mm(pg[:, 0:nn], xgT_t[q][:, c, :], Gw[p][:, c, n0:n0 + nn], c == 0, c == KC - 1, [b_xgT[q], b_G[p]], [pgb])
                        pu, pub = psbank()
                        for c in range(KC):
                            mm(pu[:, 0:nn], xgT_t[q][:, c, :], Uw[p][:, c, n0:n0 + nn], c == 0, c == KC - 1, [b_xgT[q], b_U[p]], [pub])
                        act(sgt[:, n0:n0 + nn], pg[:, 0:nn], AF.Silu, [pgb], [b_sg])
                        tt("dve", hmt[:, n0:n0 + nn], sgt[:, n0:n0 + nn], pu[:, 0:nn], ALU.mult, [b_sg, pub], [b_hm])
                    pt2, pb2 = psbank()
                    pt2b = pt2[:, :].bitcast(BF16).rearrange("p (c t) -> p c t", t=128)
                    for c in range(6):
                        tr(pt2b[:, c, :], hmt[:, c * 128:(c + 1) * 128], [b_hm], [pb2])
                    act(hmT[q][:, 0:3, :], pt2b[:, 0:3, :], AF.Copy, [pb2], [b_hmT[q]])
                    cp("dve", hmT[q][:, 3:6, :], pt2b[:, 3:6, :], [pb2], [b_hmT[q]])
                    for n in range(2):
                        py, pyb = psbank()
                        for c in range(6):
                            mm(py[:, :], hmT[q][:, c, :], Dw[p][:, c, n * 512:(n + 1) * 512], c == 0, c == 5, [b_hmT[q], b_D[p]], [pyb])
                        if n == 0:
                            act(ybt[q][:, 0:512], py[:, :], AF.Copy, [pyb], [b_ybt[q]])
                        else:
                            cp("dve", ybt[q][:, 512:1024], py[:, :], [pyb], [b_ybt[q]])
                    dma_sp("ybst", yb_d[t * 128:(t + 1) * 128, :], ybt[q], reads=[b_ybt[q]], writes=[b_yb])
                S.alias(b_gat, b_G + b_xg + b_xgT)
                for i in range(NT):
                    for k in range(2):
                        gi = (i * 2 + k) % 4
                        S.dma("pool", "gat%d" % gi, lambda e, i=i, k=k, gi=gi: e.indirect_dma_start(
                            out=gat_t[gi], out_offset=None, in_=yb_d[:, :],
                            in_offset=bass.IndirectOffsetOnAxis(ap=desti[:, i, k:k + 1], axis=0)), reads=[b_yb, b_dest_i[i]], writes=[b_gat[gi]])
                        stt("dve", X[:, i, :], gat_t[gi], wts[:, i, k:k + 1], X[:, i, :], ALU.mult, ALU.add, [b_gat[gi], b_oh_i[i], Xb[i]], [Xb[i]])

            gat_t = [HT[:, i * 2048:(i + 1) * 2048].bitcast(F32) for i in range(4)]
            b_gat = S.bufs(4, "gat")
            steps.append((None, moe_blocks))

            def moe_end(slot, sbuf_):
                S.alias([b_big], [b_rt, b_oh, b_route] + b_rt_i + b_oh_i + b_dest_i + b_D + b_hmT)
                S.alias([b_u], [b_sg])
                S.alias(hn_b, [b_hm, b_hmT[0]])
                S.alias([YRb], b_U + b_ybt + b_hr)
                S.alias([hTb], b_gat + b_G + b_xg + b_xgT)

            steps.append((None, moe_end))
            if stop_after == (l, "moe"):
                return True
            return False

        ztile = sb("ztile", [128, 512], BF16)
        b_zt = S.buf("zt")
        S.op("pool", lambda e: e.memset(ztile[:, :], 0.0), writes=[b_zt])
        U_pad = sb("U_pad", [128, S_LEN + 4], BF16)
        b_u = S.buf("u")
        b_xb = S.buf("xb_d")
        b_yb = S.buf("yb_d")
        S.op("pool", lambda e: e.memset(U_pad[:, :], 0.0), writes=[b_u])

        stopped = False
        for li_, l in enumerate(layer_ids):
            stopped = layer_steps(li_, l)
            if stopped:
                break

        def epilogue(slot, sbuf_):
            if final and not stopped:
                gfin, _ = carve(BIG, 0, D, F32)
                otile = [carve(BIG, 2 * D + i * 2 * D, D, F32)[0] for i in range(2)]
                b_gf = S.buf("gfin")
                b_ot = S.bufs(2, "otile")
                S.alias([b_gf] + b_ot, [b_big])
                dma_sp("c2", gfin, gfin_d[:, :], writes=[b_gf])
                rstd_tiles(lambda i: X[:, i, :], Xb, NT, 1.0 / D)
                for i in range(NT):
                    p = i % 2
                    act(otile[p], X[:, i, :], AF.Copy, [Xb[i], b_rstd], [b_ot[p]], scale=rstd[:, i:i + 1])
                    tt("dve", otile[p], otile[p], gfin, ALU.mult, [b_ot[p], b_gf], [b_ot[p]])
                    dma_sp("out", out_d[i * 128:(i + 1) * 128, :], otile[p], reads=[b_ot[p]], writes=[b_out])
            else:
                for i in range(NT):
                    dma_sp("out", out_d[i * 128:(i + 1) * 128, :], X[:, i, :], reads=[Xb[i]], writes=[b_out])

        b_out = S.buf("out")
        if max_steps is not None:
            del steps[max_steps:]
        steps.append((None, epilogue))

        wsteps = [k for k, (ld, _) in enumerate(steps) if ld is not None]
        issued = 0
        done = 0
        for k, (ld, cpf) in enumerate(steps):
            while issued < len(wsteps) and issued < done + NSLOT:
                sl = issued % NSLOT
                steps[wsteps[issued]][0](WS[sl], WSb[sl])
                issued += 1
            if ld is not None:
                sl = done % NSLOT
                cpf(WS[sl], WSb[sl])
                done += 1
            else:
                cpf(None, None)
        S.wait_all("sp", [b_out])
        if os.environ.get('WAITCS'):
            for b_ in dbg_bufs:
                S.wait_all("sp", [b_])
        S.finalize()
        dl = S.simulate()
        if dl is not None:
            raise RuntimeError("sync deadlock: %r" % (dl,))
        S.emit()
    return nc, S.ninst


def _consts():
    ident = np.eye(128, dtype=np.float32)
    ones = np.ones((128, 128), np.float32)
    rrot = np.zeros((128, 128), np.float32)
    for c in range(2):
        for i in range(8):
            rrot[c * 64 + 8 + i, c * 64 + i] = -1.0
            rrot[c * 64 + i, c * 64 + 8 + i] = 1.0
    ustr = np.triu(np.ones((128, 128), np.float32), 1)
    cmat = np.concatenate([ident, ones, rrot, ustr], axis=1)
    inv = np.float32(500000.0) ** (-np.arange(0, 16, 2, dtype=np.float32) / np.float32(16))
    ang = np.arange(S_LEN, dtype=np.float32)[:, None] * inv[None, :]
    cs, sn = np.cos(ang).astype(np.float32), np.sin(ang).astype(np.float32)
    cosT = np.ones((128, S_LEN), np.float32)
    sinT = np.zeros((128, S_LEN), np.float32)
    for c in range(2):
        for i in range(16):
            cosT[c * 64 + i] = cs[:, i % 8]
            sinT[c * 64 + i] = sn[:, i % 8]
    cossin = np.concatenate([cosT, sinT], axis=1)
    thr = np.broadcast_to((np.arange(64, dtype=np.float32) * float(RB))[None, :], (128, 64))
    pc = np.arange(8, dtype=np.float32)[None, :] * 128.0 + np.arange(128, dtype=np.float32)[:, None]
    cmisc = np.ascontiguousarray(np.concatenate([thr, pc], axis=1))
    return np.ascontiguousarray(cmat.astype(ml_dtypes.bfloat16)), np.ascontiguousarray(cossin.astype(ml_dtypes.bfloat16)), cmisc


def _pack_small(inp, layer_ids):
    cols = []
    for l in layer_ids:
        def colmaj(v):
            return np.asarray(v, np.float32).reshape(8, 128).T
        cols.append(colmaj(inp["norm_mix"][l]))
        cols.append(colmaj(inp["norm_cross"][l]))
        cols.append(colmaj(inp["norm_ffn"][l]))
        for k in range(3):
            cols.append(colmaj(inp["conv_w"][l][k]))
        cols.append(np.asarray(inp["subln"][l], np.float32).reshape(128, 1))
    cols.append(np.asarray(inp["norm_mem"], np.float32).reshape(8, 128).T)
    gcols = np.ascontiguousarray(np.concatenate(cols, axis=1))
    lam = []
    for l in layer_ids:
        lam.append(np.concatenate([inp["lambda_q1"][l], inp["lambda_k1"][l], inp["lambda_q2"][l], inp["lambda_k2"][l]]))
    lamv = np.ascontiguousarray(np.broadcast_to(np.concatenate(lam)[None, :].astype(np.float32), (128, len(layer_ids) * 256)))
    gfin = np.ascontiguousarray(np.broadcast_to(np.asarray(inp["norm_final"], np.float32)[None, :], (128, D)))
    return gcols, lamv, gfin


_PROG_CACHE = {}


def _run(inp, x_shards, layer_ids, final, stop_after=None, cores=None, max_steps=None):
    key = (tuple(layer_ids), final, stop_after, max_steps)
    if key not in _PROG_CACHE:
        _PROG_CACHE[key] = build(list(layer_ids), final, stop_after, max_steps)[0]
    nc = _PROG_CACHE[key]
    cmat, cossin, cmisc = _consts()
    gcols, lamv, gfin = _pack_small(inp, layer_ids)
    ls = list(layer_ids)
    sl = slice(ls[0], ls[-1] + 1)
    w_rt = np.ascontiguousarray(np.concatenate([inp["w_router_group"][sl], inp["w_router_expert"][sl]], axis=-1))
    shared = {
        "w_in": np.ascontiguousarray(inp["w_in"][sl]), "w_branch": np.ascontiguousarray(inp["w_branch"][sl]),
        "w_o": np.ascontiguousarray(inp["w_o"][sl]), "w_cq": np.ascontiguousarray(inp["w_cq"][sl]),
        "w_ckv": np.ascontiguousarray(inp["w_ckv"][sl]), "w_co": np.ascontiguousarray(inp["w_co"][sl]),
        "w_rt": w_rt, "w_exp_gate": np.ascontiguousarray(inp["w_exp_gate"][sl]),
        "w_exp_up": np.ascontiguousarray(inp["w_exp_up"][sl]), "w_exp_down": np.ascontiguousarray(inp["w_exp_down"][sl]),
        "gcols": gcols, "lamv": lamv, "gfinal": gfin, "cmat": cmat, "cossin": cossin, "cmisc": cmisc,
    }
    cores = list(range(len(x_shards))) if cores is None else cores
    in_maps = []
    for b in range(len(x_shards)):
        m = dict(shared)
        m["x"] = np.ascontiguousarray(x_shards[b])
        m["mem"] = np.ascontiguousarray(inp["mem"][b])
        in_maps.append(m)
    res = run_bass_kernel_spmd(nc, in_maps, core_ids=cores)
    return [r["out"] for r in res.results]


MODE = "fused"


def kernel(**inputs):
    inp = {k: np.asarray(v) for k, v in inputs.items()}
    xs = [inp["x"][b] for b in range(inp["x"].shape[0])]
    if MODE == "fused":
        outs = _run(inp, xs, list(range(DEPTH)), True)
    else:
        for l in range(DEPTH):
            xs = _run(inp, xs, [l], l == DEPTH - 1)
        outs = xs
    return np.stack(outs, axis=0).astype(np.float32)
```

```python
import math
import os
import numpy as np
import ml_dtypes
HEADCUT = int(os.environ.get('HEADCUT', '99'))
from contextlib import ExitStack
import concourse.bass as bass
import concourse.mybir as mybir
from concourse.bass_utils import run_bass_kernel_spmd

F32 = mybir.dt.float32
BF16 = mybir.dt.bfloat16
I32 = mybir.dt.int32
ALU = mybir.AluOpType
AF = mybir.ActivationFunctionType
AX = mybir.AxisListType

D = 1024
S_LEN = 2048
NT = 16
KC = 8
NMEM = 256
DEPTH = 4
NEXP = 32
DEXP = 768
NBLK = 48
RB = 256
NROW = NBLK * RB
EPS = 1e-6
NEG = -1.0e30


class Buf:
    __slots__ = ("name", "w", "r", "excl")

    def __init__(self, name):
        self.name = name
        self.w = None
        self.r = {}
        self.excl = False


class Sched:
    ENG = ("pe", "act", "dve", "pool", "sp")

    def __init__(self, nc, stack):
        self.nc = nc
        self.stack = stack
        self.rec = {e: [] for e in self.ENG}
        self.sem = {e: stack.enter_context(nc.semaphore("s_" + e)) for e in self.ENG}
        self.cnt = {e: 0 for e in self.ENG}
        self.seen = {e: {} for e in self.ENG}
        self.dsem = {}
        self.dcnt = {}
        self.nbuf = 0
        self.ninst = 0

    def buf(self, name=None):
        self.nbuf += 1
        return Buf(name or "b%d" % self.nbuf)

    def bufs(self, n, name="b"):
        return [self.buf("%s%d" % (name, i)) for i in range(n)]

    def _deps(self, eng, reads, writes, skipkey=None):
        deps = {}

        def add(k, v):
            if k == skipkey:
                return
            if deps.get(k, 0) < v:
                deps[k] = v

        for b in reads:
            if b.w is not None:
                add(*b.w)
            if b.excl:
                for k, v in b.r.items():
                    if k != ("e", eng):
                        add(k, v)
        for b in writes:
            if b.w is not None:
                add(*b.w)
            for k, v in b.r.items():
                add(k, v)
        out = []
        seen = self.seen[eng]
        for k, v in deps.items():
            if eng == "pe" and k == ("e", "pe"):
                continue
            if k[0] == "d":
                v = self.dcnt[k[1]]
            if seen.get(k, 0) >= v:
                continue
            seen[k] = v
            out.append((k, v))
        return out

    def _post(self, ev, reads, writes):
        k, v = ev
        for b in reads:
            if b.r.get(k, 0) < v:
                b.r[k] = v
        for b in writes:
            b.w = ev
            b.r = {}

    def op(self, eng, fn, reads=(), writes=()):
        waits = self._deps(eng, reads, writes)
        self.cnt[eng] += 1
        ev = (("e", eng), self.cnt[eng])
        self.rec[eng].append(("op", waits, fn, self.cnt[eng]))
        self._post(ev, reads, writes)
        self.ninst += 1
        return ev

    def dma(self, eng, key, fn, reads=(), writes=()):
        if key not in self.dsem:
            self.dsem[key] = self.stack.enter_context(self.nc.semaphore("d_" + str(key)))
            self.dcnt[key] = 0
        waits = self._deps(eng, reads, writes, skipkey=("d", key))
        self.dcnt[key] += 16
        ev = (("d", key), self.dcnt[key])
        self.rec[eng].append(("dma", waits, fn, key))
        self._post(ev, reads, writes)
        self.ninst += 1
        return ev

    def alias(self, new_bufs, old_bufs):
        for nb in new_bufs:
            for ob in old_bufs:
                if ob.w is not None:
                    k, v = ob.w
                    if nb.r.get(k, 0) < v:
                        nb.r[k] = v
                for k, v in ob.r.items():
                    if nb.r.get(k, 0) < v:
                        nb.r[k] = v

    def wait_all(self, eng, bufs):
        waits = self._deps(eng, bufs, bufs)
        self.rec[eng].append(("wait", waits, None, None))

    def finalize(self):
        waited = {e: set() for e in self.ENG}
        for eng in self.ENG:
            for kind, waits, fn, x in self.rec[eng]:
                for k, v in waits:
                    if k[0] == "e":
                        waited[k[1]].add(v)
        rank = {e: {v: i + 1 for i, v in enumerate(sorted(waited[e]))} for e in self.ENG}
        self.prog = {}
        for eng in self.ENG:
            pl = []
            for kind, waits, fn, x in self.rec[eng]:
                rw = []
                for k, v in waits:
                    if k[0] == "e":
                        rw.append((self.sem[k[1]], rank[k[1]][v]))
                    else:
                        rw.append((self.dsem[k[1]], v))
                if kind == "op":
                    pl.append((rw, fn, self.sem[eng] if x in waited[eng] else None, 1))
                elif kind == "dma":
                    pl.append((rw, fn, self.dsem[x], 16))
                else:
                    pl.append((rw, None, None, 0))
            self.prog[eng] = pl

    def simulate(self):
        val = {}
        pc = {e: 0 for e in self.ENG}
        prog = self.prog
        while True:
            progressed = False
            for e in self.ENG:
                while pc[e] < len(prog[e]):
                    waits, fn, sem, inc = prog[e][pc[e]]
                    if all(val.get(id(s_), 0) >= v for s_, v in waits):
                        if sem is not None:
                            val[id(sem)] = val.get(id(sem), 0) + inc
                        pc[e] += 1
                        progressed = True
                    else:
                        break
            if all(pc[e] == len(prog[e]) for e in self.ENG):
                return None
            if not progressed:
                return {e: (pc[e], len(prog[e])) for e in self.ENG}

    def emit(self):
        nc = self.nc
        prog = self.prog

        def run(e, pl):
            for waits, fn, sem, inc in pl:
                for s_, v in waits:
                    e.wait_ge(s_, v)
                if fn is not None:
                    ins = fn(e)
                    if sem is not None:
                        ins.then_inc(sem, inc)

        with nc.Block() as block:
            @block.tensor
            def _(e):
                run(e, prog["pe"])

            @block.scalar
            def _(e):
                run(e, prog["act"])

            @block.vector
            def _(e):
                run(e, prog["dve"])

            @block.gpsimd
            def _(e):
                run(e, prog["pool"])

            @block.sync
            def _(e):
                run(e, prog["sp"])


def lambda_init(l):
    return 0.8 - 0.6 * math.exp(-0.3 * l)


GC_MIX, GC_CROSS, GC_FFN, GC_CONV, GC_SUBLN, GC_PER = 0, 8, 16, 24, 48, 49


def build(layer_ids, final, stop_after=None, max_steps=None):
    L = len(layer_ids)
    nc = bass.Bass("TRN2", target_bir_lowering=False)

    def din(name, shape, dt=F32):
        return nc.dram_tensor(name, shape, dt, kind="ExternalInput").ap()

    x_in = din("x", [S_LEN, D])
    mem_in = din("mem", [NMEM, D])
    w_in = din("w_in", [L, D, 8192])
    w_br = din("w_branch", [L, 2, D, D])
    w_o = din("w_o", [L, D, D])
    w_cq = din("w_cq", [L, D, D])
    w_ckv = din("w_ckv", [L, D, 2 * D])
    w_co = din("w_co", [L, D, D])
    w_rt = din("w_rt", [L, D, 36])
    w_eg = din("w_exp_gate", [L, NEXP, D, DEXP])
    w_eu = din("w_exp_up", [L, NEXP, D, DEXP])
    w_ed = din("w_exp_down", [L, NEXP, DEXP, D])
    gcols_d = din("gcols", [128, L * GC_PER + 8])
    lamv_d = din("lamv", [128, L * 256])
    gfin_d = din("gfinal", [128, D])
    cmat_d = din("cmat", [128, 4 * 128], BF16)
    cossin_d = din("cossin", [128, 2 * S_LEN], BF16)
    cmisc_d = din("cmisc", [128, 64 + 8])
    out_d = nc.dram_tensor("out", [S_LEN, D], F32, kind="ExternalOutput").ap()
    xb_d = nc.dram_tensor("xb_scr", [NROW, D], BF16, kind="Internal").ap()
    yb_d = nc.dram_tensor("yb_scr", [NROW, D], F32, kind="Internal").ap()

    st = ExitStack()
    with st:
        S = Sched(nc, st)

        def sb(name, shape, dt):
            return st.enter_context(nc.sbuf_tensor("sb_" + name, shape, dt))

        X = sb("X", [128, NT, D], F32)
        HT = sb("HT", [128, KC * S_LEN], BF16)
        YR = sb("YR", [128, KC * S_LEN], BF16)
        NBIG = 19 * 1024
        BIG = sb("BIG", [128, NBIG], BF16)
        NSLOT = 3
        WS = [sb("WS%d" % i, [128, 4096], BF16) for i in range(NSLOT)]
        cmat = sb("cmat", [128, 4 * 128], BF16)
        gcols = sb("gcols", [128, L * GC_PER + 8], F32)
        cmisc = sb("cmisc", [128, 72], F32)
        memT = sb("memT", [128, KC, NMEM], BF16)
        stat = sb("stat", [128, 64], F32)
        lamc = sb("lamc", [128, 2 * L], F32)
        sublnS = sb("sublnS", [128, L], F32)
        psall = st.enter_context(nc.psum_tensor("psall", [128, 8 * 512], F32))
        PS = [psall[:, i * 512:(i + 1) * 512] for i in range(8)]
        PSB = S.bufs(8, "ps")
        for b_ in PSB:
            b_.excl = True

        ident = cmat[:, 0:128]
        ones = cmat[:, 128:256]
        rrot = cmat[:, 256:384]
        ustr = cmat[:, 384:512]
        thr = cmisc[:, 0:64]
        pcv = cmisc[:, 64:72]

        Xb = S.bufs(NT, "X")
        hTb = S.buf("hT")
        YRb = S.buf("YR")
        WSb = S.bufs(NSLOT, "WS")
        b_const = S.buf("const")
        b_memT = S.buf("memT")
        b_stat = S.buf("stat")

        hT = HT[:, :].rearrange("p (c t) -> p c t", c=KC)

        def carve(region, off, n, dt=BF16):
            if dt == BF16:
                return region[:, off:off + n], off + n
            assert off % 2 == 0
            return region[:, off:off + 2 * n].bitcast(dt), off + 2 * n

        psrr = [0]

        def psbank():
            i = psrr[0] % 8
            psrr[0] += 1
            return PS[i], PSB[i]

        def dma_sp(key, out, in_, reads=(), writes=()):
            return S.dma("sp", key, lambda e: e.dma_start(out=out, in_=in_), reads=reads, writes=writes)

        def dma_pool(key, out, in_, reads=(), writes=()):
            return S.dma("pool", key, lambda e: e.dma_start(out=out, in_=in_), reads=reads, writes=writes)

        def mm(out, lhsT, rhs, start, stop, reads, writes):
            S.op("pe", lambda e: e.matmul(out, lhsT=lhsT, rhs=rhs, start=start, stop=stop),
                 reads=reads, writes=writes)

        def tr(out, in_, reads, writes):
            S.op("pe", lambda e: e.transpose(out=out, in_=in_, identity=ident), reads=list(reads) + [b_const], writes=writes)

        def act(out, in_, func, reads, writes, scale=1.0, bias=None, accum=None):
            kw = {}
            if bias is not None:
                kw["bias"] = bias
            if accum is not None:
                kw["accum_out"] = accum
            S.op("act", lambda e: e.activation(out=out, in_=in_, func=func, scale=scale, **kw), reads=reads, writes=writes)

        def tt(eng, out, in0, in1, op, reads, writes):
            S.op(eng, lambda e: e.tensor_tensor(out=out, in0=in0, in1=in1, op=op), reads=reads, writes=writes)

        def ts(eng, out, in0, s1, op0, reads, writes, s2=None, op1=None, accum=None):
            kw = {}
            if accum is not None:
                kw["accum_out"] = accum
            if op1 is None:
                S.op(eng, lambda e: e.tensor_scalar(out=out, in0=in0, scalar1=s1, scalar2=None, op0=op0, **kw), reads=reads, writes=writes)
            else:
                S.op(eng, lambda e: e.tensor_scalar(out=out, in0=in0, scalar1=s1, scalar2=s2, op0=op0, op1=op1, **kw), reads=reads, writes=writes)

        def stt(eng, out, in0, scalar, in1, op0, op1, reads, writes):
            S.op(eng, lambda e: e.scalar_tensor_tensor(out=out, in0=in0, scalar=scalar, in1=in1, op0=op0, op1=op1), reads=reads, writes=writes)

        def cp(eng, out, in_, reads, writes):
            S.op(eng, lambda e: e.tensor_copy(out=out, in_=in_), reads=reads, writes=writes)

        dma_sp("c0", cmat[:, :], cmat_d[:, :], writes=[b_const])
        dma_sp("c1", gcols[:, :], gcols_d[:, :], writes=[b_const])
        dma_sp("c1", cmisc[:, :], cmisc_d[:, :], writes=[b_const])
        for i in range(NT):
            dma_sp("xin", X[:, i, :], x_in[i * 128:(i + 1) * 128, :], writes=[Xb[i]])

        b_big = S.buf("bigscratch")
        lamv, _ = carve(BIG, 0, L * 256, F32)
        ltmp, _ = carve(BIG, 2 * L * 256, 64, F32)
        dma_sp("c3", lamv, lamv_d[:, :], writes=[b_big])
        for li_, l in enumerate(layer_ids):
            base = li_ * 256
            for j in range(2):
                S.op("dve", lambda e, base=base, j=j: e.tensor_tensor(out=ltmp, in0=lamv[:, base + j * 128:base + j * 128 + 64],
                                                                      in1=lamv[:, base + j * 128 + 64:base + j * 128 + 128], op=ALU.mult),
                     reads=[b_big], writes=[b_big])
                S.op("dve", lambda e, j=j: e.tensor_reduce(out=stat[:, j:j + 1], in_=ltmp, axis=AX.X, op=ALU.add),
                     reads=[b_big], writes=[b_stat])
            act(stat[:, 2:4], stat[:, 0:2], AF.Exp, [b_stat], [b_stat])
            tt("dve", stat[:, 4:5], stat[:, 3:4], stat[:, 2:3], ALU.subtract, [b_stat], [b_stat])
            ts("dve", lamc[:, li_:li_ + 1], stat[:, 4:5], -lambda_init(l), ALU.add, [b_stat], [b_const])
            ts("dve", sublnS[:, li_:li_ + 1], gcols[:, li_ * GC_PER + GC_SUBLN:li_ * GC_PER + GC_SUBLN + 1],
               1.0 - lambda_init(l), ALU.mult, [b_const], [b_const])

        ssq = sb("ssq", [128, NT], F32)
        rstd = sb("rstd", [128, NT], F32)
        b_ssq = S.buf("ssq")
        b_rstd = S.buf("rstd")
        hn_t = [sb("hn%d" % i, [128, D], BF16) for i in range(2)]
        hn_b = S.bufs(2, "hn")

        def rstd_tiles(src_ap_fn, src_bufs, ntiles, inv_n):
            for i in range(ntiles):
                act(hn_t[0][:, :], src_ap_fn(i), AF.Square, [src_bufs[i]], [hn_b[0], b_ssq], accum=ssq[:, i:i + 1])
            ts("dve", rstd[:, 0:ntiles], ssq[:, 0:ntiles], inv_n, ALU.mult, [b_ssq], [b_rstd], s2=EPS, op1=ALU.add)
            act(rstd[:, 0:ntiles], rstd[:, 0:ntiles], AF.Ln, [b_rstd], [b_rstd])
            act(rstd[:, 0:ntiles], rstd[:, 0:ntiles], AF.Exp, [b_rstd], [b_rstd], scale=-0.5)

        def norm_to_fm(src_ap_fn, src_bufs, ntiles, gcol0, dstT, dst_buf, keep_rows=None):
            rstd_tiles(src_ap_fn, src_bufs, ntiles, 1.0 / D)
            for i in range(ntiles):
                if keep_rows is None:
                    hn, hb = hn_t[i % 2], hn_b[i % 2]
                    hn_ap = hn[:, :]
                else:
                    hn_ap, hb = keep_rows(i)
                act(hn_ap, src_ap_fn(i), AF.Copy, [src_bufs[i], b_rstd], [hb], scale=rstd[:, i:i + 1])
                pt, pb = psbank()
                ptb = pt[:, :].bitcast(BF16).rearrange("p (c t) -> p c t", t=128)
                for c in range(KC):
                    tr(ptb[:, c, :], hn_ap[:, c * 128:(c + 1) * 128], [hb], [pb])
                for c in range(KC):
                    eng = "dve" if c % 2 == 0 else "act"
                    if eng == "dve":
                        ts("dve", dstT[:, c, i * 128:(i + 1) * 128], ptb[:, c, :], gcols[:, gcol0 + c:gcol0 + c + 1], ALU.mult,
                           [pb, b_const], [dst_buf])
                    else:
                        act(dstT[:, c, i * 128:(i + 1) * 128], ptb[:, c, :], AF.Copy, [pb, b_const], [dst_buf],
                            scale=gcols[:, gcol0 + c:gcol0 + c + 1])

        memrows, _ = carve(BIG, 4096, 2 * D, F32)
        memrows = memrows.rearrange("p (i d) -> p i d", i=2)
        b_memrows = S.bufs(2, "memrows")
        for i in range(2):
            dma_sp("c2", memrows[:, i, :], mem_in[i * 128:(i + 1) * 128, :], writes=[b_memrows[i]])
        norm_to_fm(lambda i: memrows[:, i, :], b_memrows, 2, L * GC_PER, memT, b_memT)

        dbg_bufs = []
        steps = []

        def wload(slot, sbuf_, pieces):
            key = "w%d" % [i for i in range(NSLOT) if WS[i] is slot][0]
            for dst, src in pieces:
                dma_pool(key, dst(slot), src, writes=[sbuf_])

        def w3(slot, off, ncols):
            return slot[:, off:off + KC * ncols].rearrange("p (c n) -> p c n", c=KC)

        def dram_cols(w2d, c0, ncols):
            return w2d[:, c0:c0 + ncols].rearrange("(c p) n -> p c n", p=128)

        def layer_steps(li_, l):
            g0 = li_ * GC_PER
            win_l = w_in[li_]
            off = 0
            cosb, off = carve(BIG, off, S_LEN)
            sinb, off = carve(BIG, off, S_LEN)
            qT, off = carve(BIG, off, S_LEN)
            kT, off = carve(BIG, off, S_LEN)
            vtm, off = carve(BIG, off, NT * 128)
            vtm = vtm.rearrange("p (i e) -> p i e", i=NT)
            NE = 3
            Et2 = []
            for _ in range(NE):
                a, off = carve(BIG, off, 1024)
                Et2.append(a)
            Et = [a[:, 0:512] for a in Et2]
            NTMP = 6
            Tm = []
            for _ in range(NTMP):
                a, off = carve(BIG, off, 512, F32)
                Tm.append(a)
            assert off <= NBIG, off
            b_cs = S.buf("cossin")
            dbg_bufs.append(b_cs)
            b_q = S.buf("qT")
            b_k = S.buf("kT")
            b_v = S.buf("v")
            Eb = S.bufs(NE, "E")
            Tb = S.bufs(NTMP, "T")
            rrE = [0]
            rrT = [0]

            def getE():
                i = rrE[0] % NE
                rrE[0] += 1
                return Et[i], Eb[i]

            def getE2():
                i = rrE[0] % NE
                rrE[0] += 1
                return Et2[i], Eb[i]

            def getT():
                i = rrT[0] % (NTMP - 2)
                rrT[0] += 1
                return Tm[i], Tb[i]

            Esum = [(Tm[NTMP - 2], Tb[NTMP - 2]), (Tm[NTMP - 1], Tb[NTMP - 1])]

            yT = YR[:, :].rearrange("p (c t) -> p c t", c=KC)
            b_y = [S.buf("y%d" % c) for c in range(KC)]

            def mixer_begin(slot, sbuf_):
                S.alias([b_cs, b_q, b_k, b_v] + Eb + Tb, [b_big])
                S.alias(b_y, [YRb])
                dma_sp("cs", cosb, cossin_d[:, 0:S_LEN], writes=[b_cs])
                dma_sp("cs", sinb, cossin_d[:, S_LEN:2 * S_LEN], writes=[b_cs])
                if os.environ.get('CSTOUCH'):
                    cp("dve", stat[:, 60:61], cosb[:, 0:1], [b_cs], [b_stat])
                norm_to_fm(lambda i: X[:, i, :], Xb, NT, g0 + GC_MIX, hT, hTb)
                for r in range(NROW // 128):
                    for hh in range(2):
                        dma_sp("xbz", xb_d[r * 128:(r + 1) * 128, hh * 512:(hh + 1) * 512], ztile[:, :], reads=[b_zt], writes=[b_xb])

            steps.append((None, mixer_begin))

            def head_step(h):
                def load(slot, sbuf_):
                    wload(slot, sbuf_, [
                        (lambda s: w3(s, 0, 128), dram_cols(win_l, h * 128, 128)),
                        (lambda s: w3(s, 1024, 128), dram_cols(win_l, 1024 + h * 128, 128)),
                        (lambda s: w3(s, 2048, 128), dram_cols(win_l, 2048 + h * 128, 128)),
                    ])

                def compute(slot, sbuf_):
                    wq = w3(slot, 0, 128)
                    wk = w3(slot, 1024, 128)
                    wv = w3(slot, 2048, 128)
                    for (wt, dst, db) in ((wq, qT, b_q), (wk, kT, b_k)):
                        for tb in range(4):
                            tsl = slice(tb * 512, (tb + 1) * 512)
                            pp, pb = psbank()
                            for c in range(KC):
                                mm(pp[:, :], wt[:, c, :], hT[:, c, tsl], c == 0, c == KC - 1, [sbuf_, hTb], [pb])
                            qb, qbb = getE()
                            act(qb, pp[:, :], AF.Copy, [pb], [qbb])
                            if HEADCUT == 0:
                                continue
                            ROT = int(os.environ.get('ROT', '9'))
                            p2, p2b = psbank()
                            P2V = os.environ.get('P2V', '')
                            if P2V == 'ident':
                                mm(p2[:, :], ident, qb, True, True, [b_const, qbb], [p2b])
                            elif P2V == 'rhs':
                                mm(p2[:, :], rrot, hT[:, 0, tsl], True, True, [b_const, hTb], [p2b])
                            elif P2V == 'evac':
                                mm(p2[:, :], rrot, qb, True, True, [b_const, qbb], [p2b])
                                t9, t9b = getT()
                                cp("dve", t9, p2[:, :], [p2b], [t9b])
                            else:
                                mm(p2[:, :], rrot, qb, True, True, [b_const, qbb], [p2b])
                            if ROT < 2:
                                continue
                            t1, t1b = getT()
                            if os.environ.get('ROTV') == 'sb':
                                tt("dve", t1, qb, cosb[:, tsl], ALU.mult, [qbb, b_cs], [t1b])
                            elif os.environ.get('ROTV') == 'hT':
                                tt("dve", t1, pp[:, :], hT[:, 0, tsl], ALU.mult, [pb, hTb], [t1b])
                            elif os.environ.get('ROTV') == 'nocs':
                                tt("dve", t1, pp[:, :], qb, ALU.mult, [pb, qbb], [t1b])
                            else:
                                tt("dve", t1, pp[:, :], cosb[:, tsl], ALU.mult, [pb, b_cs], [t1b])
                            if ROT < 3:
                                continue
                            t2, t2b = getT()
                            tt("dve", t2, p2[:, :], sinb[:, tsl], ALU.mult, [p2b, b_cs], [t2b])
                            if ROT < 4:
                                continue
                            tt("pool", dst[:, tsl], t1, t2, ALU.add, [t1b, t2b], [db])
                    if HEADCUT < 2:
                        return
                    for i4 in range(4):
                        pp, pb = psbank()
                        for ii in range(4):
                            i = i4 * 4 + ii
                            for c in range(KC):
                                mm(pp[:, ii * 128:(ii + 1) * 128], hT[:, c, i * 128:(i + 1) * 128], wv[:, c, :],
                                   c == 0, c == KC - 1, [sbuf_, hTb], [pb])
                        act(vtm[:, i4 * 4:(i4 + 1) * 4, :], pp[:, :].rearrange("p (i e) -> p i e", i=4), AF.Copy, [pb], [b_v])
                    if HEADCUT < 3:
                        return
                    pending = [None]
                    for qb_ in range(4):
                        qsl = slice(qb_ * 512, (qb_ + 1) * 512)
                        pO = [(PS[4], PSB[4]), (PS[5], PSB[5])]
                        pR = [(PS[6], PSB[6]), (PS[7], PSB[7])]
                        LA = 2
                        Spair = [(0, 1), (2, 3)]
                        Es = {}
                        for i in range(NT + LA):
                            if i < NT:
                                kb = i
                                ba, bb = Spair[i % 2]
                                for c, bk in ((0, ba), (1, bb)):
                                    mm(PS[bk][:, :], kT[c * 64:(c + 1) * 64, kb * 128:(kb + 1) * 128], qT[c * 64:(c + 1) * 64, qsl],
                                       True, True, [b_k, b_q], [PSB[bk]])
                                E2, E2b = getE2()
                                act(E2, psall[:, ba * 512:(ba + 2) * 512], AF.Exp, [PSB[ba], PSB[bb]], [E2b], scale=0.125)
                                Es[i] = (E2, E2b)
                            j = i - LA
                            if j >= 0:
                                kb = j
                                E2, E2b = Es.pop(j)
                                for c in range(2):
                                    E = E2[:, c * 512:(c + 1) * 512]
                                    mm(pO[c][0][:, :], vtm[:, kb, :], E, kb == 0, kb == NT - 1, [b_v, E2b], [pO[c][1]])
                                    es_, esb_ = Esum[c]
                                    eng_ = "dve" if c == 0 else "pool"
                                    if kb == 0:
                                        cp(eng_, es_, E, [E2b], [esb_])
                                    else:
                                        tt(eng_, es_, es_, E, ALU.add, [esb_, E2b], [esb_])
                            if i == 5 and pending[0] is not None:
                                pending[0]()
                                pending[0] = None
                        ri = []
                        for c in range(2):
                            e16, e16b = getE()
                            act(e16, Esum[c][0], AF.Copy, [Esum[c][1]], [e16b])
                            mm(pR[c][0][:, :], ones, e16, True, True, [b_const, e16b], [pR[c][1]])
                        for c in range(2):
                            r_, rb_ = getT()
                            act(r_, pR[c][0][:, :], AF.Ln, [pR[c][1]], [rb_])
                            act(r_, r_, AF.Exp, [rb_], [rb_], scale=-1.0)
                            ri.append((r_, rb_))
                        t0, t0b = getT()
                        tt("dve", t0, pO[0][0][:, :], ri[0][0], ALU.mult, [pO[0][1], ri[0][1]], [t0b])
                        t1, t1b = getT()
                        tt("dve", t1, pO[1][0][:, :], ri[1][0], ALU.mult, [pO[1][1], ri[1][1]], [t1b])
                        o_, ob_ = getT()
                        stt("dve", o_, t1, lamc[:, li_:li_ + 1], t0, ALU.mult, ALU.add, [t1b, t0b, b_const], [ob_])
                        sqf, sqb = getT()
                        sq = sqf.bitcast(BF16)[:, 0:512]
                        tt("pool", sq, o_, o_, ALU.mult, [ob_], [sqb])

                        def fin2(o_=o_, ob_=ob_, sq=sq, sqb=sqb, qsl=qsl):
                            pq, pqb = (PS[6], PSB[6])
                            mm(pq[:, :], ones, sq, True, True, [b_const, sqb], [pqb])
                            rs, rsb = getT()
                            ts("dve", rs, pq[:, :], 1.0 / 128, ALU.mult, [pqb], [rsb], s2=EPS, op1=ALU.add)
                            act(rs, rs, AF.Ln, [rsb], [rsb])
                            act(rs, rs, AF.Exp, [rsb], [rsb], scale=-0.5)
                            stt("dve", yT[:, h, qsl], o_, sublnS[:, li_:li_ + 1], rs, ALU.mult, ALU.mult, [ob_, rsb, b_const], [b_y[h]])

                        pending[0] = fin2
                    if pending[0] is not None:
                        pending[0]()
                        pending[0] = None

                return load, compute

            psS = [(PS[i], PSB[i]) for i in range(4)]

            for h in range(8):
                steps.append(head_step(h))

            offc = 0
            Mreg, offc = carve(BIG, offc, KC * S_LEN)
            Mv = Mreg.rearrange("p (c t) -> p c t", c=KC)
            CT = []
            for _ in range(3):
                a, offc = carve(BIG, offc, 512, F32)
                CT.append(a)
            assert offc <= NBIG
            b_M = [S.buf("M%d" % c) for c in range(KC)]
            CTb = S.bufs(3, "CT")
            rrC = [0]

            def getC():
                i = rrC[0] % 3
                rrC[0] += 1
                return CT[i], CTb[i]

            def phaseC_begin(slot, sbuf_):
                S.alias(b_M + CTb, [b_cs, b_q, b_k, b_v] + Eb + Tb)

            steps.append((None, phaseC_begin))

            def branch_step(n, j):
                def load(slot, sbuf_):
                    wload(slot, sbuf_, [
                        (lambda s: w3(s, 0, 128), dram_cols(w_br[li_, n], j * 128, 128)),
                        (lambda s: w3(s, 1024, 128), dram_cols(win_l, 6144 + n * 1024 + j * 128, 128)),
                    ])

                def compute(slot, sbuf_):
                    wb = w3(slot, 0, 128)
                    wg = w3(slot, 1024, 128)
                    for tb in range(4):
                        tsl = slice(tb * 512, (tb + 1) * 512)
                        pg, pgb = psbank()
                        for c in range(KC):
                            mm(pg[:, :], wg[:, c, :], hT[:, c, tsl], c == 0, c == KC - 1, [sbuf_, hTb], [pgb])
                        pbr, pbrb = psbank()
                        for c in range(KC):
                            mm(pbr[:, :], wb[:, c, :], yT[:, c, tsl], c == 0, c == KC - 1, [sbuf_, b_y[c]], [pbrb])
                        sg, sgb = getC()
                        act(sg, pg[:, :], AF.Sigmoid, [pgb], [sgb])
                        if n == 0:
                            tt("dve", Mv[:, j, tsl], sg, pbr[:, :], ALU.mult, [sgb, pbrb], [b_M[j]])
                        else:
                            t_, tb_ = getC()
                            tt("dve", t_, sg, pbr[:, :], ALU.mult, [sgb, pbrb], [tb_])
                            tt("pool", Mv[:, j, tsl], Mv[:, j, tsl], t_, ALU.add, [b_M[j], tb_], [b_M[j]])

                return load, compute

            for j in range(KC):
                steps.append(branch_step(0, j))


            def conv_step(j):
                def load(slot, sbuf_):
                    wload(slot, sbuf_, [
                        (lambda s: w3(s, 0, 128), dram_cols(win_l, 3072 + j * 128, 128)),
                        (lambda s: w3(s, 1024, 128), dram_cols(win_l, 4096 + j * 128, 128)),
                        (lambda s: w3(s, 2048, 128), dram_cols(win_l, 5120 + j * 128, 128)),
                    ])

                def compute(slot, sbuf_):
                    wcb = w3(slot, 0, 128)
                    wcc = w3(slot, 1024, 128)
                    wcx = w3(slot, 2048, 128)
                    u = U_pad
                    for tb in range(4):
                        tsl = slice(tb * 512, (tb + 1) * 512)
                        pc_, pcb = psbank()
                        for c in range(KC):
                            mm(pc_[:, :], wcc[:, c, :], hT[:, c, tsl], c == 0, c == KC - 1, [sbuf_, hTb], [pcb])
                        px, pxb = psbank()
                        for c in range(KC):
                            mm(px[:, :], wcx[:, c, :], hT[:, c, tsl], c == 0, c == KC - 1, [sbuf_, hTb], [pxb])
                        t_, tb_ = getC()
                        act(t_, pc_[:, :], AF.Copy, [pcb], [tb_])
                        tt("dve", u[:, 1 + tb * 512:1 + (tb + 1) * 512], t_, px[:, :], ALU.mult, [tb_, pxb], [b_u])
                    cw = g0 + GC_CONV
                    for tb in range(4):
                        tsl = slice(tb * 512, (tb + 1) * 512)
                        acc, accb = getC()
                        act(acc, u[:, 1 + tb * 512:1 + (tb + 1) * 512], AF.Copy, [b_u, b_const], [accb], scale=gcols[:, cw + 8 + j:cw + 8 + j + 1])
                        stt("dve", acc, u[:, tb * 512:(tb + 1) * 512], gcols[:, cw + j:cw + j + 1], acc, ALU.mult, ALU.add, [b_u, accb, b_const], [accb])
                        stt("dve", acc, u[:, 2 + tb * 512:2 + (tb + 1) * 512], gcols[:, cw + 16 + j:cw + 16 + j + 1], acc, ALU.mult, ALU.add, [b_u, accb, b_const], [accb])
                        pb_, pbb = psbank()
                        for c in range(KC):
                            mm(pb_[:, :], wcb[:, c, :], hT[:, c, tsl], c == 0, c == KC - 1, [sbuf_, hTb], [pbb])
                        tt("dve", yT[:, j, tsl], pb_[:, :], acc, ALU.mult, [pbb, accb], [b_y[j]])

                return load, compute

            for j in range(KC):
                steps.append(conv_step(j))
            for j in range(KC):
                steps.append(branch_step(1, j))

            def out_proj_step(wmat, half, srcT, src_bufs):
                def load(slot, sbuf_):
                    wload(slot, sbuf_, [(lambda s: w3(s, 0, 512), dram_cols(wmat, half * 512, 512))])

                def compute(slot, sbuf_):
                    wt = w3(slot, 0, 512)
                    for i in range(NT):
                        pp, pb = psbank()
                        for c in range(KC):
                            mm(pp[:, :], srcT[:, c, i * 128:(i + 1) * 128], wt[:, c, :], c == 0, c == KC - 1, [sbuf_, src_bufs[c]], [pb])
                        tt("dve", X[:, i, half * 512:(half + 1) * 512], X[:, i, half * 512:(half + 1) * 512], pp[:, :], ALU.add,
                           [Xb[i], pb], [Xb[i]])

                return load, compute

            for half in range(2):
                steps.append(out_proj_step(w_o[li_], half, Mv, b_M))

            def mixer_end(slot, sbuf_):
                S.alias([b_big], b_M + CTb)
                S.alias([YRb], b_y)

            steps.append((None, mixer_end))
            if stop_after == (l, "mix"):
                return True

            offx = 0
            kcT, offx = carve(BIG, offx, KC * NMEM)
            kcT = kcT.rearrange("p (c m) -> p c m", c=KC)
            vc, offx = carve(BIG, offx, 2 * D)
            vc = vc.rearrange("p (i d) -> p i d", i=2)
            XE = []
            for _ in range(6):
                a, offx = carve(BIG, offx, 512)
                XE.append(a)
            XT = []
            for _ in range(4):
                a, offx = carve(BIG, offx, 512, F32)
                XT.append(a)
            assert offx <= NBIG
            b_kc = S.buf("kcT")
            b_vc = S.buf("vc")
            XEb = S.bufs(6, "XE")
            XTb = S.bufs(4, "XT")
            rrx = [0, 0]

            def getXE():
                i = rrx[0] % 6
                rrx[0] += 1
                return XE[i], XEb[i]

            def getXT():
                i = rrx[1] % 4
                rrx[1] += 1
                return XT[i], XTb[i]

            qcT = YR[:, :].rearrange("p (c t) -> p c t", c=KC)
            b_qc = [S.buf("qc%d" % c) for c in range(KC)]
            ocT = HT[:, :].rearrange("p (c t) -> p c t", c=KC)
            b_oc = [S.buf("oc%d" % c) for c in range(KC)]

            def cross_begin(slot, sbuf_):
                S.alias([b_kc, b_vc] + XEb + XTb, [b_big])
                S.alias(b_qc, [YRb])
                norm_to_fm(lambda i: X[:, i, :], Xb, NT, g0 + GC_CROSS, hT, hTb)

            steps.append((None, cross_begin))

            def ckv_step(part):
                def load(slot, sbuf_):
                    wload(slot, sbuf_, [(lambda s: w3(s, 0, 512), dram_cols(w_ckv[li_], part * 512, 512))])

                def compute(slot, sbuf_):
                    wt = w3(slot, 0, 512)
                    if part < 2:
                        for jj in range(4):
                            j = part * 4 + jj
                            pp, pb = psbank()
                            for c in range(KC):
                                mm(pp[:, 0:NMEM], wt[:, c, jj * 128:(jj + 1) * 128], memT[:, c, :], c == 0, c == KC - 1, [sbuf_, b_memT], [pb])
                            act(kcT[:, j, :], pp[:, 0:NMEM], AF.Copy, [pb], [b_kc])
                    else:
                        for i in range(2):
                            pp, pb = psbank()
                            for c in range(KC):
                                mm(pp[:, :], memT[:, c, i * 128:(i + 1) * 128], wt[:, c, :], c == 0, c == KC - 1, [sbuf_, b_memT], [pb])
                            act(vc[:, i, (part - 2) * 512:(part - 1) * 512], pp[:, :], AF.Copy, [pb], [b_vc])

                return load, compute

            for part in range(4):
                steps.append(ckv_step(part))

            def cq_step(half):
                def load(slot, sbuf_):
                    wload(slot, sbuf_, [(lambda s: w3(s, 0, 512), dram_cols(w_cq[li_], half * 512, 512))])

                def compute(slot, sbuf_):
                    wt = w3(slot, 0, 512)
                    for jj in range(4):
                        j = half * 4 + jj
                        for tb in range(4):
                            tsl = slice(tb * 512, (tb + 1) * 512)
                            pp, pb = psbank()
                            for c in range(KC):
                                mm(pp[:, :], wt[:, c, jj * 128:(jj + 1) * 128], hT[:, c, tsl], c == 0, c == KC - 1, [sbuf_, hTb], [pb])
                            if (jj + tb) % 2 == 0:
                                act(qcT[:, j, tsl], pp[:, :], AF.Copy, [pb], [b_qc[j]])
                            else:
                                cp("dve", qcT[:, j, tsl], pp[:, :], [pb], [b_qc[j]])

                return load, compute

            for half in range(2):
                steps.append(cq_step(half))

            def cross_attn(slot, sbuf_):
                S.alias(b_oc, [hTb])
                for h in range(4):
                    for tb in range(4):
                        tsl = slice(tb * 512, (tb + 1) * 512)
                        pO = [psbank(), psbank()]
                        pR = psbank()
                        for mb in range(2):
                            pS, pSb = psbank()
                            for cc in range(2):
                                mm(pS[:, :], kcT[:, 2 * h + cc, mb * 128:(mb + 1) * 128], qcT[:, 2 * h + cc, tsl], cc == 0, cc == 1,
                                   [b_kc, b_qc[2 * h + cc]], [pSb])
                            E, Eb_ = getXE()
                            act(E, pS[:, :], AF.Exp, [pSb], [Eb_], scale=1.0 / 16)
                            for ee in range(2):
                                mm(pO[ee][0][:, :], vc[:, mb, h * 256 + ee * 128:h * 256 + (ee + 1) * 128], E, mb == 0, mb == 1,
                                   [b_vc, Eb_], [pO[ee][1]])
                            mm(pR[0][:, :], ones, E, mb == 0, mb == 1, [b_const, Eb_], [pR[1]])
                        r_, rb_ = getXT()
                        S.op("dve", lambda e, r_=r_, pR=pR: e.reciprocal(out=r_, in_=pR[0][:, :]), reads=[pR[1]], writes=[rb_])
                        for ee in range(2):
                            tt("dve", ocT[:, 2 * h + ee, tsl], pO[ee][0][:, :], r_, ALU.mult, [pO[ee][1], rb_], [b_oc[2 * h + ee]])

            steps.append((None, cross_attn))
            for half in range(2):
                steps.append(out_proj_step(w_co[li_], half, ocT, b_oc))

            def cross_end(slot, sbuf_):
                S.alias([b_big], [b_kc, b_vc] + XEb + XTb)
                S.alias([YRb], b_qc)
                S.alias([hTb], b_oc)

            steps.append((None, cross_end))
            if stop_after == (l, "cross"):
                return True

            hrows = YR[:, :].rearrange("p (i d) -> p i d", i=NT)
            b_hr = S.bufs(NT, "hrows")
            offm = 0
            oh12, offm = carve(BIG, offm, NT * 64, F32)
            oh12 = oh12.rearrange("p (i e) -> p i e", i=NT)
            mohb, offm = carve(BIG, offm, NT * 32)
            mohb = mohb.rearrange("p (i e) -> p i e", i=NT)
            wts, offm = carve(BIG, offm, NT * 2, F32)
            wts = wts.rearrange("p (i k) -> p i k", i=NT)
            desti, offm = carve(BIG, offm, NT * 2, I32)
            desti = desti.rearrange("p (i k) -> p i k", i=NT)
            rt, offm = carve(BIG, offm, 320, F32)
            cnt, offm = carve(BIG, offm, 32, F32)
            padA, offm = carve(BIG, offm, 32, F32)
            padB, offm = carve(BIG, offm, 32, F32)
            pstart, offm = carve(BIG, offm, 32, F32)
            pend, offm = carve(BIG, offm, 32, F32)
            eblk, offm = carve(BIG, offm, NBLK, F32)
            idxgu, offm = carve(BIG, offm, NBLK * 8, I32)
            idxd, offm = carve(BIG, offm, NBLK * 8, I32)
            idxgu = idxgu.rearrange("p (b c) -> p b c", b=NBLK)
            idxd = idxd.rearrange("p (b c) -> p b c", b=NBLK)
            idxgu_f, offt = carve(BIG, offm, NBLK * 8, F32)
            idxd_f, offt = carve(BIG, offt, NBLK * 8, F32)
            idxgu_f = idxgu_f.rearrange("p (b c) -> p b c", b=NBLK)
            idxd_f = idxd_f.rearrange("p (b c) -> p b c", b=NBLK)
            rtall, offt = carve(BIG, offt, NT * 208, F32)
            rtall = rtall.rearrange("p (i n) -> p i n", i=NT)
            assert offt <= NBIG, offt
            offm0 = offm
            b_rt = S.buf("rt")
            b_rt_i = S.bufs(NT, "rti")
            b_oh_i = S.bufs(NT, "ohi")
            b_dest_i = S.bufs(NT, "desti")
            b_oh = S.buf("oh")
            b_route = S.buf("route")

            def moe_begin(slot, sbuf_):
                S.alias([b_rt, b_oh, b_route] + b_rt_i + b_oh_i + b_dest_i, [b_big])
                S.alias(b_hr, [YRb])

            steps.append((None, moe_begin))

            def router_step():
                def load(slot, sbuf_):
                    wload(slot, sbuf_, [(lambda s: s[:, 0:KC * 36].rearrange("p (c n) -> p c n", c=KC),
                                         w_rt[li_].rearrange("(c p) n -> p c n", p=128))])

                def compute(slot, sbuf_):
                    wr = slot[:, 0:KC * 36].rearrange("p (c n) -> p c n", c=KC)
                    norm_to_fm(lambda i: X[:, i, :], Xb, NT, g0 + GC_FFN, hT, hTb,
                               keep_rows=lambda i: (hrows[:, i, :], b_hr[i]))
                    def rt_tile(i):
                        rt = rtall[:, i, :]
                        yield
                        lg = rt[:, 0:36]
                        yield
                        pp, pb = psbank()
                        yield
                        for c in range(KC):
                            mm(pp[:, 0:36], hT[:, c, i * 128:(i + 1) * 128], wr[:, c, :], c == 0, c == KC - 1, [sbuf_, hTb], [pb])
                        yield
                        R_ = [b_rt_i[i]]
                        yield
                        act(lg, pp[:, 0:36], AF.Copy, [pb], R_)
                        yield
                        mxg = rt[:, 40:41]
                        yield
                        S.op("dve", lambda e, mxg=mxg: e.tensor_reduce(out=mxg, in_=rt[:, 0:4], axis=AX.X, op=ALU.max), reads=R_, writes=R_)
                        yield
                        ohg = rt[:, 44:48]
                        yield
                        ts("dve", ohg, rt[:, 0:4], mxg, ALU.is_equal, R_, R_)
                        yield
                        nmx = rt[:, 41:42]
                        yield
                        ts("dve", nmx, mxg, -1.0, ALU.mult, R_, R_)
                        yield
                        sumg = rt[:, 42:43]
                        yield
                        act(rt[:, 48:52], rt[:, 0:4], AF.Exp, R_, R_, bias=nmx, accum=sumg)
                        yield
                        gw = rt[:, 43:44]
                        yield
                        S.op("dve", lambda e, gw=gw, sumg=sumg: e.reciprocal(out=gw, in_=sumg), reads=R_, writes=R_)
                        yield
                        pen = rt[:, 52:56]
                        yield
                        ts("dve", pen, ohg, -1.0, ALU.add, R_, R_, s2=-NEG, op1=ALU.mult)
                        yield
                        lem = rt[:, 64:96]
                        yield
                        for g in range(4):
                            ts("dve", lem[:, g * 8:(g + 1) * 8], rt[:, 4 + g * 8:4 + (g + 1) * 8], pen[:, g:g + 1], ALU.add, R_, R_)
                        yield
                        m1 = rt[:, 56:57]
                        yield
                        S.op("dve", lambda e, m1=m1, lem=lem: e.tensor_reduce(out=m1, in_=lem, axis=AX.X, op=ALU.max), reads=R_, writes=R_)
                        yield
                        ts("dve", oh12[:, i, 0:32], lem, m1, ALU.is_equal, R_, [b_oh_i[i]])
                        yield
                        lem2 = rt[:, 96:128]
                        yield
                        stt("dve", lem2, oh12[:, i, 0:32], NEG, lem, ALU.mult, ALU.add, [b_oh_i[i], b_rt_i[i]], R_)
                        yield
                        m2 = rt[:, 57:58]
                        yield
                        S.op("dve", lambda e, m2=m2, lem2=lem2: e.tensor_reduce(out=m2, in_=lem2, axis=AX.X, op=ALU.max), reads=R_, writes=R_)
                        yield
                        ts("dve", oh12[:, i, 32:64], lem2, m2, ALU.is_equal, R_, [b_oh_i[i]])
                        yield
                        dd = rt[:, 58:59]
                        yield
                        tt("dve", dd, m2, m1, ALU.subtract, R_, R_)
                        yield
                        ed = rt[:, 59:60]
                        yield
                        act(ed, dd, AF.Exp, R_, R_)
                        yield
                        den = rt[:, 60:61]
                        yield
                        ts("dve", den, ed, 1.0, ALU.add, R_, R_)
                        yield
                        w1 = rt[:, 61:62]
                        yield
                        S.op("dve", lambda e, w1=w1, den=den: e.reciprocal(out=w1, in_=den), reads=R_, writes=R_)
                        yield
                        tt("dve", wts[:, i, 0:1], w1, gw, ALU.mult, R_, [b_oh_i[i]])
                        yield
                        w2 = rt[:, 62:63]
                        yield
                        tt("dve", w2, ed, w1, ALU.mult, R_, R_)
                        yield
                        tt("dve", wts[:, i, 1:2], w2, gw, ALU.mult, R_, [b_oh_i[i]])
                        yield
                        tt("dve", mohb[:, i, :], oh12[:, i, 0:32], oh12[:, i, 32:64], ALU.add, [b_oh_i[i]], [b_oh_i[i]])
                        yield


                    def run_interleaved(gens):
                        gens = list(gens)
                        while gens:
                            for g_ in list(gens):
                                try:
                                    next(g_)
                                except StopIteration:
                                    gens.remove(g_)

                    run_interleaved([rt_tile(i) for i in range(0, 8)])
                    run_interleaved([rt_tile(i) for i in range(8, NT)])
                    pc_, pcb = psbank()
                    for i in range(NT):
                        mm(pc_[:, 0:32], ones, mohb[:, i, :], i == 0, i == NT - 1, [b_const, b_oh_i[i]], [pcb])
                    Q_ = [b_route]
                    cp("dve", cnt, pc_[:, 0:32], [pcb], Q_)
                    ts("dve", padA, cnt, 1.0 / RB, ALU.mult, Q_, Q_, s2=(RB - 1.0) / RB - 0.5 + 0.5 / RB, op1=ALU.add)
                    cp("dve", padB.bitcast(I32), padA, Q_, Q_)
                    cp("dve", padA, padB.bitcast(I32), Q_, Q_)
                    ts("dve", padA, padA, float(RB), ALU.mult, Q_, Q_)
                    cp("dve", pstart, padA, Q_, Q_)
                    a, b_ = padA, padB
                    for s_ in (1, 2, 4, 8, 16):
                        cp("dve", b_[:, 0:s_], a[:, 0:s_], Q_, Q_)
                        tt("dve", b_[:, s_:32], a[:, s_:32], a[:, 0:32 - s_], ALU.add, Q_, Q_)
                        a, b_ = b_, a
                    cp("dve", pend, a, Q_, Q_)
                    tt("dve", pstart, pend, pstart, ALU.subtract, Q_, Q_)
                    def dest_tile(i):
                        rt = rtall[:, i, :]
                        yield
                        pp, pb = psbank()
                        yield
                        mm(pp[:, 0:32], ustr, mohb[:, i, :], True, i == 0, [b_const, b_oh_i[i]], [pb])
                        yield
                        for j in range(i):
                            mm(pp[:, 0:32], ones, mohb[:, j, :], False, j == i - 1, [b_const, b_oh_i[j]], [pb])
                        yield
                        tmp = rt[:, 128:160]
                        yield
                        tt("dve", tmp, pp[:, 0:32], pstart, ALU.add, [pb, b_route], [b_rt_i[i]])
                        yield
                        for k in range(2):
                            prod = rt[:, 160:192]
                            df = rt[:, 192 + k:193 + k]
                            tt("dve", prod, tmp, oh12[:, i, k * 32:(k + 1) * 32], ALU.mult, [b_rt_i[i], b_oh_i[i]], [b_rt_i[i]])
                            S.op("dve", lambda e, df=df, prod=prod: e.tensor_reduce(out=df, in_=prod, axis=AX.X, op=ALU.add), reads=[b_rt_i[i]], writes=[b_rt_i[i]])
                            cp("dve", desti[:, i, k:k + 1], df, [b_rt_i[i]], [b_dest_i[i]])
                        yield


                    run_interleaved([dest_tile(i) for i in range(0, 8)])
                    run_interleaved([dest_tile(i) for i in range(8, NT)])
                    S.op("dve", lambda e: e.memset(eblk, 0.0), writes=Q_)
                    for e_ in range(NEXP):
                        stt("dve", eblk, thr[:, 0:NBLK], pend[:, e_:e_ + 1], eblk, ALU.is_ge, ALU.add, [b_const] + Q_, Q_)
                    ts("dve", eblk, eblk, float(NEXP - 1), ALU.min, Q_, Q_, s2=float(li_ * NEXP), op1=ALU.add)
                    for c in range(8):
                        ts("dve", idxgu_f[:, :, c], eblk, float(D), ALU.mult, Q_, Q_, s2=pcv[:, c:c + 1], op1=ALU.add)
                    for c in range(6):
                        ts("dve", idxd_f[:, :, c], eblk, float(DEXP), ALU.mult, Q_, Q_, s2=pcv[:, c:c + 1], op1=ALU.add)
                    cp("dve", idxgu[:, :, :], idxgu_f[:, :, :], Q_, Q_)
                    cp("dve", idxd[:, :, 0:6], idxd_f[:, :, 0:6], Q_, Q_)
                    for i in range(NT):
                        for k in range(2):
                            S.dma("pool", "scat", lambda e, i=i, k=k: e.indirect_dma_start(
                                out=xb_d[:, :], out_offset=bass.IndirectOffsetOnAxis(ap=desti[:, i, k:k + 1], axis=0),
                                in_=hrows[:, i, :], in_offset=None), reads=[b_hr[i], b_dest_i[i]], writes=[b_xb])

                return load, compute

            steps.append(router_step())

            Gw = [HT[:, g * 6144:(g + 1) * 6144].rearrange("p (c n) -> p c n", c=8) for g in range(2)]
            xg_t = [HT[:, 12288 + i * 1024:12288 + (i + 1) * 1024] for i in range(2)]
            xgT_t = [HT[:, 14336 + i * 1024:14336 + (i + 1) * 1024].rearrange("p (c t) -> p c t", c=8) for i in range(2)]
            Uw = [YR[:, g * 6144:(g + 1) * 6144].rearrange("p (c n) -> p c n", c=8) for g in range(2)]
            ybt = [YR[:, 12288 + i * 2048:12288 + (i + 1) * 2048].bitcast(F32) for i in range(2)]
            offd = offm0 + (offm0 % 2)
            Dw = []
            for g in range(2):
                a, offd = carve(BIG, offd, 6 * D)
                Dw.append(a.rearrange("p (c n) -> p c n", c=6))
            sgt = U_pad[:, 2:2 + 2 * DEXP].bitcast(F32)
            hmt = hn_t[0][:, 0:DEXP]
            hmT = [hn_t[1][:, 0:DEXP].rearrange("p (c t) -> p c t", c=6)]
            a, offd = carve(BIG, offd, DEXP)
            hmT.append(a.rearrange("p (c t) -> p c t", c=6))
            assert offd <= NBIG, offd
            b_G = S.bufs(2, "Gw")
            b_U = S.bufs(2, "Uw")
            b_D = S.bufs(2, "Dw")
            b_xg = S.bufs(2, "xg")
            b_xgT = S.bufs(2, "xgT")
            b_ybt = S.bufs(2, "ybt")
            b_sg = S.buf("sg")
            b_hm = S.buf("hm")
            b_hmT = S.bufs(2, "hmT")
            weg = w_eg.rearrange("l e d n -> (l e d) n")
            weu = w_eu.rearrange("l e d n -> (l e d) n")
            wed = w_ed.rearrange("l e d n -> (l e d) n")

            def moe_blocks(slot, sbuf_):
                S.alias(b_G + b_xg + b_xgT, [hTb])
                S.alias(b_U + b_ybt, b_hr)
                S.alias(b_D + [b_hmT[1]], [b_big, b_route] + b_rt_i)
                S.alias([b_sg], [b_u])
                S.alias([b_hm, b_hmT[0]], hn_b)

                def issue_w(b):
                    p = b % 2
                    for c in range(8):
                        S.dma("pool", "G%d" % p, lambda e, c=c, p=p, b=b: e.indirect_dma_start(
                            out=Gw[p][:, c, :], out_offset=None, in_=weg,
                            in_offset=bass.IndirectOffsetOnAxis(ap=idxgu[:, b, c:c + 1], axis=0)), reads=[b_route], writes=[b_G[p]])
                    for c in range(8):
                        S.dma("pool", "U%d" % p, lambda e, c=c, p=p, b=b: e.indirect_dma_start(
                            out=Uw[p][:, c, :], out_offset=None, in_=weu,
                            in_offset=bass.IndirectOffsetOnAxis(ap=idxgu[:, b, c:c + 1], axis=0)), reads=[b_route], writes=[b_U[p]])
                    for c in range(6):
                        S.dma("pool", "D%d" % p, lambda e, c=c, p=p, b=b: e.indirect_dma_start(
                            out=Dw[p][:, c, :], out_offset=None, in_=wed,
                            in_offset=bass.IndirectOffsetOnAxis(ap=idxd[:, b, c:c + 1], axis=0)), reads=[b_route], writes=[b_D[p]])

                def issue_x(t):
                    q = t % 2
                    dma_sp("xg%d" % q, xg_t[q], xb_d[t * 128:(t + 1) * 128, :], reads=[b_xb], writes=[b_xg[q]])

                NTIL = NROW // 128
                SUB = RB // 128
                issue_w(0)
                issue_x(0)
                for t in range(NTIL):
                    b = t // SUB
                    p = b % 2
                    q = t % 2
                    if t % SUB == 0 and b + 1 < NBLK:
                        issue_w(b + 1)
                    if t + 1 < NTIL:
                        issue_x(t + 1)
                    pt, pb = psbank()
                    ptb = pt[:, :].bitcast(BF16).rearrange("p (c t) -> p c t", t=128)
                    for c in range(KC):
                        tr(ptb[:, c, :], xg_t[q][:, c * 128:(c + 1) * 128], [b_xg[q]], [pb])
                    for c in range(KC):
                        gc = gcols[:, g0 + GC_FFN + c:g0 + GC_FFN + c + 1]
                        if c % 2 == 0:
                            ts("dve", xgT_t[q][:, c, :], ptb[:, c, :], gc, ALU.mult, [pb, b_const], [b_xgT[q]])
                        else:
                            act(xgT_t[q][:, c, :], ptb[:, c, :], AF.Copy, [pb, b_const], [b_xgT[q]], scale=gc)
                    for (n0, nn) in ((0, 512), (512, 256)):
                        pg, pgb = psbank()
                        for c in range(KC):
                            mm(pg[:, 0:nn], xgT_t[q][:, c, :], Gw[p][:, c, n0:n0 + nn], c == 0, c == KC - 1, [b_xgT[q], b_G[p]], [pgb])
                        pu, pub = psbank()
                        for c in range(KC):
                            mm(pu[:, 0:nn], xgT_t[q][:, c, :], Uw[p][:, c, n0:n0 + nn], c == 0, c == KC - 1, [b_xgT[q], b_U[p]], [pub])
                        act(sgt[:, n0:n0 + nn], pg[:, 0:nn], AF.Silu, [pgb], [b_sg])
                        tt("dve", hmt[:, n0:n0 + nn], sgt[:, n0:n0 + nn], pu[:, 0:nn], ALU.mult, [b_sg, pub], [b_hm])
                    pt2, pb2 = psbank()
                    pt2b = pt2[:, :].bitcast(BF16).rearrange("p (c t) -> p c t", t=128)
                    for c in range(6):
                        tr(pt2b[:, c, :], hmt[:, c * 128:(c + 1) * 128], [b_hm], [pb2])
                    act(hmT[q][:, 0:3, :], pt2b[:, 0:3, :], AF.Copy, [pb2], [b_hmT[q]])
                    cp("dve", hmT[q][:, 3:6, :], pt2b[:, 3:6, :], [pb2], [b_hmT[q]])
                    for n in range(2):
                        py, pyb = psbank()
                        for c in range(6):
                            mm(py[:, :], hmT[q][:, c, :], Dw[p][:, c, n * 512:(n + 1) * 512], c == 0, c == 5, [b_hmT[q], b_D[p]], [pyb])
                        if n == 0:
                            act(ybt[q][:, 0:512], py[:, :], AF.Copy, [pyb], [b_ybt[q]])
                        else:
                            cp("dve", ybt[q][:, 512:1024], py[:, :], [pyb], [b_ybt[q]])
                    dma_sp("ybst", yb_d[t * 128:(t + 1) * 128, :], ybt[q], reads=[b_ybt[q]], writes=[b_yb])
                S.alias(b_gat, b_G + b_xg + b_xgT)
                for i in range(NT):
                    for k in range(2):
                        gi = (i * 2 + k) % 4
                        S.dma("pool", "gat%d" % gi, lambda e, i=i, k=k, gi=gi: e.indirect_dma_start(
                            out=gat_t[gi], out_offset=None, in_=yb_d[:, :],
                            in_offset=bass.IndirectOffsetOnAxis(ap=desti[:, i, k:k + 1], axis=0)), reads=[b_yb, b_dest_i[i]], writes=[b_gat[gi]])
                        stt("dve", X[:, i, :], gat_t[gi], wts[:, i, k:k + 1], X[:, i, :], ALU.mult, ALU.add, [b_gat[gi], b_oh_i[i], Xb[i]], [Xb[i]])

            gat_t = [HT[:, i * 2048:(i + 1) * 2048].bitcast(F32) for i in range(4)]
            b_gat = S.bufs(4, "gat")
            steps.append((None, moe_blocks))

            def moe_end(slot, sbuf_):
                S.alias([b_big], [b_rt, b_oh, b_route] + b_rt_i + b_oh_i + b_dest_i + b_D + b_hmT)
                S.alias([b_u], [b_sg])
                S.alias(hn_b, [b_hm, b_hmT[0]])
                S.alias([YRb], b_U + b_ybt + b_hr)
                S.alias([hTb], b_gat + b_G + b_xg + b_xgT)

            steps.append((None, moe_end))
            if stop_after == (l, "moe"):
                return True
            return False

        ztile = sb("ztile", [128, 512], BF16)
        b_zt = S.buf("zt")
        S.op("pool", lambda e: e.memset(ztile[:, :], 0.0), writes=[b_zt])
        U_pad = sb("U_pad", [128, S_LEN + 4], BF16)
        b_u = S.buf("u")
        b_xb = S.buf("xb_d")
        b_yb = S.buf("yb_d")
        S.op("pool", lambda e: e.memset(U_pad[:, :], 0.0), writes=[b_u])

        stopped = False
        for li_, l in enumerate(layer_ids):
            stopped = layer_steps(li_, l)
            if stopped:
                break

        def epilogue(slot, sbuf_):
            if final and not stopped:
                gfin, _ = carve(BIG, 0, D, F32)
                otile = [carve(BIG, 2 * D + i * 2 * D, D, F32)[0] for i in range(2)]
                b_gf = S.buf("gfin")
                b_ot = S.bufs(2, "otile")
                S.alias([b_gf] + b_ot, [b_big])
                dma_sp("c2", gfin, gfin_d[:, :], writes=[b_gf])
                rstd_tiles(lambda i: X[:, i, :], Xb, NT, 1.0 / D)
                for i in range(NT):
                    p = i % 2
                    act(otile[p], X[:, i, :], AF.Copy, [Xb[i], b_rstd], [b_ot[p]], scale=rstd[:, i:i + 1])
                    tt("dve", otile[p], otile[p], gfin, ALU.mult, [b_ot[p], b_gf], [b_ot[p]])
                    dma_sp("out", out_d[i * 128:(i + 1) * 128, :], otile[p], reads=[b_ot[p]], writes=[b_out])
            else:
                for i in range(NT):
                    dma_sp("out", out_d[i * 128:(i + 1) * 128, :], X[:, i, :], reads=[Xb[i]], writes=[b_out])

        b_out = S.buf("out")
        if max_steps is not None:
            del steps[max_steps:]
        steps.append((None, epilogue))

        wsteps = [k for k, (ld, _) in enumerate(steps) if ld is not None]
        issued = 0
        done = 0
        for k, (ld, cpf) in enumerate(steps):
            while issued < len(wsteps) and issued < done + NSLOT:
                sl = issued % NSLOT
                steps[wsteps[issued]][0](WS[sl], WSb[sl])
                issued += 1
            if ld is not None:
                sl = done % NSLOT
                cpf(WS[sl], WSb[sl])
                done += 1
            else:
                cpf(None, None)
        S.wait_all("sp", [b_out])
        if os.environ.get('WAITCS'):
            for b_ in dbg_bufs:
                S.wait_all("sp", [b_])
        S.finalize()
        dl = S.simulate()
        if dl is not None:
            raise RuntimeError("sync deadlock: %r" % (dl,))
        S.emit()
    return nc, S.ninst


def _consts():
    ident = np.eye(128, dtype=np.float32)
    ones = np.ones((128, 128), np.float32)
    rrot = np.zeros((128, 128), np.float32)
    for c in range(2):
        for i in range(8):
            rrot[c * 64 + 8 + i, c * 64 + i] = -1.0
            rrot[c * 64 + i, c * 64 + 8 + i] = 1.0
    ustr = np.triu(np.ones((128, 128), np.float32), 1)
    cmat = np.concatenate([ident, ones, rrot, ustr], axis=1)
    inv = np.float32(500000.0) ** (-np.arange(0, 16, 2, dtype=np.float32) / np.float32(16))
    ang = np.arange(S_LEN, dtype=np.float32)[:, None] * inv[None, :]
    cs, sn = np.cos(ang).astype(np.float32), np.sin(ang).astype(np.float32)
    cosT = np.ones((128, S_LEN), np.float32)
    sinT = np.zeros((128, S_LEN), np.float32)
    for c in range(2):
        for i in range(16):
            cosT[c * 64 + i] = cs[:, i % 8]
            sinT[c * 64 + i] = sn[:, i % 8]
    cossin = np.concatenate([cosT, sinT], axis=1)
    thr = np.broadcast_to((np.arange(64, dtype=np.float32) * float(RB))[None, :], (128, 64))
    pc = np.arange(8, dtype=np.float32)[None, :] * 128.0 + np.arange(128, dtype=np.float32)[:, None]
    cmisc = np.ascontiguousarray(np.concatenate([thr, pc], axis=1))
    return np.ascontiguousarray(cmat.astype(ml_dtypes.bfloat16)), np.ascontiguousarray(cossin.astype(ml_dtypes.bfloat16)), cmisc


def _pack_small(inp, layer_ids):
    cols = []
    for l in layer_ids:
        def colmaj(v):
            return np.asarray(v, np.float32).reshape(8, 128).T
        cols.append(colmaj(inp["norm_mix"][l]))
        cols.append(colmaj(inp["norm_cross"][l]))
        cols.append(colmaj(inp["norm_ffn"][l]))
        for k in range(3):
            cols.append(colmaj(inp["conv_w"][l][k]))
        cols.append(np.asarray(inp["subln"][l], np.float32).reshape(128, 1))
    cols.append(np.asarray(inp["norm_mem"], np.float32).reshape(8, 128).T)
    gcols = np.ascontiguousarray(np.concatenate(cols, axis=1))
    lam = []
    for l in layer_ids:
        lam.append(np.concatenate([inp["lambda_q1"][l], inp["lambda_k1"][l], inp["lambda_q2"][l], inp["lambda_k2"][l]]))
    lamv = np.ascontiguousarray(np.broadcast_to(np.concatenate(lam)[None, :].astype(np.float32), (128, len(layer_ids) * 256)))
    gfin = np.ascontiguousarray(np.broadcast_to(np.asarray(inp["norm_final"], np.float32)[None, :], (128, D)))
    return gcols, lamv, gfin


_PROG_CACHE = {}


def _run(inp, x_shards, layer_ids, final, stop_after=None, cores=None, max_steps=None):
    key = (tuple(layer_ids), final, stop_after, max_steps)
    if key not in _PROG_CACHE:
        _PROG_CACHE[key] = build(list(layer_ids), final, stop_after, max_steps)[0]
    nc = _PROG_CACHE[key]
    cmat, cossin, cmisc = _consts()
    gcols, lamv, gfin = _pack_small(inp, layer_ids)
    ls = list(layer_ids)
    sl = slice(ls[0], ls[-1] + 1)
    w_rt = np.ascontiguousarray(np.concatenate([inp["w_router_group"][sl], inp["w_router_expert"][sl]], axis=-1))
    shared = {
        "w_in": np.ascontiguousarray(inp["w_in"][sl]), "w_branch": np.ascontiguousarray(inp["w_branch"][sl]),
        "w_o": np.ascontiguousarray(inp["w_o"][sl]), "w_cq": np.ascontiguousarray(inp["w_cq"][sl]),
        "w_ckv": np.ascontiguousarray(inp["w_ckv"][sl]), "w_co": np.ascontiguousarray(inp["w_co"][sl]),
        "w_rt": w_rt, "w_exp_gate": np.ascontiguousarray(inp["w_exp_gate"][sl]),
        "w_exp_up": np.ascontiguousarray(inp["w_exp_up"][sl]), "w_exp_down": np.ascontiguousarray(inp["w_exp_down"][sl]),
        "gcols": gcols, "lamv": lamv, "gfinal": gfin, "cmat": cmat, "cossin": cossin, "cmisc": cmisc,
    }
    cores = list(range(len(x_shards))) if cores is None else cores
    in_maps = []
    for b in range(len(x_shards)):
        m = dict(shared)
        m["x"] = np.ascontiguousarray(x_shards[b])
        m["mem"] = np.ascontiguousarray(inp["mem"][b])
        in_maps.append(m)
    res = run_bass_kernel_spmd(nc, in_maps, core_ids=cores)
    return [r["out"] for r in res.results]


MODE = "fused"


def kernel(**inputs):
    inp = {k: np.asarray(v) for k, v in inputs.items()}
    xs = [inp["x"][b] for b in range(inp["x"].shape[0])]
    if MODE == "fused":
        outs = _run(inp, xs, list(range(DEPTH)), True)
    else:
        for l in range(DEPTH):
            xs = _run(inp, xs, [l], l == DEPTH - 1)
        outs = xs
    return np.stack(outs, axis=0).astype(np.float32)
```

```python
import math
import os
import numpy as np
import ml_dtypes
HEADCUT = int(os.environ.get('HEADCUT', '99'))
from contextlib import ExitStack
import concourse.bass as bass
import concourse.mybir as mybir
from concourse.bass_utils import run_bass_kernel_spmd

F32 = mybir.dt.float32
BF16 = mybir.dt.bfloat16
I32 = mybir.dt.int32
ALU = mybir.AluOpType
AF = mybir.ActivationFunctionType
AX = mybir.AxisListType

D = 1024
S_LEN = 2048
NT = 16
KC = 8
NMEM = 256
DEPTH = 4
NEXP = 32
DEXP = 768
NBLK = 48
RB = 256
NROW = NBLK * RB
EPS = 1e-6
NEG = -1.0e30


class Buf:
    __slots__ = ("name", "w", "r", "excl")

    def __init__(self, name):
        self.name = name
        self.w = None
        self.r = {}
        self.excl = False


class Sched:
    ENG = ("pe", "act", "dve", "pool", "sp")

    def __init__(self, nc, stack):
        self.nc = nc
        self.stack = stack
        self.rec = {e: [] for e in self.ENG}
        self.sem = {e: stack.enter_context(nc.semaphore("s_" + e)) for e in self.ENG}
        self.cnt = {e: 0 for e in self.ENG}
        self.seen = {e: {} for e in self.ENG}
        self.dsem = {}
        self.dcnt = {}
        self.nbuf = 0
        self.ninst = 0

    def buf(self, name=None):
        self.nbuf += 1
        return Buf(name or "b%d" % self.nbuf)

    def bufs(self, n, name="b"):
        return [self.buf("%s%d" % (name, i)) for i in range(n)]

    def _deps(self, eng, reads, writes, skipkey=None):
        deps = {}

        def add(k, v):
            if k == skipkey:
                return
            if deps.get(k, 0) < v:
                deps[k] = v

        for b in reads:
            if b.w is not None:
                add(*b.w)
            if b.excl:
                for k, v in b.r.items():
                    if k != ("e", eng):
                        add(k, v)
        for b in writes:
            if b.w is not None:
                add(*b.w)
            for k, v in b.r.items():
                add(k, v)
        out = []
        seen = self.seen[eng]
        for k, v in deps.items():
            if eng == "pe" and k == ("e", "pe"):
                continue
            if k[0] == "d":
                v = self.dcnt[k[1]]
            if seen.get(k, 0) >= v:
                continue
            seen[k] = v
            out.append((k, v))
        return out

    def _post(self, ev, reads, writes):
        k, v = ev
        for b in reads:
            if b.r.get(k, 0) < v:
                b.r[k] = v
        for b in writes:
            b.w = ev
            b.r = {}

    def op(self, eng, fn, reads=(), writes=()):
        waits = self._deps(eng, reads, writes)
        self.cnt[eng] += 1
        ev = (("e", eng), self.cnt[eng])
        self.rec[eng].append(("op", waits, fn, self.cnt[eng]))
        self._post(ev, reads, writes)
        self.ninst += 1
        return ev

    def dma(self, eng, key, fn, reads=(), writes=()):
        if key not in self.dsem:
            self.dsem[key] = self.stack.enter_context(self.nc.semaphore("d_" + str(key)))
            self.dcnt[key] = 0
        waits = self._deps(eng, reads, writes, skipkey=("d", key))
        self.dcnt[key] += 16
        ev = (("d", key), self.dcnt[key])
        self.rec[eng].append(("dma", waits, fn, key))
        self._post(ev, reads, writes)
        self.ninst += 1
        return ev

    def alias(self, new_bufs, old_bufs):
        for nb in new_bufs:
            for ob in old_bufs:
                if ob.w is not None:
                    k, v = ob.w
                    if nb.r.get(k, 0) < v:
                        nb.r[k] = v
                for k, v in ob.r.items():
                    if nb.r.get(k, 0) < v:
                        nb.r[k] = v

    def wait_all(self, eng, bufs):
        waits = self._deps(eng, bufs, bufs)
        self.rec[eng].append(("wait", waits, None, None))

    def finalize(self):
        waited = {e: set() for e in self.ENG}
        for eng in self.ENG:
            for kind, waits, fn, x in self.rec[eng]:
                for k, v in waits:
                    if k[0] == "e":
                        waited[k[1]].add(v)
        rank = {e: {v: i + 1 for i, v in enumerate(sorted(waited[e]))} for e in self.ENG}
        self.prog = {}
        for eng in self.ENG:
            pl = []
            for kind, waits, fn, x in self.rec[eng]:
                rw = []
                for k, v in waits:
                    if k[0] == "e":
                        rw.append((self.sem[k[1]], rank[k[1]][v]))
                    else:
                        rw.append((self.dsem[k[1]], v))
                if kind == "op":
                    pl.append((rw, fn, self.sem[eng] if x in waited[eng] else None, 1))
                elif kind == "dma":
                    pl.append((rw, fn, self.dsem[x], 16))
                else:
                    pl.append((rw, None, None, 0))
            self.prog[eng] = pl

    def simulate(self):
        val = {}
        pc = {e: 0 for e in self.ENG}
        prog = self.prog
        while True:
            progressed = False
            for e in self.ENG:
                while pc[e] < len(prog[e]):
                    waits, fn, sem, inc = prog[e][pc[e]]
                    if all(val.get(id(s_), 0) >= v for s_, v in waits):
                        if sem is not None:
                            val[id(sem)] = val.get(id(sem), 0) + inc
                        pc[e] += 1
                        progressed = True
                    else:
                        break
            if all(pc[e] == len(prog[e]) for e in self.ENG):
                return None
            if not progressed:
                return {e: (pc[e], len(prog[e])) for e in self.ENG}

    def emit(self):
        nc = self.nc
        prog = self.prog

        def run(e, pl):
            for waits, fn, sem, inc in pl:
                for s_, v in waits:
                    e.wait_ge(s_, v)
                if fn is not None:
                    ins = fn(e)
                    if sem is not None:
                        ins.then_inc(sem, inc)

        with nc.Block() as block:
            @block.tensor
            def _(e):
                run(e, prog["pe"])

            @block.scalar
            def _(e):
                run(e, prog["act"])

            @block.vector
            def _(e):
                run(e, prog["dve"])

            @block.gpsimd
            def _(e):
                run(e, prog["pool"])

            @block.sync
            def _(e):
                run(e, prog["sp"])


def lambda_init(l):
    return 0.8 - 0.6 * math.exp(-0.3 * l)


GC_MIX, GC_CROSS, GC_FFN, GC_CONV, GC_SUBLN, GC_PER = 0, 8, 16, 24, 48, 49


def build(layer_ids, final, stop_after=None, max_steps=None):
    L = len(layer_ids)
    nc = bass.Bass("TRN2", target_bir_lowering=False)

    def din(name, shape, dt=F32):
        return nc.dram_tensor(name, shape, dt, kind="ExternalInput").ap()

    x_in = din("x", [S_LEN, D])
    mem_in = din("mem", [NMEM, D])
    w_in = din("w_in", [L, D, 8192])
    w_br = din("w_branch", [L, 2, D, D])
    w_o = din("w_o", [L, D, D])
    w_cq = din("w_cq", [L, D, D])
    w_ckv = din("w_ckv", [L, D, 2 * D])
    w_co = din("w_co", [L, D, D])
    w_rt = din("w_rt", [L, D, 36])
    w_eg = din("w_exp_gate", [L, NEXP, D, DEXP])
    w_eu = din("w_exp_up", [L, NEXP, D, DEXP])
    w_ed = din("w_exp_down", [L, NEXP, DEXP, D])
    gcols_d = din("gcols", [128, L * GC_PER + 8])
    lamv_d = din("lamv", [128, L * 256])
    gfin_d = din("gfinal", [128, D])
    cmat_d = din("cmat", [128, 4 * 128], BF16)
    cossin_d = din("cossin", [128, 2 * S_LEN], BF16)
    cmisc_d = din("cmisc", [128, 64 + 8])
    out_d = nc.dram_tensor("out", [S_LEN, D], F32, kind="ExternalOutput").ap()
    xb_d = nc.dram_tensor("xb_scr", [NROW, D], BF16, kind="Internal").ap()
    yb_d = nc.dram_tensor("yb_scr", [NROW, D], F32, kind="Internal").ap()

    st = ExitStack()
    with st:
        S = Sched(nc, st)

        def sb(name, shape, dt):
            return st.enter_context(nc.sbuf_tensor("sb_" + name, shape, dt))

        X = sb("X", [128, NT, D], F32)
        HT = sb("HT", [128, KC * S_LEN], BF16)
        YR = sb("YR", [128, KC * S_LEN], BF16)
        NBIG = 19 * 1024
        BIG = sb("BIG", [128, NBIG], BF16)
        NSLOT = 3
        WS = [sb("WS%d" % i, [128, 4096], BF16) for i in range(NSLOT)]
        cmat = sb("cmat", [128, 4 * 128], BF16)
        gcols = sb("gcols", [128, L * GC_PER + 8], F32)
        cmisc = sb("cmisc", [128, 72], F32)
        memT = sb("memT", [128, KC, NMEM], BF16)
        stat = sb("stat", [128, 64], F32)
        lamc = sb("lamc", [128, 2 * L], F32)
        sublnS = sb("sublnS", [128, L], F32)
        psall = st.enter_context(nc.psum_tensor("psall", [128, 8 * 512], F32))
        PS = [psall[:, i * 512:(i + 1) * 512] for i in range(8)]
        PSB = S.bufs(8, "ps")
        for b_ in PSB:
            b_.excl = True

        ident = cmat[:, 0:128]
        ones = cmat[:, 128:256]
        rrot = cmat[:, 256:384]
        ustr = cmat[:, 384:512]
        thr = cmisc[:, 0:64]
        pcv = cmisc[:, 64:72]

        Xb = S.bufs(NT, "X")
        hTb = S.buf("hT")
        YRb = S.buf("YR")
        WSb = S.bufs(NSLOT, "WS")
        b_const = S.buf("const")
        b_memT = S.buf("memT")
        b_stat = S.buf("stat")

        hT = HT[:, :].rearrange("p (c t) -> p c t", c=KC)

        def carve(region, off, n, dt=BF16):
            if dt == BF16:
                return region[:, off:off + n], off + n
            assert off % 2 == 0
            return region[:, off:off + 2 * n].bitcast(dt), off + 2 * n

        psrr = [0]

        def psbank():
            i = psrr[0] % 8
            psrr[0] += 1
            return PS[i], PSB[i]

        def dma_sp(key, out, in_, reads=(), writes=()):
            return S.dma("sp", key, lambda e: e.dma_start(out=out, in_=in_), reads=reads, writes=writes)

        def dma_pool(key, out, in_, reads=(), writes=()):
            return S.dma("pool", key, lambda e: e.dma_start(out=out, in_=in_), reads=reads, writes=writes)

        def mm(out, lhsT, rhs, start, stop, reads, writes):
            S.op("pe", lambda e: e.matmul(out, lhsT=lhsT, rhs=rhs, start=start, stop=stop),
                 reads=reads, writes=writes)

        def tr(out, in_, reads, writes):
            S.op("pe", lambda e: e.transpose(out=out, in_=in_, identity=ident), reads=list(reads) + [b_const], writes=writes)

        def act(out, in_, func, reads, writes, scale=1.0, bias=None, accum=None):
            kw = {}
            if bias is not None:
                kw["bias"] = bias
            if accum is not None:
                kw["accum_out"] = accum
            S.op("act", lambda e: e.activation(out=out, in_=in_, func=func, scale=scale, **kw), reads=reads, writes=writes)

        def tt(eng, out, in0, in1, op, reads, writes):
            S.op(eng, lambda e: e.tensor_tensor(out=out, in0=in0, in1=in1, op=op), reads=reads, writes=writes)

        def ts(eng, out, in0, s1, op0, reads, writes, s2=None, op1=None, accum=None):
            kw = {}
            if accum is not None:
                kw["accum_out"] = accum
            if op1 is None:
                S.op(eng, lambda e: e.tensor_scalar(out=out, in0=in0, scalar1=s1, scalar2=None, op0=op0, **kw), reads=reads, writes=writes)
            else:
                S.op(eng, lambda e: e.tensor_scalar(out=out, in0=in0, scalar1=s1, scalar2=s2, op0=op0, op1=op1, **kw), reads=reads, writes=writes)

        def stt(eng, out, in0, scalar, in1, op0, op1, reads, writes):
            S.op(eng, lambda e: e.scalar_tensor_tensor(out=out, in0=in0, scalar=scalar, in1=in1, op0=op0, op1=op1), reads=reads, writes=writes)

        def cp(eng, out, in_, reads, writes):
            S.op(eng, lambda e: e.tensor_copy(out=out, in_=in_), reads=reads, writes=writes)

        dma_sp("c0", cmat[:, :], cmat_d[:, :], writes=[b_const])
        dma_sp("c1", gcols[:, :], gcols_d[:, :], writes=[b_const])
        dma_sp("c1", cmisc[:, :], cmisc_d[:, :], writes=[b_const])
        for i in range(NT):
            dma_sp("xin", X[:, i, :], x_in[i * 128:(i + 1) * 128, :], writes=[Xb[i]])

        b_big = S.buf("bigscratch")
        lamv, _ = carve(BIG, 0, L * 256, F32)
        ltmp, _ = carve(BIG, 2 * L * 256, 64, F32)
        dma_sp("c3", lamv, lamv_d[:, :], writes=[b_big])
        for li_, l in enumerate(layer_ids):
            base = li_ * 256
            for j in range(2):
                S.op("dve", lambda e, base=base, j=j: e.tensor_tensor(out=ltmp, in0=lamv[:, base + j * 128:base + j * 128 + 64],
                                                                      in1=lamv[:, base + j * 128 + 64:base + j * 128 + 128], op=ALU.mult),
                     reads=[b_big], writes=[b_big])
                S.op("dve", lambda e, j=j: e.tensor_reduce(out=stat[:, j:j + 1], in_=ltmp, axis=AX.X, op=ALU.add),
                     reads=[b_big], writes=[b_stat])
            act(stat[:, 2:4], stat[:, 0:2], AF.Exp, [b_stat], [b_stat])
            tt("dve", stat[:, 4:5], stat[:, 3:4], stat[:, 2:3], ALU.subtract, [b_stat], [b_stat])
            ts("dve", lamc[:, li_:li_ + 1], stat[:, 4:5], -lambda_init(l), ALU.add, [b_stat], [b_const])
            ts("dve", sublnS[:, li_:li_ + 1], gcols[:, li_ * GC_PER + GC_SUBLN:li_ * GC_PER + GC_SUBLN + 1],
               1.0 - lambda_init(l), ALU.mult, [b_const], [b_const])

        ssq = sb("ssq", [128, NT], F32)
        rstd = sb("rstd", [128, NT], F32)
        b_ssq = S.buf("ssq")
        b_rstd = S.buf("rstd")
        hn_t = [sb("hn%d" % i, [128, D], BF16) for i in range(2)]
        hn_b = S.bufs(2, "hn")

        def rstd_tiles(src_ap_fn, src_bufs, ntiles, inv_n):
            for i in range(ntiles):
                act(hn_t[0][:, :], src_ap_fn(i), AF.Square, [src_bufs[i]], [hn_b[0], b_ssq], accum=ssq[:, i:i + 1])
            ts("dve", rstd[:, 0:ntiles], ssq[:, 0:ntiles], inv_n, ALU.mult, [b_ssq], [b_rstd], s2=EPS, op1=ALU.add)
            act(rstd[:, 0:ntiles], rstd[:, 0:ntiles], AF.Ln, [b_rstd], [b_rstd])
            act(rstd[:, 0:ntiles], rstd[:, 0:ntiles], AF.Exp, [b_rstd], [b_rstd], scale=-0.5)

        def norm_to_fm(src_ap_fn, src_bufs, ntiles, gcol0, dstT, dst_buf, keep_rows=None):
            rstd_tiles(src_ap_fn, src_bufs, ntiles, 1.0 / D)
            for i in range(ntiles):
                if keep_rows is None:
                    hn, hb = hn_t[i % 2], hn_b[i % 2]
                    hn_ap = hn[:, :]
                else:
                    hn_ap, hb = keep_rows(i)
                act(hn_ap, src_ap_fn(i), AF.Copy, [src_bufs[i], b_rstd], [hb], scale=rstd[:, i:i + 1])
                pt, pb = psbank()
                ptb = pt[:, :].bitcast(BF16).rearrange("p (c t) -> p c t", t=128)
                for c in range(KC):
                    tr(ptb[:, c, :], hn_ap[:, c * 128:(c + 1) * 128], [hb], [pb])
                for c in range(KC):
                    eng = "dve" if c % 2 == 0 else "act"
                    if eng == "dve":
                        ts("dve", dstT[:, c, i * 128:(i + 1) * 128], ptb[:, c, :], gcols[:, gcol0 + c:gcol0 + c + 1], ALU.mult,
                           [pb, b_const], [dst_buf])
                    else:
                        act(dstT[:, c, i * 128:(i + 1) * 128], ptb[:, c, :], AF.Copy, [pb, b_const], [dst_buf],
                            scale=gcols[:, gcol0 + c:gcol0 + c + 1])

        memrows, _ = carve(BIG, 4096, 2 * D, F32)
        memrows = memrows.rearrange("p (i d) -> p i d", i=2)
        b_memrows = S.bufs(2, "memrows")
        for i in range(2):
            dma_sp("c2", memrows[:, i, :], mem_in[i * 128:(i + 1) * 128, :], writes=[b_memrows[i]])
        norm_to_fm(lambda i: memrows[:, i, :], b_memrows, 2, L * GC_PER, memT, b_memT)

        dbg_bufs = []
        steps = []

        def wload(slot, sbuf_, pieces):
            key = "w%d" % [i for i in range(NSLOT) if WS[i] is slot][0]
            for dst, src in pieces:
                dma_pool(key, dst(slot), src, writes=[sbuf_])

        def w3(slot, off, ncols):
            return slot[:, off:off + KC * ncols].rearrange("p (c n) -> p c n", c=KC)

        def dram_cols(w2d, c0, ncols):
            return w2d[:, c0:c0 + ncols].rearrange("(c p) n -> p c n", p=128)

        def layer_steps(li_, l):
            g0 = li_ * GC_PER
            win_l = w_in[li_]
            off = 0
            cosb, off = carve(BIG, off, S_LEN)
            sinb, off = carve(BIG, off, S_LEN)
            qT, off = carve(BIG, off, S_LEN)
            kT, off = carve(BIG, off, S_LEN)
            vtm, off = carve(BIG, off, NT * 128)
            vtm = vtm.rearrange("p (i e) -> p i e", i=NT)
            NE = 3
            Et2 = []
            for _ in range(NE):
                a, off = carve(BIG, off, 1024)
                Et2.append(a)
            Et = [a[:, 0:512] for a in Et2]
            NTMP = 6
            Tm = []
            for _ in range(NTMP):
                a, off = carve(BIG, off, 512, F32)
                Tm.append(a)
            assert off <= NBIG, off
            b_cs = S.buf("cossin")
            dbg_bufs.append(b_cs)
            b_q = S.buf("qT")
            b_k = S.buf("kT")
            b_v = S.buf("v")
            Eb = S.bufs(NE, "E")
            Tb = S.bufs(NTMP, "T")
            rrE = [0]
            rrT = [0]

            def getE():
                i = rrE[0] % NE
                rrE[0] += 1
                return Et[i], Eb[i]

            def getE2():
                i = rrE[0] % NE
                rrE[0] += 1
                return Et2[i], Eb[i]

            def getT():
                i = rrT[0] % (NTMP - 2)
                rrT[0] += 1
                return Tm[i], Tb[i]

            Esum = [(Tm[NTMP - 2], Tb[NTMP - 2]), (Tm[NTMP - 1], Tb[NTMP - 1])]

            yT = YR[:, :].rearrange("p (c t) -> p c t", c=KC)
            b_y = [S.buf("y%d" % c) for c in range(KC)]

            def mixer_begin(slot, sbuf_):
                S.alias([b_cs, b_q, b_k, b_v] + Eb + Tb, [b_big])
                S.alias(b_y, [YRb])
                dma_sp("cs", cosb, cossin_d[:, 0:S_LEN], writes=[b_cs])
                dma_sp("cs", sinb, cossin_d[:, S_LEN:2 * S_LEN], writes=[b_cs])
                if os.environ.get('CSTOUCH'):
                    cp("dve", stat[:, 60:61], cosb[:, 0:1], [b_cs], [b_stat])
                norm_to_fm(lambda i: X[:, i, :], Xb, NT, g0 + GC_MIX, hT, hTb)
                for r in range(NROW // 128):
                    for hh in range(2):
                        dma_sp("xbz", xb_d[r * 128:(r + 1) * 128, hh * 512:(hh + 1) * 512], ztile[:, :], reads=[b_zt], writes=[b_xb])

            steps.append((None, mixer_begin))

            def head_step(h):
                def load(slot, sbuf_):
                    wload(slot, sbuf_, [
                        (lambda s: w3(s, 0, 128), dram_cols(win_l, h * 128, 128)),
                        (lambda s: w3(s, 1024, 128), dram_cols(win_l, 1024 + h * 128, 128)),
                        (lambda s: w3(s, 2048, 128), dram_cols(win_l, 2048 + h * 128, 128)),
                    ])

                def compute(slot, sbuf_):
                    wq = w3(slot, 0, 128)
                    wk = w3(slot, 1024, 128)
                    wv = w3(slot, 2048, 128)
                    def rot_tail(pp, pb, qb, qbb, tsl, dst, db):
                        p2, p2b = psbank()
                        mm(p2[:, :], rrot, qb, True, True, [b_const, qbb], [p2b])
                        t1, t1b = getT()
                        tt("dve", t1, pp[:, :], cosb[:, tsl], ALU.mult, [pb, b_cs], [t1b])
                        t2, t2b = getT()
                        tt("dve", t2, p2[:, :], sinb[:, tsl], ALU.mult, [p2b, b_cs], [t2b])
                        tt("pool", dst[:, tsl], t1, t2, ALU.add, [t1b, t2b], [db])

                    prev = None
                    for (wt, dst, db) in ((wq, qT, b_q), (wk, kT, b_k)):
                        for tb in range(4):
                            tsl = slice(tb * 512, (tb + 1) * 512)
                            pp, pb = psbank()
                            for c in range(KC):
                                mm(pp[:, :], wt[:, c, :], hT[:, c, tsl], c == 0, c == KC - 1, [sbuf_, hTb], [pb])
                            qb, qbb = getE()
                            act(qb, pp[:, :], AF.Copy, [pb], [qbb])
                            if prev is not None:
                                rot_tail(*prev)
                            prev = (pp, pb, qb, qbb, tsl, dst, db)
                    rot_tail(*prev)
                    if HEADCUT < 2:
                        return
                    for i4 in range(4):
                        pp, pb = psbank()
                        for ii in range(4):
                            i = i4 * 4 + ii
                            for c in range(KC):
                                mm(pp[:, ii * 128:(ii + 1) * 128], hT[:, c, i * 128:(i + 1) * 128], wv[:, c, :],
                                   c == 0, c == KC - 1, [sbuf_, hTb], [pb])
                        act(vtm[:, i4 * 4:(i4 + 1) * 4, :], pp[:, :].rearrange("p (i e) -> p i e", i=4), AF.Copy, [pb], [b_v])
                    if HEADCUT < 3:
                        return
                    pending = [None]
                    for qb_ in range(4):
                        qsl = slice(qb_ * 512, (qb_ + 1) * 512)
                        pO = [(PS[4], PSB[4]), (PS[5], PSB[5])]
                        pR = [(PS[6], PSB[6]), (PS[7], PSB[7])]
                        LA = 2
                        Spair = [(0, 1), (2, 3)]
                        Es = {}
                        for i in range(NT + LA):
                            if i < NT:
                                kb = i
                                ba, bb = Spair[i % 2]
                                for c, bk in ((0, ba), (1, bb)):
                                    mm(PS[bk][:, :], kT[c * 64:(c + 1) * 64, kb * 128:(kb + 1) * 128], qT[c * 64:(c + 1) * 64, qsl],
                                       True, True, [b_k, b_q], [PSB[bk]])
                                E2, E2b = getE2()
                                act(E2, psall[:, ba * 512:(ba + 2) * 512], AF.Exp, [PSB[ba], PSB[bb]], [E2b], scale=0.125)
                                Es[i] = (E2, E2b)
                            j = i - LA
                            if j >= 0:
                                kb = j
                                E2, E2b = Es.pop(j)
                                for c in range(2):
                                    E = E2[:, c * 512:(c + 1) * 512]
                                    mm(pO[c][0][:, :], vtm[:, kb, :], E, kb == 0, kb == NT - 1, [b_v, E2b], [pO[c][1]])
                                    es_, esb_ = Esum[c]
                                    eng_ = "dve" if c == 0 else "pool"
                                    if kb == 0:
                                        cp(eng_, es_, E, [E2b], [esb_])
                                    else:
                                        tt(eng_, es_, es_, E, ALU.add, [esb_, E2b], [esb_])
                            if i == 5 and pending[0] is not None:
                                pending[0]()
                                pending[0] = None
                        ri = []
                        for c in range(2):
                            e16, e16b = getE()
                            act(e16, Esum[c][0], AF.Copy, [Esum[c][1]], [e16b])
                            mm(pR[c][0][:, :], ones, e16, True, True, [b_const, e16b], [pR[c][1]])
                        for c in range(2):
                            r_, rb_ = getT()
                            act(r_, pR[c][0][:, :], AF.Ln, [pR[c][1]], [rb_])
                            act(r_, r_, AF.Exp, [rb_], [rb_], scale=-1.0)
                            ri.append((r_, rb_))
                        t0, t0b = getT()
                        tt("dve", t0, pO[0][0][:, :], ri[0][0], ALU.mult, [pO[0][1], ri[0][1]], [t0b])
                        t1, t1b = getT()
                        tt("dve", t1, pO[1][0][:, :], ri[1][0], ALU.mult, [pO[1][1], ri[1][1]], [t1b])
                        o_, ob_ = getT()
                        stt("dve", o_, t1, lamc[:, li_:li_ + 1], t0, ALU.mult, ALU.add, [t1b, t0b, b_const], [ob_])
                        sqf, sqb = getT()
                        sq = sqf.bitcast(BF16)[:, 0:512]
                        tt("pool", sq, o_, o_, ALU.mult, [ob_], [sqb])

                        def fin2(o_=o_, ob_=ob_, sq=sq, sqb=sqb, qsl=qsl):
                            pq, pqb = (PS[6], PSB[6])
                            mm(pq[:, :], ones, sq, True, True, [b_const, sqb], [pqb])
                            rs, rsb = getT()
                            ts("dve", rs, pq[:, :], 1.0 / 128, ALU.mult, [pqb], [rsb], s2=EPS, op1=ALU.add)
                            act(rs, rs, AF.Ln, [rsb], [rsb])
                            act(rs, rs, AF.Exp, [rsb], [rsb], scale=-0.5)
                            stt("dve", yT[:, h, qsl], o_, sublnS[:, li_:li_ + 1], rs, ALU.mult, ALU.mult, [ob_, rsb, b_const], [b_y[h]])

                        pending[0] = fin2
                    if pending[0] is not None:
                        pending[0]()
                        pending[0] = None

                return load, compute

            psS = [(PS[i], PSB[i]) for i in range(4)]

            for h in range(8):
                steps.append(head_step(h))

            offc = 0
            Mreg, offc = carve(BIG, offc, KC * S_LEN)
            Mv = Mreg.rearrange("p (c t) -> p c t", c=KC)
            CT = []
            for _ in range(3):
                a, offc = carve(BIG, offc, 512, F32)
                CT.append(a)
            assert offc <= NBIG
            b_M = [S.buf("M%d" % c) for c in range(KC)]
            CTb = S.bufs(3, "CT")
            rrC = [0]

            def getC():
                i = rrC[0] % 3
                rrC[0] += 1
                return CT[i], CTb[i]

            def phaseC_begin(slot, sbuf_):
                S.alias(b_M + CTb, [b_cs, b_q, b_k, b_v] + Eb + Tb)

            steps.append((None, phaseC_begin))

            def branch_step(n, j):
                def load(slot, sbuf_):
                    wload(slot, sbuf_, [
                        (lambda s: w3(s, 0, 128), dram_cols(w_br[li_, n], j * 128, 128)),
                        (lambda s: w3(s, 1024, 128), dram_cols(win_l, 6144 + n * 1024 + j * 128, 128)),
                    ])

                def compute(slot, sbuf_):
                    wb = w3(slot, 0, 128)
                    wg = w3(slot, 1024, 128)
                    for tb in range(4):
                        tsl = slice(tb * 512, (tb + 1) * 512)
                        pg, pgb = psbank()
                        for c in range(KC):
                            mm(pg[:, :], wg[:, c, :], hT[:, c, tsl], c == 0, c == KC - 1, [sbuf_, hTb], [pgb])
                        pbr, pbrb = psbank()
                        for c in range(KC):
                            mm(pbr[:, :], wb[:, c, :], yT[:, c, tsl], c == 0, c == KC - 1, [sbuf_, b_y[c]], [pbrb])
                        sg, sgb = getC()
                        act(sg, pg[:, :], AF.Sigmoid, [pgb], [sgb])
                        if n == 0:
                            tt("dve", Mv[:, j, tsl], sg, pbr[:, :], ALU.mult, [sgb, pbrb], [b_M[j]])
                        else:
                            t_, tb_ = getC()
                            tt("dve", t_, sg, pbr[:, :], ALU.mult, [sgb, pbrb], [tb_])
                            tt("pool", Mv[:, j, tsl], Mv[:, j, tsl], t_, ALU.add, [b_M[j], tb_], [b_M[j]])

                return load, compute

            for j in range(KC):
                steps.append(branch_step(0, j))


            def conv_step(j):
                def load(slot, sbuf_):
                    wload(slot, sbuf_, [
                        (lambda s: w3(s, 0, 128), dram_cols(win_l, 3072 + j * 128, 128)),
                        (lambda s: w3(s, 1024, 128), dram_cols(win_l, 4096 + j * 128, 128)),
                        (lambda s: w3(s, 2048, 128), dram_cols(win_l, 5120 + j * 128, 128)),
                    ])

                def compute(slot, sbuf_):
                    wcb = w3(slot, 0, 128)
                    wcc = w3(slot, 1024, 128)
                    wcx = w3(slot, 2048, 128)
                    u = U_pad
                    for tb in range(4):
                        tsl = slice(tb * 512, (tb + 1) * 512)
                        pc_, pcb = psbank()
                        for c in range(KC):
                            mm(pc_[:, :], wcc[:, c, :], hT[:, c, tsl], c == 0, c == KC - 1, [sbuf_, hTb], [pcb])
                        px, pxb = psbank()
                        for c in range(KC):
                            mm(px[:, :], wcx[:, c, :], hT[:, c, tsl], c == 0, c == KC - 1, [sbuf_, hTb], [pxb])
                        t_, tb_ = getC()
                        act(t_, pc_[:, :], AF.Copy, [pcb], [tb_])
                        tt("dve", u[:, 1 + tb * 512:1 + (tb + 1) * 512], t_, px[:, :], ALU.mult, [tb_, pxb], [b_u])
                    cw = g0 + GC_CONV
                    for tb in range(4):
                        tsl = slice(tb * 512, (tb + 1) * 512)
                        acc, accb = getC()
                        act(acc, u[:, 1 + tb * 512:1 + (tb + 1) * 512], AF.Copy, [b_u, b_const], [accb], scale=gcols[:, cw + 8 + j:cw + 8 + j + 1])
                        stt("dve", acc, u[:, tb * 512:(tb + 1) * 512], gcols[:, cw + j:cw + j + 1], acc, ALU.mult, ALU.add, [b_u, accb, b_const], [accb])
                        stt("dve", acc, u[:, 2 + tb * 512:2 + (tb + 1) * 512], gcols[:, cw + 16 + j:cw + 16 + j + 1], acc, ALU.mult, ALU.add, [b_u, accb, b_const], [accb])
                        pb_, pbb = psbank()
                        for c in range(KC):
                            mm(pb_[:, :], wcb[:, c, :], hT[:, c, tsl], c == 0, c == KC - 1, [sbuf_, hTb], [pbb])
                        tt("dve", yT[:, j, tsl], pb_[:, :], acc, ALU.mult, [pbb, accb], [b_y[j]])

                return load, compute

            for j in range(KC):
                steps.append(conv_step(j))
            for j in range(KC):
                steps.append(branch_step(1, j))

            def out_proj_step(wmat, half, srcT, src_bufs):
                def load(slot, sbuf_):
                    wload(slot, sbuf_, [(lambda s: w3(s, 0, 512), dram_cols(wmat, half * 512, 512))])

                def compute(slot, sbuf_):
                    wt = w3(slot, 0, 512)
                    for i in range(NT):
                        pp, pb = psbank()
                        for c in range(KC):
                            mm(pp[:, :], srcT[:, c, i * 128:(i + 1) * 128], wt[:, c, :], c == 0, c == KC - 1, [sbuf_, src_bufs[c]], [pb])
                        tt("dve", X[:, i, half * 512:(half + 1) * 512], X[:, i, half * 512:(half + 1) * 512], pp[:, :], ALU.add,
                           [Xb[i], pb], [Xb[i]])

                return load, compute

            for half in range(2):
                steps.append(out_proj_step(w_o[li_], half, Mv, b_M))

            def mixer_end(slot, sbuf_):
                S.alias([b_big], b_M + CTb)
                S.alias([YRb], b_y)

            steps.append((None, mixer_end))
            if stop_after == (l, "mix"):
                return True

            offx = 0
            kcT, offx = carve(BIG, offx, KC * NMEM)
            kcT = kcT.rearrange("p (c m) -> p c m", c=KC)
            vc, offx = carve(BIG, offx, 2 * D)
            vc = vc.rearrange("p (i d) -> p i d", i=2)
            XE = []
            for _ in range(6):
                a, offx = carve(BIG, offx, 512)
                XE.append(a)
            XT = []
            for _ in range(4):
                a, offx = carve(BIG, offx, 512, F32)
                XT.append(a)
            assert offx <= NBIG
            b_kc = S.buf("kcT")
            b_vc = S.buf("vc")
            XEb = S.bufs(6, "XE")
            XTb = S.bufs(4, "XT")
            rrx = [0, 0]

            def getXE():
                i = rrx[0] % 6
                rrx[0] += 1
                return XE[i], XEb[i]

            def getXT():
                i = rrx[1] % 4
                rrx[1] += 1
                return XT[i], XTb[i]

            qcT = YR[:, :].rearrange("p (c t) -> p c t", c=KC)
            b_qc = [S.buf("qc%d" % c) for c in range(KC)]
            ocT = HT[:, :].rearrange("p (c t) -> p c t", c=KC)
            b_oc = [S.buf("oc%d" % c) for c in range(KC)]

            def cross_begin(slot, sbuf_):
                S.alias([b_kc, b_vc] + XEb + XTb, [b_big])
                S.alias(b_qc, [YRb])
                norm_to_fm(lambda i: X[:, i, :], Xb, NT, g0 + GC_CROSS, hT, hTb)

            steps.append((None, cross_begin))

            def ckv_step(part):
                def load(slot, sbuf_):
                    wload(slot, sbuf_, [(lambda s: w3(s, 0, 512), dram_cols(w_ckv[li_], part * 512, 512))])

                def compute(slot, sbuf_):
                    wt = w3(slot, 0, 512)
                    if part < 2:
                        for jj in range(4):
                            j = part * 4 + jj
                            pp, pb = psbank()
                            for c in range(KC):
                                mm(pp[:, 0:NMEM], wt[:, c, jj * 128:(jj + 1) * 128], memT[:, c, :], c == 0, c == KC - 1, [sbuf_, b_memT], [pb])
                            act(kcT[:, j, :], pp[:, 0:NMEM], AF.Copy, [pb], [b_kc])
                    else:
                        for i in range(2):
                            pp, pb = psbank()
                            for c in range(KC):
                                mm(pp[:, :], memT[:, c, i * 128:(i + 1) * 128], wt[:, c, :], c == 0, c == KC - 1, [sbuf_, b_memT], [pb])
                            act(vc[:, i, (part - 2) * 512:(part - 1) * 512], pp[:, :], AF.Copy, [pb], [b_vc])

                return load, compute

            for part in range(4):
                steps.append(ckv_step(part))

            def cq_step(half):
                def load(slot, sbuf_):
                    wload(slot, sbuf_, [(lambda s: w3(s, 0, 512), dram_cols(w_cq[li_], half * 512, 512))])

                def compute(slot, sbuf_):
                    wt = w3(slot, 0, 512)
                    for jj in range(4):
                        j = half * 4 + jj
                        for tb in range(4):
                            tsl = slice(tb * 512, (tb + 1) * 512)
                            pp, pb = psbank()
                            for c in range(KC):
                                mm(pp[:, :], wt[:, c, jj * 128:(jj + 1) * 128], hT[:, c, tsl], c == 0, c == KC - 1, [sbuf_, hTb], [pb])
                            if (jj + tb) % 2 == 0:
                                act(qcT[:, j, tsl], pp[:, :], AF.Copy, [pb], [b_qc[j]])
                            else:
                                cp("dve", qcT[:, j, tsl], pp[:, :], [pb], [b_qc[j]])

                return load, compute

            for half in range(2):
                steps.append(cq_step(half))

            def cross_attn(slot, sbuf_):
                S.alias(b_oc, [hTb])
                for h in range(4):
                    for tb in range(4):
                        tsl = slice(tb * 512, (tb + 1) * 512)
                        pO = [psbank(), psbank()]
                        pR = psbank()
                        for mb in range(2):
                            pS, pSb = psbank()
                            for cc in range(2):
                                mm(pS[:, :], kcT[:, 2 * h + cc, mb * 128:(mb + 1) * 128], qcT[:, 2 * h + cc, tsl], cc == 0, cc == 1,
                                   [b_kc, b_qc[2 * h + cc]], [pSb])
                            E, Eb_ = getXE()
                            act(E, pS[:, :], AF.Exp, [pSb], [Eb_], scale=1.0 / 16)
                            for ee in range(2):
                                mm(pO[ee][0][:, :], vc[:, mb, h * 256 + ee * 128:h * 256 + (ee + 1) * 128], E, mb == 0, mb == 1,
                                   [b_vc, Eb_], [pO[ee][1]])
                            mm(pR[0][:, :], ones, E, mb == 0, mb == 1, [b_const, Eb_], [pR[1]])
                        r_, rb_ = getXT()
                        S.op("dve", lambda e, r_=r_, pR=pR: e.reciprocal(out=r_, in_=pR[0][:, :]), reads=[pR[1]], writes=[rb_])
                        for ee in range(2):
                            tt("dve", ocT[:, 2 * h + ee, tsl], pO[ee][0][:, :], r_, ALU.mult, [pO[ee][1], rb_], [b_oc[2 * h + ee]])

            steps.append((None, cross_attn))
            for half in range(2):
                steps.append(out_proj_step(w_co[li_], half, ocT, b_oc))

            def cross_end(slot, sbuf_):
                S.alias([b_big], [b_kc, b_vc] + XEb + XTb)
                S.alias([YRb], b_qc)
                S.alias([hTb], b_oc)

            steps.append((None, cross_end))
            if stop_after == (l, "cross"):
                return True

            hrows = YR[:, :].rearrange("p (i d) -> p i d", i=NT)
            b_hr = S.bufs(NT, "hrows")
            offm = 0
            oh12, offm = carve(BIG, offm, NT * 64, F32)
            oh12 = oh12.rearrange("p (i e) -> p i e", i=NT)
            mohb, offm = carve(BIG, offm, NT * 32)
            mohb = mohb.rearrange("p (i e) -> p i e", i=NT)
            wts, offm = carve(BIG, offm, NT * 2, F32)
            wts = wts.rearrange("p (i k) -> p i k", i=NT)
            desti, offm = carve(BIG, offm, NT * 2, I32)
            desti = desti.rearrange("p (i k) -> p i k", i=NT)
            rt, offm = carve(BIG, offm, 320, F32)
            cnt, offm = carve(BIG, offm, 32, F32)
            padA, offm = carve(BIG, offm, 32, F32)
            padB, offm = carve(BIG, offm, 32, F32)
            pstart, offm = carve(BIG, offm, 32, F32)
            pend, offm = carve(BIG, offm, 32, F32)
            eblk, offm = carve(BIG, offm, NBLK, F32)
            idxgu, offm = carve(BIG, offm, NBLK * 8, I32)
            idxd, offm = carve(BIG, offm, NBLK * 8, I32)
            idxgu = idxgu.rearrange("p (b c) -> p b c", b=NBLK)
            idxd = idxd.rearrange("p (b c) -> p b c", b=NBLK)
            idxgu_f, offt = carve(BIG, offm, NBLK * 8, F32)
            idxd_f, offt = carve(BIG, offt, NBLK * 8, F32)
            idxgu_f = idxgu_f.rearrange("p (b c) -> p b c", b=NBLK)
            idxd_f = idxd_f.rearrange("p (b c) -> p b c", b=NBLK)
            rtall, offt = carve(BIG, offt, NT * 208, F32)
            rtall = rtall.rearrange("p (i n) -> p i n", i=NT)
            assert offt <= NBIG, offt
            offm0 = offm
            b_rt = S.buf("rt")
            b_rt_i = S.bufs(NT, "rti")
            b_oh_i = S.bufs(NT, "ohi")
            b_dest_i = S.bufs(NT, "desti")
            b_oh = S.buf("oh")
            b_route = S.buf("route")

            def moe_begin(slot, sbuf_):
                S.alias([b_rt, b_oh, b_route] + b_rt_i + b_oh_i + b_dest_i, [b_big])
                S.alias(b_hr, [YRb])

            steps.append((None, moe_begin))

            def router_step():
                def load(slot, sbuf_):
                    wload(slot, sbuf_, [(lambda s: s[:, 0:KC * 36].rearrange("p (c n) -> p c n", c=KC),
                                         w_rt[li_].rearrange("(c p) n -> p c n", p=128))])

                def compute(slot, sbuf_):
                    wr = slot[:, 0:KC * 36].rearrange("p (c n) -> p c n", c=KC)
                    norm_to_fm(lambda i: X[:, i, :], Xb, NT, g0 + GC_FFN, hT, hTb,
                               keep_rows=lambda i: (hrows[:, i, :], b_hr[i]))
                    def rt_tile(i):
                        rt = rtall[:, i, :]
                        yield
                        lg = rt[:, 0:36]
                        yield
                        pp, pb = psbank()
                        yield
                        for c in range(KC):
                            mm(pp[:, 0:36], hT[:, c, i * 128:(i + 1) * 128], wr[:, c, :], c == 0, c == KC - 1, [sbuf_, hTb], [pb])
                        yield
                        R_ = [b_rt_i[i]]
                        yield
                        act(lg, pp[:, 0:36], AF.Copy, [pb], R_)
                        yield
                        mxg = rt[:, 40:41]
                        yield
                        S.op("dve", lambda e, mxg=mxg: e.tensor_reduce(out=mxg, in_=rt[:, 0:4], axis=AX.X, op=ALU.max), reads=R_, writes=R_)
                        yield
                        ohg = rt[:, 44:48]
                        yield
                        ts("dve", ohg, rt[:, 0:4], mxg, ALU.is_equal, R_, R_)
                        yield
                        nmx = rt[:, 41:42]
                        yield
                        ts("dve", nmx, mxg, -1.0, ALU.mult, R_, R_)
                        yield
                        sumg = rt[:, 42:43]
                        yield
                        act(rt[:, 48:52], rt[:, 0:4], AF.Exp, R_, R_, bias=nmx, accum=sumg)
                        yield
                        gw = rt[:, 43:44]
                        yield
                        S.op("dve", lambda e, gw=gw, sumg=sumg: e.reciprocal(out=gw, in_=sumg), reads=R_, writes=R_)
                        yield
                        pen = rt[:, 52:56]
                        yield
                        ts("dve", pen, ohg, -1.0, ALU.add, R_, R_, s2=-NEG, op1=ALU.mult)
                        yield
                        lem = rt[:, 64:96]
                        yield
                        for g in range(4):
                            ts("dve", lem[:, g * 8:(g + 1) * 8], rt[:, 4 + g * 8:4 + (g + 1) * 8], pen[:, g:g + 1], ALU.add, R_, R_)
                        yield
                        m1 = rt[:, 56:57]
                        yield
                        S.op("dve", lambda e, m1=m1, lem=lem: e.tensor_reduce(out=m1, in_=lem, axis=AX.X, op=ALU.max), reads=R_, writes=R_)
                        yield
                        ts("dve", oh12[:, i, 0:32], lem, m1, ALU.is_equal, R_, [b_oh_i[i]])
                        yield
                        lem2 = rt[:, 96:128]
                        yield
                        stt("dve", lem2, oh12[:, i, 0:32], NEG, lem, ALU.mult, ALU.add, [b_oh_i[i], b_rt_i[i]], R_)
                        yield
                        m2 = rt[:, 57:58]
                        yield
                        S.op("dve", lambda e, m2=m2, lem2=lem2: e.tensor_reduce(out=m2, in_=lem2, axis=AX.X, op=ALU.max), reads=R_, writes=R_)
                        yield
                        ts("dve", oh12[:, i, 32:64], lem2, m2, ALU.is_equal, R_, [b_oh_i[i]])
                        yield
                        dd = rt[:, 58:59]
                        yield
                        tt("dve", dd, m2, m1, ALU.subtract, R_, R_)
                        yield
                        ed = rt[:, 59:60]
                        yield
                        act(ed, dd, AF.Exp, R_, R_)
                        yield
                        den = rt[:, 60:61]
                        yield
                        ts("dve", den, ed, 1.0, ALU.add, R_, R_)
                        yield
                        w1 = rt[:, 61:62]
                        yield
                        S.op("dve", lambda e, w1=w1, den=den: e.reciprocal(out=w1, in_=den), reads=R_, writes=R_)
                        yield
                        tt("dve", wts[:, i, 0:1], w1, gw, ALU.mult, R_, [b_oh_i[i]])
                        yield
                        w2 = rt[:, 62:63]
                        yield
                        tt("dve", w2, ed, w1, ALU.mult, R_, R_)
                        yield
                        tt("dve", wts[:, i, 1:2], w2, gw, ALU.mult, R_, [b_oh_i[i]])
                        yield
                        tt("dve", mohb[:, i, :], oh12[:, i, 0:32], oh12[:, i, 32:64], ALU.add, [b_oh_i[i]], [b_oh_i[i]])
                        yield


                    def run_interleaved(gens):
                        gens = list(gens)
                        while gens:
                            for g_ in list(gens):
                                try:
                                    next(g_)
                                except StopIteration:
                                    gens.remove(g_)

                    run_interleaved([rt_tile(i) for i in range(0, 8)])
                    run_interleaved([rt_tile(i) for i in range(8, NT)])
                    pc_, pcb = psbank()
                    for i in range(NT):
                        mm(pc_[:, 0:32], ones, mohb[:, i, :], i == 0, i == NT - 1, [b_const, b_oh_i[i]], [pcb])
                    Q_ = [b_route]
                    cp("dve", cnt, pc_[:, 0:32], [pcb], Q_)
                    ts("dve", padA, cnt, 1.0 / RB, ALU.mult, Q_, Q_, s2=(RB - 1.0) / RB - 0.5 + 0.5 / RB, op1=ALU.add)
                    cp("dve", padB.bitcast(I32), padA, Q_, Q_)
                    cp("dve", padA, padB.bitcast(I32), Q_, Q_)
                    ts("dve", padA, padA, float(RB), ALU.mult, Q_, Q_)
                    cp("dve", pstart, padA, Q_, Q_)
                    a, b_ = padA, padB
                    for s_ in (1, 2, 4, 8, 16):
                        cp("dve", b_[:, 0:s_], a[:, 0:s_], Q_, Q_)
                        tt("dve", b_[:, s_:32], a[:, s_:32], a[:, 0:32 - s_], ALU.add, Q_, Q_)
                        a, b_ = b_, a
                    cp("dve", pend, a, Q_, Q_)
                    tt("dve", pstart, pend, pstart, ALU.subtract, Q_, Q_)
                    def dest_tile(i):
                        rt = rtall[:, i, :]
                        yield
                        pp, pb = psbank()
                        yield
                        mm(pp[:, 0:32], ustr, mohb[:, i, :], True, i == 0, [b_const, b_oh_i[i]], [pb])
                        yield
                        for j in range(i):
                            mm(pp[:, 0:32], ones, mohb[:, j, :], False, j == i - 1, [b_const, b_oh_i[j]], [pb])
                        yield
                        tmp = rt[:, 128:160]
                        yield
                        tt("dve", tmp, pp[:, 0:32], pstart, ALU.add, [pb, b_route], [b_rt_i[i]])
                        yield
                        for k in range(2):
                            prod = rt[:, 160:192]
                            df = rt[:, 192 + k:193 + k]
                            tt("dve", prod, tmp, oh12[:, i, k * 32:(k + 1) * 32], ALU.mult, [b_rt_i[i], b_oh_i[i]], [b_rt_i[i]])
                            S.op("dve", lambda e, df=df, prod=prod: e.tensor_reduce(out=df, in_=prod, axis=AX.X, op=ALU.add), reads=[b_rt_i[i]], writes=[b_rt_i[i]])
                            cp("dve", desti[:, i, k:k + 1], df, [b_rt_i[i]], [b_dest_i[i]])
                        yield


                    run_interleaved([dest_tile(i) for i in range(0, 8)])
                    run_interleaved([dest_tile(i) for i in range(8, NT)])
                    S.op("dve", lambda e: e.memset(eblk, 0.0), writes=Q_)
                    for e_ in range(NEXP):
                        stt("dve", eblk, thr[:, 0:NBLK], pend[:, e_:e_ + 1], eblk, ALU.is_ge, ALU.add, [b_const] + Q_, Q_)
                    ts("dve", eblk, eblk, float(NEXP - 1), ALU.min, Q_, Q_, s2=float(li_ * NEXP), op1=ALU.add)
                    for c in range(8):
                        ts("dve", idxgu_f[:, :, c], eblk, float(D), ALU.mult, Q_, Q_, s2=pcv[:, c:c + 1], op1=ALU.add)
                    for c in range(6):
                        ts("dve", idxd_f[:, :, c], eblk, float(DEXP), ALU.mult, Q_, Q_, s2=pcv[:, c:c + 1], op1=ALU.add)
                    cp("dve", idxgu[:, :, :], idxgu_f[:, :, :], Q_, Q_)
                    cp("dve", idxd[:, :, 0:6], idxd_f[:, :, 0:6], Q_, Q_)
                    for i in range(NT):
                        for k in range(2):
                            S.dma("pool", "scat", lambda e, i=i, k=k: e.indirect_dma_start(
                                out=xb_d[:, :], out_offset=bass.IndirectOffsetOnAxis(ap=desti[:, i, k:k + 1], axis=0),
                                in_=hrows[:, i, :], in_offset=None), reads=[b_hr[i], b_dest_i[i]], writes=[b_xb])

                return load, compute

            steps.append(router_step())

            Gw = [HT[:, g * 6144:(g + 1) * 6144].rearrange("p (c n) -> p c n", c=8) for g in range(2)]
            xg_t = [HT[:, 12288 + i * 1024:12288 + (i + 1) * 1024] for i in range(2)]
            xgT_t = [HT[:, 14336 + i * 1024:14336 + (i + 1) * 1024].rearrange("p (c t) -> p c t", c=8) for i in range(2)]
            Uw = [YR[:, g * 6144:(g + 1) * 6144].rearrange("p (c n) -> p c n", c=8) for g in range(2)]
            ybt = [YR[:, 12288 + i * 2048:12288 + (i + 1) * 2048].bitcast(F32) for i in range(2)]
            offd = offm0 + (offm0 % 2)
            Dw = []
            for g in range(2):
                a, offd = carve(BIG, offd, 6 * D)
                Dw.append(a.rearrange("p (c n) -> p c n", c=6))
            sgt = U_pad[:, 2:2 + 2 * DEXP].bitcast(F32)
            hmt = hn_t[0][:, 0:DEXP]
            hmT = [hn_t[1][:, 0:DEXP].rearrange("p (c t) -> p c t", c=6)]
            a, offd = carve(BIG, offd, DEXP)
            hmT.append(a.rearrange("p (c t) -> p c t", c=6))
            assert offd <= NBIG, offd
            b_G = S.bufs(2, "Gw")
            b_U = S.bufs(2, "Uw")
            b_D = S.bufs(2, "Dw")
            b_xg = S.bufs(2, "xg")
            b_xgT = S.bufs(2, "xgT")
            b_ybt = S.bufs(2, "ybt")
            b_sg = S.buf("sg")
            b_hm = S.buf("hm")
            b_hmT = S.bufs(2, "hmT")
            weg = w_eg.rearrange("l e d n -> (l e d) n")
            weu = w_eu.rearrange("l e d n -> (l e d) n")
            wed = w_ed.rearrange("l e d n -> (l e d) n")

            def moe_blocks(slot, sbuf_):
                S.alias(b_G + b_xg + b_xgT, [hTb])
                S.alias(b_U + b_ybt, b_hr)
                S.alias(b_D + [b_hmT[1]], [b_big, b_route] + b_rt_i)
                S.alias([b_sg], [b_u])
                S.alias([b_hm, b_hmT[0]], hn_b)

                def issue_w(b):
                    p = b % 2
                    for c in range(8):
                        S.dma("pool", "G%d" % p, lambda e, c=c, p=p, b=b: e.indirect_dma_start(
                            out=Gw[p][:, c, :], out_offset=None, in_=weg,
                            in_offset=bass.IndirectOffsetOnAxis(ap=idxgu[:, b, c:c + 1], axis=0)), reads=[b_route], writes=[b_G[p]])
                    for c in range(8):
                        S.dma("pool", "U%d" % p, lambda e, c=c, p=p, b=b: e.indirect_dma_start(
                            out=Uw[p][:, c, :], out_offset=None, in_=weu,
                            in_offset=bass.IndirectOffsetOnAxis(ap=idxgu[:, b, c:c + 1], axis=0)), reads=[b_route], writes=[b_U[p]])
                    for c in range(6):
                        S.dma("pool", "D%d" % p, lambda e, c=c, p=p, b=b: e.indirect_dma_start(
                            out=Dw[p][:, c, :], out_offset=None, in_=wed,
                            in_offset=bass.IndirectOffsetOnAxis(ap=idxd[:, b, c:c + 1], axis=0)), reads=[b_route], writes=[b_D[p]])

                def issue_x(t):
                    q = t % 2
                    dma_sp("xg%d" % q, xg_t[q], xb_d[t * 128:(t + 1) * 128, :], reads=[b_xb], writes=[b_xg[q]])

                NTIL = NROW // 128
                SUB = RB // 128
                issue_w(0)
                issue_x(0)
                for t in range(NTIL):
                    b = t // SUB
                    p = b % 2
                    q = t % 2
                    if t % SUB == 0 and b + 1 < NBLK:
                        issue_w(b + 1)
                    if t + 1 < NTIL:
                        issue_x(t + 1)
                    pt, pb = psbank()
                    ptb = pt[:, :].bitcast(BF16).rearrange("p (c t) -> p c t", t=128)
                    for c in range(KC):
                        tr(ptb[:, c, :], xg_t[q][:, c * 128:(c + 1) * 128], [b_xg[q]], [pb])
                    for c in range(KC):
                        gc = gcols[:, g0 + GC_FFN + c:g0 + GC_FFN + c + 1]
                        if c % 2 == 0:
                            ts("dve", xgT_t[q][:, c, :], ptb[:, c, :], gc, ALU.mult, [pb, b_const], [b_xgT[q]])
                        else:
                            act(xgT_t[q][:, c, :], ptb[:, c, :], AF.Copy, [pb, b_const], [b_xgT[q]], scale=gc)
                    for (n0, nn) in ((0, 512), (512, 256)):
                        pg, pgb = psbank()
                        for c in range(KC):
                            mm(pg[:, 0:nn], xgT_t[q][:, c, :], Gw[p][:, c, n0:n0 + nn], c == 0, c == KC - 1, [b_xgT[q], b_G[p]], [pgb])
                        pu, pub = psbank()
                        for c in range(KC):
                            mm(pu[:, 0:nn], xgT_t[q][:, c, :], Uw[p][:, c, n0:n0 + nn], c == 0, c == KC - 1, [b_xgT[q], b_U[p]], [pub])
                        act(sgt[:, n0:n0 + nn], pg[:, 0:nn], AF.Silu, [pgb], [b_sg])
                        tt("dve", hmt[:, n0:n0 + nn], sgt[:, n0:n0 + nn], pu[:, 0:nn], ALU.mult, [b_sg, pub], [b_hm])
                    pt2, pb2 = psbank()
                    pt2b = pt2[:, :].bitcast(BF16).rearrange("p (c t) -> p c t", t=128)
                    for c in range(6):
                        tr(pt2b[:, c, :], hmt[:, c * 128:(c + 1) * 128], [b_hm], [pb2])
                    act(hmT[q][:, 0:3, :], pt2b[:, 0:3, :], AF.Copy, [pb2], [b_hmT[q]])
                    cp("dve", hmT[q][:, 3:6, :], pt2b[:, 3:6, :], [pb2], [b_hmT[q]])
                    for n in range(2):
                        py, pyb = psbank()
                        for c in range(6):
                            mm(py[:, :], hmT[q][:, c, :], Dw[p][:, c, n * 512:(n + 1) * 512], c == 0, c == 5, [b_hmT[q], b_D[p]], [pyb])
                        if n == 0:
                            act(ybt[q][:, 0:512], py[:, :], AF.Copy, [pyb], [b_ybt[q]])
                        else:
                            cp("dve", ybt[q][:, 512:1024], py[:, :], [pyb], [b_ybt[q]])
                    dma_sp("ybst", yb_d[t * 128:(t + 1) * 128, :], ybt[q], reads=[b_ybt[q]], writes=[b_yb])
                S.alias(b_gat, b_G + b_xg + b_xgT)
                for i in range(NT):
                    for k in range(2):
                        gi = (i * 2 + k) % 4
                        S.dma("pool", "gat%d" % gi, lambda e, i=i, k=k, gi=gi: e.indirect_dma_start(
                            out=gat_t[gi], out_offset=None, in_=yb_d[:, :],
                            in_offset=bass.IndirectOffsetOnAxis(ap=desti[:, i, k:k + 1], axis=0)), reads=[b_yb, b_dest_i[i]], writes=[b_gat[gi]])
                        stt("dve", X[:, i, :], gat_t[gi], wts[:, i, k:k + 1], X[:, i, :], ALU.mult, ALU.add, [b_gat[gi], b_oh_i[i], Xb[i]], [Xb[i]])

            gat_t = [HT[:, i * 2048:(i + 1) * 2048].bitcast(F32) for i in range(4)]
            b_gat = S.bufs(4, "gat")
            steps.append((None, moe_blocks))

            def moe_end(slot, sbuf_):
                S.alias([b_big], [b_rt, b_oh, b_route] + b_rt_i + b_oh_i + b_dest_i + b_D + b_hmT)
                S.alias([b_u], [b_sg])
                S.alias(hn_b, [b_hm, b_hmT[0]])
                S.alias([YRb], b_U + b_ybt + b_hr)
                S.alias([hTb], b_gat + b_G + b_xg + b_xgT)

            steps.append((None, moe_end))
            if stop_after == (l, "moe"):
                return True
            return False

        ztile = sb("ztile", [128, 512], BF16)
        b_zt = S.buf("zt")
        S.op("pool", lambda e: e.memset(ztile[:, :], 0.0), writes=[b_zt])
        U_pad = sb("U_pad", [128, S_LEN + 4], BF16)
        b_u = S.buf("u")
        b_xb = S.buf("xb_d")
        b_yb = S.buf("yb_d")
        S.op("pool", lambda e: e.memset(U_pad[:, :], 0.0), writes=[b_u])

        stopped = False
        for li_, l in enumerate(layer_ids):
            stopped = layer_steps(li_, l)
            if stopped:
                break

        def epilogue(slot, sbuf_):
            if final and not stopped:
                gfin, _ = carve(BIG, 0, D, F32)
                otile = [carve(BIG, 2 * D + i * 2 * D, D, F32)[0] for i in range(2)]
                b_gf = S.buf("gfin")
                b_ot = S.bufs(2, "otile")
                S.alias([b_gf] + b_ot, [b_big])
                dma_sp("c2", gfin, gfin_d[:, :], writes=[b_gf])
                rstd_tiles(lambda i: X[:, i, :], Xb, NT, 1.0 / D)
                for i in range(NT):
                    p = i % 2
                    act(otile[p], X[:, i, :], AF.Copy, [Xb[i], b_rstd], [b_ot[p]], scale=rstd[:, i:i + 1])
                    tt("dve", otile[p], otile[p], gfin, ALU.mult, [b_ot[p], b_gf], [b_ot[p]])
                    dma_sp("out", out_d[i * 128:(i + 1) * 128, :], otile[p], reads=[b_ot[p]], writes=[b_out])
            else:
                for i in range(NT):
                    dma_sp("out", out_d[i * 128:(i + 1) * 128, :], X[:, i, :], reads=[Xb[i]], writes=[b_out])

        b_out = S.buf("out")
        if max_steps is not None:
            del steps[max_steps:]
        steps.append((None, epilogue))

        wsteps = [k for k, (ld, _) in enumerate(steps) if ld is not None]
        issued = 0
        done = 0
        for k, (ld, cpf) in enumerate(steps):
            while issued < len(wsteps) and issued < done + NSLOT:
                sl = issued % NSLOT
                steps[wsteps[issued]][0](WS[sl], WSb[sl])
                issued += 1
            if ld is not None:
                sl = done % NSLOT
                cpf(WS[sl], WSb[sl])
                done += 1
            else:
                cpf(None, None)
        S.wait_all("sp", [b_out])
        if os.environ.get('WAITCS'):
            for b_ in dbg_bufs:
                S.wait_all("sp", [b_])
        S.finalize()
        dl = S.simulate()
        if dl is not None:
            raise RuntimeError("sync deadlock: %r" % (dl,))
        S.emit()
    return nc, S.ninst


def _consts():
    ident = np.eye(128, dtype=np.float32)
    ones = np.ones((128, 128), np.float32)
    rrot = np.zeros((128, 128), np.float32)
    for c in range(2):
        for i in range(8):
            rrot[c * 64 + 8 + i, c * 64 + i] = -1.0
            rrot[c * 64 + i, c * 64 + 8 + i] = 1.0
    ustr = np.triu(np.ones((128, 128), np.float32), 1)
    cmat = np.concatenate([ident, ones, rrot, ustr], axis=1)
    inv = np.float32(500000.0) ** (-np.arange(0, 16, 2, dtype=np.float32) / np.float32(16))
    ang = np.arange(S_LEN, dtype=np.float32)[:, None] * inv[None, :]
    cs, sn = np.cos(ang).astype(np.float32), np.sin(ang).astype(np.float32)
    cosT = np.ones((128, S_LEN), np.float32)
    sinT = np.zeros((128, S_LEN), np.float32)
    for c in range(2):
        for i in range(16):
            cosT[c * 64 + i] = cs[:, i % 8]
            sinT[c * 64 + i] = sn[:, i % 8]
    cossin = np.concatenate([cosT, sinT], axis=1)
    thr = np.broadcast_to((np.arange(64, dtype=np.float32) * float(RB))[None, :], (128, 64))
    pc = np.arange(8, dtype=np.float32)[None, :] * 128.0 + np.arange(128, dtype=np.float32)[:, None]
    cmisc = np.ascontiguousarray(np.concatenate([thr, pc], axis=1))
    return np.ascontiguousarray(cmat.astype(ml_dtypes.bfloat16)), np.ascontiguousarray(cossin.astype(ml_dtypes.bfloat16)), cmisc


def _pack_small(inp, layer_ids):
    cols = []
    for l in layer_ids:
        def colmaj(v):
            return np.asarray(v, np.float32).reshape(8, 128).T
        cols.append(colmaj(inp["norm_mix"][l]))
        cols.append(colmaj(inp["norm_cross"][l]))
        cols.append(colmaj(inp["norm_ffn"][l]))
        for k in range(3):
            cols.append(colmaj(inp["conv_w"][l][k]))
        cols.append(np.asarray(inp["subln"][l], np.float32).reshape(128, 1))
    cols.append(np.asarray(inp["norm_mem"], np.float32).reshape(8, 128).T)
    gcols = np.ascontiguousarray(np.concatenate(cols, axis=1))
    lam = []
    for l in layer_ids:
        lam.append(np.concatenate([inp["lambda_q1"][l], inp["lambda_k1"][l], inp["lambda_q2"][l], inp["lambda_k2"][l]]))
    lamv = np.ascontiguousarray(np.broadcast_to(np.concatenate(lam)[None, :].astype(np.float32), (128, len(layer_ids) * 256)))
    gfin = np.ascontiguousarray(np.broadcast_to(np.asarray(inp["norm_final"], np.float32)[None, :], (128, D)))
    return gcols, lamv, gfin


_PROG_CACHE = {}


def _run(inp, x_shards, layer_ids, final, stop_after=None, cores=None, max_steps=None):
    key = (tuple(layer_ids), final, stop_after, max_steps)
    if key not in _PROG_CACHE:
        _PROG_CACHE[key] = build(list(layer_ids), final, stop_after, max_steps)[0]
    nc = _PROG_CACHE[key]
    cmat, cossin, cmisc = _consts()
    gcols, lamv, gfin = _pack_small(inp, layer_ids)
    ls = list(layer_ids)
    sl = slice(ls[0], ls[-1] + 1)
    w_rt = np.ascontiguousarray(np.concatenate([inp["w_router_group"][sl], inp["w_router_expert"][sl]], axis=-1))
    shared = {
        "w_in": np.ascontiguousarray(inp["w_in"][sl]), "w_branch": np.ascontiguousarray(inp["w_branch"][sl]),
        "w_o": np.ascontiguousarray(inp["w_o"][sl]), "w_cq": np.ascontiguousarray(inp["w_cq"][sl]),
        "w_ckv": np.ascontiguousarray(inp["w_ckv"][sl]), "w_co": np.ascontiguousarray(inp["w_co"][sl]),
        "w_rt": w_rt, "w_exp_gate": np.ascontiguousarray(inp["w_exp_gate"][sl]),
        "w_exp_up": np.ascontiguousarray(inp["w_exp_up"][sl]), "w_exp_down": np.ascontiguousarray(inp["w_exp_down"][sl]),
        "gcols": gcols, "lamv": lamv, "gfinal": gfin, "cmat": cmat, "cossin": cossin, "cmisc": cmisc,
    }
    cores = list(range(len(x_shards))) if cores is None else cores
    in_maps = []
    for b in range(len(x_shards)):
        m = dict(shared)
        m["x"] = np.ascontiguousarray(x_shards[b])
        m["mem"] = np.ascontiguousarray(inp["mem"][b])
        in_maps.append(m)
    res = run_bass_kernel_spmd(nc, in_maps, core_ids=cores)
    return [r["out"] for r in res.results]


MODE = "fused"


def kernel(**inputs):
    inp = {k: np.asarray(v) for k, v in inputs.items()}
    xs = [inp["x"][b] for b in range(inp["x"].shape[0])]
    if MODE == "fused":
        outs = _run(inp, xs, list(range(DEPTH)), True)
    else:
        for l in range(DEPTH):
            xs = _run(inp, xs, [l], l == DEPTH - 1)
        outs = xs
    return np.stack(outs, axis=0).astype(np.float32)
```
